# Optimizing a Trainium2 kernel written in Bass

```python
import math
import jax, jax.numpy as jnp
from jax import lax
import numpy as np

D_MODEL = 1024
BATCH = 16
SEQ = 2048
DEPTH = 2

CHUNK = 64
Q_BLOCK = 128
N_MEM = 256
N_A_LAYERS = DEPTH // 2
N_B_LAYERS = DEPTH - N_A_LAYERS

MLSTM_HEADS = 4
MLSTM_QK_DIM = D_MODEL // 16
MLSTM_V_DIM = D_MODEL // 8
MLSTM_WIDTH = MLSTM_HEADS * MLSTM_V_DIM

MEM_HEADS = 4
MEM_HEAD_DIM = D_MODEL // 8
MEM_WIDTH = MEM_HEADS * MEM_HEAD_DIM

MLA_HEADS = 8
MLA_NOPE = 64
MLA_ROPE = 32
MLA_V = 64
Q_LORA = D_MODEL // 4
KV_LORA = D_MODEL // 4
MLA_WIDTH = MLA_HEADS * MLA_V

D_FF = 4 * D_MODEL
ROPE_THETA = 10000.0
LN_EPS = 1e-5
RMS_EPS = 1e-6
ALPHA = (2 * DEPTH) ** 0.25
BETA = (8 * DEPTH) ** -0.25

A_SPLITS = [MLSTM_HEADS * MLSTM_QK_DIM, MLSTM_HEADS * MLSTM_QK_DIM, MLSTM_WIDTH,
            MLSTM_WIDTH, MLSTM_HEADS, MLSTM_HEADS, MEM_WIDTH]
A_IN_WIDTH = sum(A_SPLITS)
B_IN_WIDTH = Q_LORA + MEM_WIDTH

kernel_name = "yoco_mlstm_mla_memory_deepnorm"


def _offsets(sizes):
    out, acc = [], 0
    for s in sizes[:-1]:
        acc += s
        out.append(acc)
    return out


def layer_norm(x, g, b):
    xf = x.astype(jnp.float32)
    mu = jnp.mean(xf, axis=-1, keepdims=True)
    var = jnp.mean(jnp.square(xf - mu), axis=-1, keepdims=True)
    return ((xf - mu) * lax.rsqrt(var + LN_EPS) * g + b).astype(x.dtype)


def rms_norm(x, g):
    xf = x.astype(jnp.float32)
    return (xf * lax.rsqrt(jnp.mean(jnp.square(xf), axis=-1, keepdims=True) + RMS_EPS) * g).astype(x.dtype)


def apply_rope(x, cos, sin):
    half = x.shape[-1] // 2
    x1, x2 = x[..., :half], x[..., half:]
    return jnp.concatenate([x1 * cos - x2 * sin, x2 * cos + x1 * sin], axis=-1).astype(x.dtype)


def mlstm_chunkwise(q, k, v, i_pre, f_pre):
    bsz, seq, heads, dk = q.shape
    dv = v.shape[-1]
    nc = seq // CHUNK

    def chunks(t):
        return t.reshape(bsz, nc, CHUNK, heads, t.shape[-1]).transpose(0, 3, 1, 2, 4)

    def chunks_g(t):
        return t.reshape(bsz, nc, CHUNK, heads).transpose(0, 3, 1, 2)

    qc, kc, vc = chunks(q), chunks(k), chunks(v)
    a = chunks_g(i_pre.astype(jnp.float32))
    b = jnp.cumsum(chunks_g(jax.nn.log_sigmoid(f_pre.astype(jnp.float32))), axis=-1)
    g = b[..., -1]
    logw = g[..., None] - b + a

    def step(carry, inp):
        c_st, n_st, m_st = carry
        k_c, v_c, lw, g_c = inp
        m_new = jnp.maximum(g_c + m_st, jnp.max(lw, axis=-1))
        decay = jnp.exp(g_c + m_st - m_new)
        w = jnp.exp(lw - m_new[..., None])
        c_new = decay[..., None, None] * c_st + jnp.einsum("bhl,bhlk,bhlv->bhkv", w, k_c, v_c)
        n_new = decay[..., None] * n_st + jnp.einsum("bhl,bhlk->bhk", w, k_c)
        return (c_new, n_new, m_new), (c_st, n_st, m_st)

    init = (jnp.zeros((bsz, heads, dk, dv), jnp.float32),
            jnp.zeros((bsz, heads, dk), jnp.float32),
            jnp.zeros((bsz, heads), jnp.float32))
    xs = (kc.transpose(2, 0, 1, 3, 4), vc.transpose(2, 0, 1, 3, 4),
          logw.transpose(2, 0, 1, 3), g.transpose(2, 0, 1))
    _, (c_prev, n_prev, m_prev) = lax.scan(step, init, xs)
    c_prev = c_prev.transpose(1, 2, 0, 3, 4)
    n_prev = n_prev.transpose(1, 2, 0, 3)
    m_prev = m_prev.transpose(1, 2, 0)

    causal = jnp.tril(jnp.ones((CHUNK, CHUNK), dtype=bool))
    d_log = jnp.where(causal, b[..., :, None] - b[..., None, :] + a[..., None, :], -jnp.inf)
    m_inter = b + m_prev[..., None]
    m_t = jnp.maximum(m_inter, jnp.max(d_log, axis=-1))
    p = jnp.einsum("bhclk,bhcsk->bhcls", qc, kc) * jnp.exp(d_log - m_t[..., None])
    inter = jnp.exp(m_inter - m_t)
    num = (jnp.einsum("bhcls,bhcsv->bhclv", p, vc)
           + inter[..., None] * jnp.einsum("bhclk,bhckv->bhclv", qc, c_prev))
    nq = jnp.sum(p, axis=-1) + inter * jnp.einsum("bhclk,bhck->bhcl", qc, n_prev)
    h = num / jnp.maximum(jnp.abs(nq), jnp.exp(-m_t))[..., None]
    return h.transpose(0, 2, 3, 1, 4).reshape(bsz, seq, heads, dv)


def memory_attention(q_mem, mem, w_mem_kv):
    bsz, seq, _ = q_mem.shape
    kv = (mem @ w_mem_kv).reshape(bsz, mem.shape[1], 2, MEM_HEADS, MEM_HEAD_DIM)
    q = q_mem.reshape(bsz, seq, MEM_HEADS, MEM_HEAD_DIM)
    s = jnp.einsum("bshd,bmhd->bhsm", q, kv[:, :, 0]).astype(jnp.float32) * (MEM_HEAD_DIM ** -0.5)
    p = jax.nn.softmax(s, axis=-1).astype(q.dtype)
    return jnp.einsum("bhsm,bmhd->bshd", p, kv[:, :, 1]).reshape(bsz, seq, MEM_WIDTH)


def mla_chunk_causal_attention(q_nope, q_rope, k_nope, k_rope, v):
    seq = q_nope.shape[1]
    scale = (MLA_NOPE + MLA_ROPE) ** -0.5
    outs = []
    for blk in range(seq // Q_BLOCK):
        q0, q1 = blk * Q_BLOCK, (blk + 1) * Q_BLOCK
        kend = q1
        s = (jnp.einsum("bqhd,bkhd->bhqk", q_nope[:, q0:q1], k_nope[:, :kend])
             + jnp.einsum("bqhr,bkr->bhqk", q_rope[:, q0:q1], k_rope[:, :kend]))
        s = s.astype(jnp.float32) * scale
        allowed = (jnp.arange(kend) // CHUNK)[None, :] <= (jnp.arange(q0, q1) // CHUNK)[:, None]
        p = jax.nn.softmax(jnp.where(allowed, s, -jnp.inf), axis=-1).astype(v.dtype)
        outs.append(jnp.einsum("bhqk,bkhd->bqhd", p, v[:, :kend]))
    return jnp.concatenate(outs, axis=1)


def mlstm_mem_sublayer(x, mem, w_in, b_igate, b_fgate, w_mem_kv, w_out):
    bsz, seq, _ = x.shape
    q, k, v, o_pre, i_pre, f_pre, q_mem = jnp.split(x @ w_in, _offsets(A_SPLITS), axis=-1)
    q = q.reshape(bsz, seq, MLSTM_HEADS, MLSTM_QK_DIM)
    k = k.reshape(bsz, seq, MLSTM_HEADS, MLSTM_QK_DIM) * (MLSTM_QK_DIM ** -0.5)
    v = v.reshape(bsz, seq, MLSTM_HEADS, MLSTM_V_DIM)
    h = mlstm_chunkwise(q, k, v, i_pre + b_igate, f_pre + b_fgate)
    h = h.reshape(bsz, seq, MLSTM_WIDTH) * jax.nn.sigmoid(o_pre.astype(jnp.float32))
    h_mem = memory_attention(q_mem, mem, w_mem_kv)
    return (jnp.concatenate([h.astype(x.dtype), h_mem.astype(x.dtype)], axis=-1) @ w_out).astype(x.dtype)


def shared_latent_kv(h, cos, sin, w_down, norm_g, w_uk, w_uv):
    bsz, seq, _ = h.shape
    c_kv, k_rope = jnp.split(h @ w_down, [KV_LORA], axis=-1)
    c_kv = rms_norm(c_kv, norm_g)
    k_nope = (c_kv @ w_uk).reshape(bsz, seq, MLA_HEADS, MLA_NOPE)
    v = (c_kv @ w_uv).reshape(bsz, seq, MLA_HEADS, MLA_V)
    k_rope = apply_rope(k_rope, cos, sin)
    return k_nope, k_rope, v


def mla_mem_sublayer(x, mem, cos, sin, k_nope, k_rope, v, w_in, q_norm_g, w_uq, w_mem_kv, w_out):
    bsz, seq, _ = x.shape
    c_q, q_mem = jnp.split(x @ w_in, [Q_LORA], axis=-1)
    q = (rms_norm(c_q, q_norm_g) @ w_uq).reshape(bsz, seq, MLA_HEADS, MLA_NOPE + MLA_ROPE)
    q_nope, q_rope = q[..., :MLA_NOPE], q[..., MLA_NOPE:]
    q_rope = apply_rope(q_rope, cos[:, :, None, :], sin[:, :, None, :])
    o = mla_chunk_causal_attention(q_nope, q_rope, k_nope, k_rope, v).reshape(bsz, seq, MLA_WIDTH)
    h_mem = memory_attention(q_mem, mem, w_mem_kv)
    return (jnp.concatenate([o.astype(x.dtype), h_mem.astype(x.dtype)], axis=-1) @ w_out).astype(x.dtype)


def sqrelu_ffn(x, w_up, w_down):
    return (jnp.square(jax.nn.relu(x @ w_up)) @ w_down).astype(x.dtype)


def setup_inputs(seed: int = 0) -> dict:
    key = jax.random.key(seed)
    ks = jax.random.split(key, 24)
    f32 = jnp.float32

    def w(k, shape, fan_in, scale=1.0):
        return jax.random.normal(k, shape, f32) * (scale * fan_in ** -0.5)

    def gain(k, shape):
        return 1.0 + 0.02 * jax.random.normal(k, shape, f32)

    def bias(k, shape):
        return 0.02 * jax.random.normal(k, shape, f32)

    offsets = jax.random.randint(ks[2], (BATCH, 1), 0, 4096, dtype=jnp.int32)
    positions = offsets + jnp.arange(SEQ, dtype=jnp.int32)[None, :]
    return {
        "x": jax.random.normal(ks[0], (BATCH, SEQ, D_MODEL), f32),
        "mem": jax.random.normal(ks[1], (BATCH, N_MEM, D_MODEL), f32),
        "positions": positions,
        "a_w_in": w(ks[3], (N_A_LAYERS, D_MODEL, A_IN_WIDTH), D_MODEL),
        "a_b_igate": 0.1 * jax.random.normal(ks[4], (N_A_LAYERS, MLSTM_HEADS), f32),
        "a_b_fgate": 3.0 + 0.5 * jax.random.normal(ks[5], (N_A_LAYERS, MLSTM_HEADS), f32),
        "a_w_mem_kv": w(ks[6], (N_A_LAYERS, D_MODEL, 2 * MEM_WIDTH), D_MODEL),
        "a_w_out": w(ks[7], (N_A_LAYERS, MLSTM_WIDTH + MEM_WIDTH, D_MODEL), MLSTM_WIDTH + MEM_WIDTH, BETA),
        "kv_w_down": w(ks[8], (D_MODEL, KV_LORA + MLA_ROPE), D_MODEL),
        "kv_norm_g": gain(ks[9], (KV_LORA,)),
        "kv_w_uk": w(ks[10], (KV_LORA, MLA_HEADS * MLA_NOPE), KV_LORA),
        "kv_w_uv": w(ks[11], (KV_LORA, MLA_HEADS * MLA_V), KV_LORA),
        "b_w_in": w(ks[12], (N_B_LAYERS, D_MODEL, B_IN_WIDTH), D_MODEL),
        "b_q_norm_g": gain(ks[13], (N_B_LAYERS, Q_LORA)),
        "b_w_uq": w(ks[14], (N_B_LAYERS, Q_LORA, MLA_HEADS * (MLA_NOPE + MLA_ROPE)), Q_LORA),
        "b_w_mem_kv": w(ks[15], (N_B_LAYERS, D_MODEL, 2 * MEM_WIDTH), D_MODEL),
        "b_w_out": w(ks[16], (N_B_LAYERS, MLA_WIDTH + MEM_WIDTH, D_MODEL), MLA_WIDTH + MEM_WIDTH, BETA),
        "ln1_g": gain(ks[17], (DEPTH, D_MODEL)),
        "ln1_b": bias(ks[18], (DEPTH, D_MODEL)),
        "ffn_w_up": w(ks[19], (DEPTH, D_MODEL, D_FF), D_MODEL),
        "ffn_w_down": w(ks[20], (DEPTH, D_FF, D_MODEL), D_FF, BETA),
        "ln2_g": gain(ks[21], (DEPTH, D_MODEL)),
        "ln2_b": bias(ks[22], (DEPTH, D_MODEL)),
    }


def reference(x, mem, positions, a_w_in, a_b_igate, a_b_fgate, a_w_mem_kv, a_w_out,
              kv_w_down, kv_norm_g, kv_w_uk, kv_w_uv,
              b_w_in, b_q_norm_g, b_w_uq, b_w_mem_kv, b_w_out,
              ln1_g, ln1_b, ffn_w_up, ffn_w_down, ln2_g, ln2_b):
    inv_freq = ROPE_THETA ** (-jnp.arange(0, MLA_ROPE, 2, dtype=jnp.float32) / MLA_ROPE)
    ang = positions.astype(jnp.float32)[..., None] * inv_freq
    cos, sin = jnp.cos(ang), jnp.sin(ang)

    for layer in range(DEPTH):
        if layer < N_A_LAYERS:
            mix = mlstm_mem_sublayer(x, mem, a_w_in[layer], a_b_igate[layer], a_b_fgate[layer],
                                     a_w_mem_kv[layer], a_w_out[layer])
        else:
            if layer == N_A_LAYERS:
                k_nope, k_rope, v_sh = shared_latent_kv(x, cos, sin, kv_w_down, kv_norm_g,
                                                        kv_w_uk, kv_w_uv)
            j = layer - N_A_LAYERS
            mix = mla_mem_sublayer(x, mem, cos, sin, k_nope, k_rope, v_sh, b_w_in[j], b_q_norm_g[j],
                                   b_w_uq[j], b_w_mem_kv[j], b_w_out[j])
        x = layer_norm(ALPHA * x + mix, ln1_g[layer], ln1_b[layer])
        x = layer_norm(ALPHA * x + sqrelu_ffn(x, ffn_w_up[layer], ffn_w_down[layer]),
                       ln2_g[layer], ln2_b[layer])
    return x
```

```python
import contextlib
import numpy as np
import concourse.bass as bass
import concourse.mybir as mybir
from concourse.bass_utils import run_bass_kernel_spmd

F32 = mybir.dt.float32
BF16 = mybir.dt.bfloat16
I32 = mybir.dt.int32
AF = mybir.ActivationFunctionType
ALU = mybir.AluOpType
AX = mybir.AxisListType

NCORES = 8
SEQ = 2048
D = 1024
DFF = 4096
NSEQ = 2
NTOK = NSEQ * SEQ
ST = 512
NST = NTOK // ST
ALPHA = 4.0 ** 0.25
LN_EPS = 1e-5
RMS_EPS = 1e-6


class Trk:
    __slots__ = ("name", "w", "r", "dsem", "dcnt")

    def __init__(self, name):
        self.name = name
        self.w = None
        self.r = {}
        self.dsem = None
        self.dcnt = 0


class Prog:
    SEM_ROT = 12000

    def __init__(self, nc):
        self.nc = nc
        self.eng = {"pe": nc.tensor, "act": nc.scalar, "dve": nc.vector, "pool": nc.gpsimd,
                    "sp": nc.sync}
        self.sem = {}
        self.cnt = {}
        self.seen = {e: {} for e in self.eng}
        self.nsem = 0
        for e in self.eng:
            self._new_sem(e)
        self.out_tokens = []
        self.ninstr = 0
        self.last_tok = {}
        self.rec = None
        self.inter = None
        self._acc = 0.0
        self._in_replay = False
        self.dma_toks = {}
        self.free_dsems = []
        self.phase_trks = []

    def _alloc_sem(self, name):
        self.nsem += 1
        return self.nc.alloc_semaphore(name="%s_%d" % (name, self.nsem))

    def _new_sem(self, e):
        self.sem[e] = self._alloc_sem("s_" + e)
        self.cnt[e] = 0

    def _need(self, e, tok, skip_same):
        if tok is None:
            return
        sem, c, te = tok
        if skip_same and te == e and e == "pe":
            return
        if self.seen[e].get(sem, 0) >= c:
            return
        self.eng[e].wait_ge(sem, c)
        self.seen[e][sem] = c

    def _signal(self, e, ins):
        if self.cnt[e] >= self.SEM_ROT:
            self._new_sem(e)
        self.cnt[e] += 1
        ins.then_inc(self.sem[e], 1)
        self.last_tok[e] = (self.sem[e], self.cnt[e], e)
        return self.last_tok[e]

    def full_barrier(self):
        snap = dict(self.last_tok)
        dts = list(self.dma_toks.values())
        for e in self.eng:
            for o, tok in snap.items():
                if o != e:
                    self._need(e, tok, False)
            for tok in dts:
                self._need(e, tok, False)
        for t in self.phase_trks:
            self.free_dsems.append((t.dsem, t.dcnt))
            t.dsem = None
        self.phase_trks = []
        self.dma_toks = {}

    def _after_emit(self):
        if self.inter is None or self._in_replay:
            return
        pend, rate = self.inter
        self._acc += rate
        while self._acc >= 1.0 and pend:
            self._acc -= 1.0
            self._in_replay = True
            self.replay(pend.pop(0))
            self._in_replay = False

    def replay(self, item):
        kind, args, kw = item
        saved, self.rec = self.rec, None
        getattr(self, kind)(*args, **kw)
        self.rec = saved

    def op(self, e, fn, reads=(), writes=()):
        if self.rec is not None:
            self.rec.append(("op", (e, fn), dict(reads=list(reads), writes=list(writes))))
            return None
        for t in reads:
            self._need(e, t.w, False)
        for t in writes:
            self._need(e, t.w, True)
            for tok in t.r.values():
                self._need(e, tok, True)
        ins = fn(self.eng[e])
        tok = self._signal(e, ins)
        for t in writes:
            t.w = tok
            t.r = {}
        for t in reads:
            t.r[e] = tok
        self.ninstr += 1
        self._after_emit()
        return tok

    def mm(self, out, pairs, reads=(), writes=()):
        if self.rec is not None:
            self.rec.append(("mm", (out, list(pairs)), dict(reads=list(reads), writes=list(writes))))
            return None
        e = "pe"
        for t in reads:
            self._need(e, t.w, False)
        for t in writes:
            self._need(e, t.w, True)
            for tok in t.r.values():
                self._need(e, tok, True)
        n = len(pairs)
        ins = None
        for i, (l, r) in enumerate(pairs):
            ins = self.nc.tensor.matmul(out, l, r, start=(i == 0), stop=(i == n - 1))
        tok = self._signal(e, ins)
        for t in writes:
            t.w = tok
            t.r = {}
        for t in reads:
            t.r[e] = tok
        self.ninstr += n
        self._after_emit()
        return tok

    def pe_multi(self, fns, reads=(), writes=()):
        if self.rec is not None:
            self.rec.append(("pe_multi", (list(fns),), dict(reads=list(reads), writes=list(writes))))
            return None
        e = "pe"
        for t in reads:
            self._need(e, t.w, False)
        for t in writes:
            self._need(e, t.w, True)
            for tok in t.r.values():
                self._need(e, tok, True)
        ins = None
        for f in fns:
            ins = f(self.nc.tensor)
        tok = self._signal(e, ins)
        for t in writes:
            t.w = tok
            t.r = {}
        for t in reads:
            t.r[e] = tok
        self.ninstr += len(fns)
        self._after_emit()
        return tok

    def dma(self, q, out, in_, reads=(), writes=(), is_output=False, sem_trk=None):
        if self.rec is not None:
            self.rec.append(("dma", (q, out, in_), dict(reads=list(reads), writes=list(writes),
                                                        is_output=is_output, sem_trk=sem_trk)))
            return None
        e = q
        for t in reads:
            self._need(e, t.w, False)
        for t in writes:
            self._need(e, t.w, False)
            for tok in t.r.values():
                self._need(e, tok, False)
        trk = sem_trk if sem_trk is not None else (list(writes) + list(reads))[0]
        if trk.dsem is None:
            if self.free_dsems:
                trk.dsem, trk.dcnt = self.free_dsems.pop()
            else:
                trk.dsem = self._alloc_sem("d")
                trk.dcnt = 0
            self.phase_trks.append(trk)
        trk.dcnt += 16
        self.eng[e].dma_start(out=out, in_=in_).then_inc(trk.dsem, 16)
        tok = (trk.dsem, trk.dcnt, "dma")
        self.dma_toks[trk.dsem] = tok
        for t in writes:
            t.w = tok
            t.r = {}
        for t in reads:
            t.r["dma_%s" % trk.name] = tok
        if is_output:
            self.out_tokens.append(tok)
        self.ninstr += 1
        return tok

    def barrier_all(self, trks):
        for t in trks:
            self._need("sp", t.w, False)
            for tok in t.r.values():
                self._need("sp", tok, False)

    def finish(self):
        for tok in self.out_tokens:
            self._need("sp", tok, False)


class Tile:
    def __init__(self, t, name):
        self.t = t
        self.k = Trk(name)

    def __getitem__(self, idx):
        return self.t[idx]


class Ctx:
    def __init__(self, nc):
        self.nc = nc
        self.P = Prog(nc)
        self.es = contextlib.ExitStack()
        self.nid = 0

    def sb(self, name, shape, dt, es=None):
        self.nid += 1
        nm = "%s_%d" % (name, self.nid)
        t = (es or self.es).enter_context(self.nc.sbuf_tensor(nm, list(shape), dt))
        return Tile(t, nm)

    def ps(self, name, shape, dt, es=None):
        self.nid += 1
        nm = "%s_%d" % (name, self.nid)
        t = (es or self.es).enter_context(self.nc.psum_tensor(nm, list(shape), dt))
        return Tile(t, nm)


def load_weight_cast(C, wt, dram_view, nsplit, axis):
    P = C.P
    n = wt.t.shape[axis]
    step = n // nsplit
    trks = []
    for j in range(nsplit):
        sl = [slice(None)] * 3
        sl[axis] = slice(j * step, (j + 1) * step)
        sl = tuple(sl)
        k = Trk("%s_p%d" % (wt.k.name, j))
        P.dma("pool", wt.t[sl], dram_view[sl], writes=[k])
        trks.append(k)
    return trks


def layer_norm_tile(C, S, y, g_rep, b_rep, out):
    P = C.P
    st, mv, rstd, nmr = S["ln_st"], S["ln_mv"], S["ln_rstd"], S["ln_nmr"]
    xn = y
    for hh in range(2):
        P.op("dve", lambda e, hh=hh: e.bn_stats(out=st[:, hh, :], in_=y[:, hh * 512:(hh + 1) * 512]),
             reads=[y.k], writes=[st.k])
    P.op("dve", lambda e: e.bn_aggr(out=mv[:], in_=st[:]), reads=[st.k], writes=[mv.k])
    P.op("act", lambda e: e.activation(out=rstd[:], in_=mv[:, 1:2], func=AF.Ln, bias=S["eps_ln"][:], scale=1.0),
         reads=[mv.k, S["eps_ln"].k], writes=[rstd.k])
    P.op("act", lambda e: e.activation(out=rstd[:], in_=rstd[:], func=AF.Exp, scale=-0.5),
         reads=[rstd.k], writes=[rstd.k])
    P.op("dve", lambda e: e.tensor_scalar(out=nmr[:], in0=mv[:, 0:1], scalar1=-1.0, scalar2=None, op0=ALU.mult),
         reads=[mv.k], writes=[nmr.k])
    P.op("dve", lambda e: e.scalar_tensor_tensor(out=xn[:], in0=y[:], scalar=nmr[:, 0:1], in1=g_rep[:],
                                                 op0=ALU.add, op1=ALU.mult),
         reads=[y.k, nmr.k, g_rep.k], writes=[xn.k])
    P.op("dve", lambda e: e.scalar_tensor_tensor(out=out[:], in0=xn[:], scalar=rstd[:, 0:1], in1=b_rep[:],
                                                 op0=ALU.mult, op1=ALU.add),
         reads=[xn.k, rstd.k, b_rep.k], writes=[out.k])


def transpose_tokens(C, S, xin, xT, col0):
    P = C.P
    xb = S["xb"][S["xb_i"] % 2]
    S["xb_i"] += 1
    P.op(S.get("cast_eng", "act"), (lambda e: e.tensor_copy(out=xb[:], in_=xin[:])) if S.get("cast_eng") else
         (lambda e: e.activation(out=xb[:], in_=xin[:], func=AF.Copy)), reads=[xin.k], writes=[xb.k])
    pt = S["pst"][S["pst_i"] % 2]
    S["pst_i"] += 1
    ident = S["ident"]
    P.pe_multi([lambda e, c=c: e.transpose(out=pt[:, c * 128:(c + 1) * 128], in_=xb[:, c * 128:(c + 1) * 128],
                                           identity=ident[:]) for c in range(8)],
               reads=[xb.k, ident.k], writes=[pt.k])
    P.op("dve", lambda e: e.tensor_copy(out=xT[:, :, col0:col0 + 128],
                                        in_=pt[:, :].rearrange("p (c t) -> p c t", c=8)),
         reads=[pt.k], writes=[xT.k])


def ffn_phase(C, S, x_dram, x_trks, out_dram, out_trks, w_up_d, w_down_d, g_d, b_d, is_output):
    nc, P = C.nc, C.P
    with contextlib.ExitStack() as es:
        wup = C.sb("wup", [128, 8, DFF], BF16, es)
        wdn = C.sb("wdn", [128, 32, D], BF16, es)
        g_rep = C.sb("g_rep", [128, D], F32, es)
        b_rep = C.sb("b_rep", [128, D], F32, es)
        xld = [C.sb("xld", [128, D], F32, es) for _ in range(3)]
        xT = C.sb("xT", [128, 8, ST], BF16, es)
        hT = C.sb("hT", [128, 32, ST], BF16, es)
        rl = [C.sb("rl", [128, ST], BF16, es) for _ in range(2)]
        y = [C.sb("y", [128, D], F32, es) for _ in range(2)]

        P.dma("sp", g_rep[:], g_d.partition_broadcast(128), writes=[g_rep.k])
        P.dma("sp", b_rep[:], b_d.partition_broadcast(128), writes=[b_rep.k])
        x_view = x_dram.rearrange("(s a p) d -> s a p d", a=4, p=128)
        o_view = out_dram.rearrange("(s a p) d -> s a p d", a=4, p=128)
        up_tr = load_weight_cast(C, wup, w_up_d.rearrange("(c p) f -> p c f", p=128), 8, 2)
        dn_tr = load_weight_cast(C, wdn, w_down_d.rearrange("(c p) f -> p c f", p=128), 8, 1)

        psb = S["psf"]
        nb = 0
        nl = 0
        for s in range(NST):
            for a in range(4):
                xi = xld[nl % 3]
                nl += 1
                P.dma("sp", xi[:], x_view[s, a], reads=[x_trks[s]], writes=[xi.k])
                transpose_tokens(C, S, xi, xT, a * 128)
            for fc in range(32):
                pb = psb[nb % 4]
                nb += 1
                P.mm(pb[:], [(wup[:, kc, fc * 128:(fc + 1) * 128], xT[:, kc, :]) for kc in range(8)],
                     reads=[xT.k, up_tr[fc // 4]], writes=[pb.k])
                r = rl[fc % 2]
                P.op("act", lambda e, r=r, pb=pb: e.activation(out=r[:], in_=pb[:], func=AF.Relu),
                     reads=[pb.k], writes=[r.k])
                P.op("dve", lambda e, r=r, fc=fc: e.tensor_tensor(out=hT[:, fc, :], in0=r[:], in1=r[:],
                                                                   op=ALU.mult),
                     reads=[r.k], writes=[hT.k])
            for a in range(4):
                xi = xld[nl % 3]
                nl += 1
                P.dma("sp", xi[:], x_view[s, a], reads=[x_trks[s]], writes=[xi.k])
                yy = y[a % 2]
                for dh in range(2):
                    pb = psb[nb % 4]
                    nb += 1
                    P.mm(pb[:], [(hT[:, fc, a * 128:(a + 1) * 128], wdn[:, fc, dh * 512:(dh + 1) * 512])
                                 for fc in range(32)],
                         reads=[hT.k] + dn_tr, writes=[pb.k])
                    P.op("dve", lambda e, yy=yy, pb=pb, dh=dh, xi=xi: e.scalar_tensor_tensor(
                        out=yy[:, dh * 512:(dh + 1) * 512], in0=xi[:, dh * 512:(dh + 1) * 512],
                        scalar=ALPHA, in1=pb[:], op0=ALU.mult, op1=ALU.add),
                         reads=[xi.k, pb.k], writes=[yy.k])
                layer_norm_tile(C, S, yy, g_rep, b_rep, yy)
                P.dma("pool", o_view[s, a], yy[:], reads=[yy.k], writes=[out_trks[s]],
                      is_output=is_output, sem_trk=yy.k)
        P.full_barrier()


def alloc_shared(C):
    S = {}
    S["ident"] = C.sb("ident", [128, 128], BF16)
    S["psf"] = [C.ps("psf", [128, 512], F32) for _ in range(6)]
    S["pst"] = [C.ps("pst", [128, 1024], BF16) for _ in range(2)]
    S["pst_i"] = 0
    S["nb"] = 0
    S["nrot"] = 6
    S["rot0"] = 0
    S["xb"] = [C.sb("xb", [128, D], BF16) for _ in range(2)]
    S["xb_i"] = 0
    S["ln_st"] = C.sb("ln_st", [128, 2, 6], F32)
    S["ln_mv"] = C.sb("ln_mv", [128, 2], F32)
    S["ln_rstd"] = C.sb("ln_rstd", [128, 1], F32)
    S["ln_nmr"] = C.sb("ln_nmr", [128, 1], F32)
    S["eps_ln"] = C.sb("eps_ln", [128, 1], F32)
    S["eps_rms"] = C.sb("eps_rms", [128, 1], F32)
    C.P.op("pool", lambda e: e.memset(S["eps_ln"][:], LN_EPS), writes=[S["eps_ln"].k])
    C.P.op("pool", lambda e: e.memset(S["eps_rms"][:], RMS_EPS), writes=[S["eps_rms"].k])
    return S


def nextbank(S):
    b = S["psf"][S["rot0"] + S["nb"] % S["nrot"]]
    S["nb"] += 1
    return b


def v3(ap, n):
    return ap.rearrange("p (h d) -> p h d", h=n)


def load_consts(C, S, consts_d):
    P = C.P
    S["cf"] = C.sb("cf", [128, 4, 128], F32)
    S["maskb"] = C.sb("maskb", [128, 128], BF16)
    P.dma("sp", S["cf"][:], consts_d, writes=[S["cf"].k])
    P.dma("pool", S["ident"][:], consts_d[:, 0, :], writes=[S["ident"].k])
    P.dma("pool", S["maskb"][:], consts_d[:, 1, :], writes=[S["maskb"].k])
    S["one1"] = C.sb("one1", [128, 1], F32)
    S["ln8"] = C.sb("ln8", [128, 1], F32)
    P.op("pool", lambda e: e.memset(S["one1"][:], 1.0), writes=[S["one1"].k])
    P.op("pool", lambda e: e.memset(S["ln8"][:], float(np.log(0.125))), writes=[S["ln8"].k])


def mem_kv_precompute(C, S, es, mem_d, wmk, wmk_tr, xld, KmT, Vm):
    P = C.P
    memT = C.sb("memT", [128, 8, 256], BF16, es)
    P.op("pool", lambda e: e.memset(Vm[:], 1.0), writes=[Vm.k])
    n = 0
    for q in range(NSEQ):
        for mt in range(2):
            xi = xld[n % len(xld)]
            n += 1
            P.dma("sp", xi[:], mem_d[q, mt * 128:(mt + 1) * 128, :], writes=[xi.k])
            transpose_tokens(C, S, xi, memT, mt * 128)
        for h in range(4):
            pb = nextbank(S)
            P.mm(pb[:, 0:256], [(wmk[:, kc, h * 128:(h + 1) * 128], memT[:, kc, :]) for kc in range(8)],
                 reads=[memT.k] + wmk_tr, writes=[pb.k])
            P.op("act", lambda e, pb=pb, q=q, h=h: e.activation(out=KmT[:, q, h, :], in_=pb[:, 0:256], func=AF.Copy),
                 reads=[pb.k], writes=[KmT.k])
        for mt in range(2):
            pb = nextbank(S)
            P.mm(pb[:], [(memT[:, kc, mt * 128:(mt + 1) * 128], wmk[:, kc, 512:1024]) for kc in range(8)],
                 reads=[memT.k] + wmk_tr, writes=[pb.k])
            P.op("act", lambda e, pb=pb, q=q, mt=mt: e.activation(out=Vm[:, q, mt, :, 0:128], in_=v3(pb[:], 4),
                                                                  func=AF.Copy),
                 reads=[pb.k], writes=[Vm.k])
    return memT


def mem_attention(C, S, seq, qmT, KmT, Vm, hcat, col0):
    P = C.P
    for h in range(4):
        pts = []
        for mt in range(2):
            pb = nextbank(S)
            P.mm(pb[:], [(KmT[:, seq, h, mt * 128:(mt + 1) * 128], qmT[:, h, :])],
                 reads=[KmT.k, qmT.k], writes=[pb.k])
            pt = S["PT"][S["pt_i"] % len(S["PT"])]
            S["pt_i"] += 1
            P.op("act", lambda e, pt=pt, pb=pb: e.activation(out=pt[:], in_=pb[:], func=AF.Exp, scale=128.0 ** -0.5),
                 reads=[pb.k], writes=[pt.k])
            pts.append(pt)
        for a in range(4):
            pb = nextbank(S)
            P.mm(pb[:, 0:129], [(pts[mt][:, a * 128:(a + 1) * 128], Vm[:, seq, mt, h, :]) for mt in range(2)],
                 reads=[pts[0].k, pts[1].k, Vm.k], writes=[pb.k])
            rc = S["rc"][S["rc_i"] % 2]
            S["rc_i"] += 1
            P.op("dve", lambda e, rc=rc, pb=pb: e.reciprocal(out=rc[:], in_=pb[:, 128:129]),
                 reads=[pb.k], writes=[rc.k])
            P.op("dve", lambda e, rc=rc, pb=pb, a=a, h=h: e.tensor_scalar(
                out=hcat[a][:, col0 + h * 128:col0 + (h + 1) * 128], in0=pb[:, 0:128], scalar1=rc[:, 0:1],
                scalar2=None, op0=ALU.mult), reads=[pb.k, rc.k], writes=[hcat[a].k])


def out_proj_ln(C, S, hcat, hcT, wout, wout_tr, xld, nl, x_view, x_trk, s, y, g_rep, b_rep, o_view, o_trk):
    P = C.P
    for a in range(4):
        pt = S["pst"][S["pst_i"] % 2]
        S["pst_i"] += 1
        ident = S["ident"]
        P.pe_multi([lambda e, c=c, pt=pt, a=a: e.transpose(out=pt[:, c * 128:(c + 1) * 128],
                                                         in_=hcat[a][:, c * 128:(c + 1) * 128],
                                                         identity=ident[:]) for c in range(8)],
                   reads=[hcat[a].k, ident.k], writes=[pt.k])
        P.op("dve", lambda e, pt=pt, a=a: e.tensor_copy(out=hcT[:, :, a * 128:(a + 1) * 128], in_=v3(pt[:, :], 8)),
             reads=[pt.k], writes=[hcT.k])
    for a in range(4):
        xi = xld[nl[0] % len(xld)]
        nl[0] += 1
        P.dma("sp", xi[:], x_view[s, a], reads=[x_trk], writes=[xi.k])
        yy = y[a % 2]
        for dh in range(2):
            pb = nextbank(S)
            P.mm(pb[:], [(hcT[:, kc, a * 128:(a + 1) * 128], wout[:, kc, dh * 512:(dh + 1) * 512])
                         for kc in range(8)], reads=[hcT.k] + wout_tr, writes=[pb.k])
            P.op("dve", lambda e, yy=yy, pb=pb, dh=dh, xi=xi: e.scalar_tensor_tensor(
                out=yy[:, dh * 512:(dh + 1) * 512], in0=xi[:, dh * 512:(dh + 1) * 512],
                scalar=ALPHA, in1=pb[:], op0=ALU.mult, op1=ALU.add),
                 reads=[xi.k, pb.k], writes=[yy.k])
        layer_norm_tile(C, S, yy, g_rep, b_rep, yy)
        P.dma("pool", o_view[s, a], yy[:], reads=[yy.k], writes=[o_trk], sem_trk=yy.k)


def mixer_a_phase(C, S, x_dram, x_trks, out_dram, out_trks, mem_d, w_in_d, bi_d, bf_d, wmk_d, w_out_d, g_d, b_d):
    nc, P = C.nc, C.P
    with contextlib.ExitStack() as es:
        win = C.sb("win", [128, 8, 2056], BF16, es)
        wout = C.sb("wout", [128, 8, D], BF16, es)
        g_rep = C.sb("g_rep", [128, D], F32, es)
        b_rep = C.sb("b_rep", [128, D], F32, es)
        bias_rep = C.sb("bias_rep", [128, 8], F32, es)
        xld = [C.sb("xld", [128, D], F32, es) for _ in range(3)]
        KmT = C.sb("KmT", [128, NSEQ, 4, 256], BF16, es)
        Vm = C.sb("Vm", [128, NSEQ, 2, 4, 129], BF16, es)
        P.dma("sp", g_rep[:], g_d.partition_broadcast(128), writes=[g_rep.k])
        P.dma("sp", b_rep[:], b_d.partition_broadcast(128), writes=[b_rep.k])
        P.dma("sp", bias_rep[:, 0:4], bi_d.partition_broadcast(128), writes=[bias_rep.k])
        P.dma("sp", bias_rep[:, 4:8], bf_d.partition_broadcast(128), writes=[bias_rep.k])
        with contextlib.ExitStack() as es2:
            wmk = C.sb("wmk", [128, 8, D], BF16, es2)
            wmk_tr = load_weight_cast(C, wmk, wmk_d.rearrange("(c p) f -> p c f", p=128), 2, 1)
            win_tr = load_weight_cast(C, win, w_in_d.rearrange("(c p) f -> p c f", p=128), 2, 1)
            wout_tr = load_weight_cast(C, wout, w_out_d.rearrange("(c p) f -> p c f", p=128), 2, 1)
            mem_kv_precompute(C, S, es2, mem_d, wmk, wmk_tr, xld, KmT, Vm)
            P.full_barrier()
        xT = [C.sb("xT", [128, 8, ST], BF16, es) for _ in range(2)]
        qT = [C.sb("qT", [64, 4, ST], BF16, es) for _ in range(2)]
        kT = [C.sb("kT", [64, 4, ST], BF16, es) for _ in range(2)]
        qz = [C.sb("qz", [64, 4, 4, 2, 128], BF16, es) for _ in range(2)]
        qmT = [C.sb("qmT", [128, 4, ST], BF16, es) for _ in range(2)]
        gts = C.sb("gts", [128, 8], F32, es)
        lfn = C.sb("lfn", [128, 4], F32, es)
        tadd = C.sb("tadd", [128, 4], F32, es)
        colf = C.sb("colf", [128, 4], F32, es)
        enb = C.sb("enb", [128, 4], F32, es)
        eg = [C.sb("eg", [128, 2, 4], F32, es) for _ in range(2)]
        kc_t = C.sb("kc", [128, 4, 64], BF16, es)
        vaug = [C.sb("vaug", [128, 4, 129], BF16, es) for _ in range(2)]
        e_o = C.sb("e_o", [128, 512], F32, es)
        atmp = C.sb("atmp", [128, 4, 128], BF16, es)
        AT = C.sb("AT", [128, 4, 128], BF16, es)
        Sst = C.sb("Sst", [64, 4, 129], F32, es)
        Cf = C.sb("Cf", [64, 4, 129], F32, es)
        Cb = [C.sb("Cb", [64, 4, 129], BF16, es) for _ in range(2)]
        den = C.sb("den", [128, 2, 1], F32, es)
        hcat2 = [[C.sb("hcat", [128, D], BF16, es) for _ in range(4)] for _ in range(2)]
        hcT = C.sb("hcT", [128, 8, ST], BF16, es)
        y = [C.sb("y", [128, D], F32, es) for _ in range(2)]
        S["PT"] = [C.sb("PT", [128, ST], BF16, es) for _ in range(4)]
        S["pt_i"] = 0
        S["rc"] = [C.sb("rc", [128, 1], F32, es) for _ in range(2)]
        S["rc_i"] = 0
        for qq in qz:
            P.op("pool", lambda e, qq=qq: e.memset(qq[:], 0.0), writes=[qq.k])
        for vv in vaug:
            P.op("pool", lambda e, vv=vv: e.memset(vv[:], 1.0), writes=[vv.k])

        x_view = x_dram.rearrange("(s a p) d -> s a p d", a=4, p=128)
        o_view = out_dram.rearrange("(s a p) d -> s a p d", a=4, p=128)
        cf = S["cf"]
        maskb = S["maskb"]
        nl = [0]
        NT = NST // NSEQ

        def front(s):
            b, seq = s % 2, s // NT
            xTb, qTb, kTb, qzb, qmTb = xT[b], qT[b], kT[b], qz[b], qmT[b]
            for a in range(4):
                xi = xld[nl[0] % len(xld)]
                nl[0] += 1
                P.dma("sp", xi[:], x_view[s, a], reads=[x_trks[s]], writes=[xi.k])
                transpose_tokens(C, S, xi, xTb, a * 128)
            for h in range(4):
                for (dst, c0) in ((qTb, 0), (kTb, 256)):
                    pb = nextbank(S)
                    P.mm(pb[0:64, :], [(win[:, kc, c0 + h * 64:c0 + (h + 1) * 64], xTb[:, kc, :]) for kc in range(8)],
                         reads=[xTb.k] + win_tr, writes=[pb.k])
                    P.op("act", lambda e, pb=pb, dst=dst, h=h: e.activation(out=dst[:, h, :], in_=pb[0:64, :],
                                                                            func=AF.Copy),
                         reads=[pb.k], writes=[dst.k])
                P.op("pool", lambda e, h=h: e.tensor_copy(
                    out=bass.AP(qzb.t, h * 1024, [[4096, 64], [256, 4], [192, 2], [1, 64]]),
                    in_=qTb[:, h, :].rearrange("p (a c j) -> p a c j", a=4, c=2)),
                     reads=[qTb.k], writes=[qzb.k])
                pb = nextbank(S)
                P.mm(pb[:], [(win[:, kc, 1544 + h * 128:1544 + (h + 1) * 128], xTb[:, kc, :]) for kc in range(8)],
                     reads=[xTb.k] + win_tr, writes=[pb.k])
                P.op("act", lambda e, pb=pb, h=h: e.activation(out=qmTb[:, h, :], in_=pb[:], func=AF.Copy),
                     reads=[pb.k], writes=[qmTb.k])
            mem_attention(C, S, seq, qmTb, KmT, Vm, hcat2[b], 512)

        def tail(s):
            out_proj_ln(C, S, hcat2[s % 2], hcT, wout, wout_tr, xld, nl, x_view, x_trks[s], s, y, g_rep, b_rep,
                        o_view, out_trks[s])

        def tok_loop(s):
            b = s % 2
            xTb, qTb, kTb, qzb, hcat = xT[b], qT[b], kT[b], qz[b], hcat2[b]
            for a in range(4):
                t = s * 4 + a
                par = t % 2
                first = (t % (SEQ // 128) == 0)
                cols = slice(a * 128, (a + 1) * 128)
                va = vaug[par]
                pg = nextbank(S)
                P.mm(pg[:, 0:8], [(xTb[:, kc, cols], win[:, kc, 1536:1544]) for kc in range(8)],
                     reads=[xTb.k] + win_tr, writes=[pg.k])
                P.op("dve", lambda e, pg=pg: e.tensor_tensor(out=gts[:], in0=pg[:, 0:8], in1=bias_rep[:], op=ALU.add),
                     reads=[pg.k, bias_rep.k], writes=[gts.k])
                P.op("act", lambda e: e.activation(out=lfn[:], in_=gts[:, 4:8], func=AF.Exp, scale=-1.0),
                     reads=[gts.k], writes=[lfn.k])
                P.op("act", lambda e: e.activation(out=lfn[:], in_=lfn[:], func=AF.Ln, bias=S["one1"][:], scale=1.0),
                     reads=[lfn.k, S["one1"].k], writes=[lfn.k])
                pc = nextbank(S)
                P.mm(pc[:, 0:4], [(cf[:, 1, :], lfn[:])], reads=[cf.k, lfn.k], writes=[pc.k])
                P.mm(pc[:, 4:8], [(cf[:, 2, :], lfn[:])], reads=[cf.k, lfn.k], writes=[pc.k])
                P.mm(pc[:, 8:12], [(cf[:, 3, :], lfn[:])], reads=[cf.k, lfn.k], writes=[pc.k])
                P.op("dve", lambda e, pc=pc: e.tensor_tensor(out=tadd[:], in0=gts[:, 0:4], in1=pc[:, 0:4], op=ALU.add),
                     reads=[gts.k, pc.k], writes=[tadd.k])
                P.op("act", lambda e: e.activation(out=colf[:], in_=tadd[:], func=AF.Exp, bias=S["ln8"][:], scale=1.0),
                     reads=[tadd.k, S["ln8"].k], writes=[colf.k])
                P.op("act", lambda e, pc=pc: e.activation(out=enb[:], in_=pc[:, 0:4], func=AF.Exp),
                     reads=[pc.k], writes=[enb.k])
                P.op("act", lambda e, pc=pc, par=par: e.activation(out=eg[par][:], in_=v3(pc[:, 4:12], 2),
                                                                   func=AF.Exp, scale=-1.0),
                     reads=[pc.k], writes=[eg[par].k])
                pk = nextbank(S)
                P.mm(pk[:, 0:256], [(xTb[:, kc, cols], win[:, kc, 256:512]) for kc in range(8)],
                     reads=[xTb.k] + win_tr, writes=[pk.k])
                P.op("dve", lambda e, pk=pk: e.tensor_tensor(
                    out=kc_t[:], in0=v3(pk[:, 0:256], 4), in1=colf[:, 0:4].unsqueeze(2).broadcast_to([128, 4, 64]),
                    op=ALU.mult), reads=[pk.k, colf.k], writes=[kc_t.k])
                pv = nextbank(S)
                P.mm(pv[:], [(xTb[:, kc, cols], win[:, kc, 512:1024]) for kc in range(8)],
                     reads=[xTb.k] + win_tr, writes=[pv.k])
                P.op("act", lambda e, pv=pv, va=va: e.activation(out=va[:, :, 0:128], in_=v3(pv[:], 4), func=AF.Copy),
                     reads=[pv.k], writes=[va.k])
                po = nextbank(S)
                P.mm(po[:], [(xTb[:, kc, cols], win[:, kc, 1024:1536]) for kc in range(8)],
                     reads=[xTb.k] + win_tr, writes=[po.k])
                P.op("act", lambda e, po=po: e.activation(out=e_o[:], in_=po[:], func=AF.Exp, scale=-1.0),
                     reads=[po.k], writes=[e_o.k])
                P.op("act", lambda e: e.activation(out=e_o[:], in_=e_o[:], func=AF.Ln, bias=S["one1"][:], scale=1.0),
                     reads=[e_o.k, S["one1"].k], writes=[e_o.k])
                P.op("act", lambda e: e.activation(out=e_o[:], in_=e_o[:], func=AF.Exp, scale=-1.0),
                     reads=[e_o.k], writes=[e_o.k])
                pa = nextbank(S)
                for h in range(4):
                    P.mm(pa[:, h * 128:(h + 1) * 128], [(kTb[:, h, cols], qTb[:, h, cols])],
                         reads=[kTb.k, qTb.k], writes=[pa.k])
                P.op("dve", lambda e, pa=pa: e.tensor_tensor(
                    out=atmp[:], in0=v3(pa[:], 4), in1=colf[:, 0:4].unsqueeze(2).broadcast_to([128, 4, 128]),
                    op=ALU.mult), reads=[pa.k, colf.k], writes=[atmp.k])
                P.op("pool", lambda e: e.tensor_tensor(
                    out=AT[:], in0=atmp[:], in1=maskb[:, :].unsqueeze(1).broadcast_to([128, 4, 128]), op=ALU.mult),
                     reads=[atmp.k, maskb.k], writes=[AT.k])
                for c in range(2):
                    if first and c == 0:
                        P.op("dve", lambda e: e.memset(Cf[:], 0.0), writes=[Cf.k])
                        P.op("pool", lambda e: e.memset(Cb[0][:], 0.0), writes=[Cb[0].k])
                    else:
                        egp = eg[par][0:64, 0, :] if c == 1 else eg[1 - par][0:64, 1, :]
                        egk = eg[par].k if c == 1 else eg[1 - par].k
                        P.op("dve", lambda e, egp=egp: e.tensor_tensor(
                            out=Cf[:], in0=Sst[:], in1=egp.unsqueeze(2).broadcast_to([64, 4, 129]), op=ALU.mult),
                             reads=[Sst.k, egk], writes=[Cf.k])
                        P.op("act", lambda e, c=c: e.activation(out=Cb[c][:], in_=Cf[:], func=AF.Copy),
                             reads=[Cf.k], writes=[Cb[c].k])
                    rows = slice(c * 64, (c + 1) * 64)
                    for hp in range(2):
                        pu = nextbank(S)
                        for j in range(2):
                            h = 2 * hp + j
                            P.mm(pu[0:64, j * 129:(j + 1) * 129], [(kc_t[rows, h, :], va[rows, h, :])],
                                 reads=[kc_t.k, va.k], writes=[pu.k])
                        P.op("dve", lambda e, pu=pu, hp=hp: e.tensor_tensor(
                            out=Sst[:, 2 * hp:2 * hp + 2, :], in0=Cf[:, 2 * hp:2 * hp + 2, :],
                            in1=v3(pu[0:64, 0:258], 2), op=ALU.add),
                             reads=[Cf.k, pu.k], writes=[Sst.k])
                for hp in range(2):
                    pn = nextbank(S)
                    for j in range(2):
                        h = 2 * hp + j
                        P.mm(pn[:, j * 129:(j + 1) * 129],
                             [(AT[:, h, :], va[:, h, :]),
                              (qzb[:, h, a, 0, :], Cb[0][:, h, :]),
                              (qzb[:, h, a, 1, :], Cb[1][:, h, :])],
                             reads=[AT.k, va.k, qzb.k, Cb[0].k, Cb[1].k], writes=[pn.k])
                    pn3 = v3(pn[:, 0:258], 2)
                    P.op("dve", lambda e, pn3=pn3, hp=hp: e.tensor_tensor(
                        out=den[:], in0=pn3[:, :, 128:129], in1=enb[:, 2 * hp:2 * hp + 2].unsqueeze(2),
                        op=ALU.max), reads=[pn.k, enb.k], writes=[den.k])
                    P.op("dve", lambda e, pn3=pn3: e.scalar_tensor_tensor(
                        out=den[:], in0=pn3[:, :, 128:129], scalar=-1.0, in1=den[:], op0=ALU.mult, op1=ALU.max),
                         reads=[pn.k, den.k], writes=[den.k])
                    P.op("dve", lambda e: e.reciprocal(out=den[:], in_=den[:]), reads=[den.k], writes=[den.k])
                    for j in range(2):
                        h = 2 * hp + j
                        P.op("dve", lambda e, pn=pn, j=j, h=h, a=a: e.scalar_tensor_tensor(
                            out=hcat[a][:, h * 128:(h + 1) * 128], in0=pn[:, j * 129:j * 129 + 128],
                            scalar=den[:, j, :], in1=e_o[:, h * 128:(h + 1) * 128], op0=ALU.mult, op1=ALU.mult),
                             reads=[pn.k, den.k, e_o.k], writes=[hcat[a].k])

        def side(fn, *a):
            S["rot0"], S["nrot"] = 4, 2
            fn(*a)
            S["rot0"], S["nrot"] = 0, 4

        side(front, 0)
        for s in range(NST):
            pending = []
            P.rec = pending
            if s > 0:
                side(tail, s - 1)
            if s + 1 < NST:
                side(front, s + 1)
            P.rec = None
            S["rot0"], S["nrot"] = 0, 4
            P.inter = (pending, len(pending) / 200.0 + 0.02)
            P._acc = 0.0
            tok_loop(s)
            P.inter = None
            while pending:
                P.replay(pending.pop(0))
        side(tail, NST - 1)
        P.full_barrier()
        S["rot0"], S["nrot"] = 0, 6


def rms_rows(C, S, pb, ncols, out_bf, scr):
    P = C.P
    ss, rs = S["rms_ss"], S["rms_rs"]
    if scr is None:
        scr = nextbank(S)
    P.op("dve", lambda e: e.memset(ss[:], 0.0), writes=[ss.k])
    P.op("act", lambda e: e.activation(out=scr[:, 0:ncols], in_=pb[:, 0:ncols], func=AF.Square, accum_out=ss[:]),
         reads=[pb.k], writes=[scr.k, ss.k])
    P.op("act", lambda e: e.activation(out=rs[:], in_=ss[:], func=AF.Ln, bias=S["eps_rms"][:], scale=1.0 / ncols),
         reads=[ss.k, S["eps_rms"].k], writes=[rs.k])
    P.op("act", lambda e: e.activation(out=rs[:], in_=rs[:], func=AF.Exp, scale=-0.5), reads=[rs.k], writes=[rs.k])
    P.op("dve", lambda e: e.tensor_scalar(out=out_bf[:, 0:ncols], in0=pb[:, 0:ncols], scalar1=rs[:, 0:1],
                                          scalar2=None, op0=ALU.mult), reads=[pb.k, rs.k], writes=[out_bf.k])


def transpose_cols(C, S, src_bf, nchunk, dstT, col0):
    P = C.P
    pt = S["pst"][S["pst_i"] % 2]
    S["pst_i"] += 1
    ident = S["ident"]
    P.pe_multi([lambda e, c=c: e.transpose(out=pt[:, c * 128:(c + 1) * 128], in_=src_bf[:, c * 128:(c + 1) * 128],
                                           identity=ident[:]) for c in range(nchunk)],
               reads=[src_bf.k, ident.k], writes=[pt.k])
    P.op("dve", lambda e: e.tensor_copy(out=dstT[:, 0:nchunk, col0:col0 + 128], in_=v3(pt[:, 0:nchunk * 128], nchunk)),
         reads=[pt.k], writes=[dstT.k])


def mixer_b_phase(C, S, x_dram, x_trks, out_dram, out_trks, mem_d, pos_d, wdown_d, gkv_d, wuk_d, wuv_d,
                  bwin_d, gq_d, wuq_d, wmk_d, w_out_d, g_d, b_d, cf2_d):
    nc, P = C.nc, C.P
    SC = 96.0 ** -0.5
    NT = NST // NSEQ
    with contextlib.ExitStack() as es:
        wdown = C.sb("wdown", [128, 8, 288], BF16, es)
        wdr = C.sb("wdr", [128, 8, 96], BF16, es)
        wdrot = C.sb("wdrot", [128, 8, 96], BF16, es)
        wuk = C.sb("wuk", [128, 2, 512], BF16, es)
        wuv = C.sb("wuv", [128, 2, 512], BF16, es)
        wuq = C.sb("wuq", [128, 2, 768], BF16, es)
        wuqrot = C.sb("wuqrot", [128, 2, 8, 96], BF16, es)
        gk = C.sb("gk", [128, 2], F32, es)
        gq = C.sb("gq", [128, 2], F32, es)
        bwin = C.sb("bwin", [128, 8, 768], BF16, es)
        wout = C.sb("wout", [128, 8, D], BF16, es)
        g_rep = C.sb("g_rep", [128, D], F32, es)
        b_rep = C.sb("b_rep", [128, D], F32, es)
        cf2 = C.sb("cf2", [128, 4], F32, es)
        xld = [C.sb("xld", [128, D], F32, es) for _ in range(2)]
        KmT = C.sb("KmT", [128, NSEQ, 4, 256], BF16, es)
        Vm = C.sb("Vm", [128, NSEQ, 2, 4, 129], BF16, es)
        P.dma("sp", g_rep[:], g_d.partition_broadcast(128), writes=[g_rep.k])
        P.dma("sp", b_rep[:], b_d.partition_broadcast(128), writes=[b_rep.k])
        P.dma("sp", cf2[:], cf2_d, writes=[cf2.k])
        for kc in range(2):
            P.dma("sp", gk[:, kc:kc + 1], gkv_d[kc * 128:(kc + 1) * 128].rearrange("(p o) -> p o", o=1), writes=[gk.k])
            P.dma("sp", gq[:, kc:kc + 1], gq_d[kc * 128:(kc + 1) * 128].rearrange("(p o) -> p o", o=1), writes=[gq.k])
        with contextlib.ExitStack() as es2:
            wmk = C.sb("wmk", [128, 8, D], BF16, es2)
            wst = C.sb("wst", [128, 2, 768], F32, es2)
            wmk_tr = load_weight_cast(C, wmk, wmk_d.rearrange("(c p) f -> p c f", p=128), 2, 1)
            wdown_tr = load_weight_cast(C, wdown, wdown_d.rearrange("(c p) f -> p c f", p=128), 1, 1)
            bwin_tr = load_weight_cast(C, bwin, bwin_d.rearrange("(c p) f -> p c f", p=128), 2, 1)
            wout_tr = load_weight_cast(C, wout, w_out_d.rearrange("(c p) f -> p c f", p=128), 2, 1)
            for (dst, src_d, gg, ncol) in ((wuk, wuk_d, gk, 512), (wuv, wuv_d, gk, 512), (wuq, wuq_d, gq, 768)):
                P.dma("sp", wst[:, :, 0:ncol], src_d.rearrange("(c p) f -> p c f", p=128), writes=[wst.k])
                for kc in range(2):
                    P.op("dve", lambda e, dst=dst, gg=gg, kc=kc, ncol=ncol: e.tensor_scalar(
                        out=dst[:, kc, :], in0=wst[:, kc, 0:ncol], scalar1=gg[:, kc:kc + 1], scalar2=None,
                        op0=ALU.mult), reads=[wst.k, gg.k], writes=[dst.k])
            mem_kv_precompute(C, S, es2, mem_d, wmk, wmk_tr, xld, KmT, Vm)
            P.full_barrier()
        xT = [C.sb("xT", [128, 8, ST], BF16, es) for _ in range(2)]
        uu = C.sb("uu", [96, ST], F32, es)
        u2 = C.sb("u2", [96, ST], F32, es)
        cosT = C.sb("cosT", [96, ST], F32, es)
        sinT = C.sb("sinT", [96, ST], F32, es)
        kT = C.sb("kT", [96, 8, SEQ], BF16, es)
        Vaug = C.sb("Vaug", [128, 16, 8, 65], BF16, es)
        kT_blk = [Trk("kTb%d" % i) for i in range(NT)]
        V_blk = [Trk("Vb%d" % i) for i in range(NT)]
        ckn = C.sb("ckn", [128, 256], BF16, es)
        ckT = [C.sb("ckT", [128, 2, ST], BF16, es) for _ in range(2)]
        cqT = [C.sb("cqT", [128, 2, ST], BF16, es) for _ in range(2)]
        rt1 = C.sb("rt1", [96, ST], F32, es)
        rt2 = C.sb("rt2", [96, ST], F32, es)
        kr = C.sb("kr", [96, ST], BF16, es)
        qTh = [C.sb("qTh", [96, 8, ST], BF16, es) for _ in range(2)]
        qmT = [C.sb("qmT", [128, 4, ST], BF16, es) for _ in range(2)]
        hc_all = C.sb("hc_all", [128, 4, D], BF16, es)
        y = [C.sb("y", [128, D], F32, es) for _ in range(2)]
        scr = None
        rc4 = C.sb("rc4", [128, 4, 1], F32, es)
        S["PT"] = [C.sb("PT", [128, ST], BF16, es) for _ in range(3)]
        S["pt_i"] = 0
        S["rc"] = [C.sb("rc", [128, 1], F32, es) for _ in range(2)]
        S["rc_i"] = 0
        S["rms_ss"] = C.sb("rms_ss", [128, 1], F32, es)
        S["rms_rs"] = C.sb("rms_rs", [128, 1], F32, es)
        hcat = []
        for a in range(4):
            v = Tile(hc_all.t[:, a, :], "hcv")
            v.k = hc_all.k
            hcat.append(v)

        P.op("pool", lambda e: e.memset(wdr[:], 0.0), writes=[wdr.k])
        P.op("pool", lambda e: e.memset(wdrot[:], 0.0), writes=[wdrot.k])
        P.op("pool", lambda e: e.memset(wuqrot[:], 0.0), writes=[wuqrot.k])
        P.op("pool", lambda e: e.tensor_copy(out=wdr[:, :, 64:96], in_=wdown[:, :, 256:288]),
             reads=wdown_tr, writes=[wdr.k])
        P.op("pool", lambda e: e.tensor_scalar(out=wdrot[:, :, 64:80], in0=wdown[:, :, 272:288], scalar1=-1.0,
                                               scalar2=None, op0=ALU.mult), reads=wdown_tr, writes=[wdrot.k])
        P.op("pool", lambda e: e.tensor_copy(out=wdrot[:, :, 80:96], in_=wdown[:, :, 256:272]),
             reads=wdown_tr, writes=[wdrot.k])
        wuq4 = wuq.t[:, :, :].rearrange("p c (h d) -> p c h d", h=8)
        P.op("pool", lambda e: e.tensor_scalar(out=wuqrot[:, :, :, 64:80], in0=wuq4[:, :, :, 80:96], scalar1=-1.0,
                                               scalar2=None, op0=ALU.mult), reads=[wuq.k], writes=[wuqrot.k])
        P.op("pool", lambda e: e.tensor_copy(out=wuqrot[:, :, :, 80:96], in_=wuq4[:, :, :, 64:80]),
             reads=[wuq.k], writes=[wuqrot.k])
        P.op("pool", lambda e: e.memset(Vaug[:], 1.0), writes=V_blk)
        S["rot0"], S["nrot"] = 4, 2
        S["cast_eng"] = "pool"
        sbank = S["psf"][0:2]
        sb_i = [0]

        x_view = x_dram.rearrange("(s a p) d -> s a p d", a=4, p=128)
        o_view = out_dram.rearrange("(s a p) d -> s a p d", a=4, p=128)
        nl = [0]
        R = slice(64, 96)

        def front_chunks(s):
            seq, T, b = s // NT, s % NT, s % 2
            tcols = slice(T * ST, (T + 1) * ST)
            xTb, ckTb, cqTb, qThb, qmTb = xT[b], ckT[b], cqT[b], qTh[b], qmT[b]
            ch = []

            def rope_tables():
                posi_ap = rt2[R, :].bitcast(I32)
                P.dma("sp", posi_ap, pos_d[seq, T * ST:(T + 1) * ST].partition_broadcast(32), writes=[rt2.k])
                P.op("dve", lambda e: e.tensor_copy(out=uu[R, :], in_=posi_ap), reads=[rt2.k], writes=[uu.k])
                for (dstT, shift) in ((sinT, 0.0), (cosT, 0.25)):
                    P.op("dve", lambda e, shift=shift: e.tensor_scalar(
                        out=u2[R, :], in0=uu[R, :], scalar1=cf2[R, 0:1], scalar2=shift, op0=ALU.mult, op1=ALU.add),
                         reads=[uu.k, cf2.k], writes=[u2.k])
                    P.op("dve", lambda e: e.tensor_copy(out=posi_ap, in_=u2[R, :]), reads=[u2.k], writes=[rt2.k])
                    P.op("dve", lambda e: e.tensor_copy(out=rt1[R, :], in_=posi_ap), reads=[rt2.k], writes=[rt1.k])
                    P.op("dve", lambda e: e.tensor_tensor(out=u2[R, :], in0=u2[R, :], in1=rt1[R, :], op=ALU.subtract),
                         reads=[u2.k, rt1.k], writes=[u2.k])
                    P.op("dve", lambda e: e.tensor_scalar(out=rt1[R, :], in0=u2[R, :], scalar1=0.5, scalar2=None,
                                                          op0=ALU.is_gt), reads=[u2.k], writes=[rt1.k])
                    P.op("dve", lambda e: e.tensor_tensor(out=u2[R, :], in0=u2[R, :], in1=rt1[R, :], op=ALU.subtract),
                         reads=[u2.k, rt1.k], writes=[u2.k])
                    P.op("act", lambda e, dstT=dstT: e.activation(out=dstT[R, :], in_=u2[R, :], func=AF.Sin,
                                                                  scale=float(2 * np.pi)),
                         reads=[u2.k], writes=[dstT.k])
            xis = {}

            def load_dma(a):
                xi = xld[nl[0] % len(xld)]
                nl[0] += 1
                xis[a] = xi
                P.dma("sp", xi[:], x_view[s, a], reads=[x_trks[s]], writes=[xi.k])

            def load_tr(a):
                transpose_tokens(C, S, xis[a], xTb, a * 128)

            def latents(a):
                cols = slice(a * 128, (a + 1) * 128)
                for (wt, wtr, dstT) in ((wdown, wdown_tr, ckTb), (bwin, bwin_tr, cqTb)):
                    pb = nextbank(S)
                    P.mm(pb[:, 0:256], [(xTb[:, kc, cols], wt[:, kc, 0:256]) for kc in range(8)],
                         reads=[xTb.k] + wtr, writes=[pb.k])
                    rms_rows(C, S, pb, 256, ckn, scr)
                    transpose_cols(C, S, ckn, 2, dstT, a * 128)
            ch.append(lambda: load_dma(0))
            ch.append(lambda: load_dma(1))
            ch.append(rope_tables)
            ch.append(lambda: load_tr(0))
            ch.append(lambda: load_dma(2))
            ch.append(lambda: load_tr(1))
            ch.append(lambda: load_dma(3))
            ch.append(lambda: latents(0))
            ch.append(lambda: load_tr(2))
            ch.append(lambda: latents(1))
            ch.append(lambda: load_tr(3))
            ch.append(lambda: latents(2))
            ch.append(lambda: latents(3))

            def k_nope(h0):
                for h in range(h0, h0 + 4):
                    pb = nextbank(S)
                    P.mm(pb[0:64, :], [(wuk[:, kc, h * 64:(h + 1) * 64], ckTb[:, kc, :]) for kc in range(2)],
                         reads=[wuk.k, ckTb.k], writes=[pb.k])
                    P.op("dve", lambda e, pb=pb, h=h: e.tensor_copy(out=kT[0:64, h, tcols], in_=pb[0:64, :]),
                         reads=[pb.k], writes=[kT_blk[T]])
            ch.append(lambda: k_nope(0))
            ch.append(lambda: k_nope(4))

            def k_rope():
                pA, pB = nextbank(S), nextbank(S)
                P.mm(pA[0:96, :], [(wdr[:, kc, :], xTb[:, kc, :]) for kc in range(8)], reads=[wdr.k, xTb.k],
                     writes=[pA.k])
                P.mm(pB[0:96, :], [(wdrot[:, kc, :], xTb[:, kc, :]) for kc in range(8)], reads=[wdrot.k, xTb.k],
                     writes=[pB.k])
                P.op("dve", lambda e: e.tensor_tensor(out=rt1[R, :], in0=pA[R, :], in1=cosT[R, :], op=ALU.mult),
                     reads=[pA.k, cosT.k], writes=[rt1.k])
                P.op("dve", lambda e: e.tensor_tensor(out=rt2[R, :], in0=pB[R, :], in1=sinT[R, :], op=ALU.mult),
                     reads=[pB.k, sinT.k], writes=[rt2.k])
                P.op("dve", lambda e: e.tensor_tensor(out=kr[R, :], in0=rt1[R, :], in1=rt2[R, :], op=ALU.add),
                     reads=[rt1.k, rt2.k], writes=[kr.k])
                P.op("dve", lambda e: e.tensor_copy(out=kT[R, :, tcols],
                                                    in_=kr[R, :].unsqueeze(1).broadcast_to([32, 8, ST])),
                     reads=[kr.k], writes=[kT_blk[T]])
            ch.append(k_rope)

            def v_tiles():
                for a in range(4):
                    pb = nextbank(S)
                    P.mm(pb[:], [(ckTb[:, kc, a * 128:(a + 1) * 128], wuv[:, kc, :]) for kc in range(2)],
                         reads=[ckTb.k, wuv.k], writes=[pb.k])
                    P.op("act", lambda e, pb=pb, a=a: e.activation(out=Vaug[:, 4 * T + a, :, 0:64], in_=v3(pb[:], 8),
                                                                   func=AF.Copy), reads=[pb.k], writes=[V_blk[T]])
            ch.append(v_tiles)

            def queries(h0):
                for h in range(h0, h0 + 2):
                    pA, pB = nextbank(S), nextbank(S)
                    P.mm(pA[0:96, :], [(wuq[:, kc, h * 96:(h + 1) * 96], cqTb[:, kc, :]) for kc in range(2)],
                         reads=[wuq.k, cqTb.k], writes=[pA.k])
                    P.mm(pB[0:96, :], [(wuqrot[:, kc, h, :], cqTb[:, kc, :]) for kc in range(2)],
                         reads=[wuqrot.k, cqTb.k], writes=[pB.k])
                    P.op("dve", lambda e, pA=pA, h=h: e.tensor_copy(out=qThb[0:64, h, :], in_=pA[0:64, :]),
                         reads=[pA.k], writes=[qThb.k])
                    P.op("dve", lambda e, pA=pA: e.tensor_tensor(out=rt1[R, :], in0=pA[R, :], in1=cosT[R, :],
                                                                 op=ALU.mult), reads=[pA.k, cosT.k], writes=[rt1.k])
                    P.op("dve", lambda e, pB=pB: e.tensor_tensor(out=rt2[R, :], in0=pB[R, :], in1=sinT[R, :],
                                                                 op=ALU.mult), reads=[pB.k, sinT.k], writes=[rt2.k])
                    P.op("dve", lambda e, h=h: e.tensor_tensor(out=qThb[R, h, :], in0=rt1[R, :], in1=rt2[R, :],
                                                               op=ALU.add), reads=[rt1.k, rt2.k], writes=[qThb.k])
            for h0 in range(0, 8, 2):
                ch.append(lambda h0=h0: queries(h0))

            def q_mem():
                for h in range(4):
                    pb = nextbank(S)
                    P.mm(pb[:], [(bwin[:, kc, 256 + h * 128:256 + (h + 1) * 128], xTb[:, kc, :]) for kc in range(8)],
                         reads=[xTb.k] + bwin_tr, writes=[pb.k])
                    P.op("act", lambda e, pb=pb, h=h: e.activation(out=qmTb[:, h, :], in_=pb[:], func=AF.Copy),
                         reads=[pb.k], writes=[qmTb.k])
            ch.append(q_mem)
            return ch

        def mla_head(s, h, pending, per):
            T, b = s % NT, s % 2
            qThb = qTh[b]
            nkt = 4 * T + 4
            po = S["psf"][2 + (h % 2)]
            started = [False]

            def emit_front(j):
                a_min = max(0, j - 4 * T)
                qc = slice(a_min * 128, ST)
                pb = sbank[sb_i[0] % 2]
                sb_i[0] += 1
                P.mm(pb[:, qc], [(kT[:, h, j * 128:(j + 1) * 128], qThb[:, h, qc])],
                     reads=[kT_blk[j // 4], qThb.k], writes=[pb.k])
                pt = S["PT"][S["pt_i"] % len(S["PT"])]
                S["pt_i"] += 1
                P.op("act", lambda e: e.activation(out=pt[:, qc], in_=pb[:, qc], func=AF.Exp, scale=SC),
                     reads=[pb.k], writes=[pt.k])
                if j >= 4 * T:
                    P.op("pool", lambda e: e.memset(pt[64:128, a_min * 128:a_min * 128 + 64], 0.0),
                         reads=[], writes=[pt.k])
                return (j, a_min, pt)

            def emit_back(j, a_min, pt):
                fns = []
                for a in range(a_min, 4):
                    st = not started[0]
                    started[0] = True
                    fns.append(lambda e, a=a, st=st: e.matmul(
                        po[:, a * 65:(a + 1) * 65], pt[:, a * 128:(a + 1) * 128], Vaug[:, j, h, :],
                        start=st, stop=(j == 4 * T + a), skip_group_check=True))
                P.pe_multi(fns, reads=[pt.k, V_blk[j // 4]], writes=[po.k])

            prev = None
            for j in range(nkt):
                cur = emit_front(j)
                if prev is not None:
                    emit_back(*prev)
                prev = cur
                for _ in range(per):
                    if pending:
                        P.replay(pending.pop(0))
            emit_back(*prev)
            po3 = v3(po[:, 0:260], 4)
            P.op("dve", lambda e: e.reciprocal(out=rc4[:], in_=po3[:, :, 64:65]), reads=[po.k], writes=[rc4.k])
            P.op("dve", lambda e: e.tensor_tensor(
                out=hc_all[:, :, h * 64:(h + 1) * 64], in0=po3[:, :, 0:64],
                in1=rc4[:, :, 0:1].broadcast_to([128, 4, 64]), op=ALU.mult),
                 reads=[po.k, rc4.k], writes=[hc_all.k])

        for f in front_chunks(0):
            f()
        for s in range(NST):
            seq, b = s // NT, s % 2
            pending = []
            if s + 1 < NST:
                P.rec = pending
                for f in front_chunks(s + 1):
                    f()
                P.rec = None
            mem_attention(C, S, seq, qmT[b], KmT, Vm, hcat, 512)
            nsteps = 8 * (4 * (s % NT) + 4)
            per = (len(pending) + nsteps - 1) // nsteps
            for h in range(8):
                mla_head(s, h, pending, per)
            while pending:
                P.replay(pending.pop(0))
            out_proj_ln(C, S, hcat, xT[b], wout, wout_tr, xld, nl, x_view, x_trks[s], s, y, g_rep, b_rep,
                        o_view, out_trks[s])
        P.full_barrier()
        S["rot0"], S["nrot"] = 0, 6
        S.pop("cast_eng")


def _consts_np():
    c = np.zeros((128, 4, 128), np.float32)
    c[:, 0, :] = np.eye(128, dtype=np.float32)
    s = np.arange(128)[:, None]
    l = np.arange(128)[None, :]
    c[:, 1, :] = ((s // 64 == l // 64) & (s <= l)).astype(np.float32)
    c[:, 2, :] = (s < 64).astype(np.float32) * np.ones((1, 128), np.float32)
    c[:, 3, :] = (s >= 64).astype(np.float32) * np.ones((1, 128), np.float32)
    return c


def _cf2_np():
    c = np.zeros((128, 4), np.float32)
    inv = (10000.0 ** (-np.arange(0, 32, 2, dtype=np.float32) / 32)).astype(np.float32)
    for p in range(64, 96):
        c[p, 0] = inv[(p - 64) % 16] / np.float32(2 * np.pi)
    c[:, 1] = -np.pi
    return c


W_SHAPES = {
    "a_w_in": [D, 2056], "a_b_igate": [4], "a_b_fgate": [4], "a_w_mem_kv": [D, D], "a_w_out": [D, D],
    "kv_w_down": [D, 288], "kv_norm_g": [256], "kv_w_uk": [256, 512], "kv_w_uv": [256, 512],
    "b_w_in": [D, 768], "b_q_norm_g": [256], "b_w_uq": [256, 768], "b_w_mem_kv": [D, D], "b_w_out": [D, D],
    "ln1_g": [2, D], "ln1_b": [2, D], "ffn_w_up": [2, D, DFF], "ffn_w_down": [2, DFF, D], "ln2_g": [2, D],
    "ln2_b": [2, D],
}


def build_program():
    nc = bass.Bass("TRN2", target_bir_lowering=False)
    dt = lambda n, sh: nc.dram_tensor(n, sh, F32, kind="ExternalInput").ap()
    x = dt("x", [NTOK, D])
    mem = dt("mem", [NSEQ, 256, D])
    pos = nc.dram_tensor("positions", [NSEQ, SEQ], I32, kind="ExternalInput").ap()
    w = {k: dt(k, sh) for k, sh in W_SHAPES.items()}
    consts = dt("consts", [128, 4, 128])
    cf2 = dt("cf2", [128, 4])
    out = nc.dram_tensor("out", [NTOK, D], F32, kind="ExternalOutput").ap()
    sc1 = nc.dram_tensor("scratch1", [NTOK, D], F32, kind="Internal").ap()
    sc2 = nc.dram_tensor("scratch2", [NTOK, D], F32, kind="Internal").ap()
    C = Ctx(nc)
    with nc.allow_low_precision("bf16 matmul operands, fp32 accumulation"), C.es:
        S = alloc_shared(C)
        load_consts(C, S, consts)
        xtr = [Trk("xd%d" % i) for i in range(NST)]
        t1 = [Trk("s1_%d" % i) for i in range(NST)]
        t2 = [Trk("s2_%d" % i) for i in range(NST)]
        otr = [Trk("od%d" % i) for i in range(NST)]
        mixer_a_phase(C, S, x, xtr, sc1, t1, mem, w["a_w_in"], w["a_b_igate"], w["a_b_fgate"], w["a_w_mem_kv"],
                      w["a_w_out"], w["ln1_g"][0], w["ln1_b"][0])
        ffn_phase(C, S, sc1, t1, sc2, t2, w["ffn_w_up"][0], w["ffn_w_down"][0], w["ln2_g"][0], w["ln2_b"][0], False)
        mixer_b_phase(C, S, sc2, t2, sc1, t1, mem, pos, w["kv_w_down"], w["kv_norm_g"], w["kv_w_uk"], w["kv_w_uv"],
                      w["b_w_in"], w["b_q_norm_g"], w["b_w_uq"], w["b_w_mem_kv"], w["b_w_out"], w["ln1_g"][1],
                      w["ln1_b"][1], cf2)
        ffn_phase(C, S, sc1, t1, out, otr, w["ffn_w_up"][1], w["ffn_w_down"][1], w["ln2_g"][1], w["ln2_b"][1], True)
        C.P.finish()
    return nc


def kernel(x, mem, positions, a_w_in, a_b_igate, a_b_fgate, a_w_mem_kv, a_w_out, kv_w_down, kv_norm_g, kv_w_uk,
           kv_w_uv, b_w_in, b_q_norm_g, b_w_uq, b_w_mem_kv, b_w_out, ln1_g, ln1_b, ffn_w_up, ffn_w_down, ln2_g,
           ln2_b):
    f32 = lambda a: np.ascontiguousarray(np.asarray(a), dtype=np.float32)
    shared = {
        "a_w_in": f32(a_w_in)[0], "a_b_igate": f32(a_b_igate)[0], "a_b_fgate": f32(a_b_fgate)[0],
        "a_w_mem_kv": f32(a_w_mem_kv)[0], "a_w_out": f32(a_w_out)[0], "kv_w_down": f32(kv_w_down),
        "kv_norm_g": f32(kv_norm_g), "kv_w_uk": f32(kv_w_uk), "kv_w_uv": f32(kv_w_uv), "b_w_in": f32(b_w_in)[0],
        "b_q_norm_g": f32(b_q_norm_g)[0], "b_w_uq": f32(b_w_uq)[0], "b_w_mem_kv": f32(b_w_mem_kv)[0],
        "b_w_out": f32(b_w_out)[0], "ln1_g": f32(ln1_g), "ln1_b": f32(ln1_b), "ffn_w_up": f32(ffn_w_up),
        "ffn_w_down": f32(ffn_w_down), "ln2_g": f32(ln2_g), "ln2_b": f32(ln2_b),
        "consts": _consts_np(), "cf2": _cf2_np(),
    }
    shared = {k: np.ascontiguousarray(v) for k, v in shared.items()}
    x = f32(x)
    mem = f32(mem)
    positions = np.ascontiguousarray(np.asarray(positions), dtype=np.int32)
    in_maps = []
    for c in range(NCORES):
        m = dict(shared)
        m["x"] = np.ascontiguousarray(x[c * NSEQ:(c + 1) * NSEQ].reshape(NTOK, D))
        m["mem"] = np.ascontiguousarray(mem[c * NSEQ:(c + 1) * NSEQ])
        m["positions"] = np.ascontiguousarray(positions[c * NSEQ:(c + 1) * NSEQ])
        in_maps.append(m)
    nc = build_program()
    res = run_bass_kernel_spmd(nc, in_maps, core_ids=list(range(NCORES)))
    outs = [np.asarray(r["out"], dtype=np.float32).reshape(NSEQ, SEQ, D) for r in res.results]
    return np.concatenate(outs, axis=0)
```

```python
import contextlib
import numpy as np
import concourse.bass as bass
import concourse.mybir as mybir
from concourse.bass_utils import run_bass_kernel_spmd

F32 = mybir.dt.float32
BF16 = mybir.dt.bfloat16
I32 = mybir.dt.int32
AF = mybir.ActivationFunctionType
ALU = mybir.AluOpType
AX = mybir.AxisListType

NCORES = 8
SEQ = 2048
D = 1024
DFF = 4096
NSEQ = 2
NTOK = NSEQ * SEQ
ST = 512
NST = NTOK // ST
ALPHA = 4.0 ** 0.25
LN_EPS = 1e-5
RMS_EPS = 1e-6


class Trk:
    __slots__ = ("name", "w", "r", "dsem", "dcnt")

    def __init__(self, name):
        self.name = name
        self.w = None
        self.r = {}
        self.dsem = None
        self.dcnt = 0


class Prog:
    SEM_ROT = 12000

    def __init__(self, nc):
        self.nc = nc
        self.eng = {"pe": nc.tensor, "act": nc.scalar, "dve": nc.vector, "pool": nc.gpsimd,
                    "sp": nc.sync}
        self.sem = {}
        self.cnt = {}
        self.seen = {e: {} for e in self.eng}
        self.nsem = 0
        for e in self.eng:
            self._new_sem(e)
        self.out_tokens = []
        self.ninstr = 0
        self.last_tok = {}
        self.rec = None
        self.inter = None
        self._acc = 0.0
        self._in_replay = False
        self.dma_toks = {}
        self.free_dsems = []
        self.phase_trks = []

    def _alloc_sem(self, name):
        self.nsem += 1
        return self.nc.alloc_semaphore(name="%s_%d" % (name, self.nsem))

    def _new_sem(self, e):
        self.sem[e] = self._alloc_sem("s_" + e)
        self.cnt[e] = 0

    def _need(self, e, tok, skip_same):
        if tok is None:
            return
        sem, c, te = tok
        if skip_same and te == e and e == "pe":
            return
        if self.seen[e].get(sem, 0) >= c:
            return
        self.eng[e].wait_ge(sem, c)
        self.seen[e][sem] = c

    def _signal(self, e, ins):
        if self.cnt[e] >= self.SEM_ROT:
            self._new_sem(e)
        self.cnt[e] += 1
        ins.then_inc(self.sem[e], 1)
        self.last_tok[e] = (self.sem[e], self.cnt[e], e)
        return self.last_tok[e]

    def full_barrier(self):
        snap = dict(self.last_tok)
        dts = list(self.dma_toks.values())
        for e in self.eng:
            for o, tok in snap.items():
                if o != e:
                    self._need(e, tok, False)
            for tok in dts:
                self._need(e, tok, False)
        for t in self.phase_trks:
            self.free_dsems.append((t.dsem, t.dcnt))
            t.dsem = None
        self.phase_trks = []
        self.dma_toks = {}

    def _after_emit(self):
        if self.inter is None or self._in_replay:
            return
        pend, rate = self.inter
        self._acc += rate
        while self._acc >= 1.0 and pend:
            self._acc -= 1.0
            self._in_replay = True
            self.replay(pend.pop(0))
            self._in_replay = False

    def replay(self, item):
        kind, args, kw = item
        saved, self.rec = self.rec, None
        getattr(self, kind)(*args, **kw)
        self.rec = saved

    def op(self, e, fn, reads=(), writes=()):
        if self.rec is not None:
            self.rec.append(("op", (e, fn), dict(reads=list(reads), writes=list(writes))))
            return None
        for t in reads:
            self._need(e, t.w, False)
        for t in writes:
            self._need(e, t.w, True)
            for tok in t.r.values():
                self._need(e, tok, True)
        ins = fn(self.eng[e])
        tok = self._signal(e, ins)
        for t in writes:
            t.w = tok
            t.r = {}
        for t in reads:
            t.r[e] = tok
        self.ninstr += 1
        self._after_emit()
        return tok

    def mm(self, out, pairs, reads=(), writes=()):
        if self.rec is not None:
            self.rec.append(("mm", (out, list(pairs)), dict(reads=list(reads), writes=list(writes))))
            return None
        e = "pe"
        for t in reads:
            self._need(e, t.w, False)
        for t in writes:
            self._need(e, t.w, True)
            for tok in t.r.values():
                self._need(e, tok, True)
        n = len(pairs)
        ins = None
        for i, (l, r) in enumerate(pairs):
            ins = self.nc.tensor.matmul(out, l, r, start=(i == 0), stop=(i == n - 1))
        tok = self._signal(e, ins)
        for t in writes:
            t.w = tok
            t.r = {}
        for t in reads:
            t.r[e] = tok
        self.ninstr += n
        self._after_emit()
        return tok

    def pe_multi(self, fns, reads=(), writes=()):
        if self.rec is not None:
            self.rec.append(("pe_multi", (list(fns),), dict(reads=list(reads), writes=list(writes))))
            return None
        e = "pe"
        for t in reads:
            self._need(e, t.w, False)
        for t in writes:
            self._need(e, t.w, True)
            for tok in t.r.values():
                self._need(e, tok, True)
        ins = None
        for f in fns:
            ins = f(self.nc.tensor)
        tok = self._signal(e, ins)
        for t in writes:
            t.w = tok
            t.r = {}
        for t in reads:
            t.r[e] = tok
        self.ninstr += len(fns)
        self._after_emit()
        return tok

    def dma(self, q, out, in_, reads=(), writes=(), is_output=False, sem_trk=None):
        if self.rec is not None:
            self.rec.append(("dma", (q, out, in_), dict(reads=list(reads), writes=list(writes),
                                                        is_output=is_output, sem_trk=sem_trk)))
            return None
        e = q
        for t in reads:
            self._need(e, t.w, False)
        for t in writes:
            self._need(e, t.w, False)
            for tok in t.r.values():
                self._need(e, tok, False)
        trk = sem_trk if sem_trk is not None else (list(writes) + list(reads))[0]
        if trk.dsem is None:
            if self.free_dsems:
                trk.dsem, trk.dcnt = self.free_dsems.pop()
            else:
                trk.dsem = self._alloc_sem("d")
                trk.dcnt = 0
            self.phase_trks.append(trk)
        trk.dcnt += 16
        self.eng[e].dma_start(out=out, in_=in_).then_inc(trk.dsem, 16)
        tok = (trk.dsem, trk.dcnt, "dma")
        self.dma_toks[trk.dsem] = tok
        for t in writes:
            t.w = tok
            t.r = {}
        for t in reads:
            t.r["dma_%s" % trk.name] = tok
        if is_output:
            self.out_tokens.append(tok)
        self.ninstr += 1
        return tok

    def barrier_all(self, trks):
        for t in trks:
            self._need("sp", t.w, False)
            for tok in t.r.values():
                self._need("sp", tok, False)

    def finish(self):
        for tok in self.out_tokens:
            self._need("sp", tok, False)


class Tile:
    def __init__(self, t, name):
        self.t = t
        self.k = Trk(name)

    def __getitem__(self, idx):
        return self.t[idx]


class Ctx:
    def __init__(self, nc):
        self.nc = nc
        self.P = Prog(nc)
        self.es = contextlib.ExitStack()
        self.nid = 0

    def sb(self, name, shape, dt, es=None):
        self.nid += 1
        nm = "%s_%d" % (name, self.nid)
        t = (es or self.es).enter_context(self.nc.sbuf_tensor(nm, list(shape), dt))
        return Tile(t, nm)

    def ps(self, name, shape, dt, es=None):
        self.nid += 1
        nm = "%s_%d" % (name, self.nid)
        t = (es or self.es).enter_context(self.nc.psum_tensor(nm, list(shape), dt))
        return Tile(t, nm)


def load_weight_cast(C, wt, dram_view, nsplit, axis):
    P = C.P
    n = wt.t.shape[axis]
    step = n // nsplit
    trks = []
    for j in range(nsplit):
        sl = [slice(None)] * 3
        sl[axis] = slice(j * step, (j + 1) * step)
        sl = tuple(sl)
        k = Trk("%s_p%d" % (wt.k.name, j))
        P.dma("pool", wt.t[sl], dram_view[sl], writes=[k])
        trks.append(k)
    return trks


def layer_norm_tile(C, S, y, g_rep, b_rep, out):
    P = C.P
    st, mv, rstd, nmr = S["ln_st"], S["ln_mv"], S["ln_rstd"], S["ln_nmr"]
    xn = y
    for hh in range(2):
        P.op("dve", lambda e, hh=hh: e.bn_stats(out=st[:, hh, :], in_=y[:, hh * 512:(hh + 1) * 512]),
             reads=[y.k], writes=[st.k])
    P.op("dve", lambda e: e.bn_aggr(out=mv[:], in_=st[:]), reads=[st.k], writes=[mv.k])
    P.op("act", lambda e: e.activation(out=rstd[:], in_=mv[:, 1:2], func=AF.Ln, bias=S["eps_ln"][:], scale=1.0),
         reads=[mv.k, S["eps_ln"].k], writes=[rstd.k])
    P.op("act", lambda e: e.activation(out=rstd[:], in_=rstd[:], func=AF.Exp, scale=-0.5),
         reads=[rstd.k], writes=[rstd.k])
    P.op("dve", lambda e: e.tensor_scalar(out=nmr[:], in0=mv[:, 0:1], scalar1=-1.0, scalar2=None, op0=ALU.mult),
         reads=[mv.k], writes=[nmr.k])
    P.op("dve", lambda e: e.scalar_tensor_tensor(out=xn[:], in0=y[:], scalar=nmr[:, 0:1], in1=g_rep[:],
                                                 op0=ALU.add, op1=ALU.mult),
         reads=[y.k, nmr.k, g_rep.k], writes=[xn.k])
    P.op("dve", lambda e: e.scalar_tensor_tensor(out=out[:], in0=xn[:], scalar=rstd[:, 0:1], in1=b_rep[:],
                                                 op0=ALU.mult, op1=ALU.add),
         reads=[xn.k, rstd.k, b_rep.k], writes=[out.k])


def transpose_tokens(C, S, xin, xT, col0):
    P = C.P
    xb = S["xb"][S["xb_i"] % len(S["xb"])]
    S["xb_i"] += 1
    P.op(S.get("cast_eng", "act"), (lambda e: e.tensor_copy(out=xb[:], in_=xin[:])) if S.get("cast_eng") else
         (lambda e: e.activation(out=xb[:], in_=xin[:], func=AF.Copy)), reads=[xin.k], writes=[xb.k])
    pt = S["pst"][S["pst_i"] % 2]
    S["pst_i"] += 1
    ident = S["ident"]
    P.pe_multi([lambda e, c=c: e.transpose(out=pt[:, c * 128:(c + 1) * 128], in_=xb[:, c * 128:(c + 1) * 128],
                                           identity=ident[:]) for c in range(8)],
               reads=[xb.k, ident.k], writes=[pt.k])
    P.op("dve", lambda e: e.tensor_copy(out=xT[:, :, col0:col0 + 128],
                                        in_=pt[:, :].rearrange("p (c t) -> p c t", c=8)),
         reads=[pt.k], writes=[xT.k])


def ffn_phase(C, S, x_dram, x_trks, out_dram, out_trks, w_up_d, w_down_d, g_d, b_d, is_output):
    nc, P = C.nc, C.P
    with contextlib.ExitStack() as es:
        wup = C.sb("wup", [128, 8, DFF], BF16, es)
        wdn = C.sb("wdn", [128, 32, D], BF16, es)
        g_rep = C.sb("g_rep", [128, D], F32, es)
        b_rep = C.sb("b_rep", [128, D], F32, es)
        xld = [C.sb("xld", [128, D], F32, es) for _ in range(3)]
        xT = C.sb("xT", [128, 8, ST], BF16, es)
        hT = C.sb("hT", [128, 32, ST], BF16, es)
        rl = [C.sb("rl", [128, ST], BF16, es) for _ in range(2)]
        y = [C.sb("y", [128, D], F32, es) for _ in range(2)]

        P.dma("sp", g_rep[:], g_d.partition_broadcast(128), writes=[g_rep.k])
        P.dma("sp", b_rep[:], b_d.partition_broadcast(128), writes=[b_rep.k])
        x_view = x_dram.rearrange("(s a p) d -> s a p d", a=4, p=128)
        o_view = out_dram.rearrange("(s a p) d -> s a p d", a=4, p=128)
        up_tr = load_weight_cast(C, wup, w_up_d.rearrange("(c p) f -> p c f", p=128), 8, 2)
        dn_tr = load_weight_cast(C, wdn, w_down_d.rearrange("(c p) f -> p c f", p=128), 8, 1)

        psb = S["psf"]
        nb = 0
        nl = 0
        for s in range(NST):
            for a in range(4):
                xi = xld[nl % 3]
                nl += 1
                P.dma("sp", xi[:], x_view[s, a], reads=[x_trks[s]], writes=[xi.k])
                transpose_tokens(C, S, xi, xT, a * 128)
            for fc in range(32):
                pb = psb[nb % 4]
                nb += 1
                P.mm(pb[:], [(wup[:, kc, fc * 128:(fc + 1) * 128], xT[:, kc, :]) for kc in range(8)],
                     reads=[xT.k, up_tr[fc // 4]], writes=[pb.k])
                r = rl[fc % 2]
                P.op("act", lambda e, r=r, pb=pb: e.activation(out=r[:], in_=pb[:], func=AF.Relu),
                     reads=[pb.k], writes=[r.k])
                P.op("dve", lambda e, r=r, fc=fc: e.tensor_tensor(out=hT[:, fc, :], in0=r[:], in1=r[:],
                                                                   op=ALU.mult),
                     reads=[r.k], writes=[hT.k])
            for a in range(4):
                xi = xld[nl % 3]
                nl += 1
                P.dma("sp", xi[:], x_view[s, a], reads=[x_trks[s]], writes=[xi.k])
                yy = y[a % 2]
                for dh in range(2):
                    pb = psb[nb % 4]
                    nb += 1
                    P.mm(pb[:], [(hT[:, fc, a * 128:(a + 1) * 128], wdn[:, fc, dh * 512:(dh + 1) * 512])
                                 for fc in range(32)],
                         reads=[hT.k] + dn_tr, writes=[pb.k])
                    P.op("dve", lambda e, yy=yy, pb=pb, dh=dh, xi=xi: e.scalar_tensor_tensor(
                        out=yy[:, dh * 512:(dh + 1) * 512], in0=xi[:, dh * 512:(dh + 1) * 512],
                        scalar=ALPHA, in1=pb[:], op0=ALU.mult, op1=ALU.add),
                         reads=[xi.k, pb.k], writes=[yy.k])
                layer_norm_tile(C, S, yy, g_rep, b_rep, yy)
                P.dma("pool", o_view[s, a], yy[:], reads=[yy.k], writes=[out_trks[s]],
                      is_output=is_output, sem_trk=yy.k)
        P.full_barrier()


def alloc_shared(C):
    S = {}
    S["ident"] = C.sb("ident", [128, 128], BF16)
    S["psf"] = [C.ps("psf", [128, 512], F32) for _ in range(6)]
    S["pst"] = [C.ps("pst", [128, 1024], BF16) for _ in range(2)]
    S["pst_i"] = 0
    S["nb"] = 0
    S["nrot"] = 6
    S["rot0"] = 0
    S["xb"] = [C.sb("xb", [128, D], BF16) for _ in range(1)]
    S["xb_i"] = 0
    S["ln_st"] = C.sb("ln_st", [128, 2, 6], F32)
    S["ln_mv"] = C.sb("ln_mv", [128, 2], F32)
    S["ln_rstd"] = C.sb("ln_rstd", [128, 1], F32)
    S["ln_nmr"] = C.sb("ln_nmr", [128, 1], F32)
    S["eps_ln"] = C.sb("eps_ln", [128, 1], F32)
    S["eps_rms"] = C.sb("eps_rms", [128, 1], F32)
    C.P.op("pool", lambda e: e.memset(S["eps_ln"][:], LN_EPS), writes=[S["eps_ln"].k])
    C.P.op("pool", lambda e: e.memset(S["eps_rms"][:], RMS_EPS), writes=[S["eps_rms"].k])
    return S


def nextbank(S):
    b = S["psf"][S["rot0"] + S["nb"] % S["nrot"]]
    S["nb"] += 1
    return b


def v3(ap, n):
    return ap.rearrange("p (h d) -> p h d", h=n)


def load_consts(C, S, consts_d):
    P = C.P
    S["cf"] = C.sb("cf", [128, 4, 128], F32)
    S["maskb"] = C.sb("maskb", [128, 128], BF16)
    P.dma("sp", S["cf"][:], consts_d, writes=[S["cf"].k])
    P.dma("pool", S["ident"][:], consts_d[:, 0, :], writes=[S["ident"].k])
    P.dma("pool", S["maskb"][:], consts_d[:, 1, :], writes=[S["maskb"].k])
    S["one1"] = C.sb("one1", [128, 1], F32)
    S["ln8"] = C.sb("ln8", [128, 1], F32)
    P.op("pool", lambda e: e.memset(S["one1"][:], 1.0), writes=[S["one1"].k])
    P.op("pool", lambda e: e.memset(S["ln8"][:], float(np.log(0.125))), writes=[S["ln8"].k])


def mem_kv_precompute(C, S, es, mem_d, wmk, wmk_tr, xld, KmT, Vm):
    P = C.P
    memT = C.sb("memT", [128, 8, 256], BF16, es)
    P.op("pool", lambda e: e.memset(Vm[:], 1.0), writes=[Vm.k])
    n = 0
    for q in range(NSEQ):
        for mt in range(2):
            xi = xld[n % len(xld)]
            n += 1
            P.dma("sp", xi[:], mem_d[q, mt * 128:(mt + 1) * 128, :], writes=[xi.k])
            transpose_tokens(C, S, xi, memT, mt * 128)
        for h in range(4):
            pb = nextbank(S)
            P.mm(pb[:, 0:256], [(wmk[:, kc, h * 128:(h + 1) * 128], memT[:, kc, :]) for kc in range(8)],
                 reads=[memT.k] + wmk_tr, writes=[pb.k])
            P.op("act", lambda e, pb=pb, q=q, h=h: e.activation(out=KmT[:, q, h, :], in_=pb[:, 0:256], func=AF.Copy),
                 reads=[pb.k], writes=[KmT.k])
        for mt in range(2):
            pb = nextbank(S)
            P.mm(pb[:], [(memT[:, kc, mt * 128:(mt + 1) * 128], wmk[:, kc, 512:1024]) for kc in range(8)],
                 reads=[memT.k] + wmk_tr, writes=[pb.k])
            P.op("act", lambda e, pb=pb, q=q, mt=mt: e.activation(out=Vm[:, q, mt, :, 0:128], in_=v3(pb[:], 4),
                                                                  func=AF.Copy),
                 reads=[pb.k], writes=[Vm.k])
    return memT


def mem_attention(C, S, seq, qmT, KmT, Vm, hcat, col0):
    P = C.P
    for h in range(4):
        pts = []
        for mt in range(2):
            pb = nextbank(S)
            P.mm(pb[:], [(KmT[:, seq, h, mt * 128:(mt + 1) * 128], qmT[:, h, :])],
                 reads=[KmT.k, qmT.k], writes=[pb.k])
            ptl = S.get("PTm") or S["PT"]
            pt = ptl[S["pt_i"] % len(ptl)]
            S["pt_i"] += 1
            P.op("act", lambda e, pt=pt, pb=pb: e.activation(out=pt[:], in_=pb[:], func=AF.Exp, scale=128.0 ** -0.5),
                 reads=[pb.k], writes=[pt.k])
            pts.append(pt)
        for a in range(4):
            pb = nextbank(S)
            P.mm(pb[:, 0:129], [(pts[mt][:, a * 128:(a + 1) * 128], Vm[:, seq, mt, h, :]) for mt in range(2)],
                 reads=[pts[0].k, pts[1].k, Vm.k], writes=[pb.k])
            rc = S["rc"][S["rc_i"] % 2]
            S["rc_i"] += 1
            P.op("dve", lambda e, rc=rc, pb=pb: e.reciprocal(out=rc[:], in_=pb[:, 128:129]),
                 reads=[pb.k], writes=[rc.k])
            P.op("dve", lambda e, rc=rc, pb=pb, a=a, h=h: e.tensor_scalar(
                out=hcat[a][:, col0 + h * 128:col0 + (h + 1) * 128], in0=pb[:, 0:128], scalar1=rc[:, 0:1],
                scalar2=None, op0=ALU.mult), reads=[pb.k, rc.k], writes=[hcat[a].k])


def out_proj_ln(C, S, hcat, hcT, wout, wout_tr, xld, nl, x_view, x_trk, s, y, g_rep, b_rep, o_view, o_trk):
    P = C.P
    for a in range(4):
        pt = S["pst"][S["pst_i"] % 2]
        S["pst_i"] += 1
        ident = S["ident"]
        P.pe_multi([lambda e, c=c, pt=pt, a=a: e.transpose(out=pt[:, c * 128:(c + 1) * 128],
                                                         in_=hcat[a][:, c * 128:(c + 1) * 128],
                                                         identity=ident[:]) for c in range(8)],
                   reads=[hcat[a].k, ident.k], writes=[pt.k])
        P.op("dve", lambda e, pt=pt, a=a: e.tensor_copy(out=hcT[:, :, a * 128:(a + 1) * 128], in_=v3(pt[:, :], 8)),
             reads=[pt.k], writes=[hcT.k])
    for a in range(4):
        xi = xld[nl[0] % len(xld)]
        nl[0] += 1
        P.dma("sp", xi[:], x_view[s, a], reads=[x_trk], writes=[xi.k])
        yy = y[a % 2]
        for dh in range(2):
            pb = nextbank(S)
            P.mm(pb[:], [(hcT[:, kc, a * 128:(a + 1) * 128], wout[:, kc, dh * 512:(dh + 1) * 512])
                         for kc in range(8)], reads=[hcT.k] + wout_tr, writes=[pb.k])
            P.op("dve", lambda e, yy=yy, pb=pb, dh=dh, xi=xi: e.scalar_tensor_tensor(
                out=yy[:, dh * 512:(dh + 1) * 512], in0=xi[:, dh * 512:(dh + 1) * 512],
                scalar=ALPHA, in1=pb[:], op0=ALU.mult, op1=ALU.add),
                 reads=[xi.k, pb.k], writes=[yy.k])
        layer_norm_tile(C, S, yy, g_rep, b_rep, yy)
        P.dma("pool", o_view[s, a], yy[:], reads=[yy.k], writes=[o_trk], sem_trk=yy.k)


def mixer_a_phase(C, S, x_dram, x_trks, out_dram, out_trks, mem_d, w_in_d, bi_d, bf_d, wmk_d, w_out_d, g_d, b_d):
    nc, P = C.nc, C.P
    with contextlib.ExitStack() as es:
        win = C.sb("win", [128, 8, 2056], BF16, es)
        wout = C.sb("wout", [128, 8, D], BF16, es)
        g_rep = C.sb("g_rep", [128, D], F32, es)
        b_rep = C.sb("b_rep", [128, D], F32, es)
        bias_rep = C.sb("bias_rep", [128, 8], F32, es)
        xld = [C.sb("xld", [128, D], F32, es) for _ in range(3)]
        KmT = C.sb("KmT", [128, NSEQ, 4, 256], BF16, es)
        Vm = C.sb("Vm", [128, NSEQ, 2, 4, 129], BF16, es)
        P.dma("sp", g_rep[:], g_d.partition_broadcast(128), writes=[g_rep.k])
        P.dma("sp", b_rep[:], b_d.partition_broadcast(128), writes=[b_rep.k])
        P.dma("sp", bias_rep[:, 0:4], bi_d.partition_broadcast(128), writes=[bias_rep.k])
        P.dma("sp", bias_rep[:, 4:8], bf_d.partition_broadcast(128), writes=[bias_rep.k])
        with contextlib.ExitStack() as es2:
            wmk = C.sb("wmk", [128, 8, D], BF16, es2)
            wmk_tr = load_weight_cast(C, wmk, wmk_d.rearrange("(c p) f -> p c f", p=128), 2, 1)
            win_tr = load_weight_cast(C, win, w_in_d.rearrange("(c p) f -> p c f", p=128), 2, 1)
            wout_tr = load_weight_cast(C, wout, w_out_d.rearrange("(c p) f -> p c f", p=128), 2, 1)
            mem_kv_precompute(C, S, es2, mem_d, wmk, wmk_tr, xld, KmT, Vm)
            P.full_barrier()
        xT = [C.sb("xT", [128, 8, ST], BF16, es) for _ in range(2)]
        qT = [C.sb("qT", [64, 4, ST], BF16, es) for _ in range(2)]
        kT = [C.sb("kT", [64, 4, ST], BF16, es) for _ in range(2)]
        qz = [C.sb("qz", [64, 4, 4, 2, 128], BF16, es) for _ in range(2)]
        qmT = [C.sb("qmT", [128, 4, ST], BF16, es) for _ in range(2)]
        gts = C.sb("gts", [128, 8], F32, es)
        lfn = C.sb("lfn", [128, 4], F32, es)
        tadd = C.sb("tadd", [128, 4], F32, es)
        colf = C.sb("colf", [128, 4], F32, es)
        enb = C.sb("enb", [128, 4], F32, es)
        eg = [C.sb("eg", [128, 2, 4], F32, es) for _ in range(2)]
        kc_t = C.sb("kc", [128, 4, 64], BF16, es)
        vaug = [C.sb("vaug", [128, 4, 129], BF16, es) for _ in range(2)]
        e_o = C.sb("e_o", [128, 512], F32, es)
        atmp = C.sb("atmp", [128, 4, 128], BF16, es)
        AT = C.sb("AT", [128, 4, 128], BF16, es)
        Sst = C.sb("Sst", [64, 4, 129], F32, es)
        Cf = C.sb("Cf", [64, 4, 129], F32, es)
        Cb = [C.sb("Cb", [64, 4, 129], BF16, es) for _ in range(2)]
        den = C.sb("den", [128, 2, 1], F32, es)
        hcat2 = [[C.sb("hcat", [128, D], BF16, es) for _ in range(4)] for _ in range(2)]
        hcT = C.sb("hcT", [128, 8, ST], BF16, es)
        y = [C.sb("y", [128, D], F32, es) for _ in range(2)]
        S["PT"] = [C.sb("PT", [128, ST], BF16, es) for _ in range(4)]
        S["pt_i"] = 0
        S["rc"] = [C.sb("rc", [128, 1], F32, es) for _ in range(2)]
        S["rc_i"] = 0
        for qq in qz:
            P.op("pool", lambda e, qq=qq: e.memset(qq[:], 0.0), writes=[qq.k])
        for vv in vaug:
            P.op("pool", lambda e, vv=vv: e.memset(vv[:], 1.0), writes=[vv.k])

        x_view = x_dram.rearrange("(s a p) d -> s a p d", a=4, p=128)
        o_view = out_dram.rearrange("(s a p) d -> s a p d", a=4, p=128)
        cf = S["cf"]
        maskb = S["maskb"]
        nl = [0]
        NT = NST // NSEQ

        def front(s):
            b, seq = s % 2, s // NT
            xTb, qTb, kTb, qzb, qmTb = xT[b], qT[b], kT[b], qz[b], qmT[b]
            for a in range(4):
                xi = xld[nl[0] % len(xld)]
                nl[0] += 1
                P.dma("sp", xi[:], x_view[s, a], reads=[x_trks[s]], writes=[xi.k])
                transpose_tokens(C, S, xi, xTb, a * 128)
            for h in range(4):
                for (dst, c0) in ((qTb, 0), (kTb, 256)):
                    pb = nextbank(S)
                    P.mm(pb[0:64, :], [(win[:, kc, c0 + h * 64:c0 + (h + 1) * 64], xTb[:, kc, :]) for kc in range(8)],
                         reads=[xTb.k] + win_tr, writes=[pb.k])
                    P.op("act", lambda e, pb=pb, dst=dst, h=h: e.activation(out=dst[:, h, :], in_=pb[0:64, :],
                                                                            func=AF.Copy),
                         reads=[pb.k], writes=[dst.k])
                P.op("pool", lambda e, h=h: e.tensor_copy(
                    out=bass.AP(qzb.t, h * 1024, [[4096, 64], [256, 4], [192, 2], [1, 64]]),
                    in_=qTb[:, h, :].rearrange("p (a c j) -> p a c j", a=4, c=2)),
                     reads=[qTb.k], writes=[qzb.k])
                pb = nextbank(S)
                P.mm(pb[:], [(win[:, kc, 1544 + h * 128:1544 + (h + 1) * 128], xTb[:, kc, :]) for kc in range(8)],
                     reads=[xTb.k] + win_tr, writes=[pb.k])
                P.op("act", lambda e, pb=pb, h=h: e.activation(out=qmTb[:, h, :], in_=pb[:], func=AF.Copy),
                     reads=[pb.k], writes=[qmTb.k])
            mem_attention(C, S, seq, qmTb, KmT, Vm, hcat2[b], 512)

        def tail(s):
            out_proj_ln(C, S, hcat2[s % 2], hcT, wout, wout_tr, xld, nl, x_view, x_trks[s], s, y, g_rep, b_rep,
                        o_view, out_trks[s])

        def tok_loop(s):
            b = s % 2
            xTb, qTb, kTb, qzb, hcat = xT[b], qT[b], kT[b], qz[b], hcat2[b]
            for a in range(4):
                t = s * 4 + a
                par = t % 2
                first = (t % (SEQ // 128) == 0)
                cols = slice(a * 128, (a + 1) * 128)
                va = vaug[par]
                pg = nextbank(S)
                P.mm(pg[:, 0:8], [(xTb[:, kc, cols], win[:, kc, 1536:1544]) for kc in range(8)],
                     reads=[xTb.k] + win_tr, writes=[pg.k])
                P.op("dve", lambda e, pg=pg: e.tensor_tensor(out=gts[:], in0=pg[:, 0:8], in1=bias_rep[:], op=ALU.add),
                     reads=[pg.k, bias_rep.k], writes=[gts.k])
                P.op("act", lambda e: e.activation(out=lfn[:], in_=gts[:, 4:8], func=AF.Exp, scale=-1.0),
                     reads=[gts.k], writes=[lfn.k])
                P.op("act", lambda e: e.activation(out=lfn[:], in_=lfn[:], func=AF.Ln, bias=S["one1"][:], scale=1.0),
                     reads=[lfn.k, S["one1"].k], writes=[lfn.k])
                pc = nextbank(S)
                P.mm(pc[:, 0:4], [(cf[:, 1, :], lfn[:])], reads=[cf.k, lfn.k], writes=[pc.k])
                P.mm(pc[:, 4:8], [(cf[:, 2, :], lfn[:])], reads=[cf.k, lfn.k], writes=[pc.k])
                P.mm(pc[:, 8:12], [(cf[:, 3, :], lfn[:])], reads=[cf.k, lfn.k], writes=[pc.k])
                P.op("dve", lambda e, pc=pc: e.tensor_tensor(out=tadd[:], in0=gts[:, 0:4], in1=pc[:, 0:4], op=ALU.add),
                     reads=[gts.k, pc.k], writes=[tadd.k])
                P.op("act", lambda e: e.activation(out=colf[:], in_=tadd[:], func=AF.Exp, bias=S["ln8"][:], scale=1.0),
                     reads=[tadd.k, S["ln8"].k], writes=[colf.k])
                P.op("act", lambda e, pc=pc: e.activation(out=enb[:], in_=pc[:, 0:4], func=AF.Exp),
                     reads=[pc.k], writes=[enb.k])
                P.op("act", lambda e, pc=pc, par=par: e.activation(out=eg[par][:], in_=v3(pc[:, 4:12], 2),
                                                                   func=AF.Exp, scale=-1.0),
                     reads=[pc.k], writes=[eg[par].k])
                pk = nextbank(S)
                P.mm(pk[:, 0:256], [(xTb[:, kc, cols], win[:, kc, 256:512]) for kc in range(8)],
                     reads=[xTb.k] + win_tr, writes=[pk.k])
                P.op("dve", lambda e, pk=pk: e.tensor_tensor(
                    out=kc_t[:], in0=v3(pk[:, 0:256], 4), in1=colf[:, 0:4].unsqueeze(2).broadcast_to([128, 4, 64]),
                    op=ALU.mult), reads=[pk.k, colf.k], writes=[kc_t.k])
                pv = nextbank(S)
                P.mm(pv[:], [(xTb[:, kc, cols], win[:, kc, 512:1024]) for kc in range(8)],
                     reads=[xTb.k] + win_tr, writes=[pv.k])
                P.op("act", lambda e, pv=pv, va=va: e.activation(out=va[:, :, 0:128], in_=v3(pv[:], 4), func=AF.Copy),
                     reads=[pv.k], writes=[va.k])
                po = nextbank(S)
                P.mm(po[:], [(xTb[:, kc, cols], win[:, kc, 1024:1536]) for kc in range(8)],
                     reads=[xTb.k] + win_tr, writes=[po.k])
                P.op("act", lambda e, po=po: e.activation(out=e_o[:], in_=po[:], func=AF.Exp, scale=-1.0),
                     reads=[po.k], writes=[e_o.k])
                P.op("act", lambda e: e.activation(out=e_o[:], in_=e_o[:], func=AF.Ln, bias=S["one1"][:], scale=1.0),
                     reads=[e_o.k, S["one1"].k], writes=[e_o.k])
                P.op("act", lambda e: e.activation(out=e_o[:], in_=e_o[:], func=AF.Exp, scale=-1.0),
                     reads=[e_o.k], writes=[e_o.k])
                pa = nextbank(S)
                for h in range(4):
                    P.mm(pa[:, h * 128:(h + 1) * 128], [(kTb[:, h, cols], qTb[:, h, cols])],
                         reads=[kTb.k, qTb.k], writes=[pa.k])
                P.op("dve", lambda e, pa=pa: e.tensor_tensor(
                    out=atmp[:], in0=v3(pa[:], 4), in1=colf[:, 0:4].unsqueeze(2).broadcast_to([128, 4, 128]),
                    op=ALU.mult), reads=[pa.k, colf.k], writes=[atmp.k])
                P.op("pool", lambda e: e.tensor_tensor(
                    out=AT[:], in0=atmp[:], in1=maskb[:, :].unsqueeze(1).broadcast_to([128, 4, 128]), op=ALU.mult),
                     reads=[atmp.k, maskb.k], writes=[AT.k])
                for c in range(2):
                    if first and c == 0:
                        P.op("dve", lambda e: e.memset(Cf[:], 0.0), writes=[Cf.k])
                        P.op("pool", lambda e: e.memset(Cb[0][:], 0.0), writes=[Cb[0].k])
                    else:
                        egp = eg[par][0:64, 0, :] if c == 1 else eg[1 - par][0:64, 1, :]
                        egk = eg[par].k if c == 1 else eg[1 - par].k
                        P.op("dve", lambda e, egp=egp: e.tensor_tensor(
                            out=Cf[:], in0=Sst[:], in1=egp.unsqueeze(2).broadcast_to([64, 4, 129]), op=ALU.mult),
                             reads=[Sst.k, egk], writes=[Cf.k])
                        P.op("act", lambda e, c=c: e.activation(out=Cb[c][:], in_=Cf[:], func=AF.Copy),
                             reads=[Cf.k], writes=[Cb[c].k])
                    rows = slice(c * 64, (c + 1) * 64)
                    for hp in range(2):
                        pu = nextbank(S)
                        for j in range(2):
                            h = 2 * hp + j
                            P.mm(pu[0:64, j * 129:(j + 1) * 129], [(kc_t[rows, h, :], va[rows, h, :])],
                                 reads=[kc_t.k, va.k], writes=[pu.k])
                        P.op("dve", lambda e, pu=pu, hp=hp: e.tensor_tensor(
                            out=Sst[:, 2 * hp:2 * hp + 2, :], in0=Cf[:, 2 * hp:2 * hp + 2, :],
                            in1=v3(pu[0:64, 0:258], 2), op=ALU.add),
                             reads=[Cf.k, pu.k], writes=[Sst.k])
                for hp in range(2):
                    pn = nextbank(S)
                    for j in range(2):
                        h = 2 * hp + j
                        P.mm(pn[:, j * 129:(j + 1) * 129],
                             [(AT[:, h, :], va[:, h, :]),
                              (qzb[:, h, a, 0, :], Cb[0][:, h, :]),
                              (qzb[:, h, a, 1, :], Cb[1][:, h, :])],
                             reads=[AT.k, va.k, qzb.k, Cb[0].k, Cb[1].k], writes=[pn.k])
                    pn3 = v3(pn[:, 0:258], 2)
                    P.op("dve", lambda e, pn3=pn3, hp=hp: e.tensor_tensor(
                        out=den[:], in0=pn3[:, :, 128:129], in1=enb[:, 2 * hp:2 * hp + 2].unsqueeze(2),
                        op=ALU.max), reads=[pn.k, enb.k], writes=[den.k])
                    P.op("dve", lambda e, pn3=pn3: e.scalar_tensor_tensor(
                        out=den[:], in0=pn3[:, :, 128:129], scalar=-1.0, in1=den[:], op0=ALU.mult, op1=ALU.max),
                         reads=[pn.k, den.k], writes=[den.k])
                    P.op("dve", lambda e: e.reciprocal(out=den[:], in_=den[:]), reads=[den.k], writes=[den.k])
                    for j in range(2):
                        h = 2 * hp + j
                        P.op("dve", lambda e, pn=pn, j=j, h=h, a=a: e.scalar_tensor_tensor(
                            out=hcat[a][:, h * 128:(h + 1) * 128], in0=pn[:, j * 129:j * 129 + 128],
                            scalar=den[:, j, :], in1=e_o[:, h * 128:(h + 1) * 128], op0=ALU.mult, op1=ALU.mult),
                             reads=[pn.k, den.k, e_o.k], writes=[hcat[a].k])

        def side(fn, *a):
            S["rot0"], S["nrot"] = 4, 2
            fn(*a)
            S["rot0"], S["nrot"] = 0, 4

        side(front, 0)
        for s in range(NST):
            pending = []
            P.rec = pending
            if s > 0:
                side(tail, s - 1)
            if s + 1 < NST:
                side(front, s + 1)
            P.rec = None
            S["rot0"], S["nrot"] = 0, 4
            P.inter = (pending, len(pending) / 260.0 + 0.02)
            P._acc = 0.0
            tok_loop(s)
            P.inter = None
            while pending:
                P.replay(pending.pop(0))
        side(tail, NST - 1)
        P.full_barrier()
        S["rot0"], S["nrot"] = 0, 6


def rms_rows(C, S, pb, ncols, out_bf, scr):
    P = C.P
    ss, rs = S["rms_ss"], S["rms_rs"]
    if scr is None:
        scr = nextbank(S)
    P.op("dve", lambda e: e.memset(ss[:], 0.0), writes=[ss.k])
    P.op("act", lambda e: e.activation(out=scr[:, 0:ncols], in_=pb[:, 0:ncols], func=AF.Square, accum_out=ss[:]),
         reads=[pb.k], writes=[scr.k, ss.k])
    P.op("act", lambda e: e.activation(out=rs[:], in_=ss[:], func=AF.Ln, bias=S["eps_rms"][:], scale=1.0 / ncols),
         reads=[ss.k, S["eps_rms"].k], writes=[rs.k])
    P.op("act", lambda e: e.activation(out=rs[:], in_=rs[:], func=AF.Exp, scale=-0.5), reads=[rs.k], writes=[rs.k])
    P.op("dve", lambda e: e.tensor_scalar(out=out_bf[:, 0:ncols], in0=pb[:, 0:ncols], scalar1=rs[:, 0:1],
                                          scalar2=None, op0=ALU.mult), reads=[pb.k, rs.k], writes=[out_bf.k])


def transpose_cols(C, S, src_bf, nchunk, dstT, col0):
    P = C.P
    pt = S["pst"][S["pst_i"] % 2]
    S["pst_i"] += 1
    ident = S["ident"]
    P.pe_multi([lambda e, c=c: e.transpose(out=pt[:, c * 128:(c + 1) * 128], in_=src_bf[:, c * 128:(c + 1) * 128],
                                           identity=ident[:]) for c in range(nchunk)],
               reads=[src_bf.k, ident.k], writes=[pt.k])
    P.op("dve", lambda e: e.tensor_copy(out=dstT[:, 0:nchunk, col0:col0 + 128], in_=v3(pt[:, 0:nchunk * 128], nchunk)),
         reads=[pt.k], writes=[dstT.k])


def mixer_b_phase(C, S, x_dram, x_trks, out_dram, out_trks, mem_d, pos_d, wdown_d, gkv_d, wuk_d, wuv_d,
                  bwin_d, gq_d, wuq_d, wmk_d, w_out_d, g_d, b_d, cf2_d):
    nc, P = C.nc, C.P
    SC = 96.0 ** -0.5
    NT = NST // NSEQ
    with contextlib.ExitStack() as es:
        wdown = C.sb("wdown", [128, 8, 288], BF16, es)
        wdr = C.sb("wdr", [128, 8, 96], BF16, es)
        wdrot = C.sb("wdrot", [128, 8, 96], BF16, es)
        wuk = C.sb("wuk", [128, 2, 512], BF16, es)
        wuv = C.sb("wuv", [128, 2, 512], BF16, es)
        wuq = C.sb("wuq", [128, 2, 768], BF16, es)
        wuqrot = C.sb("wuqrot", [128, 2, 8, 96], BF16, es)
        gk = C.sb("gk", [128, 2], F32, es)
        gq = C.sb("gq", [128, 2], F32, es)
        bwin = C.sb("bwin", [128, 8, 768], BF16, es)
        wout = C.sb("wout", [128, 8, D], BF16, es)
        g_rep = C.sb("g_rep", [128, D], F32, es)
        b_rep = C.sb("b_rep", [128, D], F32, es)
        cf2 = C.sb("cf2", [128, 4], F32, es)
        xld = [C.sb("xld", [128, D], F32, es) for _ in range(2)]
        KmT = C.sb("KmT", [128, NSEQ, 4, 256], BF16, es)
        Vm = C.sb("Vm", [128, NSEQ, 2, 4, 129], BF16, es)
        P.dma("sp", g_rep[:], g_d.partition_broadcast(128), writes=[g_rep.k])
        P.dma("sp", b_rep[:], b_d.partition_broadcast(128), writes=[b_rep.k])
        P.dma("sp", cf2[:], cf2_d, writes=[cf2.k])
        for kc in range(2):
            P.dma("sp", gk[:, kc:kc + 1], gkv_d[kc * 128:(kc + 1) * 128].rearrange("(p o) -> p o", o=1), writes=[gk.k])
            P.dma("sp", gq[:, kc:kc + 1], gq_d[kc * 128:(kc + 1) * 128].rearrange("(p o) -> p o", o=1), writes=[gq.k])
        with contextlib.ExitStack() as es2:
            wmk = C.sb("wmk", [128, 8, D], BF16, es2)
            wst = C.sb("wst", [128, 2, 768], F32, es2)
            wmk_tr = load_weight_cast(C, wmk, wmk_d.rearrange("(c p) f -> p c f", p=128), 2, 1)
            wdown_tr = load_weight_cast(C, wdown, wdown_d.rearrange("(c p) f -> p c f", p=128), 1, 1)
            bwin_tr = load_weight_cast(C, bwin, bwin_d.rearrange("(c p) f -> p c f", p=128), 2, 1)
            wout_tr = load_weight_cast(C, wout, w_out_d.rearrange("(c p) f -> p c f", p=128), 2, 1)
            for (dst, src_d, gg, ncol) in ((wuk, wuk_d, gk, 512), (wuv, wuv_d, gk, 512), (wuq, wuq_d, gq, 768)):
                P.dma("sp", wst[:, :, 0:ncol], src_d.rearrange("(c p) f -> p c f", p=128), writes=[wst.k])
                for kc in range(2):
                    P.op("dve", lambda e, dst=dst, gg=gg, kc=kc, ncol=ncol: e.tensor_scalar(
                        out=dst[:, kc, :], in0=wst[:, kc, 0:ncol], scalar1=gg[:, kc:kc + 1], scalar2=None,
                        op0=ALU.mult), reads=[wst.k, gg.k], writes=[dst.k])
            mem_kv_precompute(C, S, es2, mem_d, wmk, wmk_tr, xld, KmT, Vm)
            P.full_barrier()
        xT = [C.sb("xT", [128, 8, ST], BF16, es) for _ in range(2)]
        uu = C.sb("uu", [96, ST], F32, es)
        u2 = C.sb("u2", [96, ST], F32, es)
        cosT = C.sb("cosT", [96, ST], F32, es)
        sinT = C.sb("sinT", [96, ST], F32, es)
        kT = C.sb("kT", [96, 8, SEQ], BF16, es)
        Vaug = C.sb("Vaug", [128, 16, 8, 65], BF16, es)
        kT_blk = [Trk("kTb%d" % i) for i in range(NT)]
        V_blk = [Trk("Vb%d" % i) for i in range(NT)]
        ckn = C.sb("ckn", [128, 256], BF16, es)
        ckT = [C.sb("ckT", [128, 2, ST], BF16, es) for _ in range(2)]
        cqT = [C.sb("cqT", [128, 2, ST], BF16, es) for _ in range(2)]
        rt1 = C.sb("rt1", [96, ST], F32, es)
        rt2 = C.sb("rt2", [96, ST], F32, es)
        kr = C.sb("kr", [96, ST], BF16, es)
        qTh = [C.sb("qTh", [96, 8, ST], BF16, es) for _ in range(2)]
        qmT = [C.sb("qmT", [128, 4, ST], BF16, es) for _ in range(2)]
        hc_all = C.sb("hc_all", [128, 4, D], BF16, es)
        y = [C.sb("y", [128, D], F32, es) for _ in range(2)]
        scr = None
        rc4 = C.sb("rc4", [128, 4, 1], F32, es)
        S["PT"] = [C.sb("PT", [128, ST], BF16, es) for _ in range(3)]
        S["PTm"] = [C.sb("PTm", [128, ST], BF16, es) for _ in range(2)]
        S["pt_i"] = 0
        S["rc"] = [C.sb("rc", [128, 1], F32, es) for _ in range(2)]
        S["rc_i"] = 0
        S["rms_ss"] = C.sb("rms_ss", [128, 1], F32, es)
        S["rms_rs"] = C.sb("rms_rs", [128, 1], F32, es)
        hcat = []
        for a in range(4):
            v = Tile(hc_all.t[:, a, :], "hcv")
            v.k = hc_all.k
            hcat.append(v)

        P.op("pool", lambda e: e.memset(wdr[:], 0.0), writes=[wdr.k])
        P.op("pool", lambda e: e.memset(wdrot[:], 0.0), writes=[wdrot.k])
        P.op("pool", lambda e: e.memset(wuqrot[:], 0.0), writes=[wuqrot.k])
        P.op("pool", lambda e: e.tensor_copy(out=wdr[:, :, 64:96], in_=wdown[:, :, 256:288]),
             reads=wdown_tr, writes=[wdr.k])
        P.op("pool", lambda e: e.tensor_scalar(out=wdrot[:, :, 64:80], in0=wdown[:, :, 272:288], scalar1=-1.0,
                                               scalar2=None, op0=ALU.mult), reads=wdown_tr, writes=[wdrot.k])
        P.op("pool", lambda e: e.tensor_copy(out=wdrot[:, :, 80:96], in_=wdown[:, :, 256:272]),
             reads=wdown_tr, writes=[wdrot.k])
        wuq4 = wuq.t[:, :, :].rearrange("p c (h d) -> p c h d", h=8)
        P.op("pool", lambda e: e.tensor_scalar(out=wuqrot[:, :, :, 64:80], in0=wuq4[:, :, :, 80:96], scalar1=-1.0,
                                               scalar2=None, op0=ALU.mult), reads=[wuq.k], writes=[wuqrot.k])
        P.op("pool", lambda e: e.tensor_copy(out=wuqrot[:, :, :, 80:96], in_=wuq4[:, :, :, 64:80]),
             reads=[wuq.k], writes=[wuqrot.k])
        P.op("pool", lambda e: e.memset(Vaug[:], 1.0), writes=V_blk)
        S["rot0"], S["nrot"] = 4, 2
        S["cast_eng"] = "pool"
        sbank = S["psf"][0:2]
        sb_i = [0]

        x_view = x_dram.rearrange("(s a p) d -> s a p d", a=4, p=128)
        o_view = out_dram.rearrange("(s a p) d -> s a p d", a=4, p=128)
        nl = [0]
        R = slice(64, 96)

        def front_chunks(s):
            seq, T, b = s // NT, s % NT, s % 2
            tcols = slice(T * ST, (T + 1) * ST)
            xTb, ckTb, cqTb, qThb, qmTb = xT[b], ckT[b], cqT[b], qTh[b], qmT[b]
            ch = []

            def rope_tables():
                posi_ap = rt2[R, :].bitcast(I32)
                P.dma("sp", posi_ap, pos_d[seq, T * ST:(T + 1) * ST].partition_broadcast(32), writes=[rt2.k])
                P.op("dve", lambda e: e.tensor_copy(out=uu[R, :], in_=posi_ap), reads=[rt2.k], writes=[uu.k])
                for (dstT, shift) in ((sinT, 0.0), (cosT, 0.25)):
                    P.op("dve", lambda e, shift=shift: e.tensor_scalar(
                        out=u2[R, :], in0=uu[R, :], scalar1=cf2[R, 0:1], scalar2=shift, op0=ALU.mult, op1=ALU.add),
                         reads=[uu.k, cf2.k], writes=[u2.k])
                    P.op("dve", lambda e: e.tensor_copy(out=posi_ap, in_=u2[R, :]), reads=[u2.k], writes=[rt2.k])
                    P.op("dve", lambda e: e.tensor_copy(out=rt1[R, :], in_=posi_ap), reads=[rt2.k], writes=[rt1.k])
                    P.op("dve", lambda e: e.tensor_tensor(out=u2[R, :], in0=u2[R, :], in1=rt1[R, :], op=ALU.subtract),
                         reads=[u2.k, rt1.k], writes=[u2.k])
                    P.op("dve", lambda e: e.tensor_scalar(out=rt1[R, :], in0=u2[R, :], scalar1=0.5, scalar2=None,
                                                          op0=ALU.is_gt), reads=[u2.k], writes=[rt1.k])
                    P.op("dve", lambda e: e.tensor_tensor(out=u2[R, :], in0=u2[R, :], in1=rt1[R, :], op=ALU.subtract),
                         reads=[u2.k, rt1.k], writes=[u2.k])
                    P.op("act", lambda e, dstT=dstT: e.activation(out=dstT[R, :], in_=u2[R, :], func=AF.Sin,
                                                                  scale=float(2 * np.pi)),
                         reads=[u2.k], writes=[dstT.k])
            xis = {}

            def load_dma(a):
                xi = xld[nl[0] % len(xld)]
                nl[0] += 1
                xis[a] = xi
                P.dma("sp", xi[:], x_view[s, a], reads=[x_trks[s]], writes=[xi.k])

            def load_tr(a):
                transpose_tokens(C, S, xis[a], xTb, a * 128)

            def latents(a):
                cols = slice(a * 128, (a + 1) * 128)
                for (wt, wtr, dstT) in ((wdown, wdown_tr, ckTb), (bwin, bwin_tr, cqTb)):
                    pb = nextbank(S)
                    P.mm(pb[:, 0:256], [(xTb[:, kc, cols], wt[:, kc, 0:256]) for kc in range(8)],
                         reads=[xTb.k] + wtr, writes=[pb.k])
                    rms_rows(C, S, pb, 256, ckn, scr)
                    transpose_cols(C, S, ckn, 2, dstT, a * 128)
            ch.append(lambda: load_dma(0))
            ch.append(lambda: load_dma(1))
            ch.append(rope_tables)
            ch.append(lambda: load_tr(0))
            ch.append(lambda: load_dma(2))
            ch.append(lambda: load_tr(1))
            ch.append(lambda: load_dma(3))
            ch.append(lambda: latents(0))
            ch.append(lambda: load_tr(2))
            ch.append(lambda: latents(1))
            ch.append(lambda: load_tr(3))
            ch.append(lambda: latents(2))
            ch.append(lambda: latents(3))

            def k_nope(h0):
                for h in range(h0, h0 + 4):
                    pb = nextbank(S)
                    P.mm(pb[0:64, :], [(wuk[:, kc, h * 64:(h + 1) * 64], ckTb[:, kc, :]) for kc in range(2)],
                         reads=[wuk.k, ckTb.k], writes=[pb.k])
                    P.op("dve", lambda e, pb=pb, h=h: e.tensor_copy(out=kT[0:64, h, tcols], in_=pb[0:64, :]),
                         reads=[pb.k], writes=[kT_blk[T]])
            ch.append(lambda: k_nope(0))
            ch.append(lambda: k_nope(4))

            def k_rope():
                pA, pB = nextbank(S), nextbank(S)
                P.mm(pA[0:96, :], [(wdr[:, kc, :], xTb[:, kc, :]) for kc in range(8)], reads=[wdr.k, xTb.k],
                     writes=[pA.k])
                P.mm(pB[0:96, :], [(wdrot[:, kc, :], xTb[:, kc, :]) for kc in range(8)], reads=[wdrot.k, xTb.k],
                     writes=[pB.k])
                P.op("dve", lambda e: e.tensor_tensor(out=rt1[R, :], in0=pA[R, :], in1=cosT[R, :], op=ALU.mult),
                     reads=[pA.k, cosT.k], writes=[rt1.k])
                P.op("dve", lambda e: e.tensor_tensor(out=rt2[R, :], in0=pB[R, :], in1=sinT[R, :], op=ALU.mult),
                     reads=[pB.k, sinT.k], writes=[rt2.k])
                P.op("dve", lambda e: e.tensor_tensor(out=kr[R, :], in0=rt1[R, :], in1=rt2[R, :], op=ALU.add),
                     reads=[rt1.k, rt2.k], writes=[kr.k])
                P.op("dve", lambda e: e.tensor_copy(out=kT[R, :, tcols],
                                                    in_=kr[R, :].unsqueeze(1).broadcast_to([32, 8, ST])),
                     reads=[kr.k], writes=[kT_blk[T]])
            ch.append(k_rope)

            def v_tiles():
                for a in range(4):
                    pb = nextbank(S)
                    P.mm(pb[:], [(ckTb[:, kc, a * 128:(a + 1) * 128], wuv[:, kc, :]) for kc in range(2)],
                         reads=[ckTb.k, wuv.k], writes=[pb.k])
                    P.op("act", lambda e, pb=pb, a=a: e.activation(out=Vaug[:, 4 * T + a, :, 0:64], in_=v3(pb[:], 8),
                                                                   func=AF.Copy), reads=[pb.k], writes=[V_blk[T]])
            ch.append(v_tiles)

            def queries(h0):
                for h in range(h0, h0 + 2):
                    pA, pB = nextbank(S), nextbank(S)
                    P.mm(pA[0:96, :], [(wuq[:, kc, h * 96:(h + 1) * 96], cqTb[:, kc, :]) for kc in range(2)],
                         reads=[wuq.k, cqTb.k], writes=[pA.k])
                    P.mm(pB[0:96, :], [(wuqrot[:, kc, h, :], cqTb[:, kc, :]) for kc in range(2)],
                         reads=[wuqrot.k, cqTb.k], writes=[pB.k])
                    P.op("dve", lambda e, pA=pA, h=h: e.tensor_copy(out=qThb[0:64, h, :], in_=pA[0:64, :]),
                         reads=[pA.k], writes=[qThb.k])
                    P.op("dve", lambda e, pA=pA: e.tensor_tensor(out=rt1[R, :], in0=pA[R, :], in1=cosT[R, :],
                                                                 op=ALU.mult), reads=[pA.k, cosT.k], writes=[rt1.k])
                    P.op("dve", lambda e, pB=pB: e.tensor_tensor(out=rt2[R, :], in0=pB[R, :], in1=sinT[R, :],
                                                                 op=ALU.mult), reads=[pB.k, sinT.k], writes=[rt2.k])
                    P.op("dve", lambda e, h=h: e.tensor_tensor(out=qThb[R, h, :], in0=rt1[R, :], in1=rt2[R, :],
                                                               op=ALU.add), reads=[rt1.k, rt2.k], writes=[qThb.k])
            for h0 in range(0, 8, 2):
                ch.append(lambda h0=h0: queries(h0))

            def q_mem():
                for h in range(4):
                    pb = nextbank(S)
                    P.mm(pb[:], [(bwin[:, kc, 256 + h * 128:256 + (h + 1) * 128], xTb[:, kc, :]) for kc in range(8)],
                         reads=[xTb.k] + bwin_tr, writes=[pb.k])
                    P.op("act", lambda e, pb=pb, h=h: e.activation(out=qmTb[:, h, :], in_=pb[:], func=AF.Copy),
                         reads=[pb.k], writes=[qmTb.k])
            ch.append(q_mem)
            return ch

        def mla_head(s, h, pending, per):
            T, b = s % NT, s % 2
            qThb = qTh[b]
            nkt = 4 * T + 4
            po = S["psf"][2 + (h % 2)]
            started = [False]

            def emit_front(j):
                a_min = max(0, j - 4 * T)
                qc = slice(a_min * 128, ST)
                pb = sbank[sb_i[0] % 2]
                sb_i[0] += 1
                P.mm(pb[:, qc], [(kT[:, h, j * 128:(j + 1) * 128], qThb[:, h, qc])],
                     reads=[kT_blk[j // 4], qThb.k], writes=[pb.k])
                pt = S["PT"][S["pt_i"] % len(S["PT"])]
                S["pt_i"] += 1
                P.op("act", lambda e: e.activation(out=pt[:, qc], in_=pb[:, qc], func=AF.Exp, scale=SC),
                     reads=[pb.k], writes=[pt.k])
                if j >= 4 * T:
                    P.op("pool", lambda e: e.memset(pt[64:128, a_min * 128:a_min * 128 + 64], 0.0),
                         reads=[], writes=[pt.k])
                return (j, a_min, pt)

            def emit_back(j, a_min, pt):
                fns = []
                for a in range(a_min, 4):
                    st = not started[0]
                    started[0] = True
                    fns.append(lambda e, a=a, st=st: e.matmul(
                        po[:, a * 65:(a + 1) * 65], pt[:, a * 128:(a + 1) * 128], Vaug[:, j, h, :],
                        start=st, stop=(j == 4 * T + a), skip_group_check=True))
                P.pe_multi(fns, reads=[pt.k, V_blk[j // 4]], writes=[po.k])

            prev = None
            for j in range(nkt):
                cur = emit_front(j)
                if prev is not None:
                    emit_back(*prev)
                prev = cur
                for _ in range(per):
                    if pending:
                        P.replay(pending.pop(0))
            emit_back(*prev)
            po3 = v3(po[:, 0:260], 4)
            P.op("dve", lambda e: e.reciprocal(out=rc4[:], in_=po3[:, :, 64:65]), reads=[po.k], writes=[rc4.k])
            P.op("dve", lambda e: e.tensor_tensor(
                out=hc_all[:, :, h * 64:(h + 1) * 64], in0=po3[:, :, 0:64],
                in1=rc4[:, :, 0:1].broadcast_to([128, 4, 64]), op=ALU.mult),
                 reads=[po.k, rc4.k], writes=[hc_all.k])

        for f in front_chunks(0):
            f()
        for s in range(NST):
            seq, b = s // NT, s % 2
            pending = []
            P.rec = pending
            mem_attention(C, S, seq, qmT[b], KmT, Vm, hcat, 512)
            if s + 1 < NST:
                for f in front_chunks(s + 1):
                    f()
            P.rec = None
            nsteps = 8 * (4 * (s % NT) + 4)
            per = (len(pending) + nsteps - 1) // nsteps
            for h in range(8):
                mla_head(s, h, pending, per)
            while pending:
                P.replay(pending.pop(0))
            out_proj_ln(C, S, hcat, xT[b], wout, wout_tr, xld, nl, x_view, x_trks[s], s, y, g_rep, b_rep,
                        o_view, out_trks[s])
        P.full_barrier()
        S["rot0"], S["nrot"] = 0, 6
        S.pop("cast_eng")
        S.pop("PTm")


def _consts_np():
    c = np.zeros((128, 4, 128), np.float32)
    c[:, 0, :] = np.eye(128, dtype=np.float32)
    s = np.arange(128)[:, None]
    l = np.arange(128)[None, :]
    c[:, 1, :] = ((s // 64 == l // 64) & (s <= l)).astype(np.float32)
    c[:, 2, :] = (s < 64).astype(np.float32) * np.ones((1, 128), np.float32)
    c[:, 3, :] = (s >= 64).astype(np.float32) * np.ones((1, 128), np.float32)
    return c


def _cf2_np():
    c = np.zeros((128, 4), np.float32)
    inv = (10000.0 ** (-np.arange(0, 32, 2, dtype=np.float32) / 32)).astype(np.float32)
    for p in range(64, 96):
        c[p, 0] = inv[(p - 64) % 16] / np.float32(2 * np.pi)
    c[:, 1] = -np.pi
    return c


W_SHAPES = {
    "a_w_in": [D, 2056], "a_b_igate": [4], "a_b_fgate": [4], "a_w_mem_kv": [D, D], "a_w_out": [D, D],
    "kv_w_down": [D, 288], "kv_norm_g": [256], "kv_w_uk": [256, 512], "kv_w_uv": [256, 512],
    "b_w_in": [D, 768], "b_q_norm_g": [256], "b_w_uq": [256, 768], "b_w_mem_kv": [D, D], "b_w_out": [D, D],
    "ln1_g": [2, D], "ln1_b": [2, D], "ffn_w_up": [2, D, DFF], "ffn_w_down": [2, DFF, D], "ln2_g": [2, D],
    "ln2_b": [2, D],
}


def build_program():
    nc = bass.Bass("TRN2", target_bir_lowering=False)
    dt = lambda n, sh: nc.dram_tensor(n, sh, F32, kind="ExternalInput").ap()
    x = dt("x", [NTOK, D])
    mem = dt("mem", [NSEQ, 256, D])
    pos = nc.dram_tensor("positions", [NSEQ, SEQ], I32, kind="ExternalInput").ap()
    w = {k: dt(k, sh) for k, sh in W_SHAPES.items()}
    consts = dt("consts", [128, 4, 128])
    cf2 = dt("cf2", [128, 4])
    out = nc.dram_tensor("out", [NTOK, D], F32, kind="ExternalOutput").ap()
    sc1 = nc.dram_tensor("scratch1", [NTOK, D], F32, kind="Internal").ap()
    sc2 = nc.dram_tensor("scratch2", [NTOK, D], F32, kind="Internal").ap()
    C = Ctx(nc)
    with nc.allow_low_precision("bf16 matmul operands, fp32 accumulation"), C.es:
        S = alloc_shared(C)
        load_consts(C, S, consts)
        xtr = [Trk("xd%d" % i) for i in range(NST)]
        t1 = [Trk("s1_%d" % i) for i in range(NST)]
        t2 = [Trk("s2_%d" % i) for i in range(NST)]
        otr = [Trk("od%d" % i) for i in range(NST)]
        mixer_a_phase(C, S, x, xtr, sc1, t1, mem, w["a_w_in"], w["a_b_igate"], w["a_b_fgate"], w["a_w_mem_kv"],
                      w["a_w_out"], w["ln1_g"][0], w["ln1_b"][0])
        ffn_phase(C, S, sc1, t1, sc2, t2, w["ffn_w_up"][0], w["ffn_w_down"][0], w["ln2_g"][0], w["ln2_b"][0], False)
        mixer_b_phase(C, S, sc2, t2, sc1, t1, mem, pos, w["kv_w_down"], w["kv_norm_g"], w["kv_w_uk"], w["kv_w_uv"],
                      w["b_w_in"], w["b_q_norm_g"], w["b_w_uq"], w["b_w_mem_kv"], w["b_w_out"], w["ln1_g"][1],
                      w["ln1_b"][1], cf2)
        ffn_phase(C, S, sc1, t1, out, otr, w["ffn_w_up"][1], w["ffn_w_down"][1], w["ln2_g"][1], w["ln2_b"][1], True)
        C.P.finish()
    return nc


def kernel(x, mem, positions, a_w_in, a_b_igate, a_b_fgate, a_w_mem_kv, a_w_out, kv_w_down, kv_norm_g, kv_w_uk,
           kv_w_uv, b_w_in, b_q_norm_g, b_w_uq, b_w_mem_kv, b_w_out, ln1_g, ln1_b, ffn_w_up, ffn_w_down, ln2_g,
           ln2_b):
    f32 = lambda a: np.ascontiguousarray(np.asarray(a), dtype=np.float32)
    shared = {
        "a_w_in": f32(a_w_in)[0], "a_b_igate": f32(a_b_igate)[0], "a_b_fgate": f32(a_b_fgate)[0],
        "a_w_mem_kv": f32(a_w_mem_kv)[0], "a_w_out": f32(a_w_out)[0], "kv_w_down": f32(kv_w_down),
        "kv_norm_g": f32(kv_norm_g), "kv_w_uk": f32(kv_w_uk), "kv_w_uv": f32(kv_w_uv), "b_w_in": f32(b_w_in)[0],
        "b_q_norm_g": f32(b_q_norm_g)[0], "b_w_uq": f32(b_w_uq)[0], "b_w_mem_kv": f32(b_w_mem_kv)[0],
        "b_w_out": f32(b_w_out)[0], "ln1_g": f32(ln1_g), "ln1_b": f32(ln1_b), "ffn_w_up": f32(ffn_w_up),
        "ffn_w_down": f32(ffn_w_down), "ln2_g": f32(ln2_g), "ln2_b": f32(ln2_b),
        "consts": _consts_np(), "cf2": _cf2_np(),
    }
    shared = {k: np.ascontiguousarray(v) for k, v in shared.items()}
    x = f32(x)
    mem = f32(mem)
    positions = np.ascontiguousarray(np.asarray(positions), dtype=np.int32)
    in_maps = []
    for c in range(NCORES):
        m = dict(shared)
        m["x"] = np.ascontiguousarray(x[c * NSEQ:(c + 1) * NSEQ].reshape(NTOK, D))
        m["mem"] = np.ascontiguousarray(mem[c * NSEQ:(c + 1) * NSEQ])
        m["positions"] = np.ascontiguousarray(positions[c * NSEQ:(c + 1) * NSEQ])
        in_maps.append(m)
    nc = build_program()
    res = run_bass_kernel_spmd(nc, in_maps, core_ids=list(range(NCORES)))
    outs = [np.asarray(r["out"], dtype=np.float32).reshape(NSEQ, SEQ, D) for r in res.results]
    return np.concatenate(outs, axis=0)
```

```python
import contextlib
import numpy as np
import concourse.bass as bass
import concourse.mybir as mybir
from concourse.bass_utils import run_bass_kernel_spmd

F32 = mybir.dt.float32
BF16 = mybir.dt.bfloat16
I32 = mybir.dt.int32
AF = mybir.ActivationFunctionType
ALU = mybir.AluOpType
AX = mybir.AxisListType

NCORES = 8
SEQ = 2048
D = 1024
DFF = 4096
NSEQ = 2
NTOK = NSEQ * SEQ
ST = 512
NST = NTOK // ST
ALPHA = 4.0 ** 0.25
LN_EPS = 1e-5
RMS_EPS = 1e-6
KEEP_WARM = True


class Trk:
    __slots__ = ("name", "w", "r", "dsem", "dcnt")

    def __init__(self, name):
        self.name = name
        self.w = None
        self.r = {}
        self.dsem = None
        self.dcnt = 0


class Prog:
    SEM_ROT = 12000

    def __init__(self, nc):
        self.nc = nc
        self.eng = {"pe": nc.tensor, "act": nc.scalar, "dve": nc.vector, "pool": nc.gpsimd,
                    "sp": nc.sync}
        self.sem = {}
        self.cnt = {}
        self.seen = {e: {} for e in self.eng}
        self.nsem = 0
        for e in self.eng:
            self._new_sem(e)
        self.out_tokens = []
        self.ninstr = 0
        self.last_tok = {}
        self.rec = None
        self.inter = None
        self._acc = 0.0
        self._in_replay = False
        self.dma_toks = {}
        self.free_dsems = []
        self.phase_trks = []

    def _alloc_sem(self, name):
        self.nsem += 1
        return self.nc.alloc_semaphore(name="%s_%d" % (name, self.nsem))

    def _new_sem(self, e):
        self.sem[e] = self._alloc_sem("s_" + e)
        self.cnt[e] = 0

    def _need(self, e, tok, skip_same):
        if tok is None:
            return
        sem, c, te = tok
        if skip_same and te == e and e == "pe":
            return
        if self.seen[e].get(sem, 0) >= c:
            return
        self.eng[e].wait_ge(sem, c)
        self.seen[e][sem] = c

    def _signal(self, e, ins):
        if self.cnt[e] >= self.SEM_ROT:
            self._new_sem(e)
        self.cnt[e] += 1
        ins.then_inc(self.sem[e], 1)
        self.last_tok[e] = (self.sem[e], self.cnt[e], e)
        return self.last_tok[e]

    def full_barrier(self):
        snap = dict(self.last_tok)
        dts = list(self.dma_toks.values())
        for e in self.eng:
            for o, tok in snap.items():
                if o != e:
                    self._need(e, tok, False)
            for tok in dts:
                self._need(e, tok, False)
        for t in self.phase_trks:
            self.free_dsems.append((t.dsem, t.dcnt))
            t.dsem = None
        self.phase_trks = []
        self.dma_toks = {}

    def _after_emit(self):
        if self.inter is None or self._in_replay:
            return
        pend, rate = self.inter
        self._acc += rate
        while self._acc >= 1.0 and pend:
            self._acc -= 1.0
            self._in_replay = True
            self.replay(pend.pop(0))
            self._in_replay = False

    def replay(self, item):
        kind, args, kw = item
        saved, self.rec = self.rec, None
        getattr(self, kind)(*args, **kw)
        self.rec = saved

    def op(self, e, fn, reads=(), writes=()):
        if self.rec is not None:
            self.rec.append(("op", (e, fn), dict(reads=list(reads), writes=list(writes))))
            return None
        for t in reads:
            self._need(e, t.w, False)
        for t in writes:
            self._need(e, t.w, True)
            for tok in t.r.values():
                self._need(e, tok, True)
        ins = fn(self.eng[e])
        tok = self._signal(e, ins)
        for t in writes:
            t.w = tok
            t.r = {}
        for t in reads:
            t.r[e] = tok
        self.ninstr += 1
        self._after_emit()
        return tok

    def mm(self, out, pairs, reads=(), writes=()):
        if self.rec is not None:
            self.rec.append(("mm", (out, list(pairs)), dict(reads=list(reads), writes=list(writes))))
            return None
        e = "pe"
        for t in reads:
            self._need(e, t.w, False)
        for t in writes:
            self._need(e, t.w, True)
            for tok in t.r.values():
                self._need(e, tok, True)
        n = len(pairs)
        ins = None
        for i, (l, r) in enumerate(pairs):
            ins = self.nc.tensor.matmul(out, l, r, start=(i == 0), stop=(i == n - 1))
        tok = self._signal(e, ins)
        for t in writes:
            t.w = tok
            t.r = {}
        for t in reads:
            t.r[e] = tok
        self.ninstr += n
        self._after_emit()
        return tok

    def pe_multi(self, fns, reads=(), writes=()):
        if self.rec is not None:
            self.rec.append(("pe_multi", (list(fns),), dict(reads=list(reads), writes=list(writes))))
            return None
        e = "pe"
        for t in reads:
            self._need(e, t.w, False)
        for t in writes:
            self._need(e, t.w, True)
            for tok in t.r.values():
                self._need(e, tok, True)
        ins = None
        for f in fns:
            ins = f(self.nc.tensor)
        tok = self._signal(e, ins)
        for t in writes:
            t.w = tok
            t.r = {}
        for t in reads:
            t.r[e] = tok
        self.ninstr += len(fns)
        self._after_emit()
        return tok

    def dma(self, q, out, in_, reads=(), writes=(), is_output=False, sem_trk=None):
        if self.rec is not None:
            self.rec.append(("dma", (q, out, in_), dict(reads=list(reads), writes=list(writes),
                                                        is_output=is_output, sem_trk=sem_trk)))
            return None
        e = q
        for t in reads:
            self._need(e, t.w, False)
        for t in writes:
            self._need(e, t.w, False)
            for tok in t.r.values():
                self._need(e, tok, False)
        trk = sem_trk if sem_trk is not None else (list(writes) + list(reads))[0]
        if trk.dsem is None:
            if self.free_dsems:
                trk.dsem, trk.dcnt = self.free_dsems.pop()
            else:
                trk.dsem = self._alloc_sem("d")
                trk.dcnt = 0
            self.phase_trks.append(trk)
        trk.dcnt += 16
        self.eng[e].dma_start(out=out, in_=in_).then_inc(trk.dsem, 16)
        tok = (trk.dsem, trk.dcnt, "dma")
        self.dma_toks[trk.dsem] = tok
        for t in writes:
            t.w = tok
            t.r = {}
        for t in reads:
            t.r["dma_%s" % trk.name] = tok
        if is_output:
            self.out_tokens.append(tok)
        self.ninstr += 1
        return tok

    def barrier_all(self, trks):
        for t in trks:
            self._need("sp", t.w, False)
            for tok in t.r.values():
                self._need("sp", tok, False)

    def finish(self):
        for tok in self.out_tokens:
            self._need("sp", tok, False)


class Tile:
    def __init__(self, t, name):
        self.t = t
        self.k = Trk(name)

    def __getitem__(self, idx):
        return self.t[idx]


class Ctx:
    def __init__(self, nc):
        self.nc = nc
        self.P = Prog(nc)
        self.es = contextlib.ExitStack()
        self.nid = 0

    def sb(self, name, shape, dt, es=None):
        self.nid += 1
        nm = "%s_%d" % (name, self.nid)
        t = (es or self.es).enter_context(self.nc.sbuf_tensor(nm, list(shape), dt))
        return Tile(t, nm)

    def ps(self, name, shape, dt, es=None):
        self.nid += 1
        nm = "%s_%d" % (name, self.nid)
        t = (es or self.es).enter_context(self.nc.psum_tensor(nm, list(shape), dt))
        return Tile(t, nm)


def load_weight_cast(C, wt, dram_view, nsplit, axis):
    P = C.P
    n = wt.t.shape[axis]
    step = n // nsplit
    trks = []
    for j in range(nsplit):
        sl = [slice(None)] * 3
        sl[axis] = slice(j * step, (j + 1) * step)
        sl = tuple(sl)
        k = Trk("%s_p%d" % (wt.k.name, j))
        P.dma("pool", wt.t[sl], dram_view[sl], writes=[k])
        trks.append(k)
    return trks


def layer_norm_tile(C, S, y, g_rep, b_rep, out):
    P = C.P
    st, mv, rstd, nmr = S["ln_st"], S["ln_mv"], S["ln_rstd"], S["ln_nmr"]
    xn = y
    for hh in range(2):
        P.op("dve", lambda e, hh=hh: e.bn_stats(out=st[:, hh, :], in_=y[:, hh * 512:(hh + 1) * 512]),
             reads=[y.k], writes=[st.k])
    P.op("dve", lambda e: e.bn_aggr(out=mv[:], in_=st[:]), reads=[st.k], writes=[mv.k])
    P.op("act", lambda e: e.activation(out=rstd[:], in_=mv[:, 1:2], func=AF.Ln, bias=S["eps_ln"][:], scale=1.0),
         reads=[mv.k, S["eps_ln"].k], writes=[rstd.k])
    P.op("act", lambda e: e.activation(out=rstd[:], in_=rstd[:], func=AF.Exp, scale=-0.5),
         reads=[rstd.k], writes=[rstd.k])
    P.op("dve", lambda e: e.tensor_scalar(out=nmr[:], in0=mv[:, 0:1], scalar1=-1.0, scalar2=None, op0=ALU.mult),
         reads=[mv.k], writes=[nmr.k])
    P.op("dve", lambda e: e.scalar_tensor_tensor(out=xn[:], in0=y[:], scalar=nmr[:, 0:1], in1=g_rep[:],
                                                 op0=ALU.add, op1=ALU.mult),
         reads=[y.k, nmr.k, g_rep.k], writes=[xn.k])
    P.op("dve", lambda e: e.scalar_tensor_tensor(out=out[:], in0=xn[:], scalar=rstd[:, 0:1], in1=b_rep[:],
                                                 op0=ALU.mult, op1=ALU.add),
         reads=[xn.k, rstd.k, b_rep.k], writes=[out.k])


def transpose_tokens(C, S, xin, xT, col0):
    P = C.P
    xb = S["xb"][S["xb_i"] % 2]
    S["xb_i"] += 1
    P.op(S.get("cast_eng", "act"), (lambda e: e.tensor_copy(out=xb[:], in_=xin[:])) if S.get("cast_eng") else
         (lambda e: e.activation(out=xb[:], in_=xin[:], func=AF.Copy)), reads=[xin.k], writes=[xb.k])
    pt = S["pst"][S["pst_i"] % 2]
    S["pst_i"] += 1
    ident = S["ident"]
    P.pe_multi([lambda e, c=c: e.transpose(out=pt[:, c * 128:(c + 1) * 128], in_=xb[:, c * 128:(c + 1) * 128],
                                           identity=ident[:]) for c in range(8)],
               reads=[xb.k, ident.k], writes=[pt.k])
    P.op("dve", lambda e: e.tensor_copy(out=xT[:, :, col0:col0 + 128],
                                        in_=pt[:, :].rearrange("p (c t) -> p c t", c=8)),
         reads=[pt.k], writes=[xT.k])


def ffn_phase(C, S, x_dram, x_trks, out_dram, out_trks, w_up_d, w_down_d, g_d, b_d, is_output):
    nc, P = C.nc, C.P
    with contextlib.ExitStack() as es:
        wup = C.sb("wup", [128, 8, DFF], BF16, es)
        wdn = C.sb("wdn", [128, 32, D], BF16, es)
        g_rep = C.sb("g_rep", [128, D], F32, es)
        b_rep = C.sb("b_rep", [128, D], F32, es)
        xld = [C.sb("xld", [128, D], F32, es) for _ in range(3)]
        xT = C.sb("xT", [128, 8, ST], BF16, es)
        hT = C.sb("hT", [128, 32, ST], BF16, es)
        rl = [C.sb("rl", [128, ST], BF16, es) for _ in range(2)]
        y = [C.sb("y", [128, D], F32, es) for _ in range(2)]

        P.dma("sp", g_rep[:], g_d.partition_broadcast(128), writes=[g_rep.k])
        P.dma("sp", b_rep[:], b_d.partition_broadcast(128), writes=[b_rep.k])
        x_view = x_dram.rearrange("(s a p) d -> s a p d", a=4, p=128)
        o_view = out_dram.rearrange("(s a p) d -> s a p d", a=4, p=128)
        up_tr = load_weight_cast(C, wup, w_up_d.rearrange("(c p) f -> p c f", p=128), 8, 2)
        dn_tr = load_weight_cast(C, wdn, w_down_d.rearrange("(c p) f -> p c f", p=128), 8, 1)

        psb = S["psf"]
        nb = 0
        nl = 0
        for s in range(NST):
            for a in range(4):
                xi = xld[nl % 3]
                nl += 1
                P.dma("sp", xi[:], x_view[s, a], reads=[x_trks[s]], writes=[xi.k])
                transpose_tokens(C, S, xi, xT, a * 128)
            for fc in range(32):
                pb = psb[nb % 4]
                nb += 1
                P.mm(pb[:], [(wup[:, kc, fc * 128:(fc + 1) * 128], xT[:, kc, :]) for kc in range(8)],
                     reads=[xT.k, up_tr[fc // 4]], writes=[pb.k])
                r = rl[fc % 2]
                P.op("act", lambda e, r=r, pb=pb: e.activation(out=r[:], in_=pb[:], func=AF.Relu),
                     reads=[pb.k], writes=[r.k])
                P.op("dve", lambda e, r=r, fc=fc: e.tensor_tensor(out=hT[:, fc, :], in0=r[:], in1=r[:],
                                                                   op=ALU.mult),
                     reads=[r.k], writes=[hT.k])
            for a in range(4):
                xi = xld[nl % 3]
                nl += 1
                P.dma("sp", xi[:], x_view[s, a], reads=[x_trks[s]], writes=[xi.k])
                yy = y[a % 2]
                for dh in range(2):
                    pb = psb[nb % 4]
                    nb += 1
                    P.mm(pb[:], [(hT[:, fc, a * 128:(a + 1) * 128], wdn[:, fc, dh * 512:(dh + 1) * 512])
                                 for fc in range(32)],
                         reads=[hT.k] + dn_tr, writes=[pb.k])
                    P.op("dve", lambda e, yy=yy, pb=pb, dh=dh, xi=xi: e.scalar_tensor_tensor(
                        out=yy[:, dh * 512:(dh + 1) * 512], in0=xi[:, dh * 512:(dh + 1) * 512],
                        scalar=ALPHA, in1=pb[:], op0=ALU.mult, op1=ALU.add),
                         reads=[xi.k, pb.k], writes=[yy.k])
                layer_norm_tile(C, S, yy, g_rep, b_rep, yy)
                P.dma("pool", o_view[s, a], yy[:], reads=[yy.k], writes=[out_trks[s]],
                      is_output=is_output, sem_trk=yy.k)
        P.full_barrier()


def alloc_shared(C):
    S = {}
    S["ident"] = C.sb("ident", [128, 128], BF16)
    S["psf"] = [C.ps("psf", [128, 512], F32) for _ in range(6)]
    S["pst"] = [C.ps("pst", [128, 1024], BF16) for _ in range(2)]
    S["pst_i"] = 0
    S["nb"] = 0
    S["nrot"] = 6
    S["rot0"] = 0
    S["xb"] = [C.sb("xb", [128, D], BF16) for _ in range(2)]
    S["xb_i"] = 0
    S["ln_st"] = C.sb("ln_st", [128, 2, 6], F32)
    S["ln_mv"] = C.sb("ln_mv", [128, 2], F32)
    S["ln_rstd"] = C.sb("ln_rstd", [128, 1], F32)
    S["ln_nmr"] = C.sb("ln_nmr", [128, 1], F32)
    S["eps_ln"] = C.sb("eps_ln", [128, 1], F32)
    S["eps_rms"] = C.sb("eps_rms", [128, 1], F32)
    C.P.op("pool", lambda e: e.memset(S["eps_ln"][:], LN_EPS), writes=[S["eps_ln"].k])
    C.P.op("pool", lambda e: e.memset(S["eps_rms"][:], RMS_EPS), writes=[S["eps_rms"].k])
    return S


def nextbank(S):
    b = S["psf"][S["rot0"] + S["nb"] % S["nrot"]]
    S["nb"] += 1
    return b


def v3(ap, n):
    return ap.rearrange("p (h d) -> p h d", h=n)


def load_consts(C, S, consts_d):
    P = C.P
    S["cf"] = C.sb("cf", [128, 4, 128], F32)
    S["maskb"] = C.sb("maskb", [128, 128], BF16)
    P.dma("sp", S["cf"][:], consts_d, writes=[S["cf"].k])
    P.dma("pool", S["ident"][:], consts_d[:, 0, :], writes=[S["ident"].k])
    P.dma("pool", S["maskb"][:], consts_d[:, 1, :], writes=[S["maskb"].k])
    S["one1"] = C.sb("one1", [128, 1], F32)
    S["ln8"] = C.sb("ln8", [128, 1], F32)
    P.op("pool", lambda e: e.memset(S["one1"][:], 1.0), writes=[S["one1"].k])
    P.op("pool", lambda e: e.memset(S["ln8"][:], float(np.log(0.125))), writes=[S["ln8"].k])


def mem_kv_precompute(C, S, es, mem_d, wmk, wmk_tr, xld, KmT, Vm):
    P = C.P
    memT = C.sb("memT", [128, 8, 256], BF16, es)
    P.op("pool", lambda e: e.memset(Vm[:], 1.0), writes=[Vm.k])
    n = 0
    for q in range(NSEQ):
        for mt in range(2):
            xi = xld[n % len(xld)]
            n += 1
            P.dma("sp", xi[:], mem_d[q, mt * 128:(mt + 1) * 128, :], writes=[xi.k])
            transpose_tokens(C, S, xi, memT, mt * 128)
        for h in range(4):
            pb = nextbank(S)
            P.mm(pb[:, 0:256], [(wmk[:, kc, h * 128:(h + 1) * 128], memT[:, kc, :]) for kc in range(8)],
                 reads=[memT.k] + wmk_tr, writes=[pb.k])
            P.op("act", lambda e, pb=pb, q=q, h=h: e.activation(out=KmT[:, q, h, :], in_=pb[:, 0:256], func=AF.Copy),
                 reads=[pb.k], writes=[KmT.k])
        for mt in range(2):
            pb = nextbank(S)
            P.mm(pb[:], [(memT[:, kc, mt * 128:(mt + 1) * 128], wmk[:, kc, 512:1024]) for kc in range(8)],
                 reads=[memT.k] + wmk_tr, writes=[pb.k])
            P.op("act", lambda e, pb=pb, q=q, mt=mt: e.activation(out=Vm[:, q, mt, :, 0:128], in_=v3(pb[:], 4),
                                                                  func=AF.Copy),
                 reads=[pb.k], writes=[Vm.k])
    return memT


def mem_attention(C, S, seq, qmT, KmT, Vm, hcat, col0):
    P = C.P
    for h in range(4):
        pts = []
        for mt in range(2):
            pb = nextbank(S)
            P.mm(pb[:], [(KmT[:, seq, h, mt * 128:(mt + 1) * 128], qmT[:, h, :])],
                 reads=[KmT.k, qmT.k], writes=[pb.k])
            pt = S["PT"][S["pt_i"] % len(S["PT"])]
            S["pt_i"] += 1
            P.op("act", lambda e, pt=pt, pb=pb: e.activation(out=pt[:], in_=pb[:], func=AF.Exp, scale=128.0 ** -0.5),
                 reads=[pb.k], writes=[pt.k])
            pts.append(pt)
        for a in range(4):
            pb = nextbank(S)
            P.mm(pb[:, 0:129], [(pts[mt][:, a * 128:(a + 1) * 128], Vm[:, seq, mt, h, :]) for mt in range(2)],
                 reads=[pts[0].k, pts[1].k, Vm.k], writes=[pb.k])
            rc = S["rc"][S["rc_i"] % 2]
            S["rc_i"] += 1
            P.op("dve", lambda e, rc=rc, pb=pb: e.reciprocal(out=rc[:], in_=pb[:, 128:129]),
                 reads=[pb.k], writes=[rc.k])
            P.op("dve", lambda e, rc=rc, pb=pb, a=a, h=h: e.tensor_scalar(
                out=hcat[a][:, col0 + h * 128:col0 + (h + 1) * 128], in0=pb[:, 0:128], scalar1=rc[:, 0:1],
                scalar2=None, op0=ALU.mult), reads=[pb.k, rc.k], writes=[hcat[a].k])


def out_proj_ln(C, S, hcat, hcT, wout, wout_tr, xld, nl, x_view, x_trk, s, y, g_rep, b_rep, o_view, o_trk):
    P = C.P
    for a in range(4):
        pt = S["pst"][S["pst_i"] % 2]
        S["pst_i"] += 1
        ident = S["ident"]
        P.pe_multi([lambda e, c=c, pt=pt, a=a: e.transpose(out=pt[:, c * 128:(c + 1) * 128],
                                                         in_=hcat[a][:, c * 128:(c + 1) * 128],
                                                         identity=ident[:]) for c in range(8)],
                   reads=[hcat[a].k, ident.k], writes=[pt.k])
        P.op("dve", lambda e, pt=pt, a=a: e.tensor_copy(out=hcT[:, :, a * 128:(a + 1) * 128], in_=v3(pt[:, :], 8)),
             reads=[pt.k], writes=[hcT.k])
    for a in range(4):
        xi = xld[nl[0] % len(xld)]
        nl[0] += 1
        P.dma("sp", xi[:], x_view[s, a], reads=[x_trk], writes=[xi.k])
        yy = y[a % 2]
        for dh in range(2):
            pb = nextbank(S)
            P.mm(pb[:], [(hcT[:, kc, a * 128:(a + 1) * 128], wout[:, kc, dh * 512:(dh + 1) * 512])
                         for kc in range(8)], reads=[hcT.k] + wout_tr, writes=[pb.k])
            P.op("dve", lambda e, yy=yy, pb=pb, dh=dh, xi=xi: e.scalar_tensor_tensor(
                out=yy[:, dh * 512:(dh + 1) * 512], in0=xi[:, dh * 512:(dh + 1) * 512],
                scalar=ALPHA, in1=pb[:], op0=ALU.mult, op1=ALU.add),
                 reads=[xi.k, pb.k], writes=[yy.k])
        layer_norm_tile(C, S, yy, g_rep, b_rep, yy)
        P.dma("pool", o_view[s, a], yy[:], reads=[yy.k], writes=[o_trk], sem_trk=yy.k)


def mixer_a_phase(C, S, x_dram, x_trks, out_dram, out_trks, mem_d, w_in_d, bi_d, bf_d, wmk_d, w_out_d, g_d, b_d):
    nc, P = C.nc, C.P
    with contextlib.ExitStack() as es:
        win = C.sb("win", [128, 8, 2056], BF16, es)
        wout = C.sb("wout", [128, 8, D], BF16, es)
        g_rep = C.sb("g_rep", [128, D], F32, es)
        b_rep = C.sb("b_rep", [128, D], F32, es)
        bias_rep = C.sb("bias_rep", [128, 8], F32, es)
        xld = [C.sb("xld", [128, D], F32, es) for _ in range(3)]
        KmT = C.sb("KmT", [128, NSEQ, 4, 256], BF16, es)
        Vm = C.sb("Vm", [128, NSEQ, 2, 4, 129], BF16, es)
        P.dma("sp", g_rep[:], g_d.partition_broadcast(128), writes=[g_rep.k])
        P.dma("sp", b_rep[:], b_d.partition_broadcast(128), writes=[b_rep.k])
        P.dma("sp", bias_rep[:, 0:4], bi_d.partition_broadcast(128), writes=[bias_rep.k])
        P.dma("sp", bias_rep[:, 4:8], bf_d.partition_broadcast(128), writes=[bias_rep.k])
        with contextlib.ExitStack() as es2:
            wmk = C.sb("wmk", [128, 8, D], BF16, es2)
            wmk_tr = load_weight_cast(C, wmk, wmk_d.rearrange("(c p) f -> p c f", p=128), 2, 1)
            win_tr = load_weight_cast(C, win, w_in_d.rearrange("(c p) f -> p c f", p=128), 2, 1)
            wout_tr = load_weight_cast(C, wout, w_out_d.rearrange("(c p) f -> p c f", p=128), 2, 1)
            mem_kv_precompute(C, S, es2, mem_d, wmk, wmk_tr, xld, KmT, Vm)
            P.full_barrier()
        xT = [C.sb("xT", [128, 8, ST], BF16, es) for _ in range(2)]
        qT = [C.sb("qT", [64, 4, ST], BF16, es) for _ in range(2)]
        kT = [C.sb("kT", [64, 4, ST], BF16, es) for _ in range(2)]
        qz = [C.sb("qz", [64, 4, 4, 2, 128], BF16, es) for _ in range(2)]
        qmT = [C.sb("qmT", [128, 4, ST], BF16, es) for _ in range(2)]
        gts = C.sb("gts", [128, 8], F32, es)
        lfn = C.sb("lfn", [128, 4], F32, es)
        tadd = C.sb("tadd", [128, 4], F32, es)
        colf = C.sb("colf", [128, 4], F32, es)
        enb = C.sb("enb", [128, 4], F32, es)
        eg = [C.sb("eg", [128, 2, 4], F32, es) for _ in range(2)]
        kc_t = C.sb("kc", [128, 4, 64], BF16, es)
        vaug = [C.sb("vaug", [128, 4, 129], BF16, es) for _ in range(2)]
        e_o = C.sb("e_o", [128, 512], F32, es)
        atmp = C.sb("atmp", [128, 4, 128], BF16, es)
        AT = C.sb("AT", [128, 4, 128], BF16, es)
        Sst = C.sb("Sst", [64, 4, 129], F32, es)
        Cf = C.sb("Cf", [64, 4, 129], F32, es)
        Cb = [C.sb("Cb", [64, 4, 129], BF16, es) for _ in range(2)]
        den = C.sb("den", [128, 2, 1], F32, es)
        hcat2 = [[C.sb("hcat", [128, D], BF16, es) for _ in range(4)] for _ in range(2)]
        hcT = C.sb("hcT", [128, 8, ST], BF16, es)
        y = [C.sb("y", [128, D], F32, es) for _ in range(2)]
        S["PT"] = [C.sb("PT", [128, ST], BF16, es) for _ in range(4)]
        S["pt_i"] = 0
        S["rc"] = [C.sb("rc", [128, 1], F32, es) for _ in range(2)]
        S["rc_i"] = 0
        for qq in qz:
            P.op("pool", lambda e, qq=qq: e.memset(qq[:], 0.0), writes=[qq.k])
        for vv in vaug:
            P.op("pool", lambda e, vv=vv: e.memset(vv[:], 1.0), writes=[vv.k])

        x_view = x_dram.rearrange("(s a p) d -> s a p d", a=4, p=128)
        o_view = out_dram.rearrange("(s a p) d -> s a p d", a=4, p=128)
        cf = S["cf"]
        maskb = S["maskb"]
        nl = [0]
        NT = NST // NSEQ

        def front(s):
            b, seq = s % 2, s // NT
            xTb, qTb, kTb, qzb, qmTb = xT[b], qT[b], kT[b], qz[b], qmT[b]
            for a in range(4):
                xi = xld[nl[0] % len(xld)]
                nl[0] += 1
                P.dma("sp", xi[:], x_view[s, a], reads=[x_trks[s]], writes=[xi.k])
                transpose_tokens(C, S, xi, xTb, a * 128)
            for h in range(4):
                for (dst, c0) in ((qTb, 0), (kTb, 256)):
                    pb = nextbank(S)
                    P.mm(pb[0:64, :], [(win[:, kc, c0 + h * 64:c0 + (h + 1) * 64], xTb[:, kc, :]) for kc in range(8)],
                         reads=[xTb.k] + win_tr, writes=[pb.k])
                    P.op("act", lambda e, pb=pb, dst=dst, h=h: e.activation(out=dst[:, h, :], in_=pb[0:64, :],
                                                                            func=AF.Copy),
                         reads=[pb.k], writes=[dst.k])
                P.op("pool", lambda e, h=h: e.tensor_copy(
                    out=bass.AP(qzb.t, h * 1024, [[4096, 64], [256, 4], [192, 2], [1, 64]]),
                    in_=qTb[:, h, :].rearrange("p (a c j) -> p a c j", a=4, c=2)),
                     reads=[qTb.k], writes=[qzb.k])
                pb = nextbank(S)
                P.mm(pb[:], [(win[:, kc, 1544 + h * 128:1544 + (h + 1) * 128], xTb[:, kc, :]) for kc in range(8)],
                     reads=[xTb.k] + win_tr, writes=[pb.k])
                P.op("act", lambda e, pb=pb, h=h: e.activation(out=qmTb[:, h, :], in_=pb[:], func=AF.Copy),
                     reads=[pb.k], writes=[qmTb.k])
            mem_attention(C, S, seq, qmTb, KmT, Vm, hcat2[b], 512)

        def tail(s):
            out_proj_ln(C, S, hcat2[s % 2], hcT, wout, wout_tr, xld, nl, x_view, x_trks[s], s, y, g_rep, b_rep,
                        o_view, out_trks[s])

        def tok_loop(s):
            b = s % 2
            xTb, qTb, kTb, qzb, hcat = xT[b], qT[b], kT[b], qz[b], hcat2[b]
            for a in range(4):
                t = s * 4 + a
                par = t % 2
                first = (t % (SEQ // 128) == 0)
                cols = slice(a * 128, (a + 1) * 128)
                va = vaug[par]
                pg = nextbank(S)
                P.mm(pg[:, 0:8], [(xTb[:, kc, cols], win[:, kc, 1536:1544]) for kc in range(8)],
                     reads=[xTb.k] + win_tr, writes=[pg.k])
                P.op("dve", lambda e, pg=pg: e.tensor_tensor(out=gts[:], in0=pg[:, 0:8], in1=bias_rep[:], op=ALU.add),
                     reads=[pg.k, bias_rep.k], writes=[gts.k])
                P.op("act", lambda e: e.activation(out=lfn[:], in_=gts[:, 4:8], func=AF.Exp, scale=-1.0),
                     reads=[gts.k], writes=[lfn.k])
                P.op("act", lambda e: e.activation(out=lfn[:], in_=lfn[:], func=AF.Ln, bias=S["one1"][:], scale=1.0),
                     reads=[lfn.k, S["one1"].k], writes=[lfn.k])
                pc = nextbank(S)
                P.mm(pc[:, 0:4], [(cf[:, 1, :], lfn[:])], reads=[cf.k, lfn.k], writes=[pc.k])
                P.mm(pc[:, 4:8], [(cf[:, 2, :], lfn[:])], reads=[cf.k, lfn.k], writes=[pc.k])
                P.mm(pc[:, 8:12], [(cf[:, 3, :], lfn[:])], reads=[cf.k, lfn.k], writes=[pc.k])
                P.op("dve", lambda e, pc=pc: e.tensor_tensor(out=tadd[:], in0=gts[:, 0:4], in1=pc[:, 0:4], op=ALU.add),
                     reads=[gts.k, pc.k], writes=[tadd.k])
                P.op("act", lambda e: e.activation(out=colf[:], in_=tadd[:], func=AF.Exp, bias=S["ln8"][:], scale=1.0),
                     reads=[tadd.k, S["ln8"].k], writes=[colf.k])
                P.op("act", lambda e, pc=pc: e.activation(out=enb[:], in_=pc[:, 0:4], func=AF.Exp),
                     reads=[pc.k], writes=[enb.k])
                P.op("act", lambda e, pc=pc, par=par: e.activation(out=eg[par][:], in_=v3(pc[:, 4:12], 2),
                                                                   func=AF.Exp, scale=-1.0),
                     reads=[pc.k], writes=[eg[par].k])
                pk = nextbank(S)
                P.mm(pk[:, 0:256], [(xTb[:, kc, cols], win[:, kc, 256:512]) for kc in range(8)],
                     reads=[xTb.k] + win_tr, writes=[pk.k])
                P.op("dve", lambda e, pk=pk: e.tensor_tensor(
                    out=kc_t[:], in0=v3(pk[:, 0:256], 4), in1=colf[:, 0:4].unsqueeze(2).broadcast_to([128, 4, 64]),
                    op=ALU.mult), reads=[pk.k, colf.k], writes=[kc_t.k])
                pv = nextbank(S)
                P.mm(pv[:], [(xTb[:, kc, cols], win[:, kc, 512:1024]) for kc in range(8)],
                     reads=[xTb.k] + win_tr, writes=[pv.k])
                P.op("act", lambda e, pv=pv, va=va: e.activation(out=va[:, :, 0:128], in_=v3(pv[:], 4), func=AF.Copy),
                     reads=[pv.k], writes=[va.k])
                po = nextbank(S)
                P.mm(po[:], [(xTb[:, kc, cols], win[:, kc, 1024:1536]) for kc in range(8)],
                     reads=[xTb.k] + win_tr, writes=[po.k])
                P.op("act", lambda e, po=po: e.activation(out=e_o[:], in_=po[:], func=AF.Exp, scale=-1.0),
                     reads=[po.k], writes=[e_o.k])
                P.op("act", lambda e: e.activation(out=e_o[:], in_=e_o[:], func=AF.Ln, bias=S["one1"][:], scale=1.0),
                     reads=[e_o.k, S["one1"].k], writes=[e_o.k])
                P.op("act", lambda e: e.activation(out=e_o[:], in_=e_o[:], func=AF.Exp, scale=-1.0),
                     reads=[e_o.k], writes=[e_o.k])
                pa = nextbank(S)
                for h in range(4):
                    P.mm(pa[:, h * 128:(h + 1) * 128], [(kTb[:, h, cols], qTb[:, h, cols])],
                         reads=[kTb.k, qTb.k], writes=[pa.k])
                P.op("dve", lambda e, pa=pa: e.tensor_tensor(
                    out=atmp[:], in0=v3(pa[:], 4), in1=colf[:, 0:4].unsqueeze(2).broadcast_to([128, 4, 128]),
                    op=ALU.mult), reads=[pa.k, colf.k], writes=[atmp.k])
                P.op("pool", lambda e: e.tensor_tensor(
                    out=AT[:], in0=atmp[:], in1=maskb[:, :].unsqueeze(1).broadcast_to([128, 4, 128]), op=ALU.mult),
                     reads=[atmp.k, maskb.k], writes=[AT.k])
                for c in range(2):
                    if first and c == 0:
                        P.op("dve", lambda e: e.memset(Cf[:], 0.0), writes=[Cf.k])
                        P.op("pool", lambda e: e.memset(Cb[0][:], 0.0), writes=[Cb[0].k])
                    else:
                        egp = eg[par][0:64, 0, :] if c == 1 else eg[1 - par][0:64, 1, :]
                        egk = eg[par].k if c == 1 else eg[1 - par].k
                        P.op("dve", lambda e, egp=egp: e.tensor_tensor(
                            out=Cf[:], in0=Sst[:], in1=egp.unsqueeze(2).broadcast_to([64, 4, 129]), op=ALU.mult),
                             reads=[Sst.k, egk], writes=[Cf.k])
                        P.op("act", lambda e, c=c: e.activation(out=Cb[c][:], in_=Cf[:], func=AF.Copy),
                             reads=[Cf.k], writes=[Cb[c].k])
                    rows = slice(c * 64, (c + 1) * 64)
                    for hp in range(2):
                        pu = nextbank(S)
                        for j in range(2):
                            h = 2 * hp + j
                            P.mm(pu[0:64, j * 129:(j + 1) * 129], [(kc_t[rows, h, :], va[rows, h, :])],
                                 reads=[kc_t.k, va.k], writes=[pu.k])
                        P.op("dve", lambda e, pu=pu, hp=hp: e.tensor_tensor(
                            out=Sst[:, 2 * hp:2 * hp + 2, :], in0=Cf[:, 2 * hp:2 * hp + 2, :],
                            in1=v3(pu[0:64, 0:258], 2), op=ALU.add),
                             reads=[Cf.k, pu.k], writes=[Sst.k])
                for hp in range(2):
                    pn = nextbank(S)
                    for j in range(2):
                        h = 2 * hp + j
                        P.mm(pn[:, j * 129:(j + 1) * 129],
                             [(AT[:, h, :], va[:, h, :]),
                              (qzb[:, h, a, 0, :], Cb[0][:, h, :]),
                              (qzb[:, h, a, 1, :], Cb[1][:, h, :])],
                             reads=[AT.k, va.k, qzb.k, Cb[0].k, Cb[1].k], writes=[pn.k])
                    pn3 = v3(pn[:, 0:258], 2)
                    P.op("dve", lambda e, pn3=pn3, hp=hp: e.tensor_tensor(
                        out=den[:], in0=pn3[:, :, 128:129], in1=enb[:, 2 * hp:2 * hp + 2].unsqueeze(2),
                        op=ALU.max), reads=[pn.k, enb.k], writes=[den.k])
                    P.op("dve", lambda e, pn3=pn3: e.scalar_tensor_tensor(
                        out=den[:], in0=pn3[:, :, 128:129], scalar=-1.0, in1=den[:], op0=ALU.mult, op1=ALU.max),
                         reads=[pn.k, den.k], writes=[den.k])
                    P.op("dve", lambda e: e.reciprocal(out=den[:], in_=den[:]), reads=[den.k], writes=[den.k])
                    for j in range(2):
                        h = 2 * hp + j
                        P.op("dve", lambda e, pn=pn, j=j, h=h, a=a: e.scalar_tensor_tensor(
                            out=hcat[a][:, h * 128:(h + 1) * 128], in0=pn[:, j * 129:j * 129 + 128],
                            scalar=den[:, j, :], in1=e_o[:, h * 128:(h + 1) * 128], op0=ALU.mult, op1=ALU.mult),
                             reads=[pn.k, den.k, e_o.k], writes=[hcat[a].k])

        def side(fn, *a):
            S["rot0"], S["nrot"] = 4, 2
            fn(*a)
            S["rot0"], S["nrot"] = 0, 4

        side(front, 0)
        for s in range(NST):
            pending = []
            P.rec = pending
            if s > 0:
                side(tail, s - 1)
            if s + 1 < NST:
                side(front, s + 1)
            P.rec = None
            S["rot0"], S["nrot"] = 0, 4
            P.inter = (pending, len(pending) / 260.0 + 0.02)
            P._acc = 0.0
            tok_loop(s)
            P.inter = None
            while pending:
                P.replay(pending.pop(0))
        side(tail, NST - 1)
        P.full_barrier()
        S["rot0"], S["nrot"] = 0, 6


def rms_rows(C, S, pb, ncols, out_bf, scr):
    P = C.P
    ss, rs = S["rms_ss"], S["rms_rs"]
    if scr is None:
        scr = nextbank(S)
    P.op("dve", lambda e: e.memset(ss[:], 0.0), writes=[ss.k])
    P.op("act", lambda e: e.activation(out=scr[:, 0:ncols], in_=pb[:, 0:ncols], func=AF.Square, accum_out=ss[:]),
         reads=[pb.k], writes=[scr.k, ss.k])
    P.op("act", lambda e: e.activation(out=rs[:], in_=ss[:], func=AF.Ln, bias=S["eps_rms"][:], scale=1.0 / ncols),
         reads=[ss.k, S["eps_rms"].k], writes=[rs.k])
    P.op("act", lambda e: e.activation(out=rs[:], in_=rs[:], func=AF.Exp, scale=-0.5), reads=[rs.k], writes=[rs.k])
    P.op("dve", lambda e: e.tensor_scalar(out=out_bf[:, 0:ncols], in0=pb[:, 0:ncols], scalar1=rs[:, 0:1],
                                          scalar2=None, op0=ALU.mult), reads=[pb.k, rs.k], writes=[out_bf.k])


def transpose_cols(C, S, src_bf, nchunk, dstT, col0):
    P = C.P
    pt = S["pst"][S["pst_i"] % 2]
    S["pst_i"] += 1
    ident = S["ident"]
    P.pe_multi([lambda e, c=c: e.transpose(out=pt[:, c * 128:(c + 1) * 128], in_=src_bf[:, c * 128:(c + 1) * 128],
                                           identity=ident[:]) for c in range(nchunk)],
               reads=[src_bf.k, ident.k], writes=[pt.k])
    P.op("dve", lambda e: e.tensor_copy(out=dstT[:, 0:nchunk, col0:col0 + 128], in_=v3(pt[:, 0:nchunk * 128], nchunk)),
         reads=[pt.k], writes=[dstT.k])


def mixer_b_phase(C, S, x_dram, x_trks, out_dram, out_trks, mem_d, pos_d, wdown_d, gkv_d, wuk_d, wuv_d,
                  bwin_d, gq_d, wuq_d, wmk_d, w_out_d, g_d, b_d, cf2_d):
    nc, P = C.nc, C.P
    SC = 96.0 ** -0.5
    NT = NST // NSEQ
    with contextlib.ExitStack() as es:
        wdown = C.sb("wdown", [128, 8, 288], BF16, es)
        wdr = C.sb("wdr", [128, 8, 96], BF16, es)
        wdrot = C.sb("wdrot", [128, 8, 96], BF16, es)
        wuk = C.sb("wuk", [128, 2, 512], BF16, es)
        wuv = C.sb("wuv", [128, 2, 512], BF16, es)
        wuq = C.sb("wuq", [128, 2, 768], BF16, es)
        wuqrot = C.sb("wuqrot", [128, 2, 8, 96], BF16, es)
        gk = C.sb("gk", [128, 2], F32, es)
        gq = C.sb("gq", [128, 2], F32, es)
        bwin = C.sb("bwin", [128, 8, 768], BF16, es)
        wout = C.sb("wout", [128, 8, D], BF16, es)
        g_rep = C.sb("g_rep", [128, D], F32, es)
        b_rep = C.sb("b_rep", [128, D], F32, es)
        cf2 = C.sb("cf2", [128, 4], F32, es)
        xld = [C.sb("xld", [128, D], F32, es) for _ in range(2)]
        KmT = C.sb("KmT", [128, NSEQ, 4, 256], BF16, es)
        Vm = C.sb("Vm", [128, NSEQ, 2, 4, 129], BF16, es)
        P.dma("sp", g_rep[:], g_d.partition_broadcast(128), writes=[g_rep.k])
        P.dma("sp", b_rep[:], b_d.partition_broadcast(128), writes=[b_rep.k])
        P.dma("sp", cf2[:], cf2_d, writes=[cf2.k])
        for kc in range(2):
            P.dma("sp", gk[:, kc:kc + 1], gkv_d[kc * 128:(kc + 1) * 128].rearrange("(p o) -> p o", o=1), writes=[gk.k])
            P.dma("sp", gq[:, kc:kc + 1], gq_d[kc * 128:(kc + 1) * 128].rearrange("(p o) -> p o", o=1), writes=[gq.k])
        with contextlib.ExitStack() as es2:
            wmk = C.sb("wmk", [128, 8, D], BF16, es2)
            wst = C.sb("wst", [128, 2, 768], F32, es2)
            wmk_tr = load_weight_cast(C, wmk, wmk_d.rearrange("(c p) f -> p c f", p=128), 2, 1)
            wdown_tr = load_weight_cast(C, wdown, wdown_d.rearrange("(c p) f -> p c f", p=128), 1, 1)
            bwin_tr = load_weight_cast(C, bwin, bwin_d.rearrange("(c p) f -> p c f", p=128), 2, 1)
            wout_tr = load_weight_cast(C, wout, w_out_d.rearrange("(c p) f -> p c f", p=128), 2, 1)
            for (dst, src_d, gg, ncol) in ((wuk, wuk_d, gk, 512), (wuv, wuv_d, gk, 512), (wuq, wuq_d, gq, 768)):
                P.dma("sp", wst[:, :, 0:ncol], src_d.rearrange("(c p) f -> p c f", p=128), writes=[wst.k])
                for kc in range(2):
                    P.op("dve", lambda e, dst=dst, gg=gg, kc=kc, ncol=ncol: e.tensor_scalar(
                        out=dst[:, kc, :], in0=wst[:, kc, 0:ncol], scalar1=gg[:, kc:kc + 1], scalar2=None,
                        op0=ALU.mult), reads=[wst.k, gg.k], writes=[dst.k])
            mem_kv_precompute(C, S, es2, mem_d, wmk, wmk_tr, xld, KmT, Vm)
            P.full_barrier()
        xT = [C.sb("xT", [128, 8, ST], BF16, es) for _ in range(2)]
        uu = C.sb("uu", [96, ST], F32, es)
        u2 = C.sb("u2", [96, ST], F32, es)
        cosT = C.sb("cosT", [96, ST], F32, es)
        sinT = C.sb("sinT", [96, ST], F32, es)
        kT = C.sb("kT", [96, 8, SEQ], BF16, es)
        Vaug = C.sb("Vaug", [128, 16, 8, 65], BF16, es)
        kT_blk = [Trk("kTb%d" % i) for i in range(NT)]
        V_blk = [Trk("Vb%d" % i) for i in range(NT)]
        ckn = C.sb("ckn", [128, 256], BF16, es)
        ckT = [C.sb("ckT", [128, 2, ST], BF16, es) for _ in range(2)]
        cqT = [C.sb("cqT", [128, 2, ST], BF16, es) for _ in range(2)]
        rt1 = C.sb("rt1", [96, ST], F32, es)
        rt2 = C.sb("rt2", [96, ST], F32, es)
        kr = C.sb("kr", [96, ST], BF16, es)
        qTh = [C.sb("qTh", [96, 8, ST], BF16, es) for _ in range(2)]
        qmT = [C.sb("qmT", [128, 4, ST], BF16, es) for _ in range(2)]
        hc_all = C.sb("hc_all", [128, 4, D], BF16, es)
        y = [C.sb("y", [128, D], F32, es) for _ in range(2)]
        scr = None
        rc4 = C.sb("rc4", [128, 4, 1], F32, es)
        S["PT"] = [C.sb("PT", [128, ST], BF16, es) for _ in range(3)]
        S["pt_i"] = 0
        S["rc"] = [C.sb("rc", [128, 1], F32, es) for _ in range(2)]
        S["rc_i"] = 0
        S["rms_ss"] = C.sb("rms_ss", [128, 1], F32, es)
        S["rms_rs"] = C.sb("rms_rs", [128, 1], F32, es)
        hcat = []
        for a in range(4):
            v = Tile(hc_all.t[:, a, :], "hcv")
            v.k = hc_all.k
            hcat.append(v)

        P.op("pool", lambda e: e.memset(wdr[:], 0.0), writes=[wdr.k])
        P.op("pool", lambda e: e.memset(wdrot[:], 0.0), writes=[wdrot.k])
        P.op("pool", lambda e: e.memset(wuqrot[:], 0.0), writes=[wuqrot.k])
        P.op("pool", lambda e: e.tensor_copy(out=wdr[:, :, 64:96], in_=wdown[:, :, 256:288]),
             reads=wdown_tr, writes=[wdr.k])
        P.op("pool", lambda e: e.tensor_scalar(out=wdrot[:, :, 64:80], in0=wdown[:, :, 272:288], scalar1=-1.0,
                                               scalar2=None, op0=ALU.mult), reads=wdown_tr, writes=[wdrot.k])
        P.op("pool", lambda e: e.tensor_copy(out=wdrot[:, :, 80:96], in_=wdown[:, :, 256:272]),
             reads=wdown_tr, writes=[wdrot.k])
        wuq4 = wuq.t[:, :, :].rearrange("p c (h d) -> p c h d", h=8)
        P.op("pool", lambda e: e.tensor_scalar(out=wuqrot[:, :, :, 64:80], in0=wuq4[:, :, :, 80:96], scalar1=-1.0,
                                               scalar2=None, op0=ALU.mult), reads=[wuq.k], writes=[wuqrot.k])
        P.op("pool", lambda e: e.tensor_copy(out=wuqrot[:, :, :, 80:96], in_=wuq4[:, :, :, 64:80]),
             reads=[wuq.k], writes=[wuqrot.k])
        P.op("pool", lambda e: e.memset(Vaug[:], 1.0), writes=V_blk)
        S["rot0"], S["nrot"] = 4, 2
        S["cast_eng"] = "pool"
        sbank = S["psf"][0:2]
        sb_i = [0]

        x_view = x_dram.rearrange("(s a p) d -> s a p d", a=4, p=128)
        o_view = out_dram.rearrange("(s a p) d -> s a p d", a=4, p=128)
        nl = [0]
        R = slice(64, 96)

        def front_chunks(s):
            seq, T, b = s // NT, s % NT, s % 2
            tcols = slice(T * ST, (T + 1) * ST)
            xTb, ckTb, cqTb, qThb, qmTb = xT[b], ckT[b], cqT[b], qTh[b], qmT[b]
            ch = []

            def rope_tables():
                posi_ap = rt2[R, :].bitcast(I32)
                P.dma("sp", posi_ap, pos_d[seq, T * ST:(T + 1) * ST].partition_broadcast(32), writes=[rt2.k])
                P.op("dve", lambda e: e.tensor_copy(out=uu[R, :], in_=posi_ap), reads=[rt2.k], writes=[uu.k])
                for (dstT, shift) in ((sinT, 0.0), (cosT, 0.25)):
                    P.op("dve", lambda e, shift=shift: e.tensor_scalar(
                        out=u2[R, :], in0=uu[R, :], scalar1=cf2[R, 0:1], scalar2=shift, op0=ALU.mult, op1=ALU.add),
                         reads=[uu.k, cf2.k], writes=[u2.k])
                    P.op("dve", lambda e: e.tensor_copy(out=posi_ap, in_=u2[R, :]), reads=[u2.k], writes=[rt2.k])
                    P.op("dve", lambda e: e.tensor_copy(out=rt1[R, :], in_=posi_ap), reads=[rt2.k], writes=[rt1.k])
                    P.op("dve", lambda e: e.tensor_tensor(out=u2[R, :], in0=u2[R, :], in1=rt1[R, :], op=ALU.subtract),
                         reads=[u2.k, rt1.k], writes=[u2.k])
                    P.op("dve", lambda e: e.tensor_scalar(out=rt1[R, :], in0=u2[R, :], scalar1=0.5, scalar2=None,
                                                          op0=ALU.is_gt), reads=[u2.k], writes=[rt1.k])
                    P.op("dve", lambda e: e.tensor_tensor(out=u2[R, :], in0=u2[R, :], in1=rt1[R, :], op=ALU.subtract),
                         reads=[u2.k, rt1.k], writes=[u2.k])
                    P.op("act", lambda e, dstT=dstT: e.activation(out=dstT[R, :], in_=u2[R, :], func=AF.Sin,
                                                                  scale=float(2 * np.pi)),
                         reads=[u2.k], writes=[dstT.k])
            xis = {}

            def load_dma(a):
                xi = xld[nl[0] % len(xld)]
                nl[0] += 1
                xis[a] = xi
                P.dma("sp", xi[:], x_view[s, a], reads=[x_trks[s]], writes=[xi.k])

            def load_tr(a):
                transpose_tokens(C, S, xis[a], xTb, a * 128)

            def latents(a):
                cols = slice(a * 128, (a + 1) * 128)
                for (wt, wtr, dstT) in ((wdown, wdown_tr, ckTb), (bwin, bwin_tr, cqTb)):
                    pb = nextbank(S)
                    P.mm(pb[:, 0:256], [(xTb[:, kc, cols], wt[:, kc, 0:256]) for kc in range(8)],
                         reads=[xTb.k] + wtr, writes=[pb.k])
                    rms_rows(C, S, pb, 256, ckn, scr)
                    transpose_cols(C, S, ckn, 2, dstT, a * 128)
            ch.append(lambda: load_dma(0))
            ch.append(lambda: load_dma(1))
            ch.append(rope_tables)
            ch.append(lambda: load_tr(0))
            ch.append(lambda: load_dma(2))
            ch.append(lambda: load_tr(1))
            ch.append(lambda: load_dma(3))
            ch.append(lambda: latents(0))
            ch.append(lambda: load_tr(2))
            ch.append(lambda: latents(1))
            ch.append(lambda: load_tr(3))
            ch.append(lambda: latents(2))
            ch.append(lambda: latents(3))

            def k_nope(h0):
                for h in range(h0, h0 + 4):
                    pb = nextbank(S)
                    P.mm(pb[0:64, :], [(wuk[:, kc, h * 64:(h + 1) * 64], ckTb[:, kc, :]) for kc in range(2)],
                         reads=[wuk.k, ckTb.k], writes=[pb.k])
                    P.op("dve", lambda e, pb=pb, h=h: e.tensor_copy(out=kT[0:64, h, tcols], in_=pb[0:64, :]),
                         reads=[pb.k], writes=[kT_blk[T]])
            ch.append(lambda: k_nope(0))
            ch.append(lambda: k_nope(4))

            def k_rope():
                pA, pB = nextbank(S), nextbank(S)
                P.mm(pA[0:96, :], [(wdr[:, kc, :], xTb[:, kc, :]) for kc in range(8)], reads=[wdr.k, xTb.k],
                     writes=[pA.k])
                P.mm(pB[0:96, :], [(wdrot[:, kc, :], xTb[:, kc, :]) for kc in range(8)], reads=[wdrot.k, xTb.k],
                     writes=[pB.k])
                P.op("dve", lambda e: e.tensor_tensor(out=rt1[R, :], in0=pA[R, :], in1=cosT[R, :], op=ALU.mult),
                     reads=[pA.k, cosT.k], writes=[rt1.k])
                P.op("dve", lambda e: e.tensor_tensor(out=rt2[R, :], in0=pB[R, :], in1=sinT[R, :], op=ALU.mult),
                     reads=[pB.k, sinT.k], writes=[rt2.k])
                P.op("dve", lambda e: e.tensor_tensor(out=kr[R, :], in0=rt1[R, :], in1=rt2[R, :], op=ALU.add),
                     reads=[rt1.k, rt2.k], writes=[kr.k])
                P.op("dve", lambda e: e.tensor_copy(out=kT[R, :, tcols],
                                                    in_=kr[R, :].unsqueeze(1).broadcast_to([32, 8, ST])),
                     reads=[kr.k], writes=[kT_blk[T]])
            ch.append(k_rope)

            def v_tiles():
                for a in range(4):
                    pb = nextbank(S)
                    P.mm(pb[:], [(ckTb[:, kc, a * 128:(a + 1) * 128], wuv[:, kc, :]) for kc in range(2)],
                         reads=[ckTb.k, wuv.k], writes=[pb.k])
                    P.op("act", lambda e, pb=pb, a=a: e.activation(out=Vaug[:, 4 * T + a, :, 0:64], in_=v3(pb[:], 8),
                                                                   func=AF.Copy), reads=[pb.k], writes=[V_blk[T]])
            ch.append(v_tiles)

            def queries(h0):
                for h in range(h0, h0 + 2):
                    pA, pB = nextbank(S), nextbank(S)
                    P.mm(pA[0:96, :], [(wuq[:, kc, h * 96:(h + 1) * 96], cqTb[:, kc, :]) for kc in range(2)],
                         reads=[wuq.k, cqTb.k], writes=[pA.k])
                    P.mm(pB[0:96, :], [(wuqrot[:, kc, h, :], cqTb[:, kc, :]) for kc in range(2)],
                         reads=[wuqrot.k, cqTb.k], writes=[pB.k])
                    P.op("dve", lambda e, pA=pA, h=h: e.tensor_copy(out=qThb[0:64, h, :], in_=pA[0:64, :]),
                         reads=[pA.k], writes=[qThb.k])
                    P.op("dve", lambda e, pA=pA: e.tensor_tensor(out=rt1[R, :], in0=pA[R, :], in1=cosT[R, :],
                                                                 op=ALU.mult), reads=[pA.k, cosT.k], writes=[rt1.k])
                    P.op("dve", lambda e, pB=pB: e.tensor_tensor(out=rt2[R, :], in0=pB[R, :], in1=sinT[R, :],
                                                                 op=ALU.mult), reads=[pB.k, sinT.k], writes=[rt2.k])
                    P.op("dve", lambda e, h=h: e.tensor_tensor(out=qThb[R, h, :], in0=rt1[R, :], in1=rt2[R, :],
                                                               op=ALU.add), reads=[rt1.k, rt2.k], writes=[qThb.k])
            for h0 in range(0, 8, 2):
                ch.append(lambda h0=h0: queries(h0))

            def q_mem():
                for h in range(4):
                    pb = nextbank(S)
                    P.mm(pb[:], [(bwin[:, kc, 256 + h * 128:256 + (h + 1) * 128], xTb[:, kc, :]) for kc in range(8)],
                         reads=[xTb.k] + bwin_tr, writes=[pb.k])
                    P.op("act", lambda e, pb=pb, h=h: e.activation(out=qmTb[:, h, :], in_=pb[:], func=AF.Copy),
                         reads=[pb.k], writes=[qmTb.k])
            ch.append(q_mem)
            return ch

        def mla_head(s, h, pending, per):
            T, b = s % NT, s % 2
            qThb = qTh[b]
            nkt = 4 * T + 4
            po = S["psf"][2 + (h % 2)]
            started = [False]

            def emit_front(j):
                a_min = max(0, j - 4 * T)
                qc = slice(a_min * 128, ST)
                pb = sbank[sb_i[0] % 2]
                sb_i[0] += 1
                P.mm(pb[:, qc], [(kT[:, h, j * 128:(j + 1) * 128], qThb[:, h, qc])],
                     reads=[kT_blk[j // 4], qThb.k], writes=[pb.k])
                pt = S["PT"][S["pt_i"] % len(S["PT"])]
                S["pt_i"] += 1
                P.op("act", lambda e: e.activation(out=pt[:, qc], in_=pb[:, qc], func=AF.Exp, scale=SC),
                     reads=[pb.k], writes=[pt.k])
                if j >= 4 * T:
                    P.op("pool", lambda e: e.memset(pt[64:128, a_min * 128:a_min * 128 + 64], 0.0),
                         reads=[], writes=[pt.k])
                return (j, a_min, pt)

            def emit_back(j, a_min, pt):
                fns = []
                for a in range(a_min, 4):
                    st = not started[0]
                    started[0] = True
                    fns.append(lambda e, a=a, st=st: e.matmul(
                        po[:, a * 65:(a + 1) * 65], pt[:, a * 128:(a + 1) * 128], Vaug[:, j, h, :],
                        start=st, stop=(j == 4 * T + a), skip_group_check=True))
                if KEEP_WARM and started[0]:
                    P.pe_multi([lambda e: e.matmul(po[:, 260:512], kT[:, h, 0:128], qThb[:, h, 0:252],
                                                   start=False, stop=False, skip_group_check=True)],
                               reads=[kT_blk[0], qThb.k], writes=[po.k])
                P.pe_multi(fns, reads=[pt.k, V_blk[j // 4]], writes=[po.k])

            prev = None
            for j in range(nkt):
                cur = emit_front(j)
                if prev is not None:
                    emit_back(*prev)
                prev = cur
                for _ in range(per):
                    if pending:
                        P.replay(pending.pop(0))
            emit_back(*prev)
            po3 = v3(po[:, 0:260], 4)
            P.op("dve", lambda e: e.reciprocal(out=rc4[:], in_=po3[:, :, 64:65]), reads=[po.k], writes=[rc4.k])
            P.op("dve", lambda e: e.tensor_tensor(
                out=hc_all[:, :, h * 64:(h + 1) * 64], in0=po3[:, :, 0:64],
                in1=rc4[:, :, 0:1].broadcast_to([128, 4, 64]), op=ALU.mult),
                 reads=[po.k, rc4.k], writes=[hc_all.k])

        for f in front_chunks(0):
            f()
        for s in range(NST):
            seq, b = s // NT, s % 2
            pending = []
            if s + 1 < NST:
                P.rec = pending
                for f in front_chunks(s + 1):
                    f()
                P.rec = None
            mem_attention(C, S, seq, qmT[b], KmT, Vm, hcat, 512)
            nsteps = 8 * (4 * (s % NT) + 4)
            per = (len(pending) + nsteps - 1) // nsteps
            for h in range(8):
                mla_head(s, h, pending, per)
            while pending:
                P.replay(pending.pop(0))
            out_proj_ln(C, S, hcat, xT[b], wout, wout_tr, xld, nl, x_view, x_trks[s], s, y, g_rep, b_rep,
                        o_view, out_trks[s])
        P.full_barrier()
        S["rot0"], S["nrot"] = 0, 6
        S.pop("cast_eng")


def _consts_np():
    c = np.zeros((128, 4, 128), np.float32)
    c[:, 0, :] = np.eye(128, dtype=np.float32)
    s = np.arange(128)[:, None]
    l = np.arange(128)[None, :]
    c[:, 1, :] = ((s // 64 == l // 64) & (s <= l)).astype(np.float32)
    c[:, 2, :] = (s < 64).astype(np.float32) * np.ones((1, 128), np.float32)
    c[:, 3, :] = (s >= 64).astype(np.float32) * np.ones((1, 128), np.float32)
    return c


def _cf2_np():
    c = np.zeros((128, 4), np.float32)
    inv = (10000.0 ** (-np.arange(0, 32, 2, dtype=np.float32) / 32)).astype(np.float32)
    for p in range(64, 96):
        c[p, 0] = inv[(p - 64) % 16] / np.float32(2 * np.pi)
    c[:, 1] = -np.pi
    return c


W_SHAPES = {
    "a_w_in": [D, 2056], "a_b_igate": [4], "a_b_fgate": [4], "a_w_mem_kv": [D, D], "a_w_out": [D, D],
    "kv_w_down": [D, 288], "kv_norm_g": [256], "kv_w_uk": [256, 512], "kv_w_uv": [256, 512],
    "b_w_in": [D, 768], "b_q_norm_g": [256], "b_w_uq": [256, 768], "b_w_mem_kv": [D, D], "b_w_out": [D, D],
    "ln1_g": [2, D], "ln1_b": [2, D], "ffn_w_up": [2, D, DFF], "ffn_w_down": [2, DFF, D], "ln2_g": [2, D],
    "ln2_b": [2, D],
}


def build_program():
    nc = bass.Bass("TRN2", target_bir_lowering=False)
    dt = lambda n, sh: nc.dram_tensor(n, sh, F32, kind="ExternalInput").ap()
    x = dt("x", [NTOK, D])
    mem = dt("mem", [NSEQ, 256, D])
    pos = nc.dram_tensor("positions", [NSEQ, SEQ], I32, kind="ExternalInput").ap()
    w = {k: dt(k, sh) for k, sh in W_SHAPES.items()}
    consts = dt("consts", [128, 4, 128])
    cf2 = dt("cf2", [128, 4])
    out = nc.dram_tensor("out", [NTOK, D], F32, kind="ExternalOutput").ap()
    sc1 = nc.dram_tensor("scratch1", [NTOK, D], F32, kind="Internal").ap()
    sc2 = nc.dram_tensor("scratch2", [NTOK, D], F32, kind="Internal").ap()
    C = Ctx(nc)
    with nc.allow_low_precision("bf16 matmul operands, fp32 accumulation"), C.es:
        S = alloc_shared(C)
        load_consts(C, S, consts)
        xtr = [Trk("xd%d" % i) for i in range(NST)]
        t1 = [Trk("s1_%d" % i) for i in range(NST)]
        t2 = [Trk("s2_%d" % i) for i in range(NST)]
        otr = [Trk("od%d" % i) for i in range(NST)]
        mixer_a_phase(C, S, x, xtr, sc1, t1, mem, w["a_w_in"], w["a_b_igate"], w["a_b_fgate"], w["a_w_mem_kv"],
                      w["a_w_out"], w["ln1_g"][0], w["ln1_b"][0])
        ffn_phase(C, S, sc1, t1, sc2, t2, w["ffn_w_up"][0], w["ffn_w_down"][0], w["ln2_g"][0], w["ln2_b"][0], False)
        mixer_b_phase(C, S, sc2, t2, sc1, t1, mem, pos, w["kv_w_down"], w["kv_norm_g"], w["kv_w_uk"], w["kv_w_uv"],
                      w["b_w_in"], w["b_q_norm_g"], w["b_w_uq"], w["b_w_mem_kv"], w["b_w_out"], w["ln1_g"][1],
                      w["ln1_b"][1], cf2)
        ffn_phase(C, S, sc1, t1, out, otr, w["ffn_w_up"][1], w["ffn_w_down"][1], w["ln2_g"][1], w["ln2_b"][1], True)
        C.P.finish()
    return nc


def kernel(x, mem, positions, a_w_in, a_b_igate, a_b_fgate, a_w_mem_kv, a_w_out, kv_w_down, kv_norm_g, kv_w_uk,
           kv_w_uv, b_w_in, b_q_norm_g, b_w_uq, b_w_mem_kv, b_w_out, ln1_g, ln1_b, ffn_w_up, ffn_w_down, ln2_g,
           ln2_b):
    f32 = lambda a: np.ascontiguousarray(np.asarray(a), dtype=np.float32)
    shared = {
        "a_w_in": f32(a_w_in)[0], "a_b_igate": f32(a_b_igate)[0], "a_b_fgate": f32(a_b_fgate)[0],
        "a_w_mem_kv": f32(a_w_mem_kv)[0], "a_w_out": f32(a_w_out)[0], "kv_w_down": f32(kv_w_down),
        "kv_norm_g": f32(kv_norm_g), "kv_w_uk": f32(kv_w_uk), "kv_w_uv": f32(kv_w_uv), "b_w_in": f32(b_w_in)[0],
        "b_q_norm_g": f32(b_q_norm_g)[0], "b_w_uq": f32(b_w_uq)[0], "b_w_mem_kv": f32(b_w_mem_kv)[0],
        "b_w_out": f32(b_w_out)[0], "ln1_g": f32(ln1_g), "ln1_b": f32(ln1_b), "ffn_w_up": f32(ffn_w_up),
        "ffn_w_down": f32(ffn_w_down), "ln2_g": f32(ln2_g), "ln2_b": f32(ln2_b),
        "consts": _consts_np(), "cf2": _cf2_np(),
    }
    shared = {k: np.ascontiguousarray(v) for k, v in shared.items()}
    x = f32(x)
    mem = f32(mem)
    positions = np.ascontiguousarray(np.asarray(positions), dtype=np.int32)
    in_maps = []
    for c in range(NCORES):
        m = dict(shared)
        m["x"] = np.ascontiguousarray(x[c * NSEQ:(c + 1) * NSEQ].reshape(NTOK, D))
        m["mem"] = np.ascontiguousarray(mem[c * NSEQ:(c + 1) * NSEQ])
        m["positions"] = np.ascontiguousarray(positions[c * NSEQ:(c + 1) * NSEQ])
        in_maps.append(m)
    nc = build_program()
    res = run_bass_kernel_spmd(nc, in_maps, core_ids=list(range(NCORES)))
    outs = [np.asarray(r["out"], dtype=np.float32).reshape(NSEQ, SEQ, D) for r in res.results]
    return np.concatenate(outs, axis=0)
```

```python
import contextlib
import numpy as np
import concourse.bass as bass
import concourse.mybir as mybir
from concourse.bass_utils import run_bass_kernel_spmd

F32 = mybir.dt.float32
BF16 = mybir.dt.bfloat16
I32 = mybir.dt.int32
AF = mybir.ActivationFunctionType
ALU = mybir.AluOpType
AX = mybir.AxisListType

NCORES = 8
SEQ = 2048
D = 1024
DFF = 4096
NSEQ = 2
NTOK = NSEQ * SEQ
ST = 512
NST = NTOK // ST
ALPHA = 4.0 ** 0.25
LN_EPS = 1e-5
RMS_EPS = 1e-6


class Trk:
    __slots__ = ("name", "w", "r", "dsem", "dcnt")

    def __init__(self, name):
        self.name = name
        self.w = None
        self.r = {}
        self.dsem = None
        self.dcnt = 0


class Prog:
    SEM_ROT = 12000

    def __init__(self, nc):
        self.nc = nc
        self.eng = {"pe": nc.tensor, "act": nc.scalar, "dve": nc.vector, "pool": nc.gpsimd,
                    "sp": nc.sync}
        self.sem = {}
        self.cnt = {}
        self.seen = {e: {} for e in self.eng}
        self.nsem = 0
        for e in self.eng:
            self._new_sem(e)
        self.out_tokens = []
        self.ninstr = 0
        self.last_tok = {}
        self.rec = None
        self.inter = None
        self._acc = 0.0
        self._in_replay = False
        self.dma_toks = {}
        self.free_dsems = []
        self.phase_trks = []

    def _alloc_sem(self, name):
        self.nsem += 1
        return self.nc.alloc_semaphore(name="%s_%d" % (name, self.nsem))

    def _new_sem(self, e):
        self.sem[e] = self._alloc_sem("s_" + e)
        self.cnt[e] = 0

    def _need(self, e, tok, skip_same):
        if tok is None:
            return
        sem, c, te = tok
        if skip_same and te == e and e == "pe":
            return
        if self.seen[e].get(sem, 0) >= c:
            return
        self.eng[e].wait_ge(sem, c)
        self.seen[e][sem] = c

    def _signal(self, e, ins):
        if self.cnt[e] >= self.SEM_ROT:
            self._new_sem(e)
        self.cnt[e] += 1
        ins.then_inc(self.sem[e], 1)
        self.last_tok[e] = (self.sem[e], self.cnt[e], e)
        return self.last_tok[e]

    def full_barrier(self):
        snap = dict(self.last_tok)
        dts = list(self.dma_toks.values())
        for e in self.eng:
            for o, tok in snap.items():
                if o != e:
                    self._need(e, tok, False)
            for tok in dts:
                self._need(e, tok, False)
        for t in self.phase_trks:
            self.free_dsems.append((t.dsem, t.dcnt))
            t.dsem = None
        self.phase_trks = []
        self.dma_toks = {}

    def _after_emit(self):
        if self.inter is None or self._in_replay:
            return
        pend, rate = self.inter
        self._acc += rate
        while self._acc >= 1.0 and pend:
            self._acc -= 1.0
            self._in_replay = True
            self.replay(pend.pop(0))
            self._in_replay = False

    def replay(self, item):
        kind, args, kw = item
        saved, self.rec = self.rec, None
        getattr(self, kind)(*args, **kw)
        self.rec = saved

    def op(self, e, fn, reads=(), writes=()):
        if self.rec is not None:
            self.rec.append(("op", (e, fn), dict(reads=list(reads), writes=list(writes))))
            return None
        for t in reads:
            self._need(e, t.w, False)
        for t in writes:
            self._need(e, t.w, True)
            for tok in t.r.values():
                self._need(e, tok, True)
        ins = fn(self.eng[e])
        tok = self._signal(e, ins)
        for t in writes:
            t.w = tok
            t.r = {}
        for t in reads:
            t.r[e] = tok
        self.ninstr += 1
        self._after_emit()
        return tok

    def mm(self, out, pairs, reads=(), writes=()):
        if self.rec is not None:
            self.rec.append(("mm", (out, list(pairs)), dict(reads=list(reads), writes=list(writes))))
            return None
        e = "pe"
        for t in reads:
            self._need(e, t.w, False)
        for t in writes:
            self._need(e, t.w, True)
            for tok in t.r.values():
                self._need(e, tok, True)
        n = len(pairs)
        ins = None
        for i, (l, r) in enumerate(pairs):
            ins = self.nc.tensor.matmul(out, l, r, start=(i == 0), stop=(i == n - 1))
        tok = self._signal(e, ins)
        for t in writes:
            t.w = tok
            t.r = {}
        for t in reads:
            t.r[e] = tok
        self.ninstr += n
        self._after_emit()
        return tok

    def pe_multi(self, fns, reads=(), writes=()):
        if self.rec is not None:
            self.rec.append(("pe_multi", (list(fns),), dict(reads=list(reads), writes=list(writes))))
            return None
        e = "pe"
        for t in reads:
            self._need(e, t.w, False)
        for t in writes:
            self._need(e, t.w, True)
            for tok in t.r.values():
                self._need(e, tok, True)
        ins = None
        for f in fns:
            ins = f(self.nc.tensor)
        tok = self._signal(e, ins)
        for t in writes:
            t.w = tok
            t.r = {}
        for t in reads:
            t.r[e] = tok
        self.ninstr += len(fns)
        self._after_emit()
        return tok

    def dma(self, q, out, in_, reads=(), writes=(), is_output=False, sem_trk=None):
        if self.rec is not None:
            self.rec.append(("dma", (q, out, in_), dict(reads=list(reads), writes=list(writes),
                                                        is_output=is_output, sem_trk=sem_trk)))
            return None
        e = q
        for t in reads:
            self._need(e, t.w, False)
        for t in writes:
            self._need(e, t.w, False)
            for tok in t.r.values():
                self._need(e, tok, False)
        trk = sem_trk if sem_trk is not None else (list(writes) + list(reads))[0]
        if trk.dsem is None:
            if self.free_dsems:
                trk.dsem, trk.dcnt = self.free_dsems.pop()
            else:
                trk.dsem = self._alloc_sem("d")
                trk.dcnt = 0
            self.phase_trks.append(trk)
        trk.dcnt += 16
        self.eng[e].dma_start(out=out, in_=in_).then_inc(trk.dsem, 16)
        tok = (trk.dsem, trk.dcnt, "dma")
        self.dma_toks[trk.dsem] = tok
        for t in writes:
            t.w = tok
            t.r = {}
        for t in reads:
            t.r["dma_%s" % trk.name] = tok
        if is_output:
            self.out_tokens.append(tok)
        self.ninstr += 1
        return tok

    def barrier_all(self, trks):
        for t in trks:
            self._need("sp", t.w, False)
            for tok in t.r.values():
                self._need("sp", tok, False)

    def finish(self):
        for tok in self.out_tokens:
            self._need("sp", tok, False)


class Tile:
    def __init__(self, t, name):
        self.t = t
        self.k = Trk(name)

    def __getitem__(self, idx):
        return self.t[idx]


class Ctx:
    def __init__(self, nc):
        self.nc = nc
        self.P = Prog(nc)
        self.es = contextlib.ExitStack()
        self.nid = 0

    def sb(self, name, shape, dt, es=None):
        self.nid += 1
        nm = "%s_%d" % (name, self.nid)
        t = (es or self.es).enter_context(self.nc.sbuf_tensor(nm, list(shape), dt))
        return Tile(t, nm)

    def ps(self, name, shape, dt, es=None):
        self.nid += 1
        nm = "%s_%d" % (name, self.nid)
        t = (es or self.es).enter_context(self.nc.psum_tensor(nm, list(shape), dt))
        return Tile(t, nm)


def load_weight_cast(C, wt, dram_view, nsplit, axis):
    P = C.P
    n = wt.t.shape[axis]
    step = n // nsplit
    trks = []
    for j in range(nsplit):
        sl = [slice(None)] * 3
        sl[axis] = slice(j * step, (j + 1) * step)
        sl = tuple(sl)
        k = Trk("%s_p%d" % (wt.k.name, j))
        P.dma("pool", wt.t[sl], dram_view[sl], writes=[k])
        trks.append(k)
    return trks


def layer_norm_tile(C, S, y, g_rep, b_rep, out):
    P = C.P
    st, mv, rstd, nmr = S["ln_st"], S["ln_mv"], S["ln_rstd"], S["ln_nmr"]
    xn = y
    for hh in range(2):
        P.op("dve", lambda e, hh=hh: e.bn_stats(out=st[:, hh, :], in_=y[:, hh * 512:(hh + 1) * 512]),
             reads=[y.k], writes=[st.k])
    P.op("dve", lambda e: e.bn_aggr(out=mv[:], in_=st[:]), reads=[st.k], writes=[mv.k])
    P.op("act", lambda e: e.activation(out=rstd[:], in_=mv[:, 1:2], func=AF.Ln, bias=S["eps_ln"][:], scale=1.0),
         reads=[mv.k, S["eps_ln"].k], writes=[rstd.k])
    P.op("act", lambda e: e.activation(out=rstd[:], in_=rstd[:], func=AF.Exp, scale=-0.5),
         reads=[rstd.k], writes=[rstd.k])
    P.op("dve", lambda e: e.tensor_scalar(out=nmr[:], in0=mv[:, 0:1], scalar1=-1.0, scalar2=None, op0=ALU.mult),
         reads=[mv.k], writes=[nmr.k])
    P.op("dve", lambda e: e.scalar_tensor_tensor(out=xn[:], in0=y[:], scalar=nmr[:, 0:1], in1=g_rep[:],
                                                 op0=ALU.add, op1=ALU.mult),
         reads=[y.k, nmr.k, g_rep.k], writes=[xn.k])
    P.op("dve", lambda e: e.scalar_tensor_tensor(out=out[:], in0=xn[:], scalar=rstd[:, 0:1], in1=b_rep[:],
                                                 op0=ALU.mult, op1=ALU.add),
         reads=[xn.k, rstd.k, b_rep.k], writes=[out.k])


def transpose_tokens(C, S, xin, xT, col0):
    P = C.P
    xb = S["xb"][S["xb_i"] % 2]
    S["xb_i"] += 1
    P.op(S.get("cast_eng", "act"), (lambda e: e.tensor_copy(out=xb[:], in_=xin[:])) if S.get("cast_eng") else
         (lambda e: e.activation(out=xb[:], in_=xin[:], func=AF.Copy)), reads=[xin.k], writes=[xb.k])
    pt = S["pst"][S["pst_i"] % 2]
    S["pst_i"] += 1
    ident = S["ident"]
    P.pe_multi([lambda e, c=c: e.transpose(out=pt[:, c * 128:(c + 1) * 128], in_=xb[:, c * 128:(c + 1) * 128],
                                           identity=ident[:]) for c in range(8)],
               reads=[xb.k, ident.k], writes=[pt.k])
    P.op("dve", lambda e: e.tensor_copy(out=xT[:, :, col0:col0 + 128],
                                        in_=pt[:, :].rearrange("p (c t) -> p c t", c=8)),
         reads=[pt.k], writes=[xT.k])


def ffn_phase(C, S, x_dram, x_trks, out_dram, out_trks, w_up_d, w_down_d, g_d, b_d, is_output):
    nc, P = C.nc, C.P
    with contextlib.ExitStack() as es:
        wup = C.sb("wup", [128, 8, DFF], BF16, es)
        wdn = C.sb("wdn", [128, 32, D], BF16, es)
        g_rep = C.sb("g_rep", [128, D], F32, es)
        b_rep = C.sb("b_rep", [128, D], F32, es)
        xld = [C.sb("xld", [128, D], F32, es) for _ in range(3)]
        xT = C.sb("xT", [128, 8, ST], BF16, es)
        hT = C.sb("hT", [128, 32, ST], BF16, es)
        rl = [C.sb("rl", [128, ST], BF16, es) for _ in range(2)]
        y = [C.sb("y", [128, D], F32, es) for _ in range(2)]

        P.dma("sp", g_rep[:], g_d.partition_broadcast(128), writes=[g_rep.k])
        P.dma("sp", b_rep[:], b_d.partition_broadcast(128), writes=[b_rep.k])
        x_view = x_dram.rearrange("(s a p) d -> s a p d", a=4, p=128)
        o_view = out_dram.rearrange("(s a p) d -> s a p d", a=4, p=128)
        up_tr = load_weight_cast(C, wup, w_up_d.rearrange("(c p) f -> p c f", p=128), 8, 2)
        dn_tr = load_weight_cast(C, wdn, w_down_d.rearrange("(c p) f -> p c f", p=128), 8, 1)

        psb = S["psf"]
        nb = 0
        nl = 0
        for s in range(NST):
            for a in range(4):
                xi = xld[nl % 3]
                nl += 1
                P.dma("sp", xi[:], x_view[s, a], reads=[x_trks[s]], writes=[xi.k])
                transpose_tokens(C, S, xi, xT, a * 128)
            for fc in range(32):
                pb = psb[nb % 4]
                nb += 1
                P.mm(pb[:], [(wup[:, kc, fc * 128:(fc + 1) * 128], xT[:, kc, :]) for kc in range(8)],
                     reads=[xT.k, up_tr[fc // 4]], writes=[pb.k])
                r = rl[fc % 2]
                P.op("act", lambda e, r=r, pb=pb: e.activation(out=r[:], in_=pb[:], func=AF.Relu),
                     reads=[pb.k], writes=[r.k])
                P.op("dve", lambda e, r=r, fc=fc: e.tensor_tensor(out=hT[:, fc, :], in0=r[:], in1=r[:],
                                                                   op=ALU.mult),
                     reads=[r.k], writes=[hT.k])
            for a in range(4):
                xi = xld[nl % 3]
                nl += 1
                P.dma("sp", xi[:], x_view[s, a], reads=[x_trks[s]], writes=[xi.k])
                yy = y[a % 2]
                for dh in range(2):
                    pb = psb[nb % 4]
                    nb += 1
                    P.mm(pb[:], [(hT[:, fc, a * 128:(a + 1) * 128], wdn[:, fc, dh * 512:(dh + 1) * 512])
                                 for fc in range(32)],
                         reads=[hT.k] + dn_tr, writes=[pb.k])
                    P.op("dve", lambda e, yy=yy, pb=pb, dh=dh, xi=xi: e.scalar_tensor_tensor(
                        out=yy[:, dh * 512:(dh + 1) * 512], in0=xi[:, dh * 512:(dh + 1) * 512],
                        scalar=ALPHA, in1=pb[:], op0=ALU.mult, op1=ALU.add),
                         reads=[xi.k, pb.k], writes=[yy.k])
                layer_norm_tile(C, S, yy, g_rep, b_rep, yy)
                P.dma("pool", o_view[s, a], yy[:], reads=[yy.k], writes=[out_trks[s]],
                      is_output=is_output, sem_trk=yy.k)
        P.full_barrier()


def alloc_shared(C):
    S = {}
    S["ident"] = C.sb("ident", [128, 128], BF16)
    S["psf"] = [C.ps("psf", [128, 512], F32) for _ in range(6)]
    S["pst"] = [C.ps("pst", [128, 1024], BF16) for _ in range(2)]
    S["pst_i"] = 0
    S["nb"] = 0
    S["nrot"] = 6
    S["rot0"] = 0
    S["xb"] = [C.sb("xb", [128, D], BF16) for _ in range(2)]
    S["xb_i"] = 0
    S["ln_st"] = C.sb("ln_st", [128, 2, 6], F32)
    S["ln_mv"] = C.sb("ln_mv", [128, 2], F32)
    S["ln_rstd"] = C.sb("ln_rstd", [128, 1], F32)
    S["ln_nmr"] = C.sb("ln_nmr", [128, 1], F32)
    S["eps_ln"] = C.sb("eps_ln", [128, 1], F32)
    S["eps_rms"] = C.sb("eps_rms", [128, 1], F32)
    C.P.op("pool", lambda e: e.memset(S["eps_ln"][:], LN_EPS), writes=[S["eps_ln"].k])
    C.P.op("pool", lambda e: e.memset(S["eps_rms"][:], RMS_EPS), writes=[S["eps_rms"].k])
    return S


def nextbank(S):
    b = S["psf"][S["rot0"] + S["nb"] % S["nrot"]]
    S["nb"] += 1
    return b


def v3(ap, n):
    return ap.rearrange("p (h d) -> p h d", h=n)


def load_consts(C, S, consts_d):
    P = C.P
    S["cf"] = C.sb("cf", [128, 4, 128], F32)
    S["maskb"] = C.sb("maskb", [128, 128], BF16)
    P.dma("sp", S["cf"][:], consts_d, writes=[S["cf"].k])
    P.dma("pool", S["ident"][:], consts_d[:, 0, :], writes=[S["ident"].k])
    P.dma("pool", S["maskb"][:], consts_d[:, 1, :], writes=[S["maskb"].k])
    S["one1"] = C.sb("one1", [128, 1], F32)
    S["ln8"] = C.sb("ln8", [128, 1], F32)
    P.op("pool", lambda e: e.memset(S["one1"][:], 1.0), writes=[S["one1"].k])
    P.op("pool", lambda e: e.memset(S["ln8"][:], float(np.log(0.125))), writes=[S["ln8"].k])


def mem_kv_precompute(C, S, es, mem_d, wmk, wmk_tr, xld, KmT, Vm):
    P = C.P
    memT = C.sb("memT", [128, 8, 256], BF16, es)
    P.op("pool", lambda e: e.memset(Vm[:], 1.0), writes=[Vm.k])
    n = 0
    for q in range(NSEQ):
        for mt in range(2):
            xi = xld[n % len(xld)]
            n += 1
            P.dma("sp", xi[:], mem_d[q, mt * 128:(mt + 1) * 128, :], writes=[xi.k])
            transpose_tokens(C, S, xi, memT, mt * 128)
        for h in range(4):
            pb = nextbank(S)
            P.mm(pb[:, 0:256], [(wmk[:, kc, h * 128:(h + 1) * 128], memT[:, kc, :]) for kc in range(8)],
                 reads=[memT.k] + wmk_tr, writes=[pb.k])
            P.op("act", lambda e, pb=pb, q=q, h=h: e.activation(out=KmT[:, q, h, :], in_=pb[:, 0:256], func=AF.Copy),
                 reads=[pb.k], writes=[KmT.k])
        for mt in range(2):
            pb = nextbank(S)
            P.mm(pb[:], [(memT[:, kc, mt * 128:(mt + 1) * 128], wmk[:, kc, 512:1024]) for kc in range(8)],
                 reads=[memT.k] + wmk_tr, writes=[pb.k])
            P.op("act", lambda e, pb=pb, q=q, mt=mt: e.activation(out=Vm[:, q, mt, :, 0:128], in_=v3(pb[:], 4),
                                                                  func=AF.Copy),
                 reads=[pb.k], writes=[Vm.k])
    return memT


def mem_attention(C, S, seq, qmT, KmT, Vm, hcat, col0):
    P = C.P
    for h in range(4):
        pts = []
        for mt in range(2):
            pb = nextbank(S)
            P.mm(pb[:], [(KmT[:, seq, h, mt * 128:(mt + 1) * 128], qmT[:, h, :])],
                 reads=[KmT.k, qmT.k], writes=[pb.k])
            pt = S["PT"][S["pt_i"] % len(S["PT"])]
            S["pt_i"] += 1
            P.op("act", lambda e, pt=pt, pb=pb: e.activation(out=pt[:], in_=pb[:], func=AF.Exp, scale=128.0 ** -0.5),
                 reads=[pb.k], writes=[pt.k])
            pts.append(pt)
        for a in range(4):
            pb = nextbank(S)
            P.mm(pb[:, 0:129], [(pts[mt][:, a * 128:(a + 1) * 128], Vm[:, seq, mt, h, :]) for mt in range(2)],
                 reads=[pts[0].k, pts[1].k, Vm.k], writes=[pb.k])
            rc = S["rc"][S["rc_i"] % 2]
            S["rc_i"] += 1
            P.op("dve", lambda e, rc=rc, pb=pb: e.reciprocal(out=rc[:], in_=pb[:, 128:129]),
                 reads=[pb.k], writes=[rc.k])
            P.op("dve", lambda e, rc=rc, pb=pb, a=a, h=h: e.tensor_scalar(
                out=hcat[a][:, col0 + h * 128:col0 + (h + 1) * 128], in0=pb[:, 0:128], scalar1=rc[:, 0:1],
                scalar2=None, op0=ALU.mult), reads=[pb.k, rc.k], writes=[hcat[a].k])


def out_proj_ln(C, S, hcat, hcT, wout, wout_tr, xld, nl, x_view, x_trk, s, y, g_rep, b_rep, o_view, o_trk):
    P = C.P
    for a in range(4):
        pt = S["pst"][S["pst_i"] % 2]
        S["pst_i"] += 1
        ident = S["ident"]
        P.pe_multi([lambda e, c=c, pt=pt, a=a: e.transpose(out=pt[:, c * 128:(c + 1) * 128],
                                                         in_=hcat[a][:, c * 128:(c + 1) * 128],
                                                         identity=ident[:]) for c in range(8)],
                   reads=[hcat[a].k, ident.k], writes=[pt.k])
        P.op("dve", lambda e, pt=pt, a=a: e.tensor_copy(out=hcT[:, :, a * 128:(a + 1) * 128], in_=v3(pt[:, :], 8)),
             reads=[pt.k], writes=[hcT.k])
    for a in range(4):
        xi = xld[nl[0] % len(xld)]
        nl[0] += 1
        P.dma("sp", xi[:], x_view[s, a], reads=[x_trk], writes=[xi.k])
        yy = y[a % 2]
        for dh in range(2):
            pb = nextbank(S)
            P.mm(pb[:], [(hcT[:, kc, a * 128:(a + 1) * 128], wout[:, kc, dh * 512:(dh + 1) * 512])
                         for kc in range(8)], reads=[hcT.k] + wout_tr, writes=[pb.k])
            P.op("dve", lambda e, yy=yy, pb=pb, dh=dh, xi=xi: e.scalar_tensor_tensor(
                out=yy[:, dh * 512:(dh + 1) * 512], in0=xi[:, dh * 512:(dh + 1) * 512],
                scalar=ALPHA, in1=pb[:], op0=ALU.mult, op1=ALU.add),
                 reads=[xi.k, pb.k], writes=[yy.k])
        layer_norm_tile(C, S, yy, g_rep, b_rep, yy)
        P.dma("pool", o_view[s, a], yy[:], reads=[yy.k], writes=[o_trk], sem_trk=yy.k)


def mixer_a_phase(C, S, x_dram, x_trks, out_dram, out_trks, mem_d, w_in_d, bi_d, bf_d, wmk_d, w_out_d, g_d, b_d):
    nc, P = C.nc, C.P
    with contextlib.ExitStack() as es:
        win = C.sb("win", [128, 8, 2056], BF16, es)
        wout = C.sb("wout", [128, 8, D], BF16, es)
        g_rep = C.sb("g_rep", [128, D], F32, es)
        b_rep = C.sb("b_rep", [128, D], F32, es)
        bias_rep = C.sb("bias_rep", [128, 8], F32, es)
        xld = [C.sb("xld", [128, D], F32, es) for _ in range(3)]
        KmT = C.sb("KmT", [128, NSEQ, 4, 256], BF16, es)
        Vm = C.sb("Vm", [128, NSEQ, 2, 4, 129], BF16, es)
        P.dma("sp", g_rep[:], g_d.partition_broadcast(128), writes=[g_rep.k])
        P.dma("sp", b_rep[:], b_d.partition_broadcast(128), writes=[b_rep.k])
        P.dma("sp", bias_rep[:, 0:4], bi_d.partition_broadcast(128), writes=[bias_rep.k])
        P.dma("sp", bias_rep[:, 4:8], bf_d.partition_broadcast(128), writes=[bias_rep.k])
        with contextlib.ExitStack() as es2:
            wmk = C.sb("wmk", [128, 8, D], BF16, es2)
            wmk_tr = load_weight_cast(C, wmk, wmk_d.rearrange("(c p) f -> p c f", p=128), 2, 1)
            win_view = w_in_d.rearrange("(c p) f -> p c f", p=128)
            win_g = []
            for (c0, c1) in ((0, 512), (1536, 2056), (512, 1024), (1024, 1536)):
                k = Trk("win_%d" % c0)
                P.dma("pool", win[:, :, c0:c1], win_view[:, :, c0:c1], writes=[k])
                win_g.append(k)
            wqk_tr, wgm_tr, wv_tr, wo_tr = [win_g[0]], [win_g[1]], [win_g[2]], [win_g[3]]
            wout_tr = []
            mem_kv_precompute(C, S, es2, mem_d, wmk, wmk_tr, xld, KmT, Vm)
            P.full_barrier()
        xT = [C.sb("xT", [128, 8, ST], BF16, es) for _ in range(2)]
        qT = [C.sb("qT", [64, 4, ST], BF16, es) for _ in range(2)]
        kT = [C.sb("kT", [64, 4, ST], BF16, es) for _ in range(2)]
        qz = [C.sb("qz", [64, 4, 4, 2, 128], BF16, es) for _ in range(2)]
        qmT = [C.sb("qmT", [128, 4, ST], BF16, es) for _ in range(2)]
        gts = C.sb("gts", [128, 8], F32, es)
        lfn = C.sb("lfn", [128, 4], F32, es)
        tadd = C.sb("tadd", [128, 4], F32, es)
        colf = C.sb("colf", [128, 4], F32, es)
        enb = C.sb("enb", [128, 4], F32, es)
        eg = [C.sb("eg", [128, 2, 4], F32, es) for _ in range(2)]
        kc_t = C.sb("kc", [128, 4, 64], BF16, es)
        vaug = [C.sb("vaug", [128, 4, 129], BF16, es) for _ in range(2)]
        e_o = C.sb("e_o", [128, 512], F32, es)
        atmp = C.sb("atmp", [128, 4, 128], BF16, es)
        AT = C.sb("AT", [128, 4, 128], BF16, es)
        Sst = C.sb("Sst", [64, 4, 129], F32, es)
        Cf = C.sb("Cf", [64, 4, 129], F32, es)
        Cb = [C.sb("Cb", [64, 4, 129], BF16, es) for _ in range(2)]
        den = C.sb("den", [128, 2, 1], F32, es)
        hcat2 = [[C.sb("hcat", [128, D], BF16, es) for _ in range(4)] for _ in range(2)]
        hcT = C.sb("hcT", [128, 8, ST], BF16, es)
        y = [C.sb("y", [128, D], F32, es) for _ in range(2)]
        S["PT"] = [C.sb("PT", [128, ST], BF16, es) for _ in range(4)]
        S["pt_i"] = 0
        S["rc"] = [C.sb("rc", [128, 1], F32, es) for _ in range(2)]
        S["rc_i"] = 0
        for qq in qz:
            P.op("pool", lambda e, qq=qq: e.memset(qq[:], 0.0), writes=[qq.k])
        for vv in vaug:
            P.op("pool", lambda e, vv=vv: e.memset(vv[:], 1.0), writes=[vv.k])

        x_view = x_dram.rearrange("(s a p) d -> s a p d", a=4, p=128)
        o_view = out_dram.rearrange("(s a p) d -> s a p d", a=4, p=128)
        cf = S["cf"]
        maskb = S["maskb"]
        nl = [0]
        NT = NST // NSEQ

        def front(s):
            b, seq = s % 2, s // NT
            xTb, qTb, kTb, qzb, qmTb = xT[b], qT[b], kT[b], qz[b], qmT[b]
            for a in range(4):
                xi = xld[nl[0] % len(xld)]
                nl[0] += 1
                P.dma("sp", xi[:], x_view[s, a], reads=[x_trks[s]], writes=[xi.k])
                transpose_tokens(C, S, xi, xTb, a * 128)
            for h in range(4):
                for (dst, c0) in ((qTb, 0), (kTb, 256)):
                    pb = nextbank(S)
                    P.mm(pb[0:64, :], [(win[:, kc, c0 + h * 64:c0 + (h + 1) * 64], xTb[:, kc, :]) for kc in range(8)],
                         reads=[xTb.k] + wqk_tr, writes=[pb.k])
                    P.op("act", lambda e, pb=pb, dst=dst, h=h: e.activation(out=dst[:, h, :], in_=pb[0:64, :],
                                                                            func=AF.Copy),
                         reads=[pb.k], writes=[dst.k])
                P.op("pool", lambda e, h=h: e.tensor_copy(
                    out=bass.AP(qzb.t, h * 1024, [[4096, 64], [256, 4], [192, 2], [1, 64]]),
                    in_=qTb[:, h, :].rearrange("p (a c j) -> p a c j", a=4, c=2)),
                     reads=[qTb.k], writes=[qzb.k])
                pb = nextbank(S)
                P.mm(pb[:], [(win[:, kc, 1544 + h * 128:1544 + (h + 1) * 128], xTb[:, kc, :]) for kc in range(8)],
                     reads=[xTb.k] + wgm_tr, writes=[pb.k])
                P.op("act", lambda e, pb=pb, h=h: e.activation(out=qmTb[:, h, :], in_=pb[:], func=AF.Copy),
                     reads=[pb.k], writes=[qmTb.k])
            mem_attention(C, S, seq, qmTb, KmT, Vm, hcat2[b], 512)

        def tail(s):
            out_proj_ln(C, S, hcat2[s % 2], hcT, wout, wout_tr, xld, nl, x_view, x_trks[s], s, y, g_rep, b_rep,
                        o_view, out_trks[s])

        def tok_loop(s):
            b = s % 2
            xTb, qTb, kTb, qzb, hcat = xT[b], qT[b], kT[b], qz[b], hcat2[b]
            for a in range(4):
                t = s * 4 + a
                par = t % 2
                first = (t % (SEQ // 128) == 0)
                cols = slice(a * 128, (a + 1) * 128)
                va = vaug[par]
                pg = nextbank(S)
                P.mm(pg[:, 0:8], [(xTb[:, kc, cols], win[:, kc, 1536:1544]) for kc in range(8)],
                     reads=[xTb.k] + wgm_tr, writes=[pg.k])
                P.op("dve", lambda e, pg=pg: e.tensor_tensor(out=gts[:], in0=pg[:, 0:8], in1=bias_rep[:], op=ALU.add),
                     reads=[pg.k, bias_rep.k], writes=[gts.k])
                P.op("act", lambda e: e.activation(out=lfn[:], in_=gts[:, 4:8], func=AF.Exp, scale=-1.0),
                     reads=[gts.k], writes=[lfn.k])
                P.op("act", lambda e: e.activation(out=lfn[:], in_=lfn[:], func=AF.Ln, bias=S["one1"][:], scale=1.0),
                     reads=[lfn.k, S["one1"].k], writes=[lfn.k])
                pc = nextbank(S)
                P.mm(pc[:, 0:4], [(cf[:, 1, :], lfn[:])], reads=[cf.k, lfn.k], writes=[pc.k])
                P.mm(pc[:, 4:8], [(cf[:, 2, :], lfn[:])], reads=[cf.k, lfn.k], writes=[pc.k])
                P.mm(pc[:, 8:12], [(cf[:, 3, :], lfn[:])], reads=[cf.k, lfn.k], writes=[pc.k])
                P.op("dve", lambda e, pc=pc: e.tensor_tensor(out=tadd[:], in0=gts[:, 0:4], in1=pc[:, 0:4], op=ALU.add),
                     reads=[gts.k, pc.k], writes=[tadd.k])
                P.op("act", lambda e: e.activation(out=colf[:], in_=tadd[:], func=AF.Exp, bias=S["ln8"][:], scale=1.0),
                     reads=[tadd.k, S["ln8"].k], writes=[colf.k])
                P.op("act", lambda e, pc=pc: e.activation(out=enb[:], in_=pc[:, 0:4], func=AF.Exp),
                     reads=[pc.k], writes=[enb.k])
                P.op("act", lambda e, pc=pc, par=par: e.activation(out=eg[par][:], in_=v3(pc[:, 4:12], 2),
                                                                   func=AF.Exp, scale=-1.0),
                     reads=[pc.k], writes=[eg[par].k])
                pk = nextbank(S)
                P.mm(pk[:, 0:256], [(xTb[:, kc, cols], win[:, kc, 256:512]) for kc in range(8)],
                     reads=[xTb.k] + wqk_tr, writes=[pk.k])
                P.op("dve", lambda e, pk=pk: e.tensor_tensor(
                    out=kc_t[:], in0=v3(pk[:, 0:256], 4), in1=colf[:, 0:4].unsqueeze(2).broadcast_to([128, 4, 64]),
                    op=ALU.mult), reads=[pk.k, colf.k], writes=[kc_t.k])
                pv = nextbank(S)
                P.mm(pv[:], [(xTb[:, kc, cols], win[:, kc, 512:1024]) for kc in range(8)],
                     reads=[xTb.k] + wv_tr, writes=[pv.k])
                P.op("act", lambda e, pv=pv, va=va: e.activation(out=va[:, :, 0:128], in_=v3(pv[:], 4), func=AF.Copy),
                     reads=[pv.k], writes=[va.k])
                po = nextbank(S)
                P.mm(po[:], [(xTb[:, kc, cols], win[:, kc, 1024:1536]) for kc in range(8)],
                     reads=[xTb.k] + wo_tr, writes=[po.k])
                P.op("act", lambda e, po=po: e.activation(out=e_o[:], in_=po[:], func=AF.Exp, scale=-1.0),
                     reads=[po.k], writes=[e_o.k])
                P.op("act", lambda e: e.activation(out=e_o[:], in_=e_o[:], func=AF.Ln, bias=S["one1"][:], scale=1.0),
                     reads=[e_o.k, S["one1"].k], writes=[e_o.k])
                P.op("act", lambda e: e.activation(out=e_o[:], in_=e_o[:], func=AF.Exp, scale=-1.0),
                     reads=[e_o.k], writes=[e_o.k])
                pa = nextbank(S)
                for h in range(4):
                    P.mm(pa[:, h * 128:(h + 1) * 128], [(kTb[:, h, cols], qTb[:, h, cols])],
                         reads=[kTb.k, qTb.k], writes=[pa.k])
                P.op("dve", lambda e, pa=pa: e.tensor_tensor(
                    out=atmp[:], in0=v3(pa[:], 4), in1=colf[:, 0:4].unsqueeze(2).broadcast_to([128, 4, 128]),
                    op=ALU.mult), reads=[pa.k, colf.k], writes=[atmp.k])
                P.op("pool", lambda e: e.tensor_tensor(
                    out=AT[:], in0=atmp[:], in1=maskb[:, :].unsqueeze(1).broadcast_to([128, 4, 128]), op=ALU.mult),
                     reads=[atmp.k, maskb.k], writes=[AT.k])
                for c in range(2):
                    if first and c == 0:
                        P.op("dve", lambda e: e.memset(Cf[:], 0.0), writes=[Cf.k])
                        P.op("pool", lambda e: e.memset(Cb[0][:], 0.0), writes=[Cb[0].k])
                    else:
                        egp = eg[par][0:64, 0, :] if c == 1 else eg[1 - par][0:64, 1, :]
                        egk = eg[par].k if c == 1 else eg[1 - par].k
                        P.op("dve", lambda e, egp=egp: e.tensor_tensor(
                            out=Cf[:], in0=Sst[:], in1=egp.unsqueeze(2).broadcast_to([64, 4, 129]), op=ALU.mult),
                             reads=[Sst.k, egk], writes=[Cf.k])
                        P.op("act", lambda e, c=c: e.activation(out=Cb[c][:], in_=Cf[:], func=AF.Copy),
                             reads=[Cf.k], writes=[Cb[c].k])
                    rows = slice(c * 64, (c + 1) * 64)
                    for hp in range(2):
                        pu = nextbank(S)
                        for j in range(2):
                            h = 2 * hp + j
                            P.mm(pu[0:64, j * 129:(j + 1) * 129], [(kc_t[rows, h, :], va[rows, h, :])],
                                 reads=[kc_t.k, va.k], writes=[pu.k])
                        P.op("dve", lambda e, pu=pu, hp=hp: e.tensor_tensor(
                            out=Sst[:, 2 * hp:2 * hp + 2, :], in0=Cf[:, 2 * hp:2 * hp + 2, :],
                            in1=v3(pu[0:64, 0:258], 2), op=ALU.add),
                             reads=[Cf.k, pu.k], writes=[Sst.k])
                for hp in range(2):
                    pn = nextbank(S)
                    for j in range(2):
                        h = 2 * hp + j
                        P.mm(pn[:, j * 129:(j + 1) * 129],
                             [(AT[:, h, :], va[:, h, :]),
                              (qzb[:, h, a, 0, :], Cb[0][:, h, :]),
                              (qzb[:, h, a, 1, :], Cb[1][:, h, :])],
                             reads=[AT.k, va.k, qzb.k, Cb[0].k, Cb[1].k], writes=[pn.k])
                    pn3 = v3(pn[:, 0:258], 2)
                    P.op("dve", lambda e, pn3=pn3, hp=hp: e.tensor_tensor(
                        out=den[:], in0=pn3[:, :, 128:129], in1=enb[:, 2 * hp:2 * hp + 2].unsqueeze(2),
                        op=ALU.max), reads=[pn.k, enb.k], writes=[den.k])
                    P.op("dve", lambda e, pn3=pn3: e.scalar_tensor_tensor(
                        out=den[:], in0=pn3[:, :, 128:129], scalar=-1.0, in1=den[:], op0=ALU.mult, op1=ALU.max),
                         reads=[pn.k, den.k], writes=[den.k])
                    P.op("dve", lambda e: e.reciprocal(out=den[:], in_=den[:]), reads=[den.k], writes=[den.k])
                    for j in range(2):
                        h = 2 * hp + j
                        P.op("dve", lambda e, pn=pn, j=j, h=h, a=a: e.scalar_tensor_tensor(
                            out=hcat[a][:, h * 128:(h + 1) * 128], in0=pn[:, j * 129:j * 129 + 128],
                            scalar=den[:, j, :], in1=e_o[:, h * 128:(h + 1) * 128], op0=ALU.mult, op1=ALU.mult),
                             reads=[pn.k, den.k, e_o.k], writes=[hcat[a].k])

        def side(fn, *a):
            S["rot0"], S["nrot"] = 4, 2
            fn(*a)
            S["rot0"], S["nrot"] = 0, 4

        side(front, 0)
        wout_tr.extend(load_weight_cast(C, wout, w_out_d.rearrange("(c p) f -> p c f", p=128), 2, 1))
        for s in range(NST):
            pending = []
            P.rec = pending
            if s > 0:
                side(tail, s - 1)
            if s + 1 < NST:
                side(front, s + 1)
            P.rec = None
            S["rot0"], S["nrot"] = 0, 4
            P.inter = (pending, len(pending) / 260.0 + 0.02)
            P._acc = 0.0
            tok_loop(s)
            P.inter = None
            while pending:
                P.replay(pending.pop(0))
        side(tail, NST - 1)
        P.full_barrier()
        S["rot0"], S["nrot"] = 0, 6


def rms_rows(C, S, pb, ncols, out_bf, scr):
    P = C.P
    ss, rs = S["rms_ss"], S["rms_rs"]
    if scr is None:
        scr = nextbank(S)
    P.op("dve", lambda e: e.memset(ss[:], 0.0), writes=[ss.k])
    P.op("act", lambda e: e.activation(out=scr[:, 0:ncols], in_=pb[:, 0:ncols], func=AF.Square, accum_out=ss[:]),
         reads=[pb.k], writes=[scr.k, ss.k])
    P.op("act", lambda e: e.activation(out=rs[:], in_=ss[:], func=AF.Ln, bias=S["eps_rms"][:], scale=1.0 / ncols),
         reads=[ss.k, S["eps_rms"].k], writes=[rs.k])
    P.op("act", lambda e: e.activation(out=rs[:], in_=rs[:], func=AF.Exp, scale=-0.5), reads=[rs.k], writes=[rs.k])
    P.op("dve", lambda e: e.tensor_scalar(out=out_bf[:, 0:ncols], in0=pb[:, 0:ncols], scalar1=rs[:, 0:1],
                                          scalar2=None, op0=ALU.mult), reads=[pb.k, rs.k], writes=[out_bf.k])


def transpose_cols(C, S, src_bf, nchunk, dstT, col0):
    P = C.P
    pt = S["pst"][S["pst_i"] % 2]
    S["pst_i"] += 1
    ident = S["ident"]
    P.pe_multi([lambda e, c=c: e.transpose(out=pt[:, c * 128:(c + 1) * 128], in_=src_bf[:, c * 128:(c + 1) * 128],
                                           identity=ident[:]) for c in range(nchunk)],
               reads=[src_bf.k, ident.k], writes=[pt.k])
    P.op("dve", lambda e: e.tensor_copy(out=dstT[:, 0:nchunk, col0:col0 + 128], in_=v3(pt[:, 0:nchunk * 128], nchunk)),
         reads=[pt.k], writes=[dstT.k])


def mixer_b_phase(C, S, x_dram, x_trks, out_dram, out_trks, mem_d, pos_d, wdown_d, gkv_d, wuk_d, wuv_d,
                  bwin_d, gq_d, wuq_d, wmk_d, w_out_d, g_d, b_d, cf2_d):
    nc, P = C.nc, C.P
    SC = 96.0 ** -0.5
    NT = NST // NSEQ
    with contextlib.ExitStack() as es:
        wdown = C.sb("wdown", [128, 8, 288], BF16, es)
        wdr = C.sb("wdr", [128, 8, 96], BF16, es)
        wdrot = C.sb("wdrot", [128, 8, 96], BF16, es)
        wuk = C.sb("wuk", [128, 2, 512], BF16, es)
        wuv = C.sb("wuv", [128, 2, 512], BF16, es)
        wuq = C.sb("wuq", [128, 2, 768], BF16, es)
        wuqrot = C.sb("wuqrot", [128, 2, 8, 96], BF16, es)
        gk = C.sb("gk", [128, 2], F32, es)
        gq = C.sb("gq", [128, 2], F32, es)
        bwin = C.sb("bwin", [128, 8, 768], BF16, es)
        wout = C.sb("wout", [128, 8, D], BF16, es)
        g_rep = C.sb("g_rep", [128, D], F32, es)
        b_rep = C.sb("b_rep", [128, D], F32, es)
        cf2 = C.sb("cf2", [128, 4], F32, es)
        xld = [C.sb("xld", [128, D], F32, es) for _ in range(2)]
        KmT = C.sb("KmT", [128, NSEQ, 4, 256], BF16, es)
        Vm = C.sb("Vm", [128, NSEQ, 2, 4, 129], BF16, es)
        P.dma("sp", g_rep[:], g_d.partition_broadcast(128), writes=[g_rep.k])
        P.dma("sp", b_rep[:], b_d.partition_broadcast(128), writes=[b_rep.k])
        P.dma("sp", cf2[:], cf2_d, writes=[cf2.k])
        for kc in range(2):
            P.dma("sp", gk[:, kc:kc + 1], gkv_d[kc * 128:(kc + 1) * 128].rearrange("(p o) -> p o", o=1), writes=[gk.k])
            P.dma("sp", gq[:, kc:kc + 1], gq_d[kc * 128:(kc + 1) * 128].rearrange("(p o) -> p o", o=1), writes=[gq.k])
        with contextlib.ExitStack() as es2:
            wmk = C.sb("wmk", [128, 8, D], BF16, es2)
            wst = C.sb("wst", [128, 2, 768], F32, es2)
            wmk_tr = load_weight_cast(C, wmk, wmk_d.rearrange("(c p) f -> p c f", p=128), 2, 1)
            wdown_tr = load_weight_cast(C, wdown, wdown_d.rearrange("(c p) f -> p c f", p=128), 1, 1)
            bwin_tr = load_weight_cast(C, bwin, bwin_d.rearrange("(c p) f -> p c f", p=128), 2, 1)
            wout_tr = []
            for (dst, src_d, gg, ncol) in ((wuk, wuk_d, gk, 512), (wuv, wuv_d, gk, 512), (wuq, wuq_d, gq, 768)):
                P.dma("sp", wst[:, :, 0:ncol], src_d.rearrange("(c p) f -> p c f", p=128), writes=[wst.k])
                for kc in range(2):
                    P.op("dve", lambda e, dst=dst, gg=gg, kc=kc, ncol=ncol: e.tensor_scalar(
                        out=dst[:, kc, :], in0=wst[:, kc, 0:ncol], scalar1=gg[:, kc:kc + 1], scalar2=None,
                        op0=ALU.mult), reads=[wst.k, gg.k], writes=[dst.k])
            mem_kv_precompute(C, S, es2, mem_d, wmk, wmk_tr, xld, KmT, Vm)
            P.full_barrier()
        xT = [C.sb("xT", [128, 8, ST], BF16, es) for _ in range(2)]
        uu = C.sb("uu", [96, ST], F32, es)
        u2 = C.sb("u2", [96, ST], F32, es)
        cosT = C.sb("cosT", [96, ST], F32, es)
        sinT = C.sb("sinT", [96, ST], F32, es)
        kT = C.sb("kT", [96, 8, SEQ], BF16, es)
        Vaug = C.sb("Vaug", [128, 16, 8, 65], BF16, es)
        kT_blk = [Trk("kTb%d" % i) for i in range(NT)]
        V_blk = [Trk("Vb%d" % i) for i in range(NT)]
        ckn = C.sb("ckn", [128, 256], BF16, es)
        ckT = [C.sb("ckT", [128, 2, ST], BF16, es) for _ in range(2)]
        cqT = [C.sb("cqT", [128, 2, ST], BF16, es) for _ in range(2)]
        rt1 = C.sb("rt1", [96, ST], F32, es)
        rt2 = C.sb("rt2", [96, ST], F32, es)
        kr = C.sb("kr", [96, ST], BF16, es)
        qTh = [C.sb("qTh", [96, 8, ST], BF16, es) for _ in range(2)]
        qmT = [C.sb("qmT", [128, 4, ST], BF16, es) for _ in range(2)]
        hc_all = C.sb("hc_all", [128, 4, D], BF16, es)
        y = [C.sb("y", [128, D], F32, es) for _ in range(2)]
        scr = None
        rc4 = C.sb("rc4", [128, 4, 1], F32, es)
        S["PT"] = [C.sb("PT", [128, ST], BF16, es) for _ in range(3)]
        S["pt_i"] = 0
        S["rc"] = [C.sb("rc", [128, 1], F32, es) for _ in range(2)]
        S["rc_i"] = 0
        S["rms_ss"] = C.sb("rms_ss", [128, 1], F32, es)
        S["rms_rs"] = C.sb("rms_rs", [128, 1], F32, es)
        hcat = []
        for a in range(4):
            v = Tile(hc_all.t[:, a, :], "hcv")
            v.k = hc_all.k
            hcat.append(v)

        P.op("pool", lambda e: e.memset(wdr[:], 0.0), writes=[wdr.k])
        P.op("pool", lambda e: e.memset(wdrot[:], 0.0), writes=[wdrot.k])
        P.op("pool", lambda e: e.memset(wuqrot[:], 0.0), writes=[wuqrot.k])
        P.op("pool", lambda e: e.tensor_copy(out=wdr[:, :, 64:96], in_=wdown[:, :, 256:288]),
             reads=wdown_tr, writes=[wdr.k])
        P.op("pool", lambda e: e.tensor_scalar(out=wdrot[:, :, 64:80], in0=wdown[:, :, 272:288], scalar1=-1.0,
                                               scalar2=None, op0=ALU.mult), reads=wdown_tr, writes=[wdrot.k])
        P.op("pool", lambda e: e.tensor_copy(out=wdrot[:, :, 80:96], in_=wdown[:, :, 256:272]),
             reads=wdown_tr, writes=[wdrot.k])
        wuq4 = wuq.t[:, :, :].rearrange("p c (h d) -> p c h d", h=8)
        P.op("pool", lambda e: e.tensor_scalar(out=wuqrot[:, :, :, 64:80], in0=wuq4[:, :, :, 80:96], scalar1=-1.0,
                                               scalar2=None, op0=ALU.mult), reads=[wuq.k], writes=[wuqrot.k])
        P.op("pool", lambda e: e.tensor_copy(out=wuqrot[:, :, :, 80:96], in_=wuq4[:, :, :, 64:80]),
             reads=[wuq.k], writes=[wuqrot.k])
        P.op("pool", lambda e: e.memset(Vaug[:], 1.0), writes=V_blk)
        S["rot0"], S["nrot"] = 4, 2
        S["cast_eng"] = "pool"
        sbank = S["psf"][0:2]
        sb_i = [0]

        x_view = x_dram.rearrange("(s a p) d -> s a p d", a=4, p=128)
        o_view = out_dram.rearrange("(s a p) d -> s a p d", a=4, p=128)
        nl = [0]
        R = slice(64, 96)

        def front_chunks(s):
            seq, T, b = s // NT, s % NT, s % 2
            tcols = slice(T * ST, (T + 1) * ST)
            xTb, ckTb, cqTb, qThb, qmTb = xT[b], ckT[b], cqT[b], qTh[b], qmT[b]
            ch = []

            def rope_tables():
                posi_ap = rt2[R, :].bitcast(I32)
                P.dma("sp", posi_ap, pos_d[seq, T * ST:(T + 1) * ST].partition_broadcast(32), writes=[rt2.k])
                P.op("dve", lambda e: e.tensor_copy(out=uu[R, :], in_=posi_ap), reads=[rt2.k], writes=[uu.k])
                for (dstT, shift) in ((sinT, 0.0), (cosT, 0.25)):
                    P.op("dve", lambda e, shift=shift: e.tensor_scalar(
                        out=u2[R, :], in0=uu[R, :], scalar1=cf2[R, 0:1], scalar2=shift, op0=ALU.mult, op1=ALU.add),
                         reads=[uu.k, cf2.k], writes=[u2.k])
                    P.op("dve", lambda e: e.tensor_copy(out=posi_ap, in_=u2[R, :]), reads=[u2.k], writes=[rt2.k])
                    P.op("dve", lambda e: e.tensor_copy(out=rt1[R, :], in_=posi_ap), reads=[rt2.k], writes=[rt1.k])
                    P.op("dve", lambda e: e.tensor_tensor(out=u2[R, :], in0=u2[R, :], in1=rt1[R, :], op=ALU.subtract),
                         reads=[u2.k, rt1.k], writes=[u2.k])
                    P.op("dve", lambda e: e.tensor_scalar(out=rt1[R, :], in0=u2[R, :], scalar1=0.5, scalar2=None,
                                                          op0=ALU.is_gt), reads=[u2.k], writes=[rt1.k])
                    P.op("dve", lambda e: e.tensor_tensor(out=u2[R, :], in0=u2[R, :], in1=rt1[R, :], op=ALU.subtract),
                         reads=[u2.k, rt1.k], writes=[u2.k])
                    P.op("act", lambda e, dstT=dstT: e.activation(out=dstT[R, :], in_=u2[R, :], func=AF.Sin,
                                                                  scale=float(2 * np.pi)),
                         reads=[u2.k], writes=[dstT.k])
            xis = {}

            def load_dma(a):
                xi = xld[nl[0] % len(xld)]
                nl[0] += 1
                xis[a] = xi
                P.dma("sp", xi[:], x_view[s, a], reads=[x_trks[s]], writes=[xi.k])

            def load_tr(a):
                transpose_tokens(C, S, xis[a], xTb, a * 128)

            def latents(a):
                cols = slice(a * 128, (a + 1) * 128)
                for (wt, wtr, dstT) in ((wdown, wdown_tr, ckTb), (bwin, bwin_tr, cqTb)):
                    pb = nextbank(S)
                    P.mm(pb[:, 0:256], [(xTb[:, kc, cols], wt[:, kc, 0:256]) for kc in range(8)],
                         reads=[xTb.k] + wtr, writes=[pb.k])
                    rms_rows(C, S, pb, 256, ckn, scr)
                    transpose_cols(C, S, ckn, 2, dstT, a * 128)
            ch.append(lambda: load_dma(0))
            ch.append(lambda: load_dma(1))
            ch.append(rope_tables)
            ch.append(lambda: load_tr(0))
            ch.append(lambda: load_dma(2))
            ch.append(lambda: load_tr(1))
            ch.append(lambda: load_dma(3))
            ch.append(lambda: latents(0))
            ch.append(lambda: load_tr(2))
            ch.append(lambda: latents(1))
            ch.append(lambda: load_tr(3))
            ch.append(lambda: latents(2))
            ch.append(lambda: latents(3))

            def k_nope(h0):
                for h in range(h0, h0 + 4):
                    pb = nextbank(S)
                    P.mm(pb[0:64, :], [(wuk[:, kc, h * 64:(h + 1) * 64], ckTb[:, kc, :]) for kc in range(2)],
                         reads=[wuk.k, ckTb.k], writes=[pb.k])
                    P.op("dve", lambda e, pb=pb, h=h: e.tensor_copy(out=kT[0:64, h, tcols], in_=pb[0:64, :]),
                         reads=[pb.k], writes=[kT_blk[T]])
            ch.append(lambda: k_nope(0))
            ch.append(lambda: k_nope(4))

            def k_rope():
                pA, pB = nextbank(S), nextbank(S)
                P.mm(pA[0:96, :], [(wdr[:, kc, :], xTb[:, kc, :]) for kc in range(8)], reads=[wdr.k, xTb.k],
                     writes=[pA.k])
                P.mm(pB[0:96, :], [(wdrot[:, kc, :], xTb[:, kc, :]) for kc in range(8)], reads=[wdrot.k, xTb.k],
                     writes=[pB.k])
                P.op("dve", lambda e: e.tensor_tensor(out=rt1[R, :], in0=pA[R, :], in1=cosT[R, :], op=ALU.mult),
                     reads=[pA.k, cosT.k], writes=[rt1.k])
                P.op("dve", lambda e: e.tensor_tensor(out=rt2[R, :], in0=pB[R, :], in1=sinT[R, :], op=ALU.mult),
                     reads=[pB.k, sinT.k], writes=[rt2.k])
                P.op("dve", lambda e: e.tensor_tensor(out=kr[R, :], in0=rt1[R, :], in1=rt2[R, :], op=ALU.add),
                     reads=[rt1.k, rt2.k], writes=[kr.k])
                P.op("dve", lambda e: e.tensor_copy(out=kT[R, :, tcols],
                                                    in_=kr[R, :].unsqueeze(1).broadcast_to([32, 8, ST])),
                     reads=[kr.k], writes=[kT_blk[T]])
            ch.append(k_rope)

            def v_tiles():
                for a in range(4):
                    pb = nextbank(S)
                    P.mm(pb[:], [(ckTb[:, kc, a * 128:(a + 1) * 128], wuv[:, kc, :]) for kc in range(2)],
                         reads=[ckTb.k, wuv.k], writes=[pb.k])
                    P.op("act", lambda e, pb=pb, a=a: e.activation(out=Vaug[:, 4 * T + a, :, 0:64], in_=v3(pb[:], 8),
                                                                   func=AF.Copy), reads=[pb.k], writes=[V_blk[T]])
            ch.append(v_tiles)

            def queries(h0):
                for h in range(h0, h0 + 2):
                    pA, pB = nextbank(S), nextbank(S)
                    P.mm(pA[0:96, :], [(wuq[:, kc, h * 96:(h + 1) * 96], cqTb[:, kc, :]) for kc in range(2)],
                         reads=[wuq.k, cqTb.k], writes=[pA.k])
                    P.mm(pB[0:96, :], [(wuqrot[:, kc, h, :], cqTb[:, kc, :]) for kc in range(2)],
                         reads=[wuqrot.k, cqTb.k], writes=[pB.k])
                    P.op("dve", lambda e, pA=pA, h=h: e.tensor_copy(out=qThb[0:64, h, :], in_=pA[0:64, :]),
                         reads=[pA.k], writes=[qThb.k])
                    P.op("dve", lambda e, pA=pA: e.tensor_tensor(out=rt1[R, :], in0=pA[R, :], in1=cosT[R, :],
                                                                 op=ALU.mult), reads=[pA.k, cosT.k], writes=[rt1.k])
                    P.op("dve", lambda e, pB=pB: e.tensor_tensor(out=rt2[R, :], in0=pB[R, :], in1=sinT[R, :],
                                                                 op=ALU.mult), reads=[pB.k, sinT.k], writes=[rt2.k])
                    P.op("dve", lambda e, h=h: e.tensor_tensor(out=qThb[R, h, :], in0=rt1[R, :], in1=rt2[R, :],
                                                               op=ALU.add), reads=[rt1.k, rt2.k], writes=[qThb.k])
            for h0 in range(0, 8, 2):
                ch.append(lambda h0=h0: queries(h0))

            def q_mem():
                for h in range(4):
                    pb = nextbank(S)
                    P.mm(pb[:], [(bwin[:, kc, 256 + h * 128:256 + (h + 1) * 128], xTb[:, kc, :]) for kc in range(8)],
                         reads=[xTb.k] + bwin_tr, writes=[pb.k])
                    P.op("act", lambda e, pb=pb, h=h: e.activation(out=qmTb[:, h, :], in_=pb[:], func=AF.Copy),
                         reads=[pb.k], writes=[qmTb.k])
            ch.append(q_mem)
            return ch

        def mla_head(s, h, pending, per):
            T, b = s % NT, s % 2
            qThb = qTh[b]
            nkt = 4 * T + 4
            po = S["psf"][2 + (h % 2)]
            started = [False]

            def emit_front(j):
                a_min = max(0, j - 4 * T)
                qc = slice(a_min * 128, ST)
                pb = sbank[sb_i[0] % 2]
                sb_i[0] += 1
                P.mm(pb[:, qc], [(kT[:, h, j * 128:(j + 1) * 128], qThb[:, h, qc])],
                     reads=[kT_blk[j // 4], qThb.k], writes=[pb.k])
                pt = S["PT"][S["pt_i"] % len(S["PT"])]
                S["pt_i"] += 1
                P.op("act", lambda e: e.activation(out=pt[:, qc], in_=pb[:, qc], func=AF.Exp, scale=SC),
                     reads=[pb.k], writes=[pt.k])
                if j >= 4 * T:
                    P.op("pool", lambda e: e.memset(pt[64:128, a_min * 128:a_min * 128 + 64], 0.0),
                         reads=[], writes=[pt.k])
                return (j, a_min, pt)

            def emit_back(j, a_min, pt):
                fns = []
                for a in range(a_min, 4):
                    st = not started[0]
                    started[0] = True
                    fns.append(lambda e, a=a, st=st: e.matmul(
                        po[:, a * 65:(a + 1) * 65], pt[:, a * 128:(a + 1) * 128], Vaug[:, j, h, :],
                        start=st, stop=(j == 4 * T + a), skip_group_check=True))
                P.pe_multi(fns, reads=[pt.k, V_blk[j // 4]], writes=[po.k])

            prev = None
            for j in range(nkt):
                cur = emit_front(j)
                if prev is not None:
                    emit_back(*prev)
                prev = cur
                for _ in range(per):
                    if pending:
                        P.replay(pending.pop(0))
            emit_back(*prev)
            po3 = v3(po[:, 0:260], 4)
            P.op("dve", lambda e: e.reciprocal(out=rc4[:], in_=po3[:, :, 64:65]), reads=[po.k], writes=[rc4.k])
            P.op("dve", lambda e: e.tensor_tensor(
                out=hc_all[:, :, h * 64:(h + 1) * 64], in0=po3[:, :, 0:64],
                in1=rc4[:, :, 0:1].broadcast_to([128, 4, 64]), op=ALU.mult),
                 reads=[po.k, rc4.k], writes=[hc_all.k])

        for f in front_chunks(0):
            f()
        wout_tr.extend(load_weight_cast(C, wout, w_out_d.rearrange("(c p) f -> p c f", p=128), 2, 1))
        for s in range(NST):
            seq, b = s // NT, s % 2
            pending = []
            if s + 1 < NST:
                P.rec = pending
                for f in front_chunks(s + 1):
                    f()
                P.rec = None
            mem_attention(C, S, seq, qmT[b], KmT, Vm, hcat, 512)
            nsteps = 8 * (4 * (s % NT) + 4)
            per = (len(pending) + nsteps - 1) // nsteps
            for h in range(8):
                mla_head(s, h, pending, per)
            while pending:
                P.replay(pending.pop(0))
            out_proj_ln(C, S, hcat, xT[b], wout, wout_tr, xld, nl, x_view, x_trks[s], s, y, g_rep, b_rep,
                        o_view, out_trks[s])
        P.full_barrier()
        S["rot0"], S["nrot"] = 0, 6
        S.pop("cast_eng")


def _consts_np():
    c = np.zeros((128, 4, 128), np.float32)
    c[:, 0, :] = np.eye(128, dtype=np.float32)
    s = np.arange(128)[:, None]
    l = np.arange(128)[None, :]
    c[:, 1, :] = ((s // 64 == l // 64) & (s <= l)).astype(np.float32)
    c[:, 2, :] = (s < 64).astype(np.float32) * np.ones((1, 128), np.float32)
    c[:, 3, :] = (s >= 64).astype(np.float32) * np.ones((1, 128), np.float32)
    return c


def _cf2_np():
    c = np.zeros((128, 4), np.float32)
    inv = (10000.0 ** (-np.arange(0, 32, 2, dtype=np.float32) / 32)).astype(np.float32)
    for p in range(64, 96):
        c[p, 0] = inv[(p - 64) % 16] / np.float32(2 * np.pi)
    c[:, 1] = -np.pi
    return c


W_SHAPES = {
    "a_w_in": [D, 2056], "a_b_igate": [4], "a_b_fgate": [4], "a_w_mem_kv": [D, D], "a_w_out": [D, D],
    "kv_w_down": [D, 288], "kv_norm_g": [256], "kv_w_uk": [256, 512], "kv_w_uv": [256, 512],
    "b_w_in": [D, 768], "b_q_norm_g": [256], "b_w_uq": [256, 768], "b_w_mem_kv": [D, D], "b_w_out": [D, D],
    "ln1_g": [2, D], "ln1_b": [2, D], "ffn_w_up": [2, D, DFF], "ffn_w_down": [2, DFF, D], "ln2_g": [2, D],
    "ln2_b": [2, D],
}


def build_program():
    nc = bass.Bass("TRN2", target_bir_lowering=False)
    dt = lambda n, sh: nc.dram_tensor(n, sh, F32, kind="ExternalInput").ap()
    x = dt("x", [NTOK, D])
    mem = dt("mem", [NSEQ, 256, D])
    pos = nc.dram_tensor("positions", [NSEQ, SEQ], I32, kind="ExternalInput").ap()
    w = {k: dt(k, sh) for k, sh in W_SHAPES.items()}
    consts = dt("consts", [128, 4, 128])
    cf2 = dt("cf2", [128, 4])
    out = nc.dram_tensor("out", [NTOK, D], F32, kind="ExternalOutput").ap()
    sc1 = nc.dram_tensor("scratch1", [NTOK, D], F32, kind="Internal").ap()
    sc2 = nc.dram_tensor("scratch2", [NTOK, D], F32, kind="Internal").ap()
    C = Ctx(nc)
    with nc.allow_low_precision("bf16 matmul operands, fp32 accumulation"), C.es:
        S = alloc_shared(C)
        load_consts(C, S, consts)
        xtr = [Trk("xd%d" % i) for i in range(NST)]
        t1 = [Trk("s1_%d" % i) for i in range(NST)]
        t2 = [Trk("s2_%d" % i) for i in range(NST)]
        otr = [Trk("od%d" % i) for i in range(NST)]
        mixer_a_phase(C, S, x, xtr, sc1, t1, mem, w["a_w_in"], w["a_b_igate"], w["a_b_fgate"], w["a_w_mem_kv"],
                      w["a_w_out"], w["ln1_g"][0], w["ln1_b"][0])
        ffn_phase(C, S, sc1, t1, sc2, t2, w["ffn_w_up"][0], w["ffn_w_down"][0], w["ln2_g"][0], w["ln2_b"][0], False)
        mixer_b_phase(C, S, sc2, t2, sc1, t1, mem, pos, w["kv_w_down"], w["kv_norm_g"], w["kv_w_uk"], w["kv_w_uv"],
                      w["b_w_in"], w["b_q_norm_g"], w["b_w_uq"], w["b_w_mem_kv"], w["b_w_out"], w["ln1_g"][1],
                      w["ln1_b"][1], cf2)
        ffn_phase(C, S, sc1, t1, out, otr, w["ffn_w_up"][1], w["ffn_w_down"][1], w["ln2_g"][1], w["ln2_b"][1], True)
        C.P.finish()
    return nc


def kernel(x, mem, positions, a_w_in, a_b_igate, a_b_fgate, a_w_mem_kv, a_w_out, kv_w_down, kv_norm_g, kv_w_uk,
           kv_w_uv, b_w_in, b_q_norm_g, b_w_uq, b_w_mem_kv, b_w_out, ln1_g, ln1_b, ffn_w_up, ffn_w_down, ln2_g,
           ln2_b):
    f32 = lambda a: np.ascontiguousarray(np.asarray(a), dtype=np.float32)
    shared = {
        "a_w_in": f32(a_w_in)[0], "a_b_igate": f32(a_b_igate)[0], "a_b_fgate": f32(a_b_fgate)[0],
        "a_w_mem_kv": f32(a_w_mem_kv)[0], "a_w_out": f32(a_w_out)[0], "kv_w_down": f32(kv_w_down),
        "kv_norm_g": f32(kv_norm_g), "kv_w_uk": f32(kv_w_uk), "kv_w_uv": f32(kv_w_uv), "b_w_in": f32(b_w_in)[0],
        "b_q_norm_g": f32(b_q_norm_g)[0], "b_w_uq": f32(b_w_uq)[0], "b_w_mem_kv": f32(b_w_mem_kv)[0],
        "b_w_out": f32(b_w_out)[0], "ln1_g": f32(ln1_g), "ln1_b": f32(ln1_b), "ffn_w_up": f32(ffn_w_up),
        "ffn_w_down": f32(ffn_w_down), "ln2_g": f32(ln2_g), "ln2_b": f32(ln2_b),
        "consts": _consts_np(), "cf2": _cf2_np(),
    }
    shared = {k: np.ascontiguousarray(v) for k, v in shared.items()}
    x = f32(x)
    mem = f32(mem)
    positions = np.ascontiguousarray(np.asarray(positions), dtype=np.int32)
    in_maps = []
    for c in range(NCORES):
        m = dict(shared)
        m["x"] = np.ascontiguousarray(x[c * NSEQ:(c + 1) * NSEQ].reshape(NTOK, D))
        m["mem"] = np.ascontiguousarray(mem[c * NSEQ:(c + 1) * NSEQ])
        m["positions"] = np.ascontiguousarray(positions[c * NSEQ:(c + 1) * NSEQ])
        in_maps.append(m)
    nc = build_program()
    res = run_bass_kernel_spmd(nc, in_maps, core_ids=list(range(NCORES)))
    outs = [np.asarray(r["out"], dtype=np.float32).reshape(NSEQ, SEQ, D) for r in res.results]
    return np.concatenate(outs, axis=0)
```

```python
import contextlib
import numpy as np
import concourse.bass as bass
import concourse.mybir as mybir
from concourse.bass_utils import run_bass_kernel_spmd

F32 = mybir.dt.float32
BF16 = mybir.dt.bfloat16
I32 = mybir.dt.int32
AF = mybir.ActivationFunctionType
ALU = mybir.AluOpType
AX = mybir.AxisListType

NCORES = 8
SEQ = 2048
D = 1024
DFF = 4096
NSEQ = 2
NTOK = NSEQ * SEQ
ST = 512
NST = NTOK // ST
ALPHA = 4.0 ** 0.25
LN_EPS = 1e-5
RMS_EPS = 1e-6


class Trk:
    __slots__ = ("name", "w", "r", "dsem", "dcnt")

    def __init__(self, name):
        self.name = name
        self.w = None
        self.r = {}
        self.dsem = None
        self.dcnt = 0


class Prog:
    SEM_ROT = 12000

    def __init__(self, nc):
        self.nc = nc
        self.eng = {"pe": nc.tensor, "act": nc.scalar, "dve": nc.vector, "pool": nc.gpsimd,
                    "sp": nc.sync}
        self.sem = {}
        self.cnt = {}
        self.seen = {e: {} for e in self.eng}
        self.nsem = 0
        for e in self.eng:
            self._new_sem(e)
        self.out_tokens = []
        self.ninstr = 0
        self.last_tok = {}
        self.rec = None
        self.inter = None
        self._acc = 0.0
        self._in_replay = False
        self.dma_toks = {}
        self.free_dsems = []
        self.phase_trks = []

    def _alloc_sem(self, name):
        self.nsem += 1
        return self.nc.alloc_semaphore(name="%s_%d" % (name, self.nsem))

    def _new_sem(self, e):
        self.sem[e] = self._alloc_sem("s_" + e)
        self.cnt[e] = 0

    def _need(self, e, tok, skip_same):
        if tok is None:
            return
        sem, c, te = tok
        if skip_same and te == e and e == "pe":
            return
        if self.seen[e].get(sem, 0) >= c:
            return
        self.eng[e].wait_ge(sem, c)
        self.seen[e][sem] = c

    def _signal(self, e, ins):
        if self.cnt[e] >= self.SEM_ROT:
            self._new_sem(e)
        self.cnt[e] += 1
        ins.then_inc(self.sem[e], 1)
        self.last_tok[e] = (self.sem[e], self.cnt[e], e)
        return self.last_tok[e]

    def full_barrier(self):
        snap = dict(self.last_tok)
        dts = list(self.dma_toks.values())
        for e in self.eng:
            for o, tok in snap.items():
                if o != e:
                    self._need(e, tok, False)
            for tok in dts:
                self._need(e, tok, False)
        for t in self.phase_trks:
            self.free_dsems.append((t.dsem, t.dcnt))
            t.dsem = None
        self.phase_trks = []
        self.dma_toks = {}

    def _after_emit(self):
        if self.inter is None or self._in_replay:
            return
        pend, rate = self.inter
        self._acc += rate
        while self._acc >= 1.0 and pend:
            self._acc -= 1.0
            self._in_replay = True
            self.replay(pend.pop(0))
            self._in_replay = False

    def replay(self, item):
        kind, args, kw = item
        saved, self.rec = self.rec, None
        getattr(self, kind)(*args, **kw)
        self.rec = saved

    def op(self, e, fn, reads=(), writes=()):
        if self.rec is not None:
            self.rec.append(("op", (e, fn), dict(reads=list(reads), writes=list(writes))))
            return None
        for t in reads:
            self._need(e, t.w, False)
        for t in writes:
            self._need(e, t.w, True)
            for tok in t.r.values():
                self._need(e, tok, True)
        ins = fn(self.eng[e])
        tok = self._signal(e, ins)
        for t in writes:
            t.w = tok
            t.r = {}
        for t in reads:
            t.r[e] = tok
        self.ninstr += 1
        self._after_emit()
        return tok

    def mm(self, out, pairs, reads=(), writes=()):
        if self.rec is not None:
            self.rec.append(("mm", (out, list(pairs)), dict(reads=list(reads), writes=list(writes))))
            return None
        e = "pe"
        for t in reads:
            self._need(e, t.w, False)
        for t in writes:
            self._need(e, t.w, True)
            for tok in t.r.values():
                self._need(e, tok, True)
        n = len(pairs)
        ins = None
        for i, (l, r) in enumerate(pairs):
            ins = self.nc.tensor.matmul(out, l, r, start=(i == 0), stop=(i == n - 1))
        tok = self._signal(e, ins)
        for t in writes:
            t.w = tok
            t.r = {}
        for t in reads:
            t.r[e] = tok
        self.ninstr += n
        self._after_emit()
        return tok

    def pe_multi(self, fns, reads=(), writes=()):
        if self.rec is not None:
            self.rec.append(("pe_multi", (list(fns),), dict(reads=list(reads), writes=list(writes))))
            return None
        e = "pe"
        for t in reads:
            self._need(e, t.w, False)
        for t in writes:
            self._need(e, t.w, True)
            for tok in t.r.values():
                self._need(e, tok, True)
        ins = None
        for f in fns:
            ins = f(self.nc.tensor)
        tok = self._signal(e, ins)
        for t in writes:
            t.w = tok
            t.r = {}
        for t in reads:
            t.r[e] = tok
        self.ninstr += len(fns)
        self._after_emit()
        return tok

    def dma(self, q, out, in_, reads=(), writes=(), is_output=False, sem_trk=None):
        if self.rec is not None:
            self.rec.append(("dma", (q, out, in_), dict(reads=list(reads), writes=list(writes),
                                                        is_output=is_output, sem_trk=sem_trk)))
            return None
        e = q
        for t in reads:
            self._need(e, t.w, False)
        for t in writes:
            self._need(e, t.w, False)
            for tok in t.r.values():
                self._need(e, tok, False)
        trk = sem_trk if sem_trk is not None else (list(writes) + list(reads))[0]
        if trk.dsem is None:
            if self.free_dsems:
                trk.dsem, trk.dcnt = self.free_dsems.pop()
            else:
                trk.dsem = self._alloc_sem("d")
                trk.dcnt = 0
            self.phase_trks.append(trk)
        trk.dcnt += 16
        self.eng[e].dma_start(out=out, in_=in_).then_inc(trk.dsem, 16)
        tok = (trk.dsem, trk.dcnt, "dma")
        self.dma_toks[trk.dsem] = tok
        for t in writes:
            t.w = tok
            t.r = {}
        for t in reads:
            t.r["dma_%s" % trk.name] = tok
        if is_output:
            self.out_tokens.append(tok)
        self.ninstr += 1
        return tok

    def barrier_all(self, trks):
        for t in trks:
            self._need("sp", t.w, False)
            for tok in t.r.values():
                self._need("sp", tok, False)

    def finish(self):
        for tok in self.out_tokens:
            self._need("sp", tok, False)


class Tile:
    def __init__(self, t, name):
        self.t = t
        self.k = Trk(name)

    def __getitem__(self, idx):
        return self.t[idx]


class Ctx:
    def __init__(self, nc):
        self.nc = nc
        self.P = Prog(nc)
        self.es = contextlib.ExitStack()
        self.nid = 0

    def sb(self, name, shape, dt, es=None):
        self.nid += 1
        nm = "%s_%d" % (name, self.nid)
        t = (es or self.es).enter_context(self.nc.sbuf_tensor(nm, list(shape), dt))
        return Tile(t, nm)

    def ps(self, name, shape, dt, es=None):
        self.nid += 1
        nm = "%s_%d" % (name, self.nid)
        t = (es or self.es).enter_context(self.nc.psum_tensor(nm, list(shape), dt))
        return Tile(t, nm)


def load_weight_cast(C, wt, dram_view, nsplit, axis):
    P = C.P
    n = wt.t.shape[axis]
    step = n // nsplit
    trks = []
    for j in range(nsplit):
        sl = [slice(None)] * 3
        sl[axis] = slice(j * step, (j + 1) * step)
        sl = tuple(sl)
        k = Trk("%s_p%d" % (wt.k.name, j))
        P.dma("pool", wt.t[sl], dram_view[sl], writes=[k])
        trks.append(k)
    return trks


def layer_norm_tile(C, S, y, g_rep, b_rep, out):
    P = C.P
    st, mv, rstd, nmr = S["ln_st"], S["ln_mv"], S["ln_rstd"], S["ln_nmr"]
    xn = y
    for hh in range(2):
        P.op("dve", lambda e, hh=hh: e.bn_stats(out=st[:, hh, :], in_=y[:, hh * 512:(hh + 1) * 512]),
             reads=[y.k], writes=[st.k])
    P.op("dve", lambda e: e.bn_aggr(out=mv[:], in_=st[:]), reads=[st.k], writes=[mv.k])
    P.op("act", lambda e: e.activation(out=rstd[:], in_=mv[:, 1:2], func=AF.Ln, bias=S["eps_ln"][:], scale=1.0),
         reads=[mv.k, S["eps_ln"].k], writes=[rstd.k])
    P.op("act", lambda e: e.activation(out=rstd[:], in_=rstd[:], func=AF.Exp, scale=-0.5),
         reads=[rstd.k], writes=[rstd.k])
    P.op("dve", lambda e: e.tensor_scalar(out=nmr[:], in0=mv[:, 0:1], scalar1=-1.0, scalar2=None, op0=ALU.mult),
         reads=[mv.k], writes=[nmr.k])
    P.op("dve", lambda e: e.scalar_tensor_tensor(out=xn[:], in0=y[:], scalar=nmr[:, 0:1], in1=g_rep[:],
                                                 op0=ALU.add, op1=ALU.mult),
         reads=[y.k, nmr.k, g_rep.k], writes=[xn.k])
    P.op("dve", lambda e: e.scalar_tensor_tensor(out=out[:], in0=xn[:], scalar=rstd[:, 0:1], in1=b_rep[:],
                                                 op0=ALU.mult, op1=ALU.add),
         reads=[xn.k, rstd.k, b_rep.k], writes=[out.k])


def transpose_tokens(C, S, xin, xT, col0):
    P = C.P
    xb = S["xb"][S["xb_i"] % 2]
    S["xb_i"] += 1
    P.op(S.get("cast_eng", "act"), (lambda e: e.tensor_copy(out=xb[:], in_=xin[:])) if S.get("cast_eng") else
         (lambda e: e.activation(out=xb[:], in_=xin[:], func=AF.Copy)), reads=[xin.k], writes=[xb.k])
    pt = S["pst"][S["pst_i"] % 2]
    S["pst_i"] += 1
    ident = S["ident"]
    P.pe_multi([lambda e, c=c: e.transpose(out=pt[:, c * 128:(c + 1) * 128], in_=xb[:, c * 128:(c + 1) * 128],
                                           identity=ident[:]) for c in range(8)],
               reads=[xb.k, ident.k], writes=[pt.k])
    P.op("dve", lambda e: e.tensor_copy(out=xT[:, :, col0:col0 + 128],
                                        in_=pt[:, :].rearrange("p (c t) -> p c t", c=8)),
         reads=[pt.k], writes=[xT.k])


def ffn_phase(C, S, x_dram, x_trks, out_dram, out_trks, w_up_d, w_down_d, g_d, b_d, is_output):
    nc, P = C.nc, C.P
    with contextlib.ExitStack() as es:
        wup = C.sb("wup", [128, 8, DFF], BF16, es)
        wdn = C.sb("wdn", [128, 32, D], BF16, es)
        g_rep = C.sb("g_rep", [128, D], F32, es)
        b_rep = C.sb("b_rep", [128, D], F32, es)
        xld = [C.sb("xld", [128, D], F32, es) for _ in range(3)]
        xT = C.sb("xT", [128, 8, ST], BF16, es)
        hT = C.sb("hT", [128, 32, ST], BF16, es)
        rl = [C.sb("rl", [128, ST], BF16, es) for _ in range(2)]
        y = [C.sb("y", [128, D], F32, es) for _ in range(2)]

        P.dma("sp", g_rep[:], g_d.partition_broadcast(128), writes=[g_rep.k])
        P.dma("sp", b_rep[:], b_d.partition_broadcast(128), writes=[b_rep.k])
        x_view = x_dram.rearrange("(s a p) d -> s a p d", a=4, p=128)
        o_view = out_dram.rearrange("(s a p) d -> s a p d", a=4, p=128)
        up_tr = load_weight_cast(C, wup, w_up_d.rearrange("(c p) f -> p c f", p=128), 8, 2)
        dn_tr = load_weight_cast(C, wdn, w_down_d.rearrange("(c p) f -> p c f", p=128), 8, 1)

        psb = S["psf"]
        nb = 0
        nl = 0
        for s in range(NST):
            for a in range(4):
                xi = xld[nl % 3]
                nl += 1
                P.dma("sp", xi[:], x_view[s, a], reads=[x_trks[s]], writes=[xi.k])
                transpose_tokens(C, S, xi, xT, a * 128)
            for fc in range(32):
                pb = psb[nb % 4]
                nb += 1
                P.mm(pb[:], [(wup[:, kc, fc * 128:(fc + 1) * 128], xT[:, kc, :]) for kc in range(8)],
                     reads=[xT.k, up_tr[fc // 4]], writes=[pb.k])
                r = rl[fc % 2]
                P.op("act", lambda e, r=r, pb=pb: e.activation(out=r[:], in_=pb[:], func=AF.Relu),
                     reads=[pb.k], writes=[r.k])
                P.op("dve", lambda e, r=r, fc=fc: e.tensor_tensor(out=hT[:, fc, :], in0=r[:], in1=r[:],
                                                                   op=ALU.mult),
                     reads=[r.k], writes=[hT.k])
            for a in range(4):
                xi = xld[nl % 3]
                nl += 1
                P.dma("sp", xi[:], x_view[s, a], reads=[x_trks[s]], writes=[xi.k])
                yy = y[a % 2]
                for dh in range(2):
                    pb = psb[nb % 4]
                    nb += 1
                    P.mm(pb[:], [(hT[:, fc, a * 128:(a + 1) * 128], wdn[:, fc, dh * 512:(dh + 1) * 512])
                                 for fc in range(32)],
                         reads=[hT.k] + dn_tr, writes=[pb.k])
                    P.op("dve", lambda e, yy=yy, pb=pb, dh=dh, xi=xi: e.scalar_tensor_tensor(
                        out=yy[:, dh * 512:(dh + 1) * 512], in0=xi[:, dh * 512:(dh + 1) * 512],
                        scalar=ALPHA, in1=pb[:], op0=ALU.mult, op1=ALU.add),
                         reads=[xi.k, pb.k], writes=[yy.k])
                layer_norm_tile(C, S, yy, g_rep, b_rep, yy)
                P.dma("pool", o_view[s, a], yy[:], reads=[yy.k], writes=[out_trks[s]],
                      is_output=is_output, sem_trk=yy.k)
        P.full_barrier()


def alloc_shared(C):
    S = {}
    S["ident"] = C.sb("ident", [128, 128], BF16)
    S["psf"] = [C.ps("psf", [128, 512], F32) for _ in range(6)]
    S["pst"] = [C.ps("pst", [128, 1024], BF16) for _ in range(2)]
    S["pst_i"] = 0
    S["nb"] = 0
    S["nrot"] = 6
    S["rot0"] = 0
    S["xb"] = [C.sb("xb", [128, D], BF16) for _ in range(2)]
    S["xb_i"] = 0
    S["ln_st"] = C.sb("ln_st", [128, 2, 6], F32)
    S["ln_mv"] = C.sb("ln_mv", [128, 2], F32)
    S["ln_rstd"] = C.sb("ln_rstd", [128, 1], F32)
    S["ln_nmr"] = C.sb("ln_nmr", [128, 1], F32)
    S["eps_ln"] = C.sb("eps_ln", [128, 1], F32)
    S["eps_rms"] = C.sb("eps_rms", [128, 1], F32)
    C.P.op("pool", lambda e: e.memset(S["eps_ln"][:], LN_EPS), writes=[S["eps_ln"].k])
    C.P.op("pool", lambda e: e.memset(S["eps_rms"][:], RMS_EPS), writes=[S["eps_rms"].k])
    return S


def nextbank(S):
    b = S["psf"][S["rot0"] + S["nb"] % S["nrot"]]
    S["nb"] += 1
    return b


def v3(ap, n):
    return ap.rearrange("p (h d) -> p h d", h=n)


def load_consts(C, S, consts_d):
    P = C.P
    S["cf"] = C.sb("cf", [128, 4, 128], F32)
    S["maskb"] = C.sb("maskb", [128, 128], BF16)
    P.dma("sp", S["cf"][:], consts_d, writes=[S["cf"].k])
    P.dma("pool", S["ident"][:], consts_d[:, 0, :], writes=[S["ident"].k])
    P.dma("pool", S["maskb"][:], consts_d[:, 1, :], writes=[S["maskb"].k])
    S["one1"] = C.sb("one1", [128, 1], F32)
    S["ln8"] = C.sb("ln8", [128, 1], F32)
    P.op("pool", lambda e: e.memset(S["one1"][:], 1.0), writes=[S["one1"].k])
    P.op("pool", lambda e: e.memset(S["ln8"][:], float(np.log(0.125))), writes=[S["ln8"].k])


def mem_kv_precompute(C, S, es, mem_d, wmk, wmk_tr, xld, KmT, Vm):
    P = C.P
    memT = C.sb("memT", [128, 8, 256], BF16, es)
    P.op("pool", lambda e: e.memset(Vm[:], 1.0), writes=[Vm.k])
    n = 0
    for q in range(NSEQ):
        for mt in range(2):
            xi = xld[n % len(xld)]
            n += 1
            P.dma("sp", xi[:], mem_d[q, mt * 128:(mt + 1) * 128, :], writes=[xi.k])
            transpose_tokens(C, S, xi, memT, mt * 128)
        for h in range(4):
            pb = nextbank(S)
            P.mm(pb[:, 0:256], [(wmk[:, kc, h * 128:(h + 1) * 128], memT[:, kc, :]) for kc in range(8)],
                 reads=[memT.k] + wmk_tr, writes=[pb.k])
            P.op("act", lambda e, pb=pb, q=q, h=h: e.activation(out=KmT[:, q, h, :], in_=pb[:, 0:256], func=AF.Copy),
                 reads=[pb.k], writes=[KmT.k])
        for mt in range(2):
            pb = nextbank(S)
            P.mm(pb[:], [(memT[:, kc, mt * 128:(mt + 1) * 128], wmk[:, kc, 512:1024]) for kc in range(8)],
                 reads=[memT.k] + wmk_tr, writes=[pb.k])
            P.op("act", lambda e, pb=pb, q=q, mt=mt: e.activation(out=Vm[:, q, mt, :, 0:128], in_=v3(pb[:], 4),
                                                                  func=AF.Copy),
                 reads=[pb.k], writes=[Vm.k])
    return memT


def mem_attention(C, S, seq, qmT, KmT, Vm, hcat, col0):
    P = C.P
    for h in range(4):
        pts = []
        for mt in range(2):
            pb = nextbank(S)
            P.mm(pb[:], [(KmT[:, seq, h, mt * 128:(mt + 1) * 128], qmT[:, h, :])],
                 reads=[KmT.k, qmT.k], writes=[pb.k])
            pt = S["PT"][S["pt_i"] % len(S["PT"])]
            S["pt_i"] += 1
            P.op("act", lambda e, pt=pt, pb=pb: e.activation(out=pt[:], in_=pb[:], func=AF.Exp, scale=128.0 ** -0.5),
                 reads=[pb.k], writes=[pt.k])
            pts.append(pt)
        for a in range(4):
            pb = nextbank(S)
            P.mm(pb[:, 0:129], [(pts[mt][:, a * 128:(a + 1) * 128], Vm[:, seq, mt, h, :]) for mt in range(2)],
                 reads=[pts[0].k, pts[1].k, Vm.k], writes=[pb.k])
            rc = S["rc"][S["rc_i"] % 2]
            S["rc_i"] += 1
            P.op("dve", lambda e, rc=rc, pb=pb: e.reciprocal(out=rc[:], in_=pb[:, 128:129]),
                 reads=[pb.k], writes=[rc.k])
            P.op("dve", lambda e, rc=rc, pb=pb, a=a, h=h: e.tensor_scalar(
                out=hcat[a][:, col0 + h * 128:col0 + (h + 1) * 128], in0=pb[:, 0:128], scalar1=rc[:, 0:1],
                scalar2=None, op0=ALU.mult), reads=[pb.k, rc.k], writes=[hcat[a].k])


def out_proj_ln(C, S, hcat, hcT, wout, wout_tr, xld, nl, x_view, x_trk, s, y, g_rep, b_rep, o_view, o_trk):
    P = C.P
    for a in range(4):
        pt = S["pst"][S["pst_i"] % 2]
        S["pst_i"] += 1
        ident = S["ident"]
        P.pe_multi([lambda e, c=c, pt=pt, a=a: e.transpose(out=pt[:, c * 128:(c + 1) * 128],
                                                         in_=hcat[a][:, c * 128:(c + 1) * 128],
                                                         identity=ident[:]) for c in range(8)],
                   reads=[hcat[a].k, ident.k], writes=[pt.k])
        P.op("dve", lambda e, pt=pt, a=a: e.tensor_copy(out=hcT[:, :, a * 128:(a + 1) * 128], in_=v3(pt[:, :], 8)),
             reads=[pt.k], writes=[hcT.k])
    for a in range(4):
        xi = xld[nl[0] % len(xld)]
        nl[0] += 1
        P.dma("sp", xi[:], x_view[s, a], reads=[x_trk], writes=[xi.k])
        yy = y[a % 2]
        for dh in range(2):
            pb = nextbank(S)
            P.mm(pb[:], [(hcT[:, kc, a * 128:(a + 1) * 128], wout[:, kc, dh * 512:(dh + 1) * 512])
                         for kc in range(8)], reads=[hcT.k] + wout_tr, writes=[pb.k])
            P.op("dve", lambda e, yy=yy, pb=pb, dh=dh, xi=xi: e.scalar_tensor_tensor(
                out=yy[:, dh * 512:(dh + 1) * 512], in0=xi[:, dh * 512:(dh + 1) * 512],
                scalar=ALPHA, in1=pb[:], op0=ALU.mult, op1=ALU.add),
                 reads=[xi.k, pb.k], writes=[yy.k])
        layer_norm_tile(C, S, yy, g_rep, b_rep, yy)
        P.dma("pool", o_view[s, a], yy[:], reads=[yy.k], writes=[o_trk], sem_trk=yy.k)


def mixer_a_phase(C, S, x_dram, x_trks, out_dram, out_trks, mem_d, w_in_d, bi_d, bf_d, wmk_d, w_out_d, g_d, b_d):
    nc, P = C.nc, C.P
    with contextlib.ExitStack() as es:
        win = C.sb("win", [128, 8, 2056], BF16, es)
        wout = C.sb("wout", [128, 8, D], BF16, es)
        g_rep = C.sb("g_rep", [128, D], F32, es)
        b_rep = C.sb("b_rep", [128, D], F32, es)
        bias_rep = C.sb("bias_rep", [128, 8], F32, es)
        xld = [C.sb("xld", [128, D], F32, es) for _ in range(3)]
        KmT = C.sb("KmT", [128, NSEQ, 4, 256], BF16, es)
        Vm = C.sb("Vm", [128, NSEQ, 2, 4, 129], BF16, es)
        P.dma("sp", g_rep[:], g_d.partition_broadcast(128), writes=[g_rep.k])
        P.dma("sp", b_rep[:], b_d.partition_broadcast(128), writes=[b_rep.k])
        P.dma("sp", bias_rep[:, 0:4], bi_d.partition_broadcast(128), writes=[bias_rep.k])
        P.dma("sp", bias_rep[:, 4:8], bf_d.partition_broadcast(128), writes=[bias_rep.k])
        with contextlib.ExitStack() as es2:
            wmk = C.sb("wmk", [128, 8, D], BF16, es2)
            wmk_tr = load_weight_cast(C, wmk, wmk_d.rearrange("(c p) f -> p c f", p=128), 2, 1)
            win_view = w_in_d.rearrange("(c p) f -> p c f", p=128)
            win_g = []
            for (c0, c1) in ((0, 512), (1536, 2056), (512, 1024), (1024, 1536)):
                k = Trk("win_%d" % c0)
                P.dma("pool", win[:, :, c0:c1], win_view[:, :, c0:c1], writes=[k])
                win_g.append(k)
            wqk_tr, wgm_tr, wv_tr, wo_tr = [win_g[0]], [win_g[1]], [win_g[2]], [win_g[3]]
            wout_tr = []
            mem_kv_precompute(C, S, es2, mem_d, wmk, wmk_tr, xld, KmT, Vm)
            P.full_barrier()
        xT = [C.sb("xT", [128, 8, ST], BF16, es) for _ in range(2)]
        qT = [C.sb("qT", [64, 4, ST], BF16, es) for _ in range(2)]
        kT = [C.sb("kT", [64, 4, ST], BF16, es) for _ in range(2)]
        qz = [C.sb("qz", [64, 4, 4, 2, 128], BF16, es) for _ in range(2)]
        qmT = [C.sb("qmT", [128, 4, ST], BF16, es) for _ in range(2)]
        gts = C.sb("gts", [128, 8], F32, es)
        lfn = C.sb("lfn", [128, 4], F32, es)
        tadd = C.sb("tadd", [128, 4], F32, es)
        colf = C.sb("colf", [128, 4], F32, es)
        enb = C.sb("enb", [128, 4], F32, es)
        eg = [C.sb("eg", [128, 2, 4], F32, es) for _ in range(2)]
        kc_t = C.sb("kc", [128, 4, 64], BF16, es)
        vaug = [C.sb("vaug", [128, 4, 129], BF16, es) for _ in range(2)]
        e_o = C.sb("e_o", [128, 512], F32, es)
        atmp = C.sb("atmp", [128, 4, 128], BF16, es)
        AT = C.sb("AT", [128, 4, 128], BF16, es)
        Sst = C.sb("Sst", [64, 4, 129], F32, es)
        Cf = C.sb("Cf", [64, 4, 129], F32, es)
        Cb = [C.sb("Cb", [64, 4, 129], BF16, es) for _ in range(2)]
        den = C.sb("den", [128, 2, 1], F32, es)
        hcat2 = [[C.sb("hcat", [128, D], BF16, es) for _ in range(4)] for _ in range(2)]
        hcT = C.sb("hcT", [128, 8, ST], BF16, es)
        y = [C.sb("y", [128, D], F32, es) for _ in range(2)]
        S["PT"] = [C.sb("PT", [128, ST], BF16, es) for _ in range(4)]
        S["pt_i"] = 0
        S["rc"] = [C.sb("rc", [128, 1], F32, es) for _ in range(2)]
        S["rc_i"] = 0
        for qq in qz:
            P.op("pool", lambda e, qq=qq: e.memset(qq[:], 0.0), writes=[qq.k])
        for vv in vaug:
            P.op("pool", lambda e, vv=vv: e.memset(vv[:], 1.0), writes=[vv.k])

        x_view = x_dram.rearrange("(s a p) d -> s a p d", a=4, p=128)
        o_view = out_dram.rearrange("(s a p) d -> s a p d", a=4, p=128)
        cf = S["cf"]
        maskb = S["maskb"]
        nl = [0]
        NT = NST // NSEQ

        def front(s):
            b, seq = s % 2, s // NT
            xTb, qTb, kTb, qzb, qmTb = xT[b], qT[b], kT[b], qz[b], qmT[b]
            for a in range(4):
                xi = xld[nl[0] % len(xld)]
                nl[0] += 1
                P.dma("sp", xi[:], x_view[s, a], reads=[x_trks[s]], writes=[xi.k])
                transpose_tokens(C, S, xi, xTb, a * 128)
            for h in range(4):
                for (dst, c0) in ((qTb, 0), (kTb, 256)):
                    pb = nextbank(S)
                    P.mm(pb[0:64, :], [(win[:, kc, c0 + h * 64:c0 + (h + 1) * 64], xTb[:, kc, :]) for kc in range(8)],
                         reads=[xTb.k] + wqk_tr, writes=[pb.k])
                    P.op("act", lambda e, pb=pb, dst=dst, h=h: e.activation(out=dst[:, h, :], in_=pb[0:64, :],
                                                                            func=AF.Copy),
                         reads=[pb.k], writes=[dst.k])
                P.op("pool", lambda e, h=h: e.tensor_copy(
                    out=bass.AP(qzb.t, h * 1024, [[4096, 64], [256, 4], [192, 2], [1, 64]]),
                    in_=qTb[:, h, :].rearrange("p (a c j) -> p a c j", a=4, c=2)),
                     reads=[qTb.k], writes=[qzb.k])
                pb = nextbank(S)
                P.mm(pb[:], [(win[:, kc, 1544 + h * 128:1544 + (h + 1) * 128], xTb[:, kc, :]) for kc in range(8)],
                     reads=[xTb.k] + wgm_tr, writes=[pb.k])
                P.op("act", lambda e, pb=pb, h=h: e.activation(out=qmTb[:, h, :], in_=pb[:], func=AF.Copy),
                     reads=[pb.k], writes=[qmTb.k])
            mem_attention(C, S, seq, qmTb, KmT, Vm, hcat2[b], 512)

        def tail(s):
            out_proj_ln(C, S, hcat2[s % 2], hcT, wout, wout_tr, xld, nl, x_view, x_trks[s], s, y, g_rep, b_rep,
                        o_view, out_trks[s])

        def tok_loop(s):
            b = s % 2
            xTb, qTb, kTb, qzb, hcat = xT[b], qT[b], kT[b], qz[b], hcat2[b]
            for a in range(4):
                t = s * 4 + a
                par = t % 2
                first = (t % (SEQ // 128) == 0)
                cols = slice(a * 128, (a + 1) * 128)
                va = vaug[par]
                pg = nextbank(S)
                P.mm(pg[:, 0:8], [(xTb[:, kc, cols], win[:, kc, 1536:1544]) for kc in range(8)],
                     reads=[xTb.k] + wgm_tr, writes=[pg.k])
                P.op("dve", lambda e, pg=pg: e.tensor_tensor(out=gts[:], in0=pg[:, 0:8], in1=bias_rep[:], op=ALU.add),
                     reads=[pg.k, bias_rep.k], writes=[gts.k])
                P.op("act", lambda e: e.activation(out=lfn[:], in_=gts[:, 4:8], func=AF.Exp, scale=-1.0),
                     reads=[gts.k], writes=[lfn.k])
                P.op("act", lambda e: e.activation(out=lfn[:], in_=lfn[:], func=AF.Ln, bias=S["one1"][:], scale=1.0),
                     reads=[lfn.k, S["one1"].k], writes=[lfn.k])
                pc = nextbank(S)
                P.mm(pc[:, 0:4], [(cf[:, 1, :], lfn[:])], reads=[cf.k, lfn.k], writes=[pc.k])
                P.mm(pc[:, 4:8], [(cf[:, 2, :], lfn[:])], reads=[cf.k, lfn.k], writes=[pc.k])
                P.mm(pc[:, 8:12], [(cf[:, 3, :], lfn[:])], reads=[cf.k, lfn.k], writes=[pc.k])
                P.op("dve", lambda e, pc=pc: e.tensor_tensor(out=tadd[:], in0=gts[:, 0:4], in1=pc[:, 0:4], op=ALU.add),
                     reads=[gts.k, pc.k], writes=[tadd.k])
                P.op("act", lambda e: e.activation(out=colf[:], in_=tadd[:], func=AF.Exp, bias=S["ln8"][:], scale=1.0),
                     reads=[tadd.k, S["ln8"].k], writes=[colf.k])
                P.op("act", lambda e, pc=pc: e.activation(out=enb[:], in_=pc[:, 0:4], func=AF.Exp),
                     reads=[pc.k], writes=[enb.k])
                P.op("act", lambda e, pc=pc, par=par: e.activation(out=eg[par][:], in_=v3(pc[:, 4:12], 2),
                                                                   func=AF.Exp, scale=-1.0),
                     reads=[pc.k], writes=[eg[par].k])
                pk = nextbank(S)
                P.mm(pk[:, 0:256], [(xTb[:, kc, cols], win[:, kc, 256:512]) for kc in range(8)],
                     reads=[xTb.k] + wqk_tr, writes=[pk.k])
                P.op("dve", lambda e, pk=pk: e.tensor_tensor(
                    out=kc_t[:], in0=v3(pk[:, 0:256], 4), in1=colf[:, 0:4].unsqueeze(2).broadcast_to([128, 4, 64]),
                    op=ALU.mult), reads=[pk.k, colf.k], writes=[kc_t.k])
                pv = nextbank(S)
                P.mm(pv[:], [(xTb[:, kc, cols], win[:, kc, 512:1024]) for kc in range(8)],
                     reads=[xTb.k] + wv_tr, writes=[pv.k])
                P.op("act", lambda e, pv=pv, va=va: e.activation(out=va[:, :, 0:128], in_=v3(pv[:], 4), func=AF.Copy),
                     reads=[pv.k], writes=[va.k])
                po = nextbank(S)
                P.mm(po[:], [(xTb[:, kc, cols], win[:, kc, 1024:1536]) for kc in range(8)],
                     reads=[xTb.k] + wo_tr, writes=[po.k])
                P.op("act", lambda e, po=po: e.activation(out=e_o[:], in_=po[:], func=AF.Exp, scale=-1.0),
                     reads=[po.k], writes=[e_o.k])
                P.op("act", lambda e: e.activation(out=e_o[:], in_=e_o[:], func=AF.Ln, bias=S["one1"][:], scale=1.0),
                     reads=[e_o.k, S["one1"].k], writes=[e_o.k])
                P.op("act", lambda e: e.activation(out=e_o[:], in_=e_o[:], func=AF.Exp, scale=-1.0),
                     reads=[e_o.k], writes=[e_o.k])
                pa = nextbank(S)
                for h in range(4):
                    P.mm(pa[:, h * 128:(h + 1) * 128], [(kTb[:, h, cols], qTb[:, h, cols])],
                         reads=[kTb.k, qTb.k], writes=[pa.k])
                P.op("dve", lambda e, pa=pa: e.tensor_tensor(
                    out=atmp[:], in0=v3(pa[:], 4), in1=colf[:, 0:4].unsqueeze(2).broadcast_to([128, 4, 128]),
                    op=ALU.mult), reads=[pa.k, colf.k], writes=[atmp.k])
                P.op("pool", lambda e: e.tensor_tensor(
                    out=AT[:], in0=atmp[:], in1=maskb[:, :].unsqueeze(1).broadcast_to([128, 4, 128]), op=ALU.mult),
                     reads=[atmp.k, maskb.k], writes=[AT.k])
                for c in range(2):
                    if first and c == 0:
                        P.op("dve", lambda e: e.memset(Cf[:], 0.0), writes=[Cf.k])
                        P.op("pool", lambda e: e.memset(Cb[0][:], 0.0), writes=[Cb[0].k])
                    else:
                        egp = eg[par][0:64, 0, :] if c == 1 else eg[1 - par][0:64, 1, :]
                        egk = eg[par].k if c == 1 else eg[1 - par].k
                        P.op("dve", lambda e, egp=egp: e.tensor_tensor(
                            out=Cf[:], in0=Sst[:], in1=egp.unsqueeze(2).broadcast_to([64, 4, 129]), op=ALU.mult),
                             reads=[Sst.k, egk], writes=[Cf.k])
                        P.op("act", lambda e, c=c: e.activation(out=Cb[c][:], in_=Cf[:], func=AF.Copy),
                             reads=[Cf.k], writes=[Cb[c].k])
                    rows = slice(c * 64, (c + 1) * 64)
                    for hp in range(2):
                        pu = nextbank(S)
                        for j in range(2):
                            h = 2 * hp + j
                            P.mm(pu[0:64, j * 129:(j + 1) * 129], [(kc_t[rows, h, :], va[rows, h, :])],
                                 reads=[kc_t.k, va.k], writes=[pu.k])
                        P.op("dve", lambda e, pu=pu, hp=hp: e.tensor_tensor(
                            out=Sst[:, 2 * hp:2 * hp + 2, :], in0=Cf[:, 2 * hp:2 * hp + 2, :],
                            in1=v3(pu[0:64, 0:258], 2), op=ALU.add),
                             reads=[Cf.k, pu.k], writes=[Sst.k])
                for hp in range(2):
                    pn = nextbank(S)
                    for j in range(2):
                        h = 2 * hp + j
                        P.mm(pn[:, j * 129:(j + 1) * 129],
                             [(AT[:, h, :], va[:, h, :]),
                              (qzb[:, h, a, 0, :], Cb[0][:, h, :]),
                              (qzb[:, h, a, 1, :], Cb[1][:, h, :])],
                             reads=[AT.k, va.k, qzb.k, Cb[0].k, Cb[1].k], writes=[pn.k])
                    pn3 = v3(pn[:, 0:258], 2)
                    P.op("dve", lambda e, pn3=pn3, hp=hp: e.tensor_tensor(
                        out=den[:], in0=pn3[:, :, 128:129], in1=enb[:, 2 * hp:2 * hp + 2].unsqueeze(2),
                        op=ALU.max), reads=[pn.k, enb.k], writes=[den.k])
                    P.op("dve", lambda e, pn3=pn3: e.scalar_tensor_tensor(
                        out=den[:], in0=pn3[:, :, 128:129], scalar=-1.0, in1=den[:], op0=ALU.mult, op1=ALU.max),
                         reads=[pn.k, den.k], writes=[den.k])
                    P.op("dve", lambda e: e.reciprocal(out=den[:], in_=den[:]), reads=[den.k], writes=[den.k])
                    for j in range(2):
                        h = 2 * hp + j
                        P.op("dve", lambda e, pn=pn, j=j, h=h, a=a: e.scalar_tensor_tensor(
                            out=hcat[a][:, h * 128:(h + 1) * 128], in0=pn[:, j * 129:j * 129 + 128],
                            scalar=den[:, j, :], in1=e_o[:, h * 128:(h + 1) * 128], op0=ALU.mult, op1=ALU.mult),
                             reads=[pn.k, den.k, e_o.k], writes=[hcat[a].k])

        def side(fn, *a):
            S["rot0"], S["nrot"] = 4, 2
            fn(*a)
            S["rot0"], S["nrot"] = 0, 4

        side(front, 0)
        wout_tr.extend(load_weight_cast(C, wout, w_out_d.rearrange("(c p) f -> p c f", p=128), 2, 1))
        for s in range(NST):
            pending = []
            P.rec = pending
            if s > 0:
                side(tail, s - 1)
            if s + 1 < NST:
                side(front, s + 1)
            P.rec = None
            S["rot0"], S["nrot"] = 0, 4
            P.inter = (pending, len(pending) / 260.0 + 0.02)
            P._acc = 0.0
            tok_loop(s)
            P.inter = None
            while pending:
                P.replay(pending.pop(0))
        side(tail, NST - 1)
        P.full_barrier()
        S["rot0"], S["nrot"] = 0, 6


def rms_rows(C, S, pb, ncols, out_bf, scr):
    P = C.P
    ss, rs = S["rms_ss"], S["rms_rs"]
    if scr is None:
        scr = nextbank(S)
    P.op("dve", lambda e: e.memset(ss[:], 0.0), writes=[ss.k])
    P.op("act", lambda e: e.activation(out=scr[:, 0:ncols], in_=pb[:, 0:ncols], func=AF.Square, accum_out=ss[:]),
         reads=[pb.k], writes=[scr.k, ss.k])
    P.op("act", lambda e: e.activation(out=rs[:], in_=ss[:], func=AF.Ln, bias=S["eps_rms"][:], scale=1.0 / ncols),
         reads=[ss.k, S["eps_rms"].k], writes=[rs.k])
    P.op("act", lambda e: e.activation(out=rs[:], in_=rs[:], func=AF.Exp, scale=-0.5), reads=[rs.k], writes=[rs.k])
    P.op("dve", lambda e: e.tensor_scalar(out=out_bf[:, 0:ncols], in0=pb[:, 0:ncols], scalar1=rs[:, 0:1],
                                          scalar2=None, op0=ALU.mult), reads=[pb.k, rs.k], writes=[out_bf.k])


def transpose_cols(C, S, src_bf, nchunk, dstT, col0):
    P = C.P
    pt = S["pst"][S["pst_i"] % 2]
    S["pst_i"] += 1
    ident = S["ident"]
    P.pe_multi([lambda e, c=c: e.transpose(out=pt[:, c * 128:(c + 1) * 128], in_=src_bf[:, c * 128:(c + 1) * 128],
                                           identity=ident[:]) for c in range(nchunk)],
               reads=[src_bf.k, ident.k], writes=[pt.k])
    P.op("dve", lambda e: e.tensor_copy(out=dstT[:, 0:nchunk, col0:col0 + 128], in_=v3(pt[:, 0:nchunk * 128], nchunk)),
         reads=[pt.k], writes=[dstT.k])


def mixer_b_phase(C, S, x_dram, x_trks, out_dram, out_trks, mem_d, pos_d, wdown_d, gkv_d, wuk_d, wuv_d,
                  bwin_d, gq_d, wuq_d, wmk_d, w_out_d, g_d, b_d, cf2_d):
    nc, P = C.nc, C.P
    SC = 96.0 ** -0.5
    NT = NST // NSEQ
    with contextlib.ExitStack() as es:
        wdown = C.sb("wdown", [128, 8, 288], BF16, es)
        wdr = C.sb("wdr", [128, 8, 96], BF16, es)
        wdrot = C.sb("wdrot", [128, 8, 96], BF16, es)
        wuk = C.sb("wuk", [128, 2, 512], BF16, es)
        wuv = C.sb("wuv", [128, 2, 512], BF16, es)
        wuq = C.sb("wuq", [128, 2, 768], BF16, es)
        wuqrot = C.sb("wuqrot", [128, 2, 8, 96], BF16, es)
        gk = C.sb("gk", [128, 2], F32, es)
        gq = C.sb("gq", [128, 2], F32, es)
        bwin = C.sb("bwin", [128, 8, 768], BF16, es)
        wout = C.sb("wout", [128, 8, D], BF16, es)
        g_rep = C.sb("g_rep", [128, D], F32, es)
        b_rep = C.sb("b_rep", [128, D], F32, es)
        cf2 = C.sb("cf2", [128, 4], F32, es)
        xld = [C.sb("xld", [128, D], F32, es) for _ in range(2)]
        KmT = C.sb("KmT", [128, NSEQ, 4, 256], BF16, es)
        Vm = C.sb("Vm", [128, NSEQ, 2, 4, 129], BF16, es)
        kT = C.sb("kT", [96, 8, SEQ], BF16, es)
        Vaug = C.sb("Vaug", [128, 16, 8, 65], BF16, es)
        kT_blk = [Trk("kTb%d" % i) for i in range(NT)]
        V_blk = [Trk("Vb%d" % i) for i in range(NT)]
        P.dma("sp", g_rep[:], g_d.partition_broadcast(128), writes=[g_rep.k])
        P.dma("sp", b_rep[:], b_d.partition_broadcast(128), writes=[b_rep.k])
        P.dma("sp", cf2[:], cf2_d, writes=[cf2.k])
        for kc in range(2):
            P.dma("sp", gk[:, kc:kc + 1], gkv_d[kc * 128:(kc + 1) * 128].rearrange("(p o) -> p o", o=1), writes=[gk.k])
            P.dma("sp", gq[:, kc:kc + 1], gq_d[kc * 128:(kc + 1) * 128].rearrange("(p o) -> p o", o=1), writes=[gq.k])
        with contextlib.ExitStack() as es2:
            wmk = C.sb("wmk", [128, 8, D], BF16, es2)
            wst = C.sb("wst", [128, 2, 768], F32, es2)
            wmk_tr = load_weight_cast(C, wmk, wmk_d.rearrange("(c p) f -> p c f", p=128), 2, 1)
            wdown_tr = load_weight_cast(C, wdown, wdown_d.rearrange("(c p) f -> p c f", p=128), 1, 1)
            bwin_tr = load_weight_cast(C, bwin, bwin_d.rearrange("(c p) f -> p c f", p=128), 2, 1)
            wout_tr = []
            for (dst, src_d, gg, ncol) in ((wuk, wuk_d, gk, 512), (wuv, wuv_d, gk, 512), (wuq, wuq_d, gq, 768)):
                P.dma("sp", wst[:, :, 0:ncol], src_d.rearrange("(c p) f -> p c f", p=128), writes=[wst.k])
                for kc in range(2):
                    P.op("dve", lambda e, dst=dst, gg=gg, kc=kc, ncol=ncol: e.tensor_scalar(
                        out=dst[:, kc, :], in0=wst[:, kc, 0:ncol], scalar1=gg[:, kc:kc + 1], scalar2=None,
                        op0=ALU.mult), reads=[wst.k, gg.k], writes=[dst.k])
            P.op("pool", lambda e: e.memset(wdr[:], 0.0), writes=[wdr.k])
            P.op("pool", lambda e: e.memset(wdrot[:], 0.0), writes=[wdrot.k])
            P.op("pool", lambda e: e.memset(wuqrot[:], 0.0), writes=[wuqrot.k])
            P.op("pool", lambda e: e.memset(Vaug[:], 1.0), writes=V_blk)
            mem_kv_precompute(C, S, es2, mem_d, wmk, wmk_tr, xld, KmT, Vm)
            P.op("pool", lambda e: e.tensor_copy(out=wdr[:, :, 64:96], in_=wdown[:, :, 256:288]),
                 reads=wdown_tr, writes=[wdr.k])
            P.op("pool", lambda e: e.tensor_scalar(out=wdrot[:, :, 64:80], in0=wdown[:, :, 272:288], scalar1=-1.0,
                                                   scalar2=None, op0=ALU.mult), reads=wdown_tr, writes=[wdrot.k])
            P.op("pool", lambda e: e.tensor_copy(out=wdrot[:, :, 80:96], in_=wdown[:, :, 256:272]),
                 reads=wdown_tr, writes=[wdrot.k])
            wuq4 = wuq.t[:, :, :].rearrange("p c (h d) -> p c h d", h=8)
            P.op("pool", lambda e: e.tensor_scalar(out=wuqrot[:, :, :, 64:80], in0=wuq4[:, :, :, 80:96], scalar1=-1.0,
                                                   scalar2=None, op0=ALU.mult), reads=[wuq.k], writes=[wuqrot.k])
            P.op("pool", lambda e: e.tensor_copy(out=wuqrot[:, :, :, 80:96], in_=wuq4[:, :, :, 64:80]),
                 reads=[wuq.k], writes=[wuqrot.k])
            P.full_barrier()
        xT = [C.sb("xT", [128, 8, ST], BF16, es) for _ in range(2)]
        uu = C.sb("uu", [96, ST], F32, es)
        u2 = C.sb("u2", [96, ST], F32, es)
        cosT = C.sb("cosT", [96, ST], F32, es)
        sinT = C.sb("sinT", [96, ST], F32, es)
        ckn = C.sb("ckn", [128, 256], BF16, es)
        ckT = [C.sb("ckT", [128, 2, ST], BF16, es) for _ in range(2)]
        cqT = [C.sb("cqT", [128, 2, ST], BF16, es) for _ in range(2)]
        rt1 = C.sb("rt1", [96, ST], F32, es)
        rt2 = C.sb("rt2", [96, ST], F32, es)
        kr = C.sb("kr", [96, ST], BF16, es)
        qTh = [C.sb("qTh", [96, 8, ST], BF16, es) for _ in range(2)]
        qmT = [C.sb("qmT", [128, 4, ST], BF16, es) for _ in range(2)]
        hc_all = C.sb("hc_all", [128, 4, D], BF16, es)
        y = [C.sb("y", [128, D], F32, es) for _ in range(2)]
        scr = None
        rc4 = C.sb("rc4", [128, 4, 1], F32, es)
        S["PT"] = [C.sb("PT", [128, ST], BF16, es) for _ in range(3)]
        S["pt_i"] = 0
        S["rc"] = [C.sb("rc", [128, 1], F32, es) for _ in range(2)]
        S["rc_i"] = 0
        S["rms_ss"] = C.sb("rms_ss", [128, 1], F32, es)
        S["rms_rs"] = C.sb("rms_rs", [128, 1], F32, es)
        hcat = []
        for a in range(4):
            v = Tile(hc_all.t[:, a, :], "hcv")
            v.k = hc_all.k
            hcat.append(v)

        S["rot0"], S["nrot"] = 4, 2
        S["cast_eng"] = "pool"
        sbank = S["psf"][0:2]
        sb_i = [0]

        x_view = x_dram.rearrange("(s a p) d -> s a p d", a=4, p=128)
        o_view = out_dram.rearrange("(s a p) d -> s a p d", a=4, p=128)
        nl = [0]
        R = slice(64, 96)

        def front_chunks(s):
            seq, T, b = s // NT, s % NT, s % 2
            tcols = slice(T * ST, (T + 1) * ST)
            xTb, ckTb, cqTb, qThb, qmTb = xT[b], ckT[b], cqT[b], qTh[b], qmT[b]
            ch = []

            def rope_tables():
                posi_ap = rt2[R, :].bitcast(I32)
                P.dma("sp", posi_ap, pos_d[seq, T * ST:(T + 1) * ST].partition_broadcast(32), writes=[rt2.k])
                P.op("dve", lambda e: e.tensor_copy(out=uu[R, :], in_=posi_ap), reads=[rt2.k], writes=[uu.k])
                for (dstT, shift) in ((sinT, 0.0), (cosT, 0.25)):
                    P.op("dve", lambda e, shift=shift: e.tensor_scalar(
                        out=u2[R, :], in0=uu[R, :], scalar1=cf2[R, 0:1], scalar2=shift, op0=ALU.mult, op1=ALU.add),
                         reads=[uu.k, cf2.k], writes=[u2.k])
                    P.op("dve", lambda e: e.tensor_copy(out=posi_ap, in_=u2[R, :]), reads=[u2.k], writes=[rt2.k])
                    P.op("dve", lambda e: e.tensor_copy(out=rt1[R, :], in_=posi_ap), reads=[rt2.k], writes=[rt1.k])
                    P.op("dve", lambda e: e.tensor_tensor(out=u2[R, :], in0=u2[R, :], in1=rt1[R, :], op=ALU.subtract),
                         reads=[u2.k, rt1.k], writes=[u2.k])
                    P.op("dve", lambda e: e.tensor_scalar(out=rt1[R, :], in0=u2[R, :], scalar1=0.5, scalar2=None,
                                                          op0=ALU.is_gt), reads=[u2.k], writes=[rt1.k])
                    P.op("dve", lambda e: e.tensor_tensor(out=u2[R, :], in0=u2[R, :], in1=rt1[R, :], op=ALU.subtract),
                         reads=[u2.k, rt1.k], writes=[u2.k])
                    P.op("act", lambda e, dstT=dstT: e.activation(out=dstT[R, :], in_=u2[R, :], func=AF.Sin,
                                                                  scale=float(2 * np.pi)),
                         reads=[u2.k], writes=[dstT.k])
            xis = {}

            def load_dma(a):
                xi = xld[nl[0] % len(xld)]
                nl[0] += 1
                xis[a] = xi
                P.dma("sp", xi[:], x_view[s, a], reads=[x_trks[s]], writes=[xi.k])

            def load_tr(a):
                transpose_tokens(C, S, xis[a], xTb, a * 128)

            def latents(a):
                cols = slice(a * 128, (a + 1) * 128)
                for (wt, wtr, dstT) in ((wdown, wdown_tr, ckTb), (bwin, bwin_tr, cqTb)):
                    pb = nextbank(S)
                    P.mm(pb[:, 0:256], [(xTb[:, kc, cols], wt[:, kc, 0:256]) for kc in range(8)],
                         reads=[xTb.k] + wtr, writes=[pb.k])
                    rms_rows(C, S, pb, 256, ckn, scr)
                    transpose_cols(C, S, ckn, 2, dstT, a * 128)
            ch.append(lambda: load_dma(0))
            ch.append(lambda: load_dma(1))
            ch.append(rope_tables)
            ch.append(lambda: load_tr(0))
            ch.append(lambda: load_dma(2))
            ch.append(lambda: load_tr(1))
            ch.append(lambda: load_dma(3))
            ch.append(lambda: latents(0))
            ch.append(lambda: load_tr(2))
            ch.append(lambda: latents(1))
            ch.append(lambda: load_tr(3))
            ch.append(lambda: latents(2))
            ch.append(lambda: latents(3))

            def k_nope(h0):
                for h in range(h0, h0 + 4):
                    pb = nextbank(S)
                    P.mm(pb[0:64, :], [(wuk[:, kc, h * 64:(h + 1) * 64], ckTb[:, kc, :]) for kc in range(2)],
                         reads=[wuk.k, ckTb.k], writes=[pb.k])
                    P.op("dve", lambda e, pb=pb, h=h: e.tensor_copy(out=kT[0:64, h, tcols], in_=pb[0:64, :]),
                         reads=[pb.k], writes=[kT_blk[T]])
            ch.append(lambda: k_nope(0))
            ch.append(lambda: k_nope(4))

            def k_rope():
                pA, pB = nextbank(S), nextbank(S)
                P.mm(pA[0:96, :], [(wdr[:, kc, :], xTb[:, kc, :]) for kc in range(8)], reads=[wdr.k, xTb.k],
                     writes=[pA.k])
                P.mm(pB[0:96, :], [(wdrot[:, kc, :], xTb[:, kc, :]) for kc in range(8)], reads=[wdrot.k, xTb.k],
                     writes=[pB.k])
                P.op("dve", lambda e: e.tensor_tensor(out=rt1[R, :], in0=pA[R, :], in1=cosT[R, :], op=ALU.mult),
                     reads=[pA.k, cosT.k], writes=[rt1.k])
                P.op("dve", lambda e: e.tensor_tensor(out=rt2[R, :], in0=pB[R, :], in1=sinT[R, :], op=ALU.mult),
                     reads=[pB.k, sinT.k], writes=[rt2.k])
                P.op("dve", lambda e: e.tensor_tensor(out=kr[R, :], in0=rt1[R, :], in1=rt2[R, :], op=ALU.add),
                     reads=[rt1.k, rt2.k], writes=[kr.k])
                P.op("dve", lambda e: e.tensor_copy(out=kT[R, :, tcols],
                                                    in_=kr[R, :].unsqueeze(1).broadcast_to([32, 8, ST])),
                     reads=[kr.k], writes=[kT_blk[T]])
            ch.append(k_rope)

            def v_tiles():
                for a in range(4):
                    pb = nextbank(S)
                    P.mm(pb[:], [(ckTb[:, kc, a * 128:(a + 1) * 128], wuv[:, kc, :]) for kc in range(2)],
                         reads=[ckTb.k, wuv.k], writes=[pb.k])
                    P.op("act", lambda e, pb=pb, a=a: e.activation(out=Vaug[:, 4 * T + a, :, 0:64], in_=v3(pb[:], 8),
                                                                   func=AF.Copy), reads=[pb.k], writes=[V_blk[T]])
            ch.append(v_tiles)

            def queries(h0):
                for h in range(h0, h0 + 2):
                    pA, pB = nextbank(S), nextbank(S)
                    P.mm(pA[0:96, :], [(wuq[:, kc, h * 96:(h + 1) * 96], cqTb[:, kc, :]) for kc in range(2)],
                         reads=[wuq.k, cqTb.k], writes=[pA.k])
                    P.mm(pB[0:96, :], [(wuqrot[:, kc, h, :], cqTb[:, kc, :]) for kc in range(2)],
                         reads=[wuqrot.k, cqTb.k], writes=[pB.k])
                    P.op("dve", lambda e, pA=pA, h=h: e.tensor_copy(out=qThb[0:64, h, :], in_=pA[0:64, :]),
                         reads=[pA.k], writes=[qThb.k])
                    P.op("dve", lambda e, pA=pA: e.tensor_tensor(out=rt1[R, :], in0=pA[R, :], in1=cosT[R, :],
                                                                 op=ALU.mult), reads=[pA.k, cosT.k], writes=[rt1.k])
                    P.op("dve", lambda e, pB=pB: e.tensor_tensor(out=rt2[R, :], in0=pB[R, :], in1=sinT[R, :],
                                                                 op=ALU.mult), reads=[pB.k, sinT.k], writes=[rt2.k])
                    P.op("dve", lambda e, h=h: e.tensor_tensor(out=qThb[R, h, :], in0=rt1[R, :], in1=rt2[R, :],
                                                               op=ALU.add), reads=[rt1.k, rt2.k], writes=[qThb.k])
            for h0 in range(0, 8, 2):
                ch.append(lambda h0=h0: queries(h0))

            def q_mem():
                for h in range(4):
                    pb = nextbank(S)
                    P.mm(pb[:], [(bwin[:, kc, 256 + h * 128:256 + (h + 1) * 128], xTb[:, kc, :]) for kc in range(8)],
                         reads=[xTb.k] + bwin_tr, writes=[pb.k])
                    P.op("act", lambda e, pb=pb, h=h: e.activation(out=qmTb[:, h, :], in_=pb[:], func=AF.Copy),
                         reads=[pb.k], writes=[qmTb.k])
            ch.append(q_mem)
            return ch

        def mla_head(s, h, pending, per):
            T, b = s % NT, s % 2
            qThb = qTh[b]
            nkt = 4 * T + 4
            po = S["psf"][2 + (h % 2)]
            started = [False]

            def emit_front(j):
                a_min = max(0, j - 4 * T)
                qc = slice(a_min * 128, ST)
                pb = sbank[sb_i[0] % 2]
                sb_i[0] += 1
                P.mm(pb[:, qc], [(kT[:, h, j * 128:(j + 1) * 128], qThb[:, h, qc])],
                     reads=[kT_blk[j // 4], qThb.k], writes=[pb.k])
                pt = S["PT"][S["pt_i"] % len(S["PT"])]
                S["pt_i"] += 1
                P.op("act", lambda e: e.activation(out=pt[:, qc], in_=pb[:, qc], func=AF.Exp, scale=SC),
                     reads=[pb.k], writes=[pt.k])
                if j >= 4 * T:
                    P.op("pool", lambda e: e.memset(pt[64:128, a_min * 128:a_min * 128 + 64], 0.0),
                         reads=[], writes=[pt.k])
                return (j, a_min, pt)

            def emit_back(j, a_min, pt):
                fns = []
                for a in range(a_min, 4):
                    st = not started[0]
                    started[0] = True
                    fns.append(lambda e, a=a, st=st: e.matmul(
                        po[:, a * 65:(a + 1) * 65], pt[:, a * 128:(a + 1) * 128], Vaug[:, j, h, :],
                        start=st, stop=(j == 4 * T + a), skip_group_check=True))
                P.pe_multi(fns, reads=[pt.k, V_blk[j // 4]], writes=[po.k])

            prev = None
            for j in range(nkt):
                cur = emit_front(j)
                if prev is not None:
                    emit_back(*prev)
                prev = cur
                for _ in range(per):
                    if pending:
                        P.replay(pending.pop(0))
            emit_back(*prev)
            po3 = v3(po[:, 0:260], 4)
            P.op("dve", lambda e: e.reciprocal(out=rc4[:], in_=po3[:, :, 64:65]), reads=[po.k], writes=[rc4.k])
            P.op("dve", lambda e: e.tensor_tensor(
                out=hc_all[:, :, h * 64:(h + 1) * 64], in0=po3[:, :, 0:64],
                in1=rc4[:, :, 0:1].broadcast_to([128, 4, 64]), op=ALU.mult),
                 reads=[po.k, rc4.k], writes=[hc_all.k])

        for f in front_chunks(0):
            f()
        wout_tr.extend(load_weight_cast(C, wout, w_out_d.rearrange("(c p) f -> p c f", p=128), 2, 1))
        for s in range(NST):
            seq, b = s // NT, s % 2
            pending = []
            if s + 1 < NST:
                P.rec = pending
                for f in front_chunks(s + 1):
                    f()
                P.rec = None
            mem_attention(C, S, seq, qmT[b], KmT, Vm, hcat, 512)
            nsteps = 8 * (4 * (s % NT) + 4)
            per = (len(pending) + nsteps - 1) // nsteps
            for h in range(8):
                mla_head(s, h, pending, per)
            while pending:
                P.replay(pending.pop(0))
            out_proj_ln(C, S, hcat, xT[b], wout, wout_tr, xld, nl, x_view, x_trks[s], s, y, g_rep, b_rep,
                        o_view, out_trks[s])
        P.full_barrier()
        S["rot0"], S["nrot"] = 0, 6
        S.pop("cast_eng")


def _consts_np():
    c = np.zeros((128, 4, 128), np.float32)
    c[:, 0, :] = np.eye(128, dtype=np.float32)
    s = np.arange(128)[:, None]
    l = np.arange(128)[None, :]
    c[:, 1, :] = ((s // 64 == l // 64) & (s <= l)).astype(np.float32)
    c[:, 2, :] = (s < 64).astype(np.float32) * np.ones((1, 128), np.float32)
    c[:, 3, :] = (s >= 64).astype(np.float32) * np.ones((1, 128), np.float32)
    return c


def _cf2_np():
    c = np.zeros((128, 4), np.float32)
    inv = (10000.0 ** (-np.arange(0, 32, 2, dtype=np.float32) / 32)).astype(np.float32)
    for p in range(64, 96):
        c[p, 0] = inv[(p - 64) % 16] / np.float32(2 * np.pi)
    c[:, 1] = -np.pi
    return c


W_SHAPES = {
    "a_w_in": [D, 2056], "a_b_igate": [4], "a_b_fgate": [4], "a_w_mem_kv": [D, D], "a_w_out": [D, D],
    "kv_w_down": [D, 288], "kv_norm_g": [256], "kv_w_uk": [256, 512], "kv_w_uv": [256, 512],
    "b_w_in": [D, 768], "b_q_norm_g": [256], "b_w_uq": [256, 768], "b_w_mem_kv": [D, D], "b_w_out": [D, D],
    "ln1_g": [2, D], "ln1_b": [2, D], "ffn_w_up": [2, D, DFF], "ffn_w_down": [2, DFF, D], "ln2_g": [2, D],
    "ln2_b": [2, D],
}


def build_program():
    nc = bass.Bass("TRN2", target_bir_lowering=False)
    dt = lambda n, sh: nc.dram_tensor(n, sh, F32, kind="ExternalInput").ap()
    x = dt("x", [NTOK, D])
    mem = dt("mem", [NSEQ, 256, D])
    pos = nc.dram_tensor("positions", [NSEQ, SEQ], I32, kind="ExternalInput").ap()
    w = {k: dt(k, sh) for k, sh in W_SHAPES.items()}
    consts = dt("consts", [128, 4, 128])
    cf2 = dt("cf2", [128, 4])
    out = nc.dram_tensor("out", [NTOK, D], F32, kind="ExternalOutput").ap()
    sc1 = nc.dram_tensor("scratch1", [NTOK, D], F32, kind="Internal").ap()
    sc2 = nc.dram_tensor("scratch2", [NTOK, D], F32, kind="Internal").ap()
    C = Ctx(nc)
    with nc.allow_low_precision("bf16 matmul operands, fp32 accumulation"), C.es:
        S = alloc_shared(C)
        load_consts(C, S, consts)
        xtr = [Trk("xd%d" % i) for i in range(NST)]
        t1 = [Trk("s1_%d" % i) for i in range(NST)]
        t2 = [Trk("s2_%d" % i) for i in range(NST)]
        otr = [Trk("od%d" % i) for i in range(NST)]
        mixer_a_phase(C, S, x, xtr, sc1, t1, mem, w["a_w_in"], w["a_b_igate"], w["a_b_fgate"], w["a_w_mem_kv"],
                      w["a_w_out"], w["ln1_g"][0], w["ln1_b"][0])
        ffn_phase(C, S, sc1, t1, sc2, t2, w["ffn_w_up"][0], w["ffn_w_down"][0], w["ln2_g"][0], w["ln2_b"][0], False)
        mixer_b_phase(C, S, sc2, t2, sc1, t1, mem, pos, w["kv_w_down"], w["kv_norm_g"], w["kv_w_uk"], w["kv_w_uv"],
                      w["b_w_in"], w["b_q_norm_g"], w["b_w_uq"], w["b_w_mem_kv"], w["b_w_out"], w["ln1_g"][1],
                      w["ln1_b"][1], cf2)
        ffn_phase(C, S, sc1, t1, out, otr, w["ffn_w_up"][1], w["ffn_w_down"][1], w["ln2_g"][1], w["ln2_b"][1], True)
        C.P.finish()
    return nc


def kernel(x, mem, positions, a_w_in, a_b_igate, a_b_fgate, a_w_mem_kv, a_w_out, kv_w_down, kv_norm_g, kv_w_uk,
           kv_w_uv, b_w_in, b_q_norm_g, b_w_uq, b_w_mem_kv, b_w_out, ln1_g, ln1_b, ffn_w_up, ffn_w_down, ln2_g,
           ln2_b):
    f32 = lambda a: np.ascontiguousarray(np.asarray(a), dtype=np.float32)
    shared = {
        "a_w_in": f32(a_w_in)[0], "a_b_igate": f32(a_b_igate)[0], "a_b_fgate": f32(a_b_fgate)[0],
        "a_w_mem_kv": f32(a_w_mem_kv)[0], "a_w_out": f32(a_w_out)[0], "kv_w_down": f32(kv_w_down),
        "kv_norm_g": f32(kv_norm_g), "kv_w_uk": f32(kv_w_uk), "kv_w_uv": f32(kv_w_uv), "b_w_in": f32(b_w_in)[0],
        "b_q_norm_g": f32(b_q_norm_g)[0], "b_w_uq": f32(b_w_uq)[0], "b_w_mem_kv": f32(b_w_mem_kv)[0],
        "b_w_out": f32(b_w_out)[0], "ln1_g": f32(ln1_g), "ln1_b": f32(ln1_b), "ffn_w_up": f32(ffn_w_up),
        "ffn_w_down": f32(ffn_w_down), "ln2_g": f32(ln2_g), "ln2_b": f32(ln2_b),
        "consts": _consts_np(), "cf2": _cf2_np(),
    }
    shared = {k: np.ascontiguousarray(v) for k, v in shared.items()}
    x = f32(x)
    mem = f32(mem)
    positions = np.ascontiguousarray(np.asarray(positions), dtype=np.int32)
    in_maps = []
    for c in range(NCORES):
        m = dict(shared)
        m["x"] = np.ascontiguousarray(x[c * NSEQ:(c + 1) * NSEQ].reshape(NTOK, D))
        m["mem"] = np.ascontiguousarray(mem[c * NSEQ:(c + 1) * NSEQ])
        m["positions"] = np.ascontiguousarray(positions[c * NSEQ:(c + 1) * NSEQ])
        in_maps.append(m)
    nc = build_program()
    res = run_bass_kernel_spmd(nc, in_maps, core_ids=list(range(NCORES)))
    outs = [np.asarray(r["out"], dtype=np.float32).reshape(NSEQ, SEQ, D) for r in res.results]
    return np.concatenate(outs, axis=0)
```

```python
import contextlib
import numpy as np
import concourse.bass as bass
import concourse.mybir as mybir
from concourse.bass_utils import run_bass_kernel_spmd

F32 = mybir.dt.float32
BF16 = mybir.dt.bfloat16
I32 = mybir.dt.int32
AF = mybir.ActivationFunctionType
ALU = mybir.AluOpType
AX = mybir.AxisListType

NCORES = 8
SEQ = 2048
D = 1024
DFF = 4096
NSEQ = 2
NTOK = NSEQ * SEQ
ST = 512
NST = NTOK // ST
ALPHA = 4.0 ** 0.25
LN_EPS = 1e-5
RMS_EPS = 1e-6


class Trk:
    __slots__ = ("name", "w", "r", "dsem", "dcnt")

    def __init__(self, name):
        self.name = name
        self.w = None
        self.r = {}
        self.dsem = None
        self.dcnt = 0


class Prog:
    SEM_ROT = 12000

    def __init__(self, nc):
        self.nc = nc
        self.eng = {"pe": nc.tensor, "act": nc.scalar, "dve": nc.vector, "pool": nc.gpsimd,
                    "sp": nc.sync}
        self.sem = {}
        self.cnt = {}
        self.seen = {e: {} for e in self.eng}
        self.nsem = 0
        for e in self.eng:
            self._new_sem(e)
        self.out_tokens = []
        self.ninstr = 0
        self.last_tok = {}
        self.rec = None
        self.inter = None
        self._acc = 0.0
        self._in_replay = False
        self.dma_toks = {}
        self.free_dsems = []
        self.phase_trks = []

    def _alloc_sem(self, name):
        self.nsem += 1
        return self.nc.alloc_semaphore(name="%s_%d" % (name, self.nsem))

    def _new_sem(self, e):
        self.sem[e] = self._alloc_sem("s_" + e)
        self.cnt[e] = 0

    def _need(self, e, tok, skip_same):
        if tok is None:
            return
        sem, c, te = tok
        if skip_same and te == e and e == "pe":
            return
        if self.seen[e].get(sem, 0) >= c:
            return
        self.eng[e].wait_ge(sem, c)
        self.seen[e][sem] = c

    def _signal(self, e, ins):
        if self.cnt[e] >= self.SEM_ROT:
            self._new_sem(e)
        self.cnt[e] += 1
        ins.then_inc(self.sem[e], 1)
        self.last_tok[e] = (self.sem[e], self.cnt[e], e)
        return self.last_tok[e]

    def full_barrier(self):
        snap = dict(self.last_tok)
        dts = list(self.dma_toks.values())
        for e in self.eng:
            for o, tok in snap.items():
                if o != e:
                    self._need(e, tok, False)
            for tok in dts:
                self._need(e, tok, False)
        for t in self.phase_trks:
            self.free_dsems.append((t.dsem, t.dcnt))
            t.dsem = None
        self.phase_trks = []
        self.dma_toks = {}

    def _after_emit(self):
        if self.inter is None or self._in_replay:
            return
        pend, rate = self.inter
        self._acc += rate
        while self._acc >= 1.0 and pend:
            self._acc -= 1.0
            self._in_replay = True
            self.replay(pend.pop(0))
            self._in_replay = False

    def replay(self, item):
        kind, args, kw = item
        saved, self.rec = self.rec, None
        getattr(self, kind)(*args, **kw)
        self.rec = saved

    def op(self, e, fn, reads=(), writes=()):
        if self.rec is not None:
            self.rec.append(("op", (e, fn), dict(reads=list(reads), writes=list(writes))))
            return None
        for t in reads:
            self._need(e, t.w, False)
        for t in writes:
            self._need(e, t.w, True)
            for tok in t.r.values():
                self._need(e, tok, True)
        ins = fn(self.eng[e])
        tok = self._signal(e, ins)
        for t in writes:
            t.w = tok
            t.r = {}
        for t in reads:
            t.r[e] = tok
        self.ninstr += 1
        self._after_emit()
        return tok

    def mm(self, out, pairs, reads=(), writes=()):
        if self.rec is not None:
            self.rec.append(("mm", (out, list(pairs)), dict(reads=list(reads), writes=list(writes))))
            return None
        e = "pe"
        for t in reads:
            self._need(e, t.w, False)
        for t in writes:
            self._need(e, t.w, True)
            for tok in t.r.values():
                self._need(e, tok, True)
        n = len(pairs)
        ins = None
        for i, (l, r) in enumerate(pairs):
            ins = self.nc.tensor.matmul(out, l, r, start=(i == 0), stop=(i == n - 1))
        tok = self._signal(e, ins)
        for t in writes:
            t.w = tok
            t.r = {}
        for t in reads:
            t.r[e] = tok
        self.ninstr += n
        self._after_emit()
        return tok

    def pe_multi(self, fns, reads=(), writes=()):
        if self.rec is not None:
            self.rec.append(("pe_multi", (list(fns),), dict(reads=list(reads), writes=list(writes))))
            return None
        e = "pe"
        for t in reads:
            self._need(e, t.w, False)
        for t in writes:
            self._need(e, t.w, True)
            for tok in t.r.values():
                self._need(e, tok, True)
        ins = None
        for f in fns:
            ins = f(self.nc.tensor)
        tok = self._signal(e, ins)
        for t in writes:
            t.w = tok
            t.r = {}
        for t in reads:
            t.r[e] = tok
        self.ninstr += len(fns)
        self._after_emit()
        return tok

    def dma(self, q, out, in_, reads=(), writes=(), is_output=False, sem_trk=None):
        if self.rec is not None:
            self.rec.append(("dma", (q, out, in_), dict(reads=list(reads), writes=list(writes),
                                                        is_output=is_output, sem_trk=sem_trk)))
            return None
        e = q
        for t in reads:
            self._need(e, t.w, False)
        for t in writes:
            self._need(e, t.w, False)
            for tok in t.r.values():
                self._need(e, tok, False)
        trk = sem_trk if sem_trk is not None else (list(writes) + list(reads))[0]
        if trk.dsem is None:
            if self.free_dsems:
                trk.dsem, trk.dcnt = self.free_dsems.pop()
            else:
                trk.dsem = self._alloc_sem("d")
                trk.dcnt = 0
            self.phase_trks.append(trk)
        trk.dcnt += 16
        self.eng[e].dma_start(out=out, in_=in_).then_inc(trk.dsem, 16)
        tok = (trk.dsem, trk.dcnt, "dma")
        self.dma_toks[trk.dsem] = tok
        for t in writes:
            t.w = tok
            t.r = {}
        for t in reads:
            t.r["dma_%s" % trk.name] = tok
        if is_output:
            self.out_tokens.append(tok)
        self.ninstr += 1
        return tok

    def barrier_all(self, trks):
        for t in trks:
            self._need("sp", t.w, False)
            for tok in t.r.values():
                self._need("sp", tok, False)

    def finish(self):
        for tok in self.out_tokens:
            self._need("sp", tok, False)


def list_schedule(items):
    n = len(items)
    eng, dur, deps = [], [], [[] for _ in range(n)]
    last_w, readers = {}, {}
    for i, (kind, args, kw) in enumerate(items):
        if kind == "op":
            e, d = args[0], 0.6
        elif kind == "mm":
            e, d = "pe", 0.25 * len(args[1]) + 0.1
        elif kind == "pe_multi":
            e, d = "pe", 0.15 * len(args[0]) + 0.1
        else:
            e, d = "q_" + args[0], 0.1
        eng.append(e)
        dur.append(d)
        rd, wr = kw.get("reads", ()), kw.get("writes", ())
        ds = set()
        for t in rd:
            if id(t) in last_w:
                ds.add(last_w[id(t)])
        for t in wr:
            if id(t) in last_w:
                ds.add(last_w[id(t)])
            for j in readers.get(id(t), ()):
                ds.add(j)
        ds.discard(i)
        deps[i] = sorted(ds)
        for t in wr:
            last_w[id(t)] = i
            readers[id(t)] = []
        for t in rd:
            readers.setdefault(id(t), []).append(i)
    nsucc_wait = [len(d) for d in deps]
    succ = [[] for _ in range(n)]
    for i in range(n):
        for j in deps[i]:
            succ[j].append(i)
    finish = [0.0] * n
    ready_t = [0.0] * n
    efree = {}
    ready = [i for i in range(n) if nsucc_wait[i] == 0]
    order = []
    while ready:
        best, best_key = None, None
        for i in ready:
            st = max(efree.get(eng[i], 0.0), ready_t[i])
            key = (st, i)
            if best_key is None or key < best_key:
                best, best_key = i, key
        i = best
        ready.remove(i)
        st = best_key[0]
        lat = 2.5 if eng[i].startswith("q_") else 0.3
        finish[i] = st + dur[i]
        efree[eng[i]] = finish[i]
        order.append(i)
        for k in succ[i]:
            ready_t[k] = max(ready_t[k], finish[i] + lat)
            nsucc_wait[k] -= 1
            if nsucc_wait[k] == 0:
                ready.append(k)
    assert len(order) == n
    return [items[i] for i in order]


class Tile:
    def __init__(self, t, name):
        self.t = t
        self.k = Trk(name)

    def __getitem__(self, idx):
        return self.t[idx]


class Ctx:
    def __init__(self, nc):
        self.nc = nc
        self.P = Prog(nc)
        self.es = contextlib.ExitStack()
        self.nid = 0

    def sb(self, name, shape, dt, es=None):
        self.nid += 1
        nm = "%s_%d" % (name, self.nid)
        t = (es or self.es).enter_context(self.nc.sbuf_tensor(nm, list(shape), dt))
        return Tile(t, nm)

    def ps(self, name, shape, dt, es=None):
        self.nid += 1
        nm = "%s_%d" % (name, self.nid)
        t = (es or self.es).enter_context(self.nc.psum_tensor(nm, list(shape), dt))
        return Tile(t, nm)


def load_weight_cast(C, wt, dram_view, nsplit, axis):
    P = C.P
    n = wt.t.shape[axis]
    step = n // nsplit
    trks = []
    for j in range(nsplit):
        sl = [slice(None)] * 3
        sl[axis] = slice(j * step, (j + 1) * step)
        sl = tuple(sl)
        k = Trk("%s_p%d" % (wt.k.name, j))
        P.dma("pool", wt.t[sl], dram_view[sl], writes=[k])
        trks.append(k)
    return trks


def layer_norm_tile(C, S, y, g_rep, b_rep, out):
    P = C.P
    st, mv, rstd, nmr = S["ln_st"], S["ln_mv"], S["ln_rstd"], S["ln_nmr"]
    xn = y
    for hh in range(2):
        P.op("dve", lambda e, hh=hh: e.bn_stats(out=st[:, hh, :], in_=y[:, hh * 512:(hh + 1) * 512]),
             reads=[y.k], writes=[st.k])
    P.op("dve", lambda e: e.bn_aggr(out=mv[:], in_=st[:]), reads=[st.k], writes=[mv.k])
    P.op("act", lambda e: e.activation(out=rstd[:], in_=mv[:, 1:2], func=AF.Ln, bias=S["eps_ln"][:], scale=1.0),
         reads=[mv.k, S["eps_ln"].k], writes=[rstd.k])
    P.op("act", lambda e: e.activation(out=rstd[:], in_=rstd[:], func=AF.Exp, scale=-0.5),
         reads=[rstd.k], writes=[rstd.k])
    P.op("dve", lambda e: e.tensor_scalar(out=nmr[:], in0=mv[:, 0:1], scalar1=-1.0, scalar2=None, op0=ALU.mult),
         reads=[mv.k], writes=[nmr.k])
    P.op("dve", lambda e: e.scalar_tensor_tensor(out=xn[:], in0=y[:], scalar=nmr[:, 0:1], in1=g_rep[:],
                                                 op0=ALU.add, op1=ALU.mult),
         reads=[y.k, nmr.k, g_rep.k], writes=[xn.k])
    P.op("dve", lambda e: e.scalar_tensor_tensor(out=out[:], in0=xn[:], scalar=rstd[:, 0:1], in1=b_rep[:],
                                                 op0=ALU.mult, op1=ALU.add),
         reads=[xn.k, rstd.k, b_rep.k], writes=[out.k])


def transpose_tokens(C, S, xin, xT, col0):
    P = C.P
    xb = S["xb"][S["xb_i"] % len(S["xb"])]
    S["xb_i"] += 1
    P.op(S.get("cast_eng", "act"), (lambda e: e.tensor_copy(out=xb[:], in_=xin[:])) if S.get("cast_eng") else
         (lambda e: e.activation(out=xb[:], in_=xin[:], func=AF.Copy)), reads=[xin.k], writes=[xb.k])
    pt = S["pst"][S["pst_i"] % 2]
    S["pst_i"] += 1
    ident = S["ident"]
    P.pe_multi([lambda e, c=c: e.transpose(out=pt[:, c * 128:(c + 1) * 128], in_=xb[:, c * 128:(c + 1) * 128],
                                           identity=ident[:]) for c in range(8)],
               reads=[xb.k, ident.k], writes=[pt.k])
    P.op("dve", lambda e: e.tensor_copy(out=xT[:, :, col0:col0 + 128],
                                        in_=pt[:, :].rearrange("p (c t) -> p c t", c=8)),
         reads=[pt.k], writes=[xT.k])


def ffn_phase(C, S, x_dram, x_trks, out_dram, out_trks, w_up_d, w_down_d, g_d, b_d, is_output):
    nc, P = C.nc, C.P
    with contextlib.ExitStack() as es:
        wup = C.sb("wup", [128, 8, DFF], BF16, es)
        wdn = C.sb("wdn", [128, 32, D], BF16, es)
        g_rep = C.sb("g_rep", [128, D], F32, es)
        b_rep = C.sb("b_rep", [128, D], F32, es)
        xld_p = C.sb("xld_p", [128, D], F32, es)
        xld_r = C.sb("xld_r", [128, D], F32, es)
        xT2 = [C.sb("xT", [128, 8, ST], BF16, es) for _ in range(2)]
        hT = C.sb("hT", [128, 32, ST], BF16, es)
        rl = [C.sb("rl", [128, ST], BF16, es) for _ in range(2)]
        y = [C.sb("y", [128, D], F32, es) for _ in range(2)]

        P.dma("sp", g_rep[:], g_d.partition_broadcast(128), writes=[g_rep.k])
        P.dma("sp", b_rep[:], b_d.partition_broadcast(128), writes=[b_rep.k])
        x_view = x_dram.rearrange("(s a p) d -> s a p d", a=4, p=128)
        o_view = out_dram.rearrange("(s a p) d -> s a p d", a=4, p=128)
        up_tr = load_weight_cast(C, wup, w_up_d.rearrange("(c p) f -> p c f", p=128), 8, 2)
        dn_tr = load_weight_cast(C, wdn, w_down_d.rearrange("(c p) f -> p c f", p=128), 8, 1)

        psb = S["psf"]
        nb = 0

        def prep(s):
            for a in range(4):
                P.dma("sp", xld_p[:], x_view[s, a], reads=[x_trks[s]], writes=[xld_p.k])
                transpose_tokens(C, S, xld_p, xT2[s % 2], a * 128)

        prep(0)
        for s in range(NST):
            xT = xT2[s % 2]
            pending = []
            if s + 1 < NST:
                P.rec = pending
                prep(s + 1)
                P.rec = None
            P.inter = (pending, len(pending) / 90.0 + 0.01)
            P._acc = 0.0
            for fc in range(32):
                pb = psb[nb % 4]
                nb += 1
                P.mm(pb[:], [(wup[:, kc, fc * 128:(fc + 1) * 128], xT[:, kc, :]) for kc in range(8)],
                     reads=[xT.k, up_tr[fc // 4]], writes=[pb.k])
                r = rl[fc % 2]
                P.op("act", lambda e, r=r, pb=pb: e.activation(out=r[:], in_=pb[:], func=AF.Relu),
                     reads=[pb.k], writes=[r.k])
                P.op("dve", lambda e, r=r, fc=fc: e.tensor_tensor(out=hT[:, fc, :], in0=r[:], in1=r[:],
                                                                   op=ALU.mult),
                     reads=[r.k], writes=[hT.k])
            P.inter = None
            while pending:
                P.replay(pending.pop(0))
            for a in range(4):
                xi = xld_r
                P.dma("sp", xi[:], x_view[s, a], reads=[x_trks[s]], writes=[xi.k])
                yy = y[a % 2]
                for dh in range(2):
                    pb = psb[nb % 4]
                    nb += 1
                    P.mm(pb[:], [(hT[:, fc, a * 128:(a + 1) * 128], wdn[:, fc, dh * 512:(dh + 1) * 512])
                                 for fc in range(32)],
                         reads=[hT.k] + dn_tr, writes=[pb.k])
                    P.op("dve", lambda e, yy=yy, pb=pb, dh=dh, xi=xi: e.scalar_tensor_tensor(
                        out=yy[:, dh * 512:(dh + 1) * 512], in0=xi[:, dh * 512:(dh + 1) * 512],
                        scalar=ALPHA, in1=pb[:], op0=ALU.mult, op1=ALU.add),
                         reads=[xi.k, pb.k], writes=[yy.k])
                layer_norm_tile(C, S, yy, g_rep, b_rep, yy)
                P.dma("pool", o_view[s, a], yy[:], reads=[yy.k], writes=[out_trks[s]],
                      is_output=is_output, sem_trk=yy.k)
        P.full_barrier()


def alloc_shared(C):
    S = {}
    S["ident"] = C.sb("ident", [128, 128], BF16)
    S["psf"] = [C.ps("psf", [128, 512], F32) for _ in range(6)]
    S["pst"] = [C.ps("pst", [128, 1024], BF16) for _ in range(2)]
    S["pst_i"] = 0
    S["nb"] = 0
    S["nrot"] = 6
    S["rot0"] = 0
    S["xb"] = [C.sb("xb", [128, D], BF16) for _ in range(1)]
    S["xb_i"] = 0
    S["ln_st"] = C.sb("ln_st", [128, 2, 6], F32)
    S["ln_mv"] = C.sb("ln_mv", [128, 2], F32)
    S["ln_rstd"] = C.sb("ln_rstd", [128, 1], F32)
    S["ln_nmr"] = C.sb("ln_nmr", [128, 1], F32)
    S["eps_ln"] = C.sb("eps_ln", [128, 1], F32)
    S["eps_rms"] = C.sb("eps_rms", [128, 1], F32)
    C.P.op("pool", lambda e: e.memset(S["eps_ln"][:], LN_EPS), writes=[S["eps_ln"].k])
    C.P.op("pool", lambda e: e.memset(S["eps_rms"][:], RMS_EPS), writes=[S["eps_rms"].k])
    return S


def nextbank(S):
    b = S["psf"][S["rot0"] + S["nb"] % S["nrot"]]
    S["nb"] += 1
    return b


def v3(ap, n):
    return ap.rearrange("p (h d) -> p h d", h=n)


def load_consts(C, S, consts_d):
    P = C.P
    S["cf"] = C.sb("cf", [128, 4, 128], F32)
    S["maskb"] = C.sb("maskb", [128, 128], BF16)
    P.dma("sp", S["cf"][:], consts_d, writes=[S["cf"].k])
    P.dma("pool", S["ident"][:], consts_d[:, 0, :], writes=[S["ident"].k])
    P.dma("pool", S["maskb"][:], consts_d[:, 1, :], writes=[S["maskb"].k])
    S["one1"] = C.sb("one1", [128, 1], F32)
    S["ln8"] = C.sb("ln8", [128, 1], F32)
    P.op("pool", lambda e: e.memset(S["one1"][:], 1.0), writes=[S["one1"].k])
    P.op("pool", lambda e: e.memset(S["ln8"][:], float(np.log(0.125))), writes=[S["ln8"].k])


def mem_kv_precompute(C, S, es, mem_d, wmk, wmk_tr, xld, KmT, Vm):
    P = C.P
    memT = C.sb("memT", [128, 8, 256], BF16, es)
    P.op("pool", lambda e: e.memset(Vm[:], 1.0), writes=[Vm.k])
    n = 0
    for q in range(NSEQ):
        for mt in range(2):
            xi = xld[n % len(xld)]
            n += 1
            P.dma("sp", xi[:], mem_d[q, mt * 128:(mt + 1) * 128, :], writes=[xi.k])
            transpose_tokens(C, S, xi, memT, mt * 128)
        for h in range(4):
            pb = nextbank(S)
            P.mm(pb[:, 0:256], [(wmk[:, kc, h * 128:(h + 1) * 128], memT[:, kc, :]) for kc in range(8)],
                 reads=[memT.k] + wmk_tr, writes=[pb.k])
            P.op("act", lambda e, pb=pb, q=q, h=h: e.activation(out=KmT[:, q, h, :], in_=pb[:, 0:256], func=AF.Copy),
                 reads=[pb.k], writes=[KmT.k])
        for mt in range(2):
            pb = nextbank(S)
            P.mm(pb[:], [(memT[:, kc, mt * 128:(mt + 1) * 128], wmk[:, kc, 512:1024]) for kc in range(8)],
                 reads=[memT.k] + wmk_tr, writes=[pb.k])
            P.op("act", lambda e, pb=pb, q=q, mt=mt: e.activation(out=Vm[:, q, mt, :, 0:128], in_=v3(pb[:], 4),
                                                                  func=AF.Copy),
                 reads=[pb.k], writes=[Vm.k])
    return memT


def mem_attention(C, S, seq, qmT, KmT, Vm, hcat, col0):
    P = C.P
    for h in range(4):
        pts = []
        for mt in range(2):
            pb = nextbank(S)
            P.mm(pb[:], [(KmT[:, seq, h, mt * 128:(mt + 1) * 128], qmT[:, h, :])],
                 reads=[KmT.k, qmT.k], writes=[pb.k])
            pt = S["PT"][S["pt_i"] % len(S["PT"])]
            S["pt_i"] += 1
            P.op("act", lambda e, pt=pt, pb=pb: e.activation(out=pt[:], in_=pb[:], func=AF.Exp, scale=128.0 ** -0.5),
                 reads=[pb.k], writes=[pt.k])
            pts.append(pt)
        for a in range(4):
            pb = nextbank(S)
            P.mm(pb[:, 0:129], [(pts[mt][:, a * 128:(a + 1) * 128], Vm[:, seq, mt, h, :]) for mt in range(2)],
                 reads=[pts[0].k, pts[1].k, Vm.k], writes=[pb.k])
            rc = S["rc"][S["rc_i"] % 2]
            S["rc_i"] += 1
            P.op("dve", lambda e, rc=rc, pb=pb: e.reciprocal(out=rc[:], in_=pb[:, 128:129]),
                 reads=[pb.k], writes=[rc.k])
            P.op("dve", lambda e, rc=rc, pb=pb, a=a, h=h: e.tensor_scalar(
                out=hcat[a][:, col0 + h * 128:col0 + (h + 1) * 128], in0=pb[:, 0:128], scalar1=rc[:, 0:1],
                scalar2=None, op0=ALU.mult), reads=[pb.k, rc.k], writes=[hcat[a].k])


def out_proj_ln(C, S, hcat, hcT, wout, wout_tr, xld, nl, x_view, x_trk, s, y, g_rep, b_rep, o_view, o_trk):
    P = C.P
    for a in range(4):
        pt = S["pst"][S["pst_i"] % 2]
        S["pst_i"] += 1
        ident = S["ident"]
        P.pe_multi([lambda e, c=c, pt=pt, a=a: e.transpose(out=pt[:, c * 128:(c + 1) * 128],
                                                         in_=hcat[a][:, c * 128:(c + 1) * 128],
                                                         identity=ident[:]) for c in range(8)],
                   reads=[hcat[a].k, ident.k], writes=[pt.k])
        P.op("dve", lambda e, pt=pt, a=a: e.tensor_copy(out=hcT[:, :, a * 128:(a + 1) * 128], in_=v3(pt[:, :], 8)),
             reads=[pt.k], writes=[hcT.k])
    for a in range(4):
        xi = xld[nl[0] % len(xld)]
        nl[0] += 1
        P.dma("sp", xi[:], x_view[s, a], reads=[x_trk], writes=[xi.k])
        yy = y[a % 2]
        for dh in range(2):
            pb = nextbank(S)
            P.mm(pb[:], [(hcT[:, kc, a * 128:(a + 1) * 128], wout[:, kc, dh * 512:(dh + 1) * 512])
                         for kc in range(8)], reads=[hcT.k] + wout_tr, writes=[pb.k])
            P.op("dve", lambda e, yy=yy, pb=pb, dh=dh, xi=xi: e.scalar_tensor_tensor(
                out=yy[:, dh * 512:(dh + 1) * 512], in0=xi[:, dh * 512:(dh + 1) * 512],
                scalar=ALPHA, in1=pb[:], op0=ALU.mult, op1=ALU.add),
                 reads=[xi.k, pb.k], writes=[yy.k])
        layer_norm_tile(C, S, yy, g_rep, b_rep, yy)
        P.dma("pool", o_view[s, a], yy[:], reads=[yy.k], writes=[o_trk], sem_trk=yy.k)


def mixer_a_phase(C, S, x_dram, x_trks, out_dram, out_trks, mem_d, w_in_d, bi_d, bf_d, wmk_d, w_out_d, g_d, b_d):
    nc, P = C.nc, C.P
    with contextlib.ExitStack() as es:
        win = C.sb("win", [128, 8, 2056], BF16, es)
        wout = C.sb("wout", [128, 8, D], BF16, es)
        g_rep = C.sb("g_rep", [128, D], F32, es)
        b_rep = C.sb("b_rep", [128, D], F32, es)
        bias_rep = C.sb("bias_rep", [128, 8], F32, es)
        xld = [C.sb("xld", [128, D], F32, es) for _ in range(3)]
        KmT = C.sb("KmT", [128, NSEQ, 4, 256], BF16, es)
        Vm = C.sb("Vm", [128, NSEQ, 2, 4, 129], BF16, es)
        P.dma("sp", g_rep[:], g_d.partition_broadcast(128), writes=[g_rep.k])
        P.dma("sp", b_rep[:], b_d.partition_broadcast(128), writes=[b_rep.k])
        P.dma("sp", bias_rep[:, 0:4], bi_d.partition_broadcast(128), writes=[bias_rep.k])
        P.dma("sp", bias_rep[:, 4:8], bf_d.partition_broadcast(128), writes=[bias_rep.k])
        with contextlib.ExitStack() as es2:
            wmk = C.sb("wmk", [128, 8, D], BF16, es2)
            wmk_tr = load_weight_cast(C, wmk, wmk_d.rearrange("(c p) f -> p c f", p=128), 2, 1)
            win_view = w_in_d.rearrange("(c p) f -> p c f", p=128)
            win_g = []
            for (c0, c1) in ((0, 512), (1536, 2056), (512, 1024), (1024, 1536)):
                k = Trk("win_%d" % c0)
                P.dma("pool", win[:, :, c0:c1], win_view[:, :, c0:c1], writes=[k])
                win_g.append(k)
            wqk_tr, wgm_tr, wv_tr, wo_tr = [win_g[0]], [win_g[1]], [win_g[2]], [win_g[3]]
            wout_tr = []
            mem_kv_precompute(C, S, es2, mem_d, wmk, wmk_tr, xld, KmT, Vm)
            P.full_barrier()
        xT = [C.sb("xT", [128, 8, ST], BF16, es) for _ in range(2)]
        qT = [C.sb("qT", [64, 4, ST], BF16, es) for _ in range(2)]
        kT = [C.sb("kT", [64, 4, ST], BF16, es) for _ in range(2)]
        qz = [C.sb("qz", [64, 4, 4, 2, 128], BF16, es) for _ in range(2)]
        qmT = [C.sb("qmT", [128, 4, ST], BF16, es) for _ in range(2)]
        gts = C.sb("gts", [128, 8], F32, es)
        lfn = C.sb("lfn", [128, 4], F32, es)
        tadd = C.sb("tadd", [128, 4], F32, es)
        colf = C.sb("colf", [128, 4], F32, es)
        enb = C.sb("enb", [128, 4], F32, es)
        eg = [C.sb("eg", [128, 2, 4], F32, es) for _ in range(2)]
        kc_t = C.sb("kc", [128, 4, 64], BF16, es)
        vaug = [C.sb("vaug", [128, 4, 129], BF16, es) for _ in range(2)]
        e_o = C.sb("e_o", [128, 512], F32, es)
        atmp = C.sb("atmp", [128, 4, 128], BF16, es)
        AT = C.sb("AT", [128, 4, 128], BF16, es)
        Sst = C.sb("Sst", [64, 4, 129], F32, es)
        Cf = C.sb("Cf", [64, 4, 129], F32, es)
        Cb = [C.sb("Cb", [64, 4, 129], BF16, es) for _ in range(2)]
        den = C.sb("den", [128, 2, 1], F32, es)
        hcat2 = [[C.sb("hcat", [128, D], BF16, es) for _ in range(4)] for _ in range(2)]
        hcT = C.sb("hcT", [128, 8, ST], BF16, es)
        y = [C.sb("y", [128, D], F32, es) for _ in range(2)]
        S["PT"] = [C.sb("PT", [128, ST], BF16, es) for _ in range(4)]
        S["pt_i"] = 0
        S["rc"] = [C.sb("rc", [128, 1], F32, es) for _ in range(2)]
        S["rc_i"] = 0
        for qq in qz:
            P.op("pool", lambda e, qq=qq: e.memset(qq[:], 0.0), writes=[qq.k])
        for vv in vaug:
            P.op("pool", lambda e, vv=vv: e.memset(vv[:], 1.0), writes=[vv.k])

        x_view = x_dram.rearrange("(s a p) d -> s a p d", a=4, p=128)
        o_view = out_dram.rearrange("(s a p) d -> s a p d", a=4, p=128)
        cf = S["cf"]
        maskb = S["maskb"]
        nl = [0]
        NT = NST // NSEQ

        def front(s):
            b, seq = s % 2, s // NT
            xTb, qTb, kTb, qzb, qmTb = xT[b], qT[b], kT[b], qz[b], qmT[b]
            for a in range(4):
                xi = xld[nl[0] % len(xld)]
                nl[0] += 1
                P.dma("sp", xi[:], x_view[s, a], reads=[x_trks[s]], writes=[xi.k])
                transpose_tokens(C, S, xi, xTb, a * 128)
            for h in range(4):
                for (dst, c0) in ((qTb, 0), (kTb, 256)):
                    pb = nextbank(S)
                    P.mm(pb[0:64, :], [(win[:, kc, c0 + h * 64:c0 + (h + 1) * 64], xTb[:, kc, :]) for kc in range(8)],
                         reads=[xTb.k] + wqk_tr, writes=[pb.k])
                    P.op("act", lambda e, pb=pb, dst=dst, h=h: e.activation(out=dst[:, h, :], in_=pb[0:64, :],
                                                                            func=AF.Copy),
                         reads=[pb.k], writes=[dst.k])
                P.op("pool", lambda e, h=h: e.tensor_copy(
                    out=bass.AP(qzb.t, h * 1024, [[4096, 64], [256, 4], [192, 2], [1, 64]]),
                    in_=qTb[:, h, :].rearrange("p (a c j) -> p a c j", a=4, c=2)),
                     reads=[qTb.k], writes=[qzb.k])
                pb = nextbank(S)
                P.mm(pb[:], [(win[:, kc, 1544 + h * 128:1544 + (h + 1) * 128], xTb[:, kc, :]) for kc in range(8)],
                     reads=[xTb.k] + wgm_tr, writes=[pb.k])
                P.op("act", lambda e, pb=pb, h=h: e.activation(out=qmTb[:, h, :], in_=pb[:], func=AF.Copy),
                     reads=[pb.k], writes=[qmTb.k])
            mem_attention(C, S, seq, qmTb, KmT, Vm, hcat2[b], 512)

        def tail(s):
            out_proj_ln(C, S, hcat2[s % 2], hcT, wout, wout_tr, xld, nl, x_view, x_trks[s], s, y, g_rep, b_rep,
                        o_view, out_trks[s])

        def tok_loop(s):
            b = s % 2
            xTb, qTb, kTb, qzb, hcat = xT[b], qT[b], kT[b], qz[b], hcat2[b]
            for a in range(4):
                t = s * 4 + a
                par = t % 2
                first = (t % (SEQ // 128) == 0)
                cols = slice(a * 128, (a + 1) * 128)
                va = vaug[par]
                pg = nextbank(S)
                P.mm(pg[:, 0:8], [(xTb[:, kc, cols], win[:, kc, 1536:1544]) for kc in range(8)],
                     reads=[xTb.k] + wgm_tr, writes=[pg.k])
                P.op("dve", lambda e, pg=pg: e.tensor_tensor(out=gts[:], in0=pg[:, 0:8], in1=bias_rep[:], op=ALU.add),
                     reads=[pg.k, bias_rep.k], writes=[gts.k])
                P.op("act", lambda e: e.activation(out=lfn[:], in_=gts[:, 4:8], func=AF.Exp, scale=-1.0),
                     reads=[gts.k], writes=[lfn.k])
                P.op("act", lambda e: e.activation(out=lfn[:], in_=lfn[:], func=AF.Ln, bias=S["one1"][:], scale=1.0),
                     reads=[lfn.k, S["one1"].k], writes=[lfn.k])
                pc = nextbank(S)
                P.mm(pc[:, 0:4], [(cf[:, 1, :], lfn[:])], reads=[cf.k, lfn.k], writes=[pc.k])
                P.mm(pc[:, 4:8], [(cf[:, 2, :], lfn[:])], reads=[cf.k, lfn.k], writes=[pc.k])
                P.mm(pc[:, 8:12], [(cf[:, 3, :], lfn[:])], reads=[cf.k, lfn.k], writes=[pc.k])
                P.op("dve", lambda e, pc=pc: e.tensor_tensor(out=tadd[:], in0=gts[:, 0:4], in1=pc[:, 0:4], op=ALU.add),
                     reads=[gts.k, pc.k], writes=[tadd.k])
                P.op("act", lambda e: e.activation(out=colf[:], in_=tadd[:], func=AF.Exp, bias=S["ln8"][:], scale=1.0),
                     reads=[tadd.k, S["ln8"].k], writes=[colf.k])
                P.op("act", lambda e, pc=pc: e.activation(out=enb[:], in_=pc[:, 0:4], func=AF.Exp),
                     reads=[pc.k], writes=[enb.k])
                P.op("act", lambda e, pc=pc, par=par: e.activation(out=eg[par][:], in_=v3(pc[:, 4:12], 2),
                                                                   func=AF.Exp, scale=-1.0),
                     reads=[pc.k], writes=[eg[par].k])
                pk = nextbank(S)
                P.mm(pk[:, 0:256], [(xTb[:, kc, cols], win[:, kc, 256:512]) for kc in range(8)],
                     reads=[xTb.k] + wqk_tr, writes=[pk.k])
                P.op("dve", lambda e, pk=pk: e.tensor_tensor(
                    out=kc_t[:], in0=v3(pk[:, 0:256], 4), in1=colf[:, 0:4].unsqueeze(2).broadcast_to([128, 4, 64]),
                    op=ALU.mult), reads=[pk.k, colf.k], writes=[kc_t.k])
                pv = nextbank(S)
                P.mm(pv[:], [(xTb[:, kc, cols], win[:, kc, 512:1024]) for kc in range(8)],
                     reads=[xTb.k] + wv_tr, writes=[pv.k])
                P.op("act", lambda e, pv=pv, va=va: e.activation(out=va[:, :, 0:128], in_=v3(pv[:], 4), func=AF.Copy),
                     reads=[pv.k], writes=[va.k])
                po = nextbank(S)
                P.mm(po[:], [(xTb[:, kc, cols], win[:, kc, 1024:1536]) for kc in range(8)],
                     reads=[xTb.k] + wo_tr, writes=[po.k])
                P.op("act", lambda e, po=po: e.activation(out=e_o[:], in_=po[:], func=AF.Exp, scale=-1.0),
                     reads=[po.k], writes=[e_o.k])
                P.op("act", lambda e: e.activation(out=e_o[:], in_=e_o[:], func=AF.Ln, bias=S["one1"][:], scale=1.0),
                     reads=[e_o.k, S["one1"].k], writes=[e_o.k])
                P.op("act", lambda e: e.activation(out=e_o[:], in_=e_o[:], func=AF.Exp, scale=-1.0),
                     reads=[e_o.k], writes=[e_o.k])
                pa = nextbank(S)
                for h in range(4):
                    P.mm(pa[:, h * 128:(h + 1) * 128], [(kTb[:, h, cols], qTb[:, h, cols])],
                         reads=[kTb.k, qTb.k], writes=[pa.k])
                P.op("dve", lambda e, pa=pa: e.tensor_tensor(
                    out=atmp[:], in0=v3(pa[:], 4), in1=colf[:, 0:4].unsqueeze(2).broadcast_to([128, 4, 128]),
                    op=ALU.mult), reads=[pa.k, colf.k], writes=[atmp.k])
                P.op("pool", lambda e: e.tensor_tensor(
                    out=AT[:], in0=atmp[:], in1=maskb[:, :].unsqueeze(1).broadcast_to([128, 4, 128]), op=ALU.mult),
                     reads=[atmp.k, maskb.k], writes=[AT.k])
                for c in range(2):
                    if first and c == 0:
                        P.op("dve", lambda e: e.memset(Cf[:], 0.0), writes=[Cf.k])
                        P.op("pool", lambda e: e.memset(Cb[0][:], 0.0), writes=[Cb[0].k])
                    else:
                        egp = eg[par][0:64, 0, :] if c == 1 else eg[1 - par][0:64, 1, :]
                        egk = eg[par].k if c == 1 else eg[1 - par].k
                        P.op("dve", lambda e, egp=egp: e.tensor_tensor(
                            out=Cf[:], in0=Sst[:], in1=egp.unsqueeze(2).broadcast_to([64, 4, 129]), op=ALU.mult),
                             reads=[Sst.k, egk], writes=[Cf.k])
                        P.op("act", lambda e, c=c: e.activation(out=Cb[c][:], in_=Cf[:], func=AF.Copy),
                             reads=[Cf.k], writes=[Cb[c].k])
                    rows = slice(c * 64, (c + 1) * 64)
                    for hp in range(2):
                        pu = nextbank(S)
                        for j in range(2):
                            h = 2 * hp + j
                            P.mm(pu[0:64, j * 129:(j + 1) * 129], [(kc_t[rows, h, :], va[rows, h, :])],
                                 reads=[kc_t.k, va.k], writes=[pu.k])
                        P.op("dve", lambda e, pu=pu, hp=hp: e.tensor_tensor(
                            out=Sst[:, 2 * hp:2 * hp + 2, :], in0=Cf[:, 2 * hp:2 * hp + 2, :],
                            in1=v3(pu[0:64, 0:258], 2), op=ALU.add),
                             reads=[Cf.k, pu.k], writes=[Sst.k])
                for hp in range(2):
                    pn = nextbank(S)
                    for j in range(2):
                        h = 2 * hp + j
                        P.mm(pn[:, j * 129:(j + 1) * 129],
                             [(AT[:, h, :], va[:, h, :]),
                              (qzb[:, h, a, 0, :], Cb[0][:, h, :]),
                              (qzb[:, h, a, 1, :], Cb[1][:, h, :])],
                             reads=[AT.k, va.k, qzb.k, Cb[0].k, Cb[1].k], writes=[pn.k])
                    pn3 = v3(pn[:, 0:258], 2)
                    P.op("dve", lambda e, pn3=pn3, hp=hp: e.tensor_tensor(
                        out=den[:], in0=pn3[:, :, 128:129], in1=enb[:, 2 * hp:2 * hp + 2].unsqueeze(2),
                        op=ALU.max), reads=[pn.k, enb.k], writes=[den.k])
                    P.op("dve", lambda e, pn3=pn3: e.scalar_tensor_tensor(
                        out=den[:], in0=pn3[:, :, 128:129], scalar=-1.0, in1=den[:], op0=ALU.mult, op1=ALU.max),
                         reads=[pn.k, den.k], writes=[den.k])
                    P.op("dve", lambda e: e.reciprocal(out=den[:], in_=den[:]), reads=[den.k], writes=[den.k])
                    for j in range(2):
                        h = 2 * hp + j
                        P.op("dve", lambda e, pn=pn, j=j, h=h, a=a: e.scalar_tensor_tensor(
                            out=hcat[a][:, h * 128:(h + 1) * 128], in0=pn[:, j * 129:j * 129 + 128],
                            scalar=den[:, j, :], in1=e_o[:, h * 128:(h + 1) * 128], op0=ALU.mult, op1=ALU.mult),
                             reads=[pn.k, den.k, e_o.k], writes=[hcat[a].k])

        def side(fn, *a):
            S["rot0"], S["nrot"] = 4, 2
            fn(*a)
            S["rot0"], S["nrot"] = 0, 4

        first = []
        P.rec = first
        side(front, 0)
        P.rec = None
        for it in list_schedule(first):
            P.replay(it)
        wout_tr.extend(load_weight_cast(C, wout, w_out_d.rearrange("(c p) f -> p c f", p=128), 2, 1))
        for s in range(NST):
            pending = []
            P.rec = pending
            if s > 0:
                side(tail, s - 1)
            if s + 1 < NST:
                side(front, s + 1)
            P.rec = None
            pending = list_schedule(pending)
            S["rot0"], S["nrot"] = 0, 4
            P.inter = (pending, len(pending) / 260.0 + 0.02)
            P._acc = 0.0
            tok_loop(s)
            P.inter = None
            while pending:
                P.replay(pending.pop(0))
        side(tail, NST - 1)
        P.full_barrier()
        S["rot0"], S["nrot"] = 0, 6


def rms_rows(C, S, pb, ncols, out_bf, scr):
    P = C.P
    ss, rs = S["rms_ss"], S["rms_rs"]
    if scr is None:
        scr = nextbank(S)
    P.op("dve", lambda e: e.memset(ss[:], 0.0), writes=[ss.k])
    P.op("act", lambda e: e.activation(out=scr[:, 0:ncols], in_=pb[:, 0:ncols], func=AF.Square, accum_out=ss[:]),
         reads=[pb.k], writes=[scr.k, ss.k])
    P.op("act", lambda e: e.activation(out=rs[:], in_=ss[:], func=AF.Ln, bias=S["eps_rms"][:], scale=1.0 / ncols),
         reads=[ss.k, S["eps_rms"].k], writes=[rs.k])
    P.op("act", lambda e: e.activation(out=rs[:], in_=rs[:], func=AF.Exp, scale=-0.5), reads=[rs.k], writes=[rs.k])
    P.op("dve", lambda e: e.tensor_scalar(out=out_bf[:, 0:ncols], in0=pb[:, 0:ncols], scalar1=rs[:, 0:1],
                                          scalar2=None, op0=ALU.mult), reads=[pb.k, rs.k], writes=[out_bf.k])


def transpose_cols(C, S, src_bf, nchunk, dstT, col0):
    P = C.P
    pt = S["pst"][S["pst_i"] % 2]
    S["pst_i"] += 1
    ident = S["ident"]
    P.pe_multi([lambda e, c=c: e.transpose(out=pt[:, c * 128:(c + 1) * 128], in_=src_bf[:, c * 128:(c + 1) * 128],
                                           identity=ident[:]) for c in range(nchunk)],
               reads=[src_bf.k, ident.k], writes=[pt.k])
    P.op("dve", lambda e: e.tensor_copy(out=dstT[:, 0:nchunk, col0:col0 + 128], in_=v3(pt[:, 0:nchunk * 128], nchunk)),
         reads=[pt.k], writes=[dstT.k])


def mixer_b_phase(C, S, x_dram, x_trks, out_dram, out_trks, mem_d, pos_d, wdown_d, gkv_d, wuk_d, wuv_d,
                  bwin_d, gq_d, wuq_d, wmk_d, w_out_d, g_d, b_d, cf2_d):
    nc, P = C.nc, C.P
    SC = 96.0 ** -0.5
    NT = NST // NSEQ
    with contextlib.ExitStack() as es:
        wdown = C.sb("wdown", [128, 8, 288], BF16, es)
        wdr = C.sb("wdr", [128, 8, 96], BF16, es)
        wdrot = C.sb("wdrot", [128, 8, 96], BF16, es)
        wuk = C.sb("wuk", [128, 2, 512], BF16, es)
        wuv = C.sb("wuv", [128, 2, 512], BF16, es)
        wuq = C.sb("wuq", [128, 2, 768], BF16, es)
        wuqrot = C.sb("wuqrot", [128, 2, 8, 96], BF16, es)
        gk = C.sb("gk", [128, 2], F32, es)
        gq = C.sb("gq", [128, 2], F32, es)
        bwin = C.sb("bwin", [128, 8, 768], BF16, es)
        wout = C.sb("wout", [128, 8, D], BF16, es)
        g_rep = C.sb("g_rep", [128, D], F32, es)
        b_rep = C.sb("b_rep", [128, D], F32, es)
        cf2 = C.sb("cf2", [128, 4], F32, es)
        xld = [C.sb("xld", [128, D], F32, es) for _ in range(2)]
        KmT = C.sb("KmT", [128, NSEQ, 4, 256], BF16, es)
        Vm = C.sb("Vm", [128, NSEQ, 2, 4, 129], BF16, es)
        P.dma("sp", g_rep[:], g_d.partition_broadcast(128), writes=[g_rep.k])
        P.dma("sp", b_rep[:], b_d.partition_broadcast(128), writes=[b_rep.k])
        P.dma("sp", cf2[:], cf2_d, writes=[cf2.k])
        for kc in range(2):
            P.dma("sp", gk[:, kc:kc + 1], gkv_d[kc * 128:(kc + 1) * 128].rearrange("(p o) -> p o", o=1), writes=[gk.k])
            P.dma("sp", gq[:, kc:kc + 1], gq_d[kc * 128:(kc + 1) * 128].rearrange("(p o) -> p o", o=1), writes=[gq.k])
        with contextlib.ExitStack() as es2:
            wmk = C.sb("wmk", [128, 8, D], BF16, es2)
            wst = C.sb("wst", [128, 2, 768], F32, es2)
            wmk_tr = load_weight_cast(C, wmk, wmk_d.rearrange("(c p) f -> p c f", p=128), 2, 1)
            wdown_tr = load_weight_cast(C, wdown, wdown_d.rearrange("(c p) f -> p c f", p=128), 1, 1)
            bwin_tr = load_weight_cast(C, bwin, bwin_d.rearrange("(c p) f -> p c f", p=128), 2, 1)
            wout_tr = []
            for (dst, src_d, gg, ncol) in ((wuk, wuk_d, gk, 512), (wuv, wuv_d, gk, 512), (wuq, wuq_d, gq, 768)):
                P.dma("sp", wst[:, :, 0:ncol], src_d.rearrange("(c p) f -> p c f", p=128), writes=[wst.k])
                for kc in range(2):
                    P.op("dve", lambda e, dst=dst, gg=gg, kc=kc, ncol=ncol: e.tensor_scalar(
                        out=dst[:, kc, :], in0=wst[:, kc, 0:ncol], scalar1=gg[:, kc:kc + 1], scalar2=None,
                        op0=ALU.mult), reads=[wst.k, gg.k], writes=[dst.k])
            mem_kv_precompute(C, S, es2, mem_d, wmk, wmk_tr, xld, KmT, Vm)
            P.full_barrier()
        xT = [C.sb("xT", [128, 8, ST], BF16, es) for _ in range(2)]
        uu = C.sb("uu", [96, ST], F32, es)
        u2 = C.sb("u2", [96, ST], F32, es)
        cosT = C.sb("cosT", [96, ST], F32, es)
        sinT = C.sb("sinT", [96, ST], F32, es)
        kT = C.sb("kT", [96, 8, SEQ], BF16, es)
        Vaug = C.sb("Vaug", [128, 16, 8, 65], BF16, es)
        kT_blk = [Trk("kTb%d" % i) for i in range(NT)]
        V_blk = [Trk("Vb%d" % i) for i in range(NT)]
        ckn = C.sb("ckn", [128, 256], BF16, es)
        ckT = [C.sb("ckT", [128, 2, ST], BF16, es) for _ in range(2)]
        cqT = [C.sb("cqT", [128, 2, ST], BF16, es) for _ in range(2)]
        rt1 = C.sb("rt1", [96, ST], F32, es)
        rt2 = C.sb("rt2", [96, ST], F32, es)
        kr = C.sb("kr", [96, ST], BF16, es)
        qTh = [C.sb("qTh", [96, 8, ST], BF16, es) for _ in range(2)]
        qmT = [C.sb("qmT", [128, 4, ST], BF16, es) for _ in range(2)]
        hc_all = C.sb("hc_all", [128, 4, D], BF16, es)
        y = [C.sb("y", [128, D], F32, es) for _ in range(2)]
        scr = None
        rc4 = C.sb("rc4", [128, 4, 1], F32, es)
        S["PT"] = [C.sb("PT", [128, ST], BF16, es) for _ in range(3)]
        S["pt_i"] = 0
        S["rc"] = [C.sb("rc", [128, 1], F32, es) for _ in range(2)]
        S["rc_i"] = 0
        S["rms_ss"] = C.sb("rms_ss", [128, 1], F32, es)
        S["rms_rs"] = C.sb("rms_rs", [128, 1], F32, es)
        hcat = []
        for a in range(4):
            v = Tile(hc_all.t[:, a, :], "hcv")
            v.k = hc_all.k
            hcat.append(v)

        P.op("pool", lambda e: e.memset(wdr[:], 0.0), writes=[wdr.k])
        P.op("pool", lambda e: e.memset(wdrot[:], 0.0), writes=[wdrot.k])
        P.op("pool", lambda e: e.memset(wuqrot[:], 0.0), writes=[wuqrot.k])
        P.op("pool", lambda e: e.tensor_copy(out=wdr[:, :, 64:96], in_=wdown[:, :, 256:288]),
             reads=wdown_tr, writes=[wdr.k])
        P.op("pool", lambda e: e.tensor_scalar(out=wdrot[:, :, 64:80], in0=wdown[:, :, 272:288], scalar1=-1.0,
                                               scalar2=None, op0=ALU.mult), reads=wdown_tr, writes=[wdrot.k])
        P.op("pool", lambda e: e.tensor_copy(out=wdrot[:, :, 80:96], in_=wdown[:, :, 256:272]),
             reads=wdown_tr, writes=[wdrot.k])
        wuq4 = wuq.t[:, :, :].rearrange("p c (h d) -> p c h d", h=8)
        P.op("pool", lambda e: e.tensor_scalar(out=wuqrot[:, :, :, 64:80], in0=wuq4[:, :, :, 80:96], scalar1=-1.0,
                                               scalar2=None, op0=ALU.mult), reads=[wuq.k], writes=[wuqrot.k])
        P.op("pool", lambda e: e.tensor_copy(out=wuqrot[:, :, :, 80:96], in_=wuq4[:, :, :, 64:80]),
             reads=[wuq.k], writes=[wuqrot.k])
        P.op("pool", lambda e: e.memset(Vaug[:], 1.0), writes=V_blk)
        S["rot0"], S["nrot"] = 4, 2
        S["cast_eng"] = "pool"
        sbank = S["psf"][0:2]
        sb_i = [0]

        x_view = x_dram.rearrange("(s a p) d -> s a p d", a=4, p=128)
        o_view = out_dram.rearrange("(s a p) d -> s a p d", a=4, p=128)
        nl = [0]
        R = slice(64, 96)

        def front_chunks(s):
            seq, T, b = s // NT, s % NT, s % 2
            tcols = slice(T * ST, (T + 1) * ST)
            xTb, ckTb, cqTb, qThb, qmTb = xT[b], ckT[b], cqT[b], qTh[b], qmT[b]
            ch = []

            def rope_tables():
                posi_ap = rt2[R, :].bitcast(I32)
                P.dma("sp", posi_ap, pos_d[seq, T * ST:(T + 1) * ST].partition_broadcast(32), writes=[rt2.k])
                P.op("dve", lambda e: e.tensor_copy(out=uu[R, :], in_=posi_ap), reads=[rt2.k], writes=[uu.k])
                for (dstT, shift) in ((sinT, 0.0), (cosT, 0.25)):
                    P.op("dve", lambda e, shift=shift: e.tensor_scalar(
                        out=u2[R, :], in0=uu[R, :], scalar1=cf2[R, 0:1], scalar2=shift, op0=ALU.mult, op1=ALU.add),
                         reads=[uu.k, cf2.k], writes=[u2.k])
                    P.op("dve", lambda e: e.tensor_copy(out=posi_ap, in_=u2[R, :]), reads=[u2.k], writes=[rt2.k])
                    P.op("dve", lambda e: e.tensor_copy(out=rt1[R, :], in_=posi_ap), reads=[rt2.k], writes=[rt1.k])
                    P.op("dve", lambda e: e.tensor_tensor(out=u2[R, :], in0=u2[R, :], in1=rt1[R, :], op=ALU.subtract),
                         reads=[u2.k, rt1.k], writes=[u2.k])
                    P.op("dve", lambda e: e.tensor_scalar(out=rt1[R, :], in0=u2[R, :], scalar1=0.5, scalar2=None,
                                                          op0=ALU.is_gt), reads=[u2.k], writes=[rt1.k])
                    P.op("dve", lambda e: e.tensor_tensor(out=u2[R, :], in0=u2[R, :], in1=rt1[R, :], op=ALU.subtract),
                         reads=[u2.k, rt1.k], writes=[u2.k])
                    P.op("act", lambda e, dstT=dstT: e.activation(out=dstT[R, :], in_=u2[R, :], func=AF.Sin,
                                                                  scale=float(2 * np.pi)),
                         reads=[u2.k], writes=[dstT.k])
            xis = {}

            def load_dma(a):
                xi = xld[nl[0] % len(xld)]
                nl[0] += 1
                xis[a] = xi
                P.dma("sp", xi[:], x_view[s, a], reads=[x_trks[s]], writes=[xi.k])

            def load_tr(a):
                transpose_tokens(C, S, xis[a], xTb, a * 128)

            def latents(a):
                cols = slice(a * 128, (a + 1) * 128)
                for (wt, wtr, dstT) in ((wdown, wdown_tr, ckTb), (bwin, bwin_tr, cqTb)):
                    pb = nextbank(S)
                    P.mm(pb[:, 0:256], [(xTb[:, kc, cols], wt[:, kc, 0:256]) for kc in range(8)],
                         reads=[xTb.k] + wtr, writes=[pb.k])
                    rms_rows(C, S, pb, 256, ckn, scr)
                    transpose_cols(C, S, ckn, 2, dstT, a * 128)
            ch.append(lambda: load_dma(0))
            ch.append(lambda: load_dma(1))
            ch.append(rope_tables)
            ch.append(lambda: load_tr(0))
            ch.append(lambda: load_dma(2))
            ch.append(lambda: load_tr(1))
            ch.append(lambda: load_dma(3))
            ch.append(lambda: latents(0))
            ch.append(lambda: load_tr(2))
            ch.append(lambda: latents(1))
            ch.append(lambda: load_tr(3))
            ch.append(lambda: latents(2))
            ch.append(lambda: latents(3))

            def k_nope(h0):
                for h in range(h0, h0 + 4):
                    pb = nextbank(S)
                    P.mm(pb[0:64, :], [(wuk[:, kc, h * 64:(h + 1) * 64], ckTb[:, kc, :]) for kc in range(2)],
                         reads=[wuk.k, ckTb.k], writes=[pb.k])
                    P.op("dve", lambda e, pb=pb, h=h: e.tensor_copy(out=kT[0:64, h, tcols], in_=pb[0:64, :]),
                         reads=[pb.k], writes=[kT_blk[T]])
            ch.append(lambda: k_nope(0))
            ch.append(lambda: k_nope(4))

            def k_rope():
                pA, pB = nextbank(S), nextbank(S)
                P.mm(pA[0:96, :], [(wdr[:, kc, :], xTb[:, kc, :]) for kc in range(8)], reads=[wdr.k, xTb.k],
                     writes=[pA.k])
                P.mm(pB[0:96, :], [(wdrot[:, kc, :], xTb[:, kc, :]) for kc in range(8)], reads=[wdrot.k, xTb.k],
                     writes=[pB.k])
                P.op("dve", lambda e: e.tensor_tensor(out=rt1[R, :], in0=pA[R, :], in1=cosT[R, :], op=ALU.mult),
                     reads=[pA.k, cosT.k], writes=[rt1.k])
                P.op("dve", lambda e: e.tensor_tensor(out=rt2[R, :], in0=pB[R, :], in1=sinT[R, :], op=ALU.mult),
                     reads=[pB.k, sinT.k], writes=[rt2.k])
                P.op("dve", lambda e: e.tensor_tensor(out=kr[R, :], in0=rt1[R, :], in1=rt2[R, :], op=ALU.add),
                     reads=[rt1.k, rt2.k], writes=[kr.k])
                P.op("dve", lambda e: e.tensor_copy(out=kT[R, :, tcols],
                                                    in_=kr[R, :].unsqueeze(1).broadcast_to([32, 8, ST])),
                     reads=[kr.k], writes=[kT_blk[T]])
            ch.append(k_rope)

            def v_tiles():
                for a in range(4):
                    pb = nextbank(S)
                    P.mm(pb[:], [(ckTb[:, kc, a * 128:(a + 1) * 128], wuv[:, kc, :]) for kc in range(2)],
                         reads=[ckTb.k, wuv.k], writes=[pb.k])
                    P.op("act", lambda e, pb=pb, a=a: e.activation(out=Vaug[:, 4 * T + a, :, 0:64], in_=v3(pb[:], 8),
                                                                   func=AF.Copy), reads=[pb.k], writes=[V_blk[T]])
            ch.append(v_tiles)

            def queries(h0):
                for h in range(h0, h0 + 2):
                    pA, pB = nextbank(S), nextbank(S)
                    P.mm(pA[0:96, :], [(wuq[:, kc, h * 96:(h + 1) * 96], cqTb[:, kc, :]) for kc in range(2)],
                         reads=[wuq.k, cqTb.k], writes=[pA.k])
                    P.mm(pB[0:96, :], [(wuqrot[:, kc, h, :], cqTb[:, kc, :]) for kc in range(2)],
                         reads=[wuqrot.k, cqTb.k], writes=[pB.k])
                    P.op("dve", lambda e, pA=pA, h=h: e.tensor_copy(out=qThb[0:64, h, :], in_=pA[0:64, :]),
                         reads=[pA.k], writes=[qThb.k])
                    P.op("dve", lambda e, pA=pA: e.tensor_tensor(out=rt1[R, :], in0=pA[R, :], in1=cosT[R, :],
                                                                 op=ALU.mult), reads=[pA.k, cosT.k], writes=[rt1.k])
                    P.op("dve", lambda e, pB=pB: e.tensor_tensor(out=rt2[R, :], in0=pB[R, :], in1=sinT[R, :],
                                                                 op=ALU.mult), reads=[pB.k, sinT.k], writes=[rt2.k])
                    P.op("dve", lambda e, h=h: e.tensor_tensor(out=qThb[R, h, :], in0=rt1[R, :], in1=rt2[R, :],
                                                               op=ALU.add), reads=[rt1.k, rt2.k], writes=[qThb.k])
            for h0 in range(0, 8, 2):
                ch.append(lambda h0=h0: queries(h0))

            def q_mem():
                for h in range(4):
                    pb = nextbank(S)
                    P.mm(pb[:], [(bwin[:, kc, 256 + h * 128:256 + (h + 1) * 128], xTb[:, kc, :]) for kc in range(8)],
                         reads=[xTb.k] + bwin_tr, writes=[pb.k])
                    P.op("act", lambda e, pb=pb, h=h: e.activation(out=qmTb[:, h, :], in_=pb[:], func=AF.Copy),
                         reads=[pb.k], writes=[qmTb.k])
            ch.append(q_mem)
            return ch

        def mla_head(s, h, pending, per):
            T, b = s % NT, s % 2
            qThb = qTh[b]
            nkt = 4 * T + 4
            po = S["psf"][2 + (h % 2)]
            started = [False]

            def emit_front(j):
                a_min = max(0, j - 4 * T)
                qc = slice(a_min * 128, ST)
                pb = sbank[sb_i[0] % 2]
                sb_i[0] += 1
                P.mm(pb[:, qc], [(kT[:, h, j * 128:(j + 1) * 128], qThb[:, h, qc])],
                     reads=[kT_blk[j // 4], qThb.k], writes=[pb.k])
                pt = S["PT"][S["pt_i"] % len(S["PT"])]
                S["pt_i"] += 1
                P.op("act", lambda e: e.activation(out=pt[:, qc], in_=pb[:, qc], func=AF.Exp, scale=SC),
                     reads=[pb.k], writes=[pt.k])
                if j >= 4 * T:
                    P.op("pool", lambda e: e.memset(pt[64:128, a_min * 128:a_min * 128 + 64], 0.0),
                         reads=[], writes=[pt.k])
                return (j, a_min, pt)

            def emit_back(j, a_min, pt):
                fns = []
                for a in range(a_min, 4):
                    st = not started[0]
                    started[0] = True
                    fns.append(lambda e, a=a, st=st: e.matmul(
                        po[:, a * 65:(a + 1) * 65], pt[:, a * 128:(a + 1) * 128], Vaug[:, j, h, :],
                        start=st, stop=(j == 4 * T + a), skip_group_check=True))
                P.pe_multi(fns, reads=[pt.k, V_blk[j // 4]], writes=[po.k])

            prev = None
            for j in range(nkt):
                cur = emit_front(j)
                if prev is not None:
                    emit_back(*prev)
                prev = cur
                for _ in range(per):
                    if pending:
                        P.replay(pending.pop(0))
            emit_back(*prev)
            po3 = v3(po[:, 0:260], 4)
            P.op("dve", lambda e: e.reciprocal(out=rc4[:], in_=po3[:, :, 64:65]), reads=[po.k], writes=[rc4.k])
            P.op("dve", lambda e: e.tensor_tensor(
                out=hc_all[:, :, h * 64:(h + 1) * 64], in0=po3[:, :, 0:64],
                in1=rc4[:, :, 0:1].broadcast_to([128, 4, 64]), op=ALU.mult),
                 reads=[po.k, rc4.k], writes=[hc_all.k])

        first = []
        P.rec = first
        for f in front_chunks(0):
            f()
        P.rec = None
        for it in list_schedule(first):
            P.replay(it)
        wout_tr.extend(load_weight_cast(C, wout, w_out_d.rearrange("(c p) f -> p c f", p=128), 2, 1))
        for s in range(NST):
            seq, b = s // NT, s % 2
            pending = []
            if s + 1 < NST:
                P.rec = pending
                for f in front_chunks(s + 1):
                    f()
                P.rec = None
                pending = list_schedule(pending)
            mem_attention(C, S, seq, qmT[b], KmT, Vm, hcat, 512)
            nsteps = 8 * (4 * (s % NT) + 4)
            per = (len(pending) + nsteps - 1) // nsteps
            for h in range(8):
                mla_head(s, h, pending, per)
            while pending:
                P.replay(pending.pop(0))
            out_proj_ln(C, S, hcat, xT[b], wout, wout_tr, xld, nl, x_view, x_trks[s], s, y, g_rep, b_rep,
                        o_view, out_trks[s])
        P.full_barrier()
        S["rot0"], S["nrot"] = 0, 6
        S.pop("cast_eng")


def _consts_np():
    c = np.zeros((128, 4, 128), np.float32)
    c[:, 0, :] = np.eye(128, dtype=np.float32)
    s = np.arange(128)[:, None]
    l = np.arange(128)[None, :]
    c[:, 1, :] = ((s // 64 == l // 64) & (s <= l)).astype(np.float32)
    c[:, 2, :] = (s < 64).astype(np.float32) * np.ones((1, 128), np.float32)
    c[:, 3, :] = (s >= 64).astype(np.float32) * np.ones((1, 128), np.float32)
    return c


def _cf2_np():
    c = np.zeros((128, 4), np.float32)
    inv = (10000.0 ** (-np.arange(0, 32, 2, dtype=np.float32) / 32)).astype(np.float32)
    for p in range(64, 96):
        c[p, 0] = inv[(p - 64) % 16] / np.float32(2 * np.pi)
    c[:, 1] = -np.pi
    return c


W_SHAPES = {
    "a_w_in": [D, 2056], "a_b_igate": [4], "a_b_fgate": [4], "a_w_mem_kv": [D, D], "a_w_out": [D, D],
    "kv_w_down": [D, 288], "kv_norm_g": [256], "kv_w_uk": [256, 512], "kv_w_uv": [256, 512],
    "b_w_in": [D, 768], "b_q_norm_g": [256], "b_w_uq": [256, 768], "b_w_mem_kv": [D, D], "b_w_out": [D, D],
    "ln1_g": [2, D], "ln1_b": [2, D], "ffn_w_up": [2, D, DFF], "ffn_w_down": [2, DFF, D], "ln2_g": [2, D],
    "ln2_b": [2, D],
}


def build_program():
    nc = bass.Bass("TRN2", target_bir_lowering=False)
    dt = lambda n, sh: nc.dram_tensor(n, sh, F32, kind="ExternalInput").ap()
    x = dt("x", [NTOK, D])
    mem = dt("mem", [NSEQ, 256, D])
    pos = nc.dram_tensor("positions", [NSEQ, SEQ], I32, kind="ExternalInput").ap()
    w = {k: dt(k, sh) for k, sh in W_SHAPES.items()}
    consts = dt("consts", [128, 4, 128])
    cf2 = dt("cf2", [128, 4])
    out = nc.dram_tensor("out", [NTOK, D], F32, kind="ExternalOutput").ap()
    sc1 = nc.dram_tensor("scratch1", [NTOK, D], F32, kind="Internal").ap()
    sc2 = nc.dram_tensor("scratch2", [NTOK, D], F32, kind="Internal").ap()
    C = Ctx(nc)
    with nc.allow_low_precision("bf16 matmul operands, fp32 accumulation"), C.es:
        S = alloc_shared(C)
        load_consts(C, S, consts)
        xtr = [Trk("xd%d" % i) for i in range(NST)]
        t1 = [Trk("s1_%d" % i) for i in range(NST)]
        t2 = [Trk("s2_%d" % i) for i in range(NST)]
        otr = [Trk("od%d" % i) for i in range(NST)]
        mixer_a_phase(C, S, x, xtr, sc1, t1, mem, w["a_w_in"], w["a_b_igate"], w["a_b_fgate"], w["a_w_mem_kv"],
                      w["a_w_out"], w["ln1_g"][0], w["ln1_b"][0])
        ffn_phase(C, S, sc1, t1, sc2, t2, w["ffn_w_up"][0], w["ffn_w_down"][0], w["ln2_g"][0], w["ln2_b"][0], False)
        mixer_b_phase(C, S, sc2, t2, sc1, t1, mem, pos, w["kv_w_down"], w["kv_norm_g"], w["kv_w_uk"], w["kv_w_uv"],
                      w["b_w_in"], w["b_q_norm_g"], w["b_w_uq"], w["b_w_mem_kv"], w["b_w_out"], w["ln1_g"][1],
                      w["ln1_b"][1], cf2)
        ffn_phase(C, S, sc1, t1, out, otr, w["ffn_w_up"][1], w["ffn_w_down"][1], w["ln2_g"][1], w["ln2_b"][1], True)
        C.P.finish()
    return nc


def kernel(x, mem, positions, a_w_in, a_b_igate, a_b_fgate, a_w_mem_kv, a_w_out, kv_w_down, kv_norm_g, kv_w_uk,
           kv_w_uv, b_w_in, b_q_norm_g, b_w_uq, b_w_mem_kv, b_w_out, ln1_g, ln1_b, ffn_w_up, ffn_w_down, ln2_g,
           ln2_b):
    f32 = lambda a: np.ascontiguousarray(np.asarray(a), dtype=np.float32)
    shared = {
        "a_w_in": f32(a_w_in)[0], "a_b_igate": f32(a_b_igate)[0], "a_b_fgate": f32(a_b_fgate)[0],
        "a_w_mem_kv": f32(a_w_mem_kv)[0], "a_w_out": f32(a_w_out)[0], "kv_w_down": f32(kv_w_down),
        "kv_norm_g": f32(kv_norm_g), "kv_w_uk": f32(kv_w_uk), "kv_w_uv": f32(kv_w_uv), "b_w_in": f32(b_w_in)[0],
        "b_q_norm_g": f32(b_q_norm_g)[0], "b_w_uq": f32(b_w_uq)[0], "b_w_mem_kv": f32(b_w_mem_kv)[0],
        "b_w_out": f32(b_w_out)[0], "ln1_g": f32(ln1_g), "ln1_b": f32(ln1_b), "ffn_w_up": f32(ffn_w_up),
        "ffn_w_down": f32(ffn_w_down), "ln2_g": f32(ln2_g), "ln2_b": f32(ln2_b),
        "consts": _consts_np(), "cf2": _cf2_np(),
    }
    shared = {k: np.ascontiguousarray(v) for k, v in shared.items()}
    x = f32(x)
    mem = f32(mem)
    positions = np.ascontiguousarray(np.asarray(positions), dtype=np.int32)
    in_maps = []
    for c in range(NCORES):
        m = dict(shared)
        m["x"] = np.ascontiguousarray(x[c * NSEQ:(c + 1) * NSEQ].reshape(NTOK, D))
        m["mem"] = np.ascontiguousarray(mem[c * NSEQ:(c + 1) * NSEQ])
        m["positions"] = np.ascontiguousarray(positions[c * NSEQ:(c + 1) * NSEQ])
        in_maps.append(m)
    nc = build_program()
    res = run_bass_kernel_spmd(nc, in_maps, core_ids=list(range(NCORES)))
    outs = [np.asarray(r["out"], dtype=np.float32).reshape(NSEQ, SEQ, D) for r in res.results]
    return np.concatenate(outs, axis=0)
```

```python
import contextlib
import numpy as np
import concourse.bass as bass
import concourse.mybir as mybir
from concourse.bass_utils import run_bass_kernel_spmd

F32 = mybir.dt.float32
BF16 = mybir.dt.bfloat16
I32 = mybir.dt.int32
AF = mybir.ActivationFunctionType
ALU = mybir.AluOpType
AX = mybir.AxisListType

NCORES = 8
SEQ = 2048
D = 1024
DFF = 4096
NSEQ = 2
NTOK = NSEQ * SEQ
ST = 512
NST = NTOK // ST
ALPHA = 4.0 ** 0.25
LN_EPS = 1e-5
RMS_EPS = 1e-6


class Trk:
    __slots__ = ("name", "w", "r", "dsem", "dcnt")

    def __init__(self, name):
        self.name = name
        self.w = None
        self.r = {}
        self.dsem = None
        self.dcnt = 0


class Prog:
    SEM_ROT = 12000

    def __init__(self, nc):
        self.nc = nc
        self.eng = {"pe": nc.tensor, "act": nc.scalar, "dve": nc.vector, "pool": nc.gpsimd,
                    "sp": nc.sync}
        self.sem = {}
        self.cnt = {}
        self.seen = {e: {} for e in self.eng}
        self.nsem = 0
        for e in self.eng:
            self._new_sem(e)
        self.out_tokens = []
        self.ninstr = 0
        self.last_tok = {}
        self.rec = None
        self.inter = None
        self._acc = 0.0
        self._in_replay = False
        self.dma_toks = {}
        self.free_dsems = []
        self.phase_trks = []

    def _alloc_sem(self, name):
        self.nsem += 1
        return self.nc.alloc_semaphore(name="%s_%d" % (name, self.nsem))

    def _new_sem(self, e):
        self.sem[e] = self._alloc_sem("s_" + e)
        self.cnt[e] = 0

    def _need(self, e, tok, skip_same):
        if tok is None:
            return
        sem, c, te = tok
        if skip_same and te == e and e == "pe":
            return
        if self.seen[e].get(sem, 0) >= c:
            return
        self.eng[e].wait_ge(sem, c)
        self.seen[e][sem] = c

    def _signal(self, e, ins):
        if self.cnt[e] >= self.SEM_ROT:
            self._new_sem(e)
        self.cnt[e] += 1
        ins.then_inc(self.sem[e], 1)
        self.last_tok[e] = (self.sem[e], self.cnt[e], e)
        return self.last_tok[e]

    def full_barrier(self):
        snap = dict(self.last_tok)
        dts = list(self.dma_toks.values())
        for e in self.eng:
            for o, tok in snap.items():
                if o != e:
                    self._need(e, tok, False)
            for tok in dts:
                self._need(e, tok, False)
        for t in self.phase_trks:
            self.free_dsems.append((t.dsem, t.dcnt))
            t.dsem = None
        self.phase_trks = []
        self.dma_toks = {}

    def _after_emit(self):
        if self.inter is None or self._in_replay:
            return
        pend, rate = self.inter
        self._acc += rate
        while self._acc >= 1.0 and pend:
            self._acc -= 1.0
            self._in_replay = True
            self.replay(pend.pop(0))
            self._in_replay = False

    def replay(self, item):
        kind, args, kw = item
        saved, self.rec = self.rec, None
        getattr(self, kind)(*args, **kw)
        self.rec = saved

    def op(self, e, fn, reads=(), writes=()):
        if self.rec is not None:
            self.rec.append(("op", (e, fn), dict(reads=list(reads), writes=list(writes))))
            return None
        for t in reads:
            self._need(e, t.w, False)
        for t in writes:
            self._need(e, t.w, True)
            for tok in t.r.values():
                self._need(e, tok, True)
        ins = fn(self.eng[e])
        tok = self._signal(e, ins)
        for t in writes:
            t.w = tok
            t.r = {}
        for t in reads:
            t.r[e] = tok
        self.ninstr += 1
        self._after_emit()
        return tok

    def mm(self, out, pairs, reads=(), writes=()):
        if self.rec is not None:
            self.rec.append(("mm", (out, list(pairs)), dict(reads=list(reads), writes=list(writes))))
            return None
        e = "pe"
        for t in reads:
            self._need(e, t.w, False)
        for t in writes:
            self._need(e, t.w, True)
            for tok in t.r.values():
                self._need(e, tok, True)
        n = len(pairs)
        ins = None
        for i, (l, r) in enumerate(pairs):
            ins = self.nc.tensor.matmul(out, l, r, start=(i == 0), stop=(i == n - 1))
        tok = self._signal(e, ins)
        for t in writes:
            t.w = tok
            t.r = {}
        for t in reads:
            t.r[e] = tok
        self.ninstr += n
        self._after_emit()
        return tok

    def pe_multi(self, fns, reads=(), writes=()):
        if self.rec is not None:
            self.rec.append(("pe_multi", (list(fns),), dict(reads=list(reads), writes=list(writes))))
            return None
        e = "pe"
        for t in reads:
            self._need(e, t.w, False)
        for t in writes:
            self._need(e, t.w, True)
            for tok in t.r.values():
                self._need(e, tok, True)
        ins = None
        for f in fns:
            ins = f(self.nc.tensor)
        tok = self._signal(e, ins)
        for t in writes:
            t.w = tok
            t.r = {}
        for t in reads:
            t.r[e] = tok
        self.ninstr += len(fns)
        self._after_emit()
        return tok

    def dma(self, q, out, in_, reads=(), writes=(), is_output=False, sem_trk=None):
        if self.rec is not None:
            self.rec.append(("dma", (q, out, in_), dict(reads=list(reads), writes=list(writes),
                                                        is_output=is_output, sem_trk=sem_trk)))
            return None
        e = q
        for t in reads:
            self._need(e, t.w, False)
        for t in writes:
            self._need(e, t.w, False)
            for tok in t.r.values():
                self._need(e, tok, False)
        trk = sem_trk if sem_trk is not None else (list(writes) + list(reads))[0]
        if trk.dsem is None:
            if self.free_dsems:
                trk.dsem, trk.dcnt = self.free_dsems.pop()
            else:
                trk.dsem = self._alloc_sem("d")
                trk.dcnt = 0
            self.phase_trks.append(trk)
        trk.dcnt += 16
        self.eng[e].dma_start(out=out, in_=in_).then_inc(trk.dsem, 16)
        tok = (trk.dsem, trk.dcnt, "dma")
        self.dma_toks[trk.dsem] = tok
        for t in writes:
            t.w = tok
            t.r = {}
        for t in reads:
            t.r["dma_%s" % trk.name] = tok
        if is_output:
            self.out_tokens.append(tok)
        self.ninstr += 1
        return tok

    def barrier_all(self, trks):
        for t in trks:
            self._need("sp", t.w, False)
            for tok in t.r.values():
                self._need("sp", tok, False)

    def finish(self):
        for tok in self.out_tokens:
            self._need("sp", tok, False)


def list_schedule(items):
    n = len(items)
    eng, dur, deps = [], [], [[] for _ in range(n)]
    last_w, readers = {}, {}
    for i, (kind, args, kw) in enumerate(items):
        if kind == "op":
            e, d = args[0], 0.6
        elif kind == "mm":
            e, d = "pe", 0.25 * len(args[1]) + 0.1
        elif kind == "pe_multi":
            e, d = "pe", 0.15 * len(args[0]) + 0.1
        else:
            e, d = "q_" + args[0], 0.1
        eng.append(e)
        dur.append(d)
        rd, wr = kw.get("reads", ()), kw.get("writes", ())
        ds = set()
        for t in rd:
            if id(t) in last_w:
                ds.add(last_w[id(t)])
        for t in wr:
            if id(t) in last_w:
                ds.add(last_w[id(t)])
            for j in readers.get(id(t), ()):
                ds.add(j)
        ds.discard(i)
        deps[i] = sorted(ds)
        for t in wr:
            last_w[id(t)] = i
            readers[id(t)] = []
        for t in rd:
            readers.setdefault(id(t), []).append(i)
    nsucc_wait = [len(d) for d in deps]
    succ = [[] for _ in range(n)]
    for i in range(n):
        for j in deps[i]:
            succ[j].append(i)
    finish = [0.0] * n
    ready_t = [0.0] * n
    efree = {}
    ready = [i for i in range(n) if nsucc_wait[i] == 0]
    order = []
    while ready:
        best, best_key = None, None
        for i in ready:
            st = max(efree.get(eng[i], 0.0), ready_t[i])
            key = (st, i)
            if best_key is None or key < best_key:
                best, best_key = i, key
        i = best
        ready.remove(i)
        st = best_key[0]
        lat = 2.5 if eng[i].startswith("q_") else 0.3
        finish[i] = st + dur[i]
        efree[eng[i]] = finish[i]
        order.append(i)
        for k in succ[i]:
            ready_t[k] = max(ready_t[k], finish[i] + lat)
            nsucc_wait[k] -= 1
            if nsucc_wait[k] == 0:
                ready.append(k)
    assert len(order) == n
    return [items[i] for i in order]


class Tile:
    def __init__(self, t, name):
        self.t = t
        self.k = Trk(name)

    def __getitem__(self, idx):
        return self.t[idx]


class Ctx:
    def __init__(self, nc):
        self.nc = nc
        self.P = Prog(nc)
        self.es = contextlib.ExitStack()
        self.nid = 0

    def sb(self, name, shape, dt, es=None):
        self.nid += 1
        nm = "%s_%d" % (name, self.nid)
        t = (es or self.es).enter_context(self.nc.sbuf_tensor(nm, list(shape), dt))
        return Tile(t, nm)

    def ps(self, name, shape, dt, es=None):
        self.nid += 1
        nm = "%s_%d" % (name, self.nid)
        t = (es or self.es).enter_context(self.nc.psum_tensor(nm, list(shape), dt))
        return Tile(t, nm)


def load_weight_cast(C, wt, dram_view, nsplit, axis):
    P = C.P
    n = wt.t.shape[axis]
    step = n // nsplit
    trks = []
    for j in range(nsplit):
        sl = [slice(None)] * 3
        sl[axis] = slice(j * step, (j + 1) * step)
        sl = tuple(sl)
        k = Trk("%s_p%d" % (wt.k.name, j))
        P.dma("pool", wt.t[sl], dram_view[sl], writes=[k])
        trks.append(k)
    return trks


def layer_norm_tile(C, S, y, g_rep, b_rep, out):
    P = C.P
    st, mv, rstd, nmr = S["ln_st"], S["ln_mv"], S["ln_rstd"], S["ln_nmr"]
    xn = y
    for hh in range(2):
        P.op("dve", lambda e, hh=hh: e.bn_stats(out=st[:, hh, :], in_=y[:, hh * 512:(hh + 1) * 512]),
             reads=[y.k], writes=[st.k])
    P.op("dve", lambda e: e.bn_aggr(out=mv[:], in_=st[:]), reads=[st.k], writes=[mv.k])
    P.op("act", lambda e: e.activation(out=rstd[:], in_=mv[:, 1:2], func=AF.Ln, bias=S["eps_ln"][:], scale=1.0),
         reads=[mv.k, S["eps_ln"].k], writes=[rstd.k])
    P.op("act", lambda e: e.activation(out=rstd[:], in_=rstd[:], func=AF.Exp, scale=-0.5),
         reads=[rstd.k], writes=[rstd.k])
    P.op("dve", lambda e: e.tensor_scalar(out=nmr[:], in0=mv[:, 0:1], scalar1=-1.0, scalar2=None, op0=ALU.mult),
         reads=[mv.k], writes=[nmr.k])
    P.op("dve", lambda e: e.scalar_tensor_tensor(out=xn[:], in0=y[:], scalar=nmr[:, 0:1], in1=g_rep[:],
                                                 op0=ALU.add, op1=ALU.mult),
         reads=[y.k, nmr.k, g_rep.k], writes=[xn.k])
    P.op("dve", lambda e: e.scalar_tensor_tensor(out=out[:], in0=xn[:], scalar=rstd[:, 0:1], in1=b_rep[:],
                                                 op0=ALU.mult, op1=ALU.add),
         reads=[xn.k, rstd.k, b_rep.k], writes=[out.k])


def transpose_tokens(C, S, xin, xT, col0):
    P = C.P
    xb = S["xb"][S["xb_i"] % len(S["xb"])]
    S["xb_i"] += 1
    P.op(S.get("cast_eng", "act"), (lambda e: e.tensor_copy(out=xb[:], in_=xin[:])) if S.get("cast_eng") else
         (lambda e: e.activation(out=xb[:], in_=xin[:], func=AF.Copy)), reads=[xin.k], writes=[xb.k])
    pt = S["pst"][S["pst_i"] % 2]
    S["pst_i"] += 1
    ident = S["ident"]
    P.pe_multi([lambda e, c=c: e.transpose(out=pt[:, c * 128:(c + 1) * 128], in_=xb[:, c * 128:(c + 1) * 128],
                                           identity=ident[:]) for c in range(8)],
               reads=[xb.k, ident.k], writes=[pt.k])
    P.op("dve", lambda e: e.tensor_copy(out=xT[:, :, col0:col0 + 128],
                                        in_=pt[:, :].rearrange("p (c t) -> p c t", c=8)),
         reads=[pt.k], writes=[xT.k])


def ffn_phase(C, S, x_dram, x_trks, out_dram, out_trks, w_up_d, w_down_d, g_d, b_d, is_output):
    nc, P = C.nc, C.P
    with contextlib.ExitStack() as es:
        wup = C.sb("wup", [128, 8, DFF], BF16, es)
        wdn = C.sb("wdn", [128, 32, D], BF16, es)
        g_rep = C.sb("g_rep", [128, D], F32, es)
        b_rep = C.sb("b_rep", [128, D], F32, es)
        xld_p = C.sb("xld_p", [128, D], F32, es)
        xld_r = C.sb("xld_r", [128, D], F32, es)
        xT2 = [C.sb("xT", [128, 8, ST], BF16, es) for _ in range(2)]
        hT = C.sb("hT", [128, 32, ST], BF16, es)
        rl = [C.sb("rl", [128, ST], BF16, es) for _ in range(2)]
        y = [C.sb("y", [128, D], F32, es) for _ in range(2)]

        P.dma("sp", g_rep[:], g_d.partition_broadcast(128), writes=[g_rep.k])
        P.dma("sp", b_rep[:], b_d.partition_broadcast(128), writes=[b_rep.k])
        x_view = x_dram.rearrange("(s a p) d -> s a p d", a=4, p=128)
        o_view = out_dram.rearrange("(s a p) d -> s a p d", a=4, p=128)
        up_tr = load_weight_cast(C, wup, w_up_d.rearrange("(c p) f -> p c f", p=128), 8, 2)
        dn_tr = load_weight_cast(C, wdn, w_down_d.rearrange("(c p) f -> p c f", p=128), 8, 1)

        psb = S["psf"]
        nb = 0

        def prep(s):
            for a in range(4):
                P.dma("sp", xld_p[:], x_view[s, a], reads=[x_trks[s]], writes=[xld_p.k])
                transpose_tokens(C, S, xld_p, xT2[s % 2], a * 128)

        prep(0)
        for s in range(NST):
            xT = xT2[s % 2]
            pending = []
            if s + 1 < NST:
                P.rec = pending
                prep(s + 1)
                P.rec = None
            P.inter = (pending, len(pending) / 90.0 + 0.01)
            P._acc = 0.0
            for fc in range(32):
                pb = psb[nb % 4]
                nb += 1
                P.mm(pb[:], [(wup[:, kc, fc * 128:(fc + 1) * 128], xT[:, kc, :]) for kc in range(8)],
                     reads=[xT.k, up_tr[fc // 4]], writes=[pb.k])
                r = rl[fc % 2]
                P.op("act", lambda e, r=r, pb=pb: e.activation(out=r[:], in_=pb[:], func=AF.Relu),
                     reads=[pb.k], writes=[r.k])
                P.op("dve", lambda e, r=r, fc=fc: e.tensor_tensor(out=hT[:, fc, :], in0=r[:], in1=r[:],
                                                                   op=ALU.mult),
                     reads=[r.k], writes=[hT.k])
            P.inter = None
            while pending:
                P.replay(pending.pop(0))
            for a in range(4):
                xi = xld_r
                P.dma("sp", xi[:], x_view[s, a], reads=[x_trks[s]], writes=[xi.k])
                yy = y[a % 2]
                for dh in range(2):
                    pb = psb[nb % 4]
                    nb += 1
                    P.mm(pb[:], [(hT[:, fc, a * 128:(a + 1) * 128], wdn[:, fc, dh * 512:(dh + 1) * 512])
                                 for fc in range(32)],
                         reads=[hT.k] + dn_tr, writes=[pb.k])
                    P.op("dve", lambda e, yy=yy, pb=pb, dh=dh, xi=xi: e.scalar_tensor_tensor(
                        out=yy[:, dh * 512:(dh + 1) * 512], in0=xi[:, dh * 512:(dh + 1) * 512],
                        scalar=ALPHA, in1=pb[:], op0=ALU.mult, op1=ALU.add),
                         reads=[xi.k, pb.k], writes=[yy.k])
                layer_norm_tile(C, S, yy, g_rep, b_rep, yy)
                P.dma("pool", o_view[s, a], yy[:], reads=[yy.k], writes=[out_trks[s]],
                      is_output=is_output, sem_trk=yy.k)
        P.full_barrier()


def alloc_shared(C):
    S = {}
    S["ident"] = C.sb("ident", [128, 128], BF16)
    S["psf"] = [C.ps("psf", [128, 512], F32) for _ in range(6)]
    S["pst"] = [C.ps("pst", [128, 1024], BF16) for _ in range(2)]
    S["pst_i"] = 0
    S["nb"] = 0
    S["nrot"] = 6
    S["rot0"] = 0
    S["xb"] = [C.sb("xb", [128, D], BF16) for _ in range(1)]
    S["xb_i"] = 0
    S["ln_st"] = C.sb("ln_st", [128, 2, 6], F32)
    S["ln_mv"] = C.sb("ln_mv", [128, 2], F32)
    S["ln_rstd"] = C.sb("ln_rstd", [128, 1], F32)
    S["ln_nmr"] = C.sb("ln_nmr", [128, 1], F32)
    S["eps_ln"] = C.sb("eps_ln", [128, 1], F32)
    S["eps_rms"] = C.sb("eps_rms", [128, 1], F32)
    C.P.op("pool", lambda e: e.memset(S["eps_ln"][:], LN_EPS), writes=[S["eps_ln"].k])
    C.P.op("pool", lambda e: e.memset(S["eps_rms"][:], RMS_EPS), writes=[S["eps_rms"].k])
    return S


def nextbank(S):
    b = S["psf"][S["rot0"] + S["nb"] % S["nrot"]]
    S["nb"] += 1
    return b


def v3(ap, n):
    return ap.rearrange("p (h d) -> p h d", h=n)


def load_consts(C, S, consts_d):
    P = C.P
    S["cf"] = C.sb("cf", [128, 4, 128], F32)
    S["maskb"] = C.sb("maskb", [128, 128], BF16)
    P.dma("sp", S["cf"][:], consts_d, writes=[S["cf"].k])
    P.dma("pool", S["ident"][:], consts_d[:, 0, :], writes=[S["ident"].k])
    P.dma("pool", S["maskb"][:], consts_d[:, 1, :], writes=[S["maskb"].k])
    S["one1"] = C.sb("one1", [128, 1], F32)
    S["ln8"] = C.sb("ln8", [128, 1], F32)
    P.op("pool", lambda e: e.memset(S["one1"][:], 1.0), writes=[S["one1"].k])
    P.op("pool", lambda e: e.memset(S["ln8"][:], float(np.log(0.125))), writes=[S["ln8"].k])


def mem_kv_precompute(C, S, es, mem_d, wmk, wmk_tr, xld, KmT, Vm):
    P = C.P
    memT = C.sb("memT", [128, 8, 256], BF16, es)
    P.op("pool", lambda e: e.memset(Vm[:], 1.0), writes=[Vm.k])
    n = 0
    for q in range(NSEQ):
        for mt in range(2):
            xi = xld[n % len(xld)]
            n += 1
            P.dma("sp", xi[:], mem_d[q, mt * 128:(mt + 1) * 128, :], writes=[xi.k])
            transpose_tokens(C, S, xi, memT, mt * 128)
        for h in range(4):
            pb = nextbank(S)
            P.mm(pb[:, 0:256], [(wmk[:, kc, h * 128:(h + 1) * 128], memT[:, kc, :]) for kc in range(8)],
                 reads=[memT.k] + wmk_tr, writes=[pb.k])
            P.op("act", lambda e, pb=pb, q=q, h=h: e.activation(out=KmT[:, q, h, :], in_=pb[:, 0:256], func=AF.Copy),
                 reads=[pb.k], writes=[KmT.k])
        for mt in range(2):
            pb = nextbank(S)
            P.mm(pb[:], [(memT[:, kc, mt * 128:(mt + 1) * 128], wmk[:, kc, 512:1024]) for kc in range(8)],
                 reads=[memT.k] + wmk_tr, writes=[pb.k])
            P.op("act", lambda e, pb=pb, q=q, mt=mt: e.activation(out=Vm[:, q, mt, :, 0:128], in_=v3(pb[:], 4),
                                                                  func=AF.Copy),
                 reads=[pb.k], writes=[Vm.k])
    return memT


def mem_attention(C, S, seq, qmT, KmT, Vm, hcat, col0):
    P = C.P
    for h in range(4):
        pts = []
        for mt in range(2):
            pb = nextbank(S)
            P.mm(pb[:], [(KmT[:, seq, h, mt * 128:(mt + 1) * 128], qmT[:, h, :])],
                 reads=[KmT.k, qmT.k], writes=[pb.k])
            ptl = S.get("PTm") or S["PT"]
            pt = ptl[S["pt_i"] % len(ptl)]
            S["pt_i"] += 1
            P.op("act", lambda e, pt=pt, pb=pb: e.activation(out=pt[:], in_=pb[:], func=AF.Exp, scale=128.0 ** -0.5),
                 reads=[pb.k], writes=[pt.k])
            pts.append(pt)
        for a in range(4):
            pb = nextbank(S)
            P.mm(pb[:, 0:129], [(pts[mt][:, a * 128:(a + 1) * 128], Vm[:, seq, mt, h, :]) for mt in range(2)],
                 reads=[pts[0].k, pts[1].k, Vm.k], writes=[pb.k])
            rc = S["rc"][S["rc_i"] % 2]
            S["rc_i"] += 1
            P.op("dve", lambda e, rc=rc, pb=pb: e.reciprocal(out=rc[:], in_=pb[:, 128:129]),
                 reads=[pb.k], writes=[rc.k])
            P.op("dve", lambda e, rc=rc, pb=pb, a=a, h=h: e.tensor_scalar(
                out=hcat[a][:, col0 + h * 128:col0 + (h + 1) * 128], in0=pb[:, 0:128], scalar1=rc[:, 0:1],
                scalar2=None, op0=ALU.mult), reads=[pb.k, rc.k], writes=[hcat[a].k])


def out_proj_tr(C, S, hcat, hcT):
    P = C.P
    for a in range(4):
        pt = S["pst"][S["pst_i"] % 2]
        S["pst_i"] += 1
        ident = S["ident"]
        P.pe_multi([lambda e, c=c, pt=pt, a=a: e.transpose(out=pt[:, c * 128:(c + 1) * 128],
                                                         in_=hcat[a][:, c * 128:(c + 1) * 128],
                                                         identity=ident[:]) for c in range(8)],
                   reads=[hcat[a].k, ident.k], writes=[pt.k])
        P.op("dve", lambda e, pt=pt, a=a: e.tensor_copy(out=hcT[:, :, a * 128:(a + 1) * 128], in_=v3(pt[:, :], 8)),
             reads=[pt.k], writes=[hcT.k])


def out_proj_mm_ln(C, S, hcT, wout, wout_tr, xld, nl, x_view, x_trk, s, y, g_rep, b_rep, o_view, o_trk):
    P = C.P
    for a in range(4):
        xi = xld[nl[0] % len(xld)]
        nl[0] += 1
        P.dma("sp", xi[:], x_view[s, a], reads=[x_trk], writes=[xi.k])
        yy = y[a % 2]
        for dh in range(2):
            pb = nextbank(S)
            P.mm(pb[:], [(hcT[:, kc, a * 128:(a + 1) * 128], wout[:, kc, dh * 512:(dh + 1) * 512])
                         for kc in range(8)], reads=[hcT.k] + wout_tr, writes=[pb.k])
            P.op("dve", lambda e, yy=yy, pb=pb, dh=dh, xi=xi: e.scalar_tensor_tensor(
                out=yy[:, dh * 512:(dh + 1) * 512], in0=xi[:, dh * 512:(dh + 1) * 512],
                scalar=ALPHA, in1=pb[:], op0=ALU.mult, op1=ALU.add),
                 reads=[xi.k, pb.k], writes=[yy.k])
        layer_norm_tile(C, S, yy, g_rep, b_rep, yy)
        P.dma("pool", o_view[s, a], yy[:], reads=[yy.k], writes=[o_trk], sem_trk=yy.k)


def out_proj_ln(C, S, hcat, hcT, wout, wout_tr, xld, nl, x_view, x_trk, s, y, g_rep, b_rep, o_view, o_trk):
    out_proj_tr(C, S, hcat, hcT)
    out_proj_mm_ln(C, S, hcT, wout, wout_tr, xld, nl, x_view, x_trk, s, y, g_rep, b_rep, o_view, o_trk)


def mixer_a_phase(C, S, x_dram, x_trks, out_dram, out_trks, mem_d, w_in_d, bi_d, bf_d, wmk_d, w_out_d, g_d, b_d):
    nc, P = C.nc, C.P
    with contextlib.ExitStack() as es:
        win = C.sb("win", [128, 8, 2056], BF16, es)
        wout = C.sb("wout", [128, 8, D], BF16, es)
        g_rep = C.sb("g_rep", [128, D], F32, es)
        b_rep = C.sb("b_rep", [128, D], F32, es)
        bias_rep = C.sb("bias_rep", [128, 8], F32, es)
        xld = [C.sb("xld", [128, D], F32, es) for _ in range(3)]
        KmT = C.sb("KmT", [128, NSEQ, 4, 256], BF16, es)
        Vm = C.sb("Vm", [128, NSEQ, 2, 4, 129], BF16, es)
        P.dma("sp", g_rep[:], g_d.partition_broadcast(128), writes=[g_rep.k])
        P.dma("sp", b_rep[:], b_d.partition_broadcast(128), writes=[b_rep.k])
        P.dma("sp", bias_rep[:, 0:4], bi_d.partition_broadcast(128), writes=[bias_rep.k])
        P.dma("sp", bias_rep[:, 4:8], bf_d.partition_broadcast(128), writes=[bias_rep.k])
        with contextlib.ExitStack() as es2:
            wmk = C.sb("wmk", [128, 8, D], BF16, es2)
            wmk_tr = load_weight_cast(C, wmk, wmk_d.rearrange("(c p) f -> p c f", p=128), 2, 1)
            win_view = w_in_d.rearrange("(c p) f -> p c f", p=128)
            win_g = []
            for (c0, c1) in ((0, 512), (1536, 2056), (512, 1024), (1024, 1536)):
                k = Trk("win_%d" % c0)
                P.dma("pool", win[:, :, c0:c1], win_view[:, :, c0:c1], writes=[k])
                win_g.append(k)
            wqk_tr, wgm_tr, wv_tr, wo_tr = [win_g[0]], [win_g[1]], [win_g[2]], [win_g[3]]
            wout_tr = []
            mem_kv_precompute(C, S, es2, mem_d, wmk, wmk_tr, xld, KmT, Vm)
            P.full_barrier()
        xT = [C.sb("xT", [128, 8, ST], BF16, es) for _ in range(2)]
        qT = [C.sb("qT", [64, 4, ST], BF16, es) for _ in range(2)]
        kT = [C.sb("kT", [64, 4, ST], BF16, es) for _ in range(2)]
        qz = [C.sb("qz", [64, 4, 4, 2, 128], BF16, es) for _ in range(2)]
        qmT = [C.sb("qmT", [128, 4, ST], BF16, es) for _ in range(2)]
        gts = C.sb("gts", [128, 8], F32, es)
        lfn = C.sb("lfn", [128, 4], F32, es)
        tadd = C.sb("tadd", [128, 4], F32, es)
        colf = C.sb("colf", [128, 4], F32, es)
        enb = C.sb("enb", [128, 4], F32, es)
        eg = [C.sb("eg", [128, 2, 4], F32, es) for _ in range(2)]
        kc_t = C.sb("kc", [128, 4, 64], BF16, es)
        vaug = [C.sb("vaug", [128, 4, 129], BF16, es) for _ in range(2)]
        e_o = C.sb("e_o", [128, 512], F32, es)
        atmp = C.sb("atmp", [128, 4, 128], BF16, es)
        AT = C.sb("AT", [128, 4, 128], BF16, es)
        Sst = C.sb("Sst", [64, 4, 129], F32, es)
        Cf = C.sb("Cf", [64, 4, 129], F32, es)
        Cb = [C.sb("Cb", [64, 4, 129], BF16, es) for _ in range(2)]
        den = C.sb("den", [128, 2, 1], F32, es)
        hcat2 = [[C.sb("hcat", [128, D], BF16, es) for _ in range(4)] for _ in range(2)]
        hcT = C.sb("hcT", [128, 8, ST], BF16, es)
        y = [C.sb("y", [128, D], F32, es) for _ in range(2)]
        S["PT"] = [C.sb("PT", [128, ST], BF16, es) for _ in range(4)]
        S["pt_i"] = 0
        S["rc"] = [C.sb("rc", [128, 1], F32, es) for _ in range(2)]
        S["rc_i"] = 0
        for qq in qz:
            P.op("pool", lambda e, qq=qq: e.memset(qq[:], 0.0), writes=[qq.k])
        for vv in vaug:
            P.op("pool", lambda e, vv=vv: e.memset(vv[:], 1.0), writes=[vv.k])

        x_view = x_dram.rearrange("(s a p) d -> s a p d", a=4, p=128)
        o_view = out_dram.rearrange("(s a p) d -> s a p d", a=4, p=128)
        cf = S["cf"]
        maskb = S["maskb"]
        nl = [0]
        NT = NST // NSEQ

        def front(s):
            b, seq = s % 2, s // NT
            xTb, qTb, kTb, qzb, qmTb = xT[b], qT[b], kT[b], qz[b], qmT[b]
            for a in range(4):
                xi = xld[nl[0] % len(xld)]
                nl[0] += 1
                P.dma("sp", xi[:], x_view[s, a], reads=[x_trks[s]], writes=[xi.k])
                transpose_tokens(C, S, xi, xTb, a * 128)
            for h in range(4):
                for (dst, c0) in ((qTb, 0), (kTb, 256)):
                    pb = nextbank(S)
                    P.mm(pb[0:64, :], [(win[:, kc, c0 + h * 64:c0 + (h + 1) * 64], xTb[:, kc, :]) for kc in range(8)],
                         reads=[xTb.k] + wqk_tr, writes=[pb.k])
                    P.op("act", lambda e, pb=pb, dst=dst, h=h: e.activation(out=dst[:, h, :], in_=pb[0:64, :],
                                                                            func=AF.Copy),
                         reads=[pb.k], writes=[dst.k])
                P.op("pool", lambda e, h=h: e.tensor_copy(
                    out=bass.AP(qzb.t, h * 1024, [[4096, 64], [256, 4], [192, 2], [1, 64]]),
                    in_=qTb[:, h, :].rearrange("p (a c j) -> p a c j", a=4, c=2)),
                     reads=[qTb.k], writes=[qzb.k])
                pb = nextbank(S)
                P.mm(pb[:], [(win[:, kc, 1544 + h * 128:1544 + (h + 1) * 128], xTb[:, kc, :]) for kc in range(8)],
                     reads=[xTb.k] + wgm_tr, writes=[pb.k])
                P.op("act", lambda e, pb=pb, h=h: e.activation(out=qmTb[:, h, :], in_=pb[:], func=AF.Copy),
                     reads=[pb.k], writes=[qmTb.k])
            mem_attention(C, S, seq, qmTb, KmT, Vm, hcat2[b], 512)

        def tail(s):
            out_proj_ln(C, S, hcat2[s % 2], hcT, wout, wout_tr, xld, nl, x_view, x_trks[s], s, y, g_rep, b_rep,
                        o_view, out_trks[s])

        def tok_loop(s):
            b = s % 2
            xTb, qTb, kTb, qzb, hcat = xT[b], qT[b], kT[b], qz[b], hcat2[b]
            for a in range(4):
                t = s * 4 + a
                par = t % 2
                first = (t % (SEQ // 128) == 0)
                cols = slice(a * 128, (a + 1) * 128)
                va = vaug[par]
                pg = nextbank(S)
                P.mm(pg[:, 0:8], [(xTb[:, kc, cols], win[:, kc, 1536:1544]) for kc in range(8)],
                     reads=[xTb.k] + wgm_tr, writes=[pg.k])
                P.op("dve", lambda e, pg=pg: e.tensor_tensor(out=gts[:], in0=pg[:, 0:8], in1=bias_rep[:], op=ALU.add),
                     reads=[pg.k, bias_rep.k], writes=[gts.k])
                P.op("act", lambda e: e.activation(out=lfn[:], in_=gts[:, 4:8], func=AF.Exp, scale=-1.0),
                     reads=[gts.k], writes=[lfn.k])
                P.op("act", lambda e: e.activation(out=lfn[:], in_=lfn[:], func=AF.Ln, bias=S["one1"][:], scale=1.0),
                     reads=[lfn.k, S["one1"].k], writes=[lfn.k])
                pc = nextbank(S)
                P.mm(pc[:, 0:4], [(cf[:, 1, :], lfn[:])], reads=[cf.k, lfn.k], writes=[pc.k])
                P.mm(pc[:, 4:8], [(cf[:, 2, :], lfn[:])], reads=[cf.k, lfn.k], writes=[pc.k])
                P.mm(pc[:, 8:12], [(cf[:, 3, :], lfn[:])], reads=[cf.k, lfn.k], writes=[pc.k])
                P.op("dve", lambda e, pc=pc: e.tensor_tensor(out=tadd[:], in0=gts[:, 0:4], in1=pc[:, 0:4], op=ALU.add),
                     reads=[gts.k, pc.k], writes=[tadd.k])
                P.op("act", lambda e: e.activation(out=colf[:], in_=tadd[:], func=AF.Exp, bias=S["ln8"][:], scale=1.0),
                     reads=[tadd.k, S["ln8"].k], writes=[colf.k])
                P.op("act", lambda e, pc=pc: e.activation(out=enb[:], in_=pc[:, 0:4], func=AF.Exp),
                     reads=[pc.k], writes=[enb.k])
                P.op("act", lambda e, pc=pc, par=par: e.activation(out=eg[par][:], in_=v3(pc[:, 4:12], 2),
                                                                   func=AF.Exp, scale=-1.0),
                     reads=[pc.k], writes=[eg[par].k])
                pk = nextbank(S)
                P.mm(pk[:, 0:256], [(xTb[:, kc, cols], win[:, kc, 256:512]) for kc in range(8)],
                     reads=[xTb.k] + wqk_tr, writes=[pk.k])
                P.op("dve", lambda e, pk=pk: e.tensor_tensor(
                    out=kc_t[:], in0=v3(pk[:, 0:256], 4), in1=colf[:, 0:4].unsqueeze(2).broadcast_to([128, 4, 64]),
                    op=ALU.mult), reads=[pk.k, colf.k], writes=[kc_t.k])
                pv = nextbank(S)
                P.mm(pv[:], [(xTb[:, kc, cols], win[:, kc, 512:1024]) for kc in range(8)],
                     reads=[xTb.k] + wv_tr, writes=[pv.k])
                P.op("act", lambda e, pv=pv, va=va: e.activation(out=va[:, :, 0:128], in_=v3(pv[:], 4), func=AF.Copy),
                     reads=[pv.k], writes=[va.k])
                po = nextbank(S)
                P.mm(po[:], [(xTb[:, kc, cols], win[:, kc, 1024:1536]) for kc in range(8)],
                     reads=[xTb.k] + wo_tr, writes=[po.k])
                P.op("act", lambda e, po=po: e.activation(out=e_o[:], in_=po[:], func=AF.Exp, scale=-1.0),
                     reads=[po.k], writes=[e_o.k])
                P.op("act", lambda e: e.activation(out=e_o[:], in_=e_o[:], func=AF.Ln, bias=S["one1"][:], scale=1.0),
                     reads=[e_o.k, S["one1"].k], writes=[e_o.k])
                P.op("act", lambda e: e.activation(out=e_o[:], in_=e_o[:], func=AF.Exp, scale=-1.0),
                     reads=[e_o.k], writes=[e_o.k])
                pa = nextbank(S)
                for h in range(4):
                    P.mm(pa[:, h * 128:(h + 1) * 128], [(kTb[:, h, cols], qTb[:, h, cols])],
                         reads=[kTb.k, qTb.k], writes=[pa.k])
                P.op("dve", lambda e, pa=pa: e.tensor_tensor(
                    out=atmp[:], in0=v3(pa[:], 4), in1=colf[:, 0:4].unsqueeze(2).broadcast_to([128, 4, 128]),
                    op=ALU.mult), reads=[pa.k, colf.k], writes=[atmp.k])
                P.op("pool", lambda e: e.tensor_tensor(
                    out=AT[:], in0=atmp[:], in1=maskb[:, :].unsqueeze(1).broadcast_to([128, 4, 128]), op=ALU.mult),
                     reads=[atmp.k, maskb.k], writes=[AT.k])
                for c in range(2):
                    if first and c == 0:
                        P.op("dve", lambda e: e.memset(Cf[:], 0.0), writes=[Cf.k])
                        P.op("pool", lambda e: e.memset(Cb[0][:], 0.0), writes=[Cb[0].k])
                    else:
                        egp = eg[par][0:64, 0, :] if c == 1 else eg[1 - par][0:64, 1, :]
                        egk = eg[par].k if c == 1 else eg[1 - par].k
                        P.op("dve", lambda e, egp=egp: e.tensor_tensor(
                            out=Cf[:], in0=Sst[:], in1=egp.unsqueeze(2).broadcast_to([64, 4, 129]), op=ALU.mult),
                             reads=[Sst.k, egk], writes=[Cf.k])
                        P.op("act", lambda e, c=c: e.activation(out=Cb[c][:], in_=Cf[:], func=AF.Copy),
                             reads=[Cf.k], writes=[Cb[c].k])
                    rows = slice(c * 64, (c + 1) * 64)
                    for hp in range(2):
                        pu = nextbank(S)
                        for j in range(2):
                            h = 2 * hp + j
                            P.mm(pu[0:64, j * 129:(j + 1) * 129], [(kc_t[rows, h, :], va[rows, h, :])],
                                 reads=[kc_t.k, va.k], writes=[pu.k])
                        P.op("dve", lambda e, pu=pu, hp=hp: e.tensor_tensor(
                            out=Sst[:, 2 * hp:2 * hp + 2, :], in0=Cf[:, 2 * hp:2 * hp + 2, :],
                            in1=v3(pu[0:64, 0:258], 2), op=ALU.add),
                             reads=[Cf.k, pu.k], writes=[Sst.k])
                for hp in range(2):
                    pn = nextbank(S)
                    for j in range(2):
                        h = 2 * hp + j
                        P.mm(pn[:, j * 129:(j + 1) * 129],
                             [(AT[:, h, :], va[:, h, :]),
                              (qzb[:, h, a, 0, :], Cb[0][:, h, :]),
                              (qzb[:, h, a, 1, :], Cb[1][:, h, :])],
                             reads=[AT.k, va.k, qzb.k, Cb[0].k, Cb[1].k], writes=[pn.k])
                    pn3 = v3(pn[:, 0:258], 2)
                    P.op("dve", lambda e, pn3=pn3, hp=hp: e.tensor_tensor(
                        out=den[:], in0=pn3[:, :, 128:129], in1=enb[:, 2 * hp:2 * hp + 2].unsqueeze(2),
                        op=ALU.max), reads=[pn.k, enb.k], writes=[den.k])
                    P.op("dve", lambda e, pn3=pn3: e.scalar_tensor_tensor(
                        out=den[:], in0=pn3[:, :, 128:129], scalar=-1.0, in1=den[:], op0=ALU.mult, op1=ALU.max),
                         reads=[pn.k, den.k], writes=[den.k])
                    P.op("dve", lambda e: e.reciprocal(out=den[:], in_=den[:]), reads=[den.k], writes=[den.k])
                    for j in range(2):
                        h = 2 * hp + j
                        P.op("dve", lambda e, pn=pn, j=j, h=h, a=a: e.scalar_tensor_tensor(
                            out=hcat[a][:, h * 128:(h + 1) * 128], in0=pn[:, j * 129:j * 129 + 128],
                            scalar=den[:, j, :], in1=e_o[:, h * 128:(h + 1) * 128], op0=ALU.mult, op1=ALU.mult),
                             reads=[pn.k, den.k, e_o.k], writes=[hcat[a].k])

        def side(fn, *a):
            S["rot0"], S["nrot"] = 4, 2
            fn(*a)
            S["rot0"], S["nrot"] = 0, 4

        side(front, 0)
        wout_tr.extend(load_weight_cast(C, wout, w_out_d.rearrange("(c p) f -> p c f", p=128), 2, 1))
        for s in range(NST):
            pending = []
            P.rec = pending
            if s > 0:
                side(tail, s - 1)
            if s + 1 < NST:
                side(front, s + 1)
            P.rec = None
            S["rot0"], S["nrot"] = 0, 4
            P.inter = (pending, len(pending) / 260.0 + 0.02)
            P._acc = 0.0
            tok_loop(s)
            P.inter = None
            while pending:
                P.replay(pending.pop(0))
        side(tail, NST - 1)
        P.full_barrier()
        S["rot0"], S["nrot"] = 0, 6


def rms_rows(C, S, pb, ncols, out_bf, scr):
    P = C.P
    ss, rs = S["rms_ss"], S["rms_rs"]
    if scr is None:
        scr = nextbank(S)
    P.op("dve", lambda e: e.memset(ss[:], 0.0), writes=[ss.k])
    P.op("act", lambda e: e.activation(out=scr[:, 0:ncols], in_=pb[:, 0:ncols], func=AF.Square, accum_out=ss[:]),
         reads=[pb.k], writes=[scr.k, ss.k])
    P.op("act", lambda e: e.activation(out=rs[:], in_=ss[:], func=AF.Ln, bias=S["eps_rms"][:], scale=1.0 / ncols),
         reads=[ss.k, S["eps_rms"].k], writes=[rs.k])
    P.op("act", lambda e: e.activation(out=rs[:], in_=rs[:], func=AF.Exp, scale=-0.5), reads=[rs.k], writes=[rs.k])
    P.op("dve", lambda e: e.tensor_scalar(out=out_bf[:, 0:ncols], in0=pb[:, 0:ncols], scalar1=rs[:, 0:1],
                                          scalar2=None, op0=ALU.mult), reads=[pb.k, rs.k], writes=[out_bf.k])


def transpose_cols(C, S, src_bf, nchunk, dstT, col0):
    P = C.P
    pt = S["pst"][S["pst_i"] % 2]
    S["pst_i"] += 1
    ident = S["ident"]
    P.pe_multi([lambda e, c=c: e.transpose(out=pt[:, c * 128:(c + 1) * 128], in_=src_bf[:, c * 128:(c + 1) * 128],
                                           identity=ident[:]) for c in range(nchunk)],
               reads=[src_bf.k, ident.k], writes=[pt.k])
    P.op("dve", lambda e: e.tensor_copy(out=dstT[:, 0:nchunk, col0:col0 + 128], in_=v3(pt[:, 0:nchunk * 128], nchunk)),
         reads=[pt.k], writes=[dstT.k])


def mixer_b_phase(C, S, x_dram, x_trks, out_dram, out_trks, mem_d, pos_d, wdown_d, gkv_d, wuk_d, wuv_d,
                  bwin_d, gq_d, wuq_d, wmk_d, w_out_d, g_d, b_d, cf2_d):
    nc, P = C.nc, C.P
    SC = 96.0 ** -0.5
    NT = NST // NSEQ
    with contextlib.ExitStack() as es:
        wdown = C.sb("wdown", [128, 8, 288], BF16, es)
        wdr = C.sb("wdr", [128, 8, 96], BF16, es)
        wdrot = C.sb("wdrot", [128, 8, 96], BF16, es)
        wuk = C.sb("wuk", [128, 2, 512], BF16, es)
        wuv = C.sb("wuv", [128, 2, 512], BF16, es)
        wuq = C.sb("wuq", [128, 2, 768], BF16, es)
        wuqrot = C.sb("wuqrot", [128, 2, 8, 96], BF16, es)
        gk = C.sb("gk", [128, 2], F32, es)
        gq = C.sb("gq", [128, 2], F32, es)
        bwin = C.sb("bwin", [128, 8, 768], BF16, es)
        wout = C.sb("wout", [128, 8, D], BF16, es)
        g_rep = C.sb("g_rep", [128, D], F32, es)
        b_rep = C.sb("b_rep", [128, D], F32, es)
        cf2 = C.sb("cf2", [128, 4], F32, es)
        xld = [C.sb("xld", [128, D], F32, es) for _ in range(2)]
        KmT = C.sb("KmT", [128, NSEQ, 4, 256], BF16, es)
        Vm = C.sb("Vm", [128, NSEQ, 2, 4, 129], BF16, es)
        P.dma("sp", g_rep[:], g_d.partition_broadcast(128), writes=[g_rep.k])
        P.dma("sp", b_rep[:], b_d.partition_broadcast(128), writes=[b_rep.k])
        P.dma("sp", cf2[:], cf2_d, writes=[cf2.k])
        for kc in range(2):
            P.dma("sp", gk[:, kc:kc + 1], gkv_d[kc * 128:(kc + 1) * 128].rearrange("(p o) -> p o", o=1), writes=[gk.k])
            P.dma("sp", gq[:, kc:kc + 1], gq_d[kc * 128:(kc + 1) * 128].rearrange("(p o) -> p o", o=1), writes=[gq.k])
        with contextlib.ExitStack() as es2:
            wmk = C.sb("wmk", [128, 8, D], BF16, es2)
            wst = C.sb("wst", [128, 2, 768], F32, es2)
            wmk_tr = load_weight_cast(C, wmk, wmk_d.rearrange("(c p) f -> p c f", p=128), 2, 1)
            wdown_tr = load_weight_cast(C, wdown, wdown_d.rearrange("(c p) f -> p c f", p=128), 1, 1)
            bwin_tr = load_weight_cast(C, bwin, bwin_d.rearrange("(c p) f -> p c f", p=128), 2, 1)
            wout_tr = []
            for (dst, src_d, gg, ncol) in ((wuk, wuk_d, gk, 512), (wuv, wuv_d, gk, 512), (wuq, wuq_d, gq, 768)):
                P.dma("sp", wst[:, :, 0:ncol], src_d.rearrange("(c p) f -> p c f", p=128), writes=[wst.k])
                for kc in range(2):
                    P.op("dve", lambda e, dst=dst, gg=gg, kc=kc, ncol=ncol: e.tensor_scalar(
                        out=dst[:, kc, :], in0=wst[:, kc, 0:ncol], scalar1=gg[:, kc:kc + 1], scalar2=None,
                        op0=ALU.mult), reads=[wst.k, gg.k], writes=[dst.k])
            mem_kv_precompute(C, S, es2, mem_d, wmk, wmk_tr, xld, KmT, Vm)
            P.full_barrier()
        xT = [C.sb("xT", [128, 8, ST], BF16, es) for _ in range(2)]
        uu = C.sb("uu", [96, ST], F32, es)
        u2 = C.sb("u2", [96, ST], F32, es)
        cosT = C.sb("cosT", [96, ST], F32, es)
        sinT = C.sb("sinT", [96, ST], F32, es)
        kT = C.sb("kT", [96, 8, SEQ], BF16, es)
        Vaug = C.sb("Vaug", [128, 16, 8, 65], BF16, es)
        kT_blk = [Trk("kTb%d" % i) for i in range(NT)]
        V_blk = [Trk("Vb%d" % i) for i in range(NT)]
        ckn = C.sb("ckn", [128, 256], BF16, es)
        ckT = [C.sb("ckT", [128, 2, ST], BF16, es) for _ in range(2)]
        cqT = [C.sb("cqT", [128, 2, ST], BF16, es) for _ in range(2)]
        rt1 = C.sb("rt1", [96, ST], F32, es)
        rt2 = C.sb("rt2", [96, ST], F32, es)
        kr = C.sb("kr", [96, ST], BF16, es)
        qTh = [C.sb("qTh", [96, 8, ST], BF16, es) for _ in range(2)]
        qmT = [C.sb("qmT", [128, 4, ST], BF16, es) for _ in range(2)]
        hc_all = C.sb("hc_all", [128, 4, D], BF16, es)
        y = [C.sb("y", [128, D], F32, es) for _ in range(2)]
        scr = None
        rc4 = C.sb("rc4", [128, 4, 1], F32, es)
        S["PT"] = [C.sb("PT", [128, ST], BF16, es) for _ in range(3)]
        S["PTm"] = [C.sb("PTm", [128, ST], BF16, es) for _ in range(2)]
        S["pt_i"] = 0
        S["rc"] = [C.sb("rc", [128, 1], F32, es) for _ in range(2)]
        S["rc_i"] = 0
        S["rms_ss"] = C.sb("rms_ss", [128, 1], F32, es)
        S["rms_rs"] = C.sb("rms_rs", [128, 1], F32, es)
        hcat = []
        for a in range(4):
            v = Tile(hc_all.t[:, a, :], "hcv")
            v.k = hc_all.k
            hcat.append(v)

        P.op("pool", lambda e: e.memset(wdr[:], 0.0), writes=[wdr.k])
        P.op("pool", lambda e: e.memset(wdrot[:], 0.0), writes=[wdrot.k])
        P.op("pool", lambda e: e.memset(wuqrot[:], 0.0), writes=[wuqrot.k])
        P.op("pool", lambda e: e.tensor_copy(out=wdr[:, :, 64:96], in_=wdown[:, :, 256:288]),
             reads=wdown_tr, writes=[wdr.k])
        P.op("pool", lambda e: e.tensor_scalar(out=wdrot[:, :, 64:80], in0=wdown[:, :, 272:288], scalar1=-1.0,
                                               scalar2=None, op0=ALU.mult), reads=wdown_tr, writes=[wdrot.k])
        P.op("pool", lambda e: e.tensor_copy(out=wdrot[:, :, 80:96], in_=wdown[:, :, 256:272]),
             reads=wdown_tr, writes=[wdrot.k])
        wuq4 = wuq.t[:, :, :].rearrange("p c (h d) -> p c h d", h=8)
        P.op("pool", lambda e: e.tensor_scalar(out=wuqrot[:, :, :, 64:80], in0=wuq4[:, :, :, 80:96], scalar1=-1.0,
                                               scalar2=None, op0=ALU.mult), reads=[wuq.k], writes=[wuqrot.k])
        P.op("pool", lambda e: e.tensor_copy(out=wuqrot[:, :, :, 80:96], in_=wuq4[:, :, :, 64:80]),
             reads=[wuq.k], writes=[wuqrot.k])
        P.op("pool", lambda e: e.memset(Vaug[:], 1.0), writes=V_blk)
        S["rot0"], S["nrot"] = 4, 2
        S["cast_eng"] = "pool"
        sbank = S["psf"][0:2]
        sb_i = [0]

        x_view = x_dram.rearrange("(s a p) d -> s a p d", a=4, p=128)
        o_view = out_dram.rearrange("(s a p) d -> s a p d", a=4, p=128)
        nl = [0]
        R = slice(64, 96)

        def front_chunks(s):
            seq, T, b = s // NT, s % NT, s % 2
            tcols = slice(T * ST, (T + 1) * ST)
            xTb, ckTb, cqTb, qThb, qmTb = xT[b], ckT[b], cqT[b], qTh[b], qmT[b]
            ch = []

            def rope_tables():
                posi_ap = rt2[R, :].bitcast(I32)
                P.dma("sp", posi_ap, pos_d[seq, T * ST:(T + 1) * ST].partition_broadcast(32), writes=[rt2.k])
                P.op("dve", lambda e: e.tensor_copy(out=uu[R, :], in_=posi_ap), reads=[rt2.k], writes=[uu.k])
                for (dstT, shift) in ((sinT, 0.0), (cosT, 0.25)):
                    P.op("dve", lambda e, shift=shift: e.tensor_scalar(
                        out=u2[R, :], in0=uu[R, :], scalar1=cf2[R, 0:1], scalar2=shift, op0=ALU.mult, op1=ALU.add),
                         reads=[uu.k, cf2.k], writes=[u2.k])
                    P.op("dve", lambda e: e.tensor_copy(out=posi_ap, in_=u2[R, :]), reads=[u2.k], writes=[rt2.k])
                    P.op("dve", lambda e: e.tensor_copy(out=rt1[R, :], in_=posi_ap), reads=[rt2.k], writes=[rt1.k])
                    P.op("dve", lambda e: e.tensor_tensor(out=u2[R, :], in0=u2[R, :], in1=rt1[R, :], op=ALU.subtract),
                         reads=[u2.k, rt1.k], writes=[u2.k])
                    P.op("dve", lambda e: e.tensor_scalar(out=rt1[R, :], in0=u2[R, :], scalar1=0.5, scalar2=None,
                                                          op0=ALU.is_gt), reads=[u2.k], writes=[rt1.k])
                    P.op("dve", lambda e: e.tensor_tensor(out=u2[R, :], in0=u2[R, :], in1=rt1[R, :], op=ALU.subtract),
                         reads=[u2.k, rt1.k], writes=[u2.k])
                    P.op("act", lambda e, dstT=dstT: e.activation(out=dstT[R, :], in_=u2[R, :], func=AF.Sin,
                                                                  scale=float(2 * np.pi)),
                         reads=[u2.k], writes=[dstT.k])
            xis = {}

            def load_dma(a):
                xi = xld[nl[0] % len(xld)]
                nl[0] += 1
                xis[a] = xi
                P.dma("sp", xi[:], x_view[s, a], reads=[x_trks[s]], writes=[xi.k])

            def load_tr(a):
                transpose_tokens(C, S, xis[a], xTb, a * 128)

            def latents(a):
                cols = slice(a * 128, (a + 1) * 128)
                for (wt, wtr, dstT) in ((wdown, wdown_tr, ckTb), (bwin, bwin_tr, cqTb)):
                    pb = nextbank(S)
                    P.mm(pb[:, 0:256], [(xTb[:, kc, cols], wt[:, kc, 0:256]) for kc in range(8)],
                         reads=[xTb.k] + wtr, writes=[pb.k])
                    rms_rows(C, S, pb, 256, ckn, scr)
                    transpose_cols(C, S, ckn, 2, dstT, a * 128)
            ch.append(lambda: load_dma(0))
            ch.append(lambda: load_dma(1))
            ch.append(rope_tables)
            ch.append(lambda: load_tr(0))
            ch.append(lambda: load_dma(2))
            ch.append(lambda: load_tr(1))
            ch.append(lambda: load_dma(3))
            ch.append(lambda: latents(0))
            ch.append(lambda: load_tr(2))
            ch.append(lambda: latents(1))
            ch.append(lambda: load_tr(3))
            ch.append(lambda: latents(2))
            ch.append(lambda: latents(3))

            def k_nope(h0):
                for h in range(h0, h0 + 4):
                    pb = nextbank(S)
                    P.mm(pb[0:64, :], [(wuk[:, kc, h * 64:(h + 1) * 64], ckTb[:, kc, :]) for kc in range(2)],
                         reads=[wuk.k, ckTb.k], writes=[pb.k])
                    P.op("dve", lambda e, pb=pb, h=h: e.tensor_copy(out=kT[0:64, h, tcols], in_=pb[0:64, :]),
                         reads=[pb.k], writes=[kT_blk[T]])
            ch.append(lambda: k_nope(0))
            ch.append(lambda: k_nope(4))

            def k_rope():
                pA, pB = nextbank(S), nextbank(S)
                P.mm(pA[0:96, :], [(wdr[:, kc, :], xTb[:, kc, :]) for kc in range(8)], reads=[wdr.k, xTb.k],
                     writes=[pA.k])
                P.mm(pB[0:96, :], [(wdrot[:, kc, :], xTb[:, kc, :]) for kc in range(8)], reads=[wdrot.k, xTb.k],
                     writes=[pB.k])
                P.op("dve", lambda e: e.tensor_tensor(out=rt1[R, :], in0=pA[R, :], in1=cosT[R, :], op=ALU.mult),
                     reads=[pA.k, cosT.k], writes=[rt1.k])
                P.op("dve", lambda e: e.tensor_tensor(out=rt2[R, :], in0=pB[R, :], in1=sinT[R, :], op=ALU.mult),
                     reads=[pB.k, sinT.k], writes=[rt2.k])
                P.op("dve", lambda e: e.tensor_tensor(out=kr[R, :], in0=rt1[R, :], in1=rt2[R, :], op=ALU.add),
                     reads=[rt1.k, rt2.k], writes=[kr.k])
                P.op("dve", lambda e: e.tensor_copy(out=kT[R, :, tcols],
                                                    in_=kr[R, :].unsqueeze(1).broadcast_to([32, 8, ST])),
                     reads=[kr.k], writes=[kT_blk[T]])
            ch.append(k_rope)

            def v_tiles():
                for a in range(4):
                    pb = nextbank(S)
                    P.mm(pb[:], [(ckTb[:, kc, a * 128:(a + 1) * 128], wuv[:, kc, :]) for kc in range(2)],
                         reads=[ckTb.k, wuv.k], writes=[pb.k])
                    P.op("act", lambda e, pb=pb, a=a: e.activation(out=Vaug[:, 4 * T + a, :, 0:64], in_=v3(pb[:], 8),
                                                                   func=AF.Copy), reads=[pb.k], writes=[V_blk[T]])
            ch.append(v_tiles)

            def queries(h0):
                for h in range(h0, h0 + 2):
                    pA, pB = nextbank(S), nextbank(S)
                    P.mm(pA[0:96, :], [(wuq[:, kc, h * 96:(h + 1) * 96], cqTb[:, kc, :]) for kc in range(2)],
                         reads=[wuq.k, cqTb.k], writes=[pA.k])
                    P.mm(pB[0:96, :], [(wuqrot[:, kc, h, :], cqTb[:, kc, :]) for kc in range(2)],
                         reads=[wuqrot.k, cqTb.k], writes=[pB.k])
                    P.op("dve", lambda e, pA=pA, h=h: e.tensor_copy(out=qThb[0:64, h, :], in_=pA[0:64, :]),
                         reads=[pA.k], writes=[qThb.k])
                    P.op("dve", lambda e, pA=pA: e.tensor_tensor(out=rt1[R, :], in0=pA[R, :], in1=cosT[R, :],
                                                                 op=ALU.mult), reads=[pA.k, cosT.k], writes=[rt1.k])
                    P.op("dve", lambda e, pB=pB: e.tensor_tensor(out=rt2[R, :], in0=pB[R, :], in1=sinT[R, :],
                                                                 op=ALU.mult), reads=[pB.k, sinT.k], writes=[rt2.k])
                    P.op("dve", lambda e, h=h: e.tensor_tensor(out=qThb[R, h, :], in0=rt1[R, :], in1=rt2[R, :],
                                                               op=ALU.add), reads=[rt1.k, rt2.k], writes=[qThb.k])
            for h0 in range(0, 8, 2):
                ch.append(lambda h0=h0: queries(h0))

            def q_mem():
                for h in range(4):
                    pb = nextbank(S)
                    P.mm(pb[:], [(bwin[:, kc, 256 + h * 128:256 + (h + 1) * 128], xTb[:, kc, :]) for kc in range(8)],
                         reads=[xTb.k] + bwin_tr, writes=[pb.k])
                    P.op("act", lambda e, pb=pb, h=h: e.activation(out=qmTb[:, h, :], in_=pb[:], func=AF.Copy),
                         reads=[pb.k], writes=[qmTb.k])
            ch.append(q_mem)
            return ch

        def mla_head(s, h, pending, per):
            T, b = s % NT, s % 2
            qThb = qTh[b]
            nkt = 4 * T + 4
            po = S["psf"][2 + (h % 2)]
            started = [False]

            def emit_front(j):
                a_min = max(0, j - 4 * T)
                qc = slice(a_min * 128, ST)
                pb = sbank[sb_i[0] % 2]
                sb_i[0] += 1
                P.mm(pb[:, qc], [(kT[:, h, j * 128:(j + 1) * 128], qThb[:, h, qc])],
                     reads=[kT_blk[j // 4], qThb.k], writes=[pb.k])
                pt = S["PT"][S["pt_i"] % len(S["PT"])]
                S["pt_i"] += 1
                P.op("act", lambda e: e.activation(out=pt[:, qc], in_=pb[:, qc], func=AF.Exp, scale=SC),
                     reads=[pb.k], writes=[pt.k])
                if j >= 4 * T:
                    P.op("pool", lambda e: e.memset(pt[64:128, a_min * 128:a_min * 128 + 64], 0.0),
                         reads=[], writes=[pt.k])
                return (j, a_min, pt)

            def emit_back(j, a_min, pt):
                fns = []
                for a in range(a_min, 4):
                    st = not started[0]
                    started[0] = True
                    fns.append(lambda e, a=a, st=st: e.matmul(
                        po[:, a * 65:(a + 1) * 65], pt[:, a * 128:(a + 1) * 128], Vaug[:, j, h, :],
                        start=st, stop=(j == 4 * T + a), skip_group_check=True))
                P.pe_multi(fns, reads=[pt.k, V_blk[j // 4]], writes=[po.k])

            prev = None
            for j in range(nkt):
                cur = emit_front(j)
                if prev is not None:
                    emit_back(*prev)
                prev = cur
                for _ in range(per):
                    if pending:
                        P.replay(pending.pop(0))
            emit_back(*prev)
            po3 = v3(po[:, 0:260], 4)
            P.op("dve", lambda e: e.reciprocal(out=rc4[:], in_=po3[:, :, 64:65]), reads=[po.k], writes=[rc4.k])
            P.op("dve", lambda e: e.tensor_tensor(
                out=hc_all[:, :, h * 64:(h + 1) * 64], in0=po3[:, :, 0:64],
                in1=rc4[:, :, 0:1].broadcast_to([128, 4, 64]), op=ALU.mult),
                 reads=[po.k, rc4.k], writes=[hc_all.k])

        first = []
        P.rec = first
        for f in front_chunks(0):
            f()
        P.rec = None
        for it in list_schedule(first):
            P.replay(it)
        wout_tr.extend(load_weight_cast(C, wout, w_out_d.rearrange("(c p) f -> p c f", p=128), 2, 1))
        for s in range(NST):
            seq, b = s // NT, s % 2
            pending = []
            P.rec = pending
            mem_attention(C, S, seq, qmT[b], KmT, Vm, hcat, 512)
            if s > 0:
                out_proj_mm_ln(C, S, xT[(s - 1) % 2], wout, wout_tr, xld, nl, x_view, x_trks[s - 1], s - 1, y,
                               g_rep, b_rep, o_view, out_trks[s - 1])
            if s + 1 < NST:
                for f in front_chunks(s + 1):
                    f()
            P.rec = None
            pending = list_schedule(pending)
            nsteps = 8 * (4 * (s % NT) + 4)
            per = (len(pending) + nsteps - 1) // nsteps
            for h in range(8):
                mla_head(s, h, pending, per)
            while pending:
                P.replay(pending.pop(0))
            out_proj_tr(C, S, hcat, xT[b])
        out_proj_mm_ln(C, S, xT[(NST - 1) % 2], wout, wout_tr, xld, nl, x_view, x_trks[NST - 1], NST - 1, y,
                       g_rep, b_rep, o_view, out_trks[NST - 1])
        S.pop("PTm")
        P.full_barrier()
        S["rot0"], S["nrot"] = 0, 6
        S.pop("cast_eng")


def _consts_np():
    c = np.zeros((128, 4, 128), np.float32)
    c[:, 0, :] = np.eye(128, dtype=np.float32)
    s = np.arange(128)[:, None]
    l = np.arange(128)[None, :]
    c[:, 1, :] = ((s // 64 == l // 64) & (s <= l)).astype(np.float32)
    c[:, 2, :] = (s < 64).astype(np.float32) * np.ones((1, 128), np.float32)
    c[:, 3, :] = (s >= 64).astype(np.float32) * np.ones((1, 128), np.float32)
    return c


def _cf2_np():
    c = np.zeros((128, 4), np.float32)
    inv = (10000.0 ** (-np.arange(0, 32, 2, dtype=np.float32) / 32)).astype(np.float32)
    for p in range(64, 96):
        c[p, 0] = inv[(p - 64) % 16] / np.float32(2 * np.pi)
    c[:, 1] = -np.pi
    return c


W_SHAPES = {
    "a_w_in": [D, 2056], "a_b_igate": [4], "a_b_fgate": [4], "a_w_mem_kv": [D, D], "a_w_out": [D, D],
    "kv_w_down": [D, 288], "kv_norm_g": [256], "kv_w_uk": [256, 512], "kv_w_uv": [256, 512],
    "b_w_in": [D, 768], "b_q_norm_g": [256], "b_w_uq": [256, 768], "b_w_mem_kv": [D, D], "b_w_out": [D, D],
    "ln1_g": [2, D], "ln1_b": [2, D], "ffn_w_up": [2, D, DFF], "ffn_w_down": [2, DFF, D], "ln2_g": [2, D],
    "ln2_b": [2, D],
}


def build_program():
    nc = bass.Bass("TRN2", target_bir_lowering=False)
    dt = lambda n, sh: nc.dram_tensor(n, sh, F32, kind="ExternalInput").ap()
    x = dt("x", [NTOK, D])
    mem = dt("mem", [NSEQ, 256, D])
    pos = nc.dram_tensor("positions", [NSEQ, SEQ], I32, kind="ExternalInput").ap()
    w = {k: dt(k, sh) for k, sh in W_SHAPES.items()}
    consts = dt("consts", [128, 4, 128])
    cf2 = dt("cf2", [128, 4])
    out = nc.dram_tensor("out", [NTOK, D], F32, kind="ExternalOutput").ap()
    sc1 = nc.dram_tensor("scratch1", [NTOK, D], F32, kind="Internal").ap()
    sc2 = nc.dram_tensor("scratch2", [NTOK, D], F32, kind="Internal").ap()
    C = Ctx(nc)
    with nc.allow_low_precision("bf16 matmul operands, fp32 accumulation"), C.es:
        S = alloc_shared(C)
        load_consts(C, S, consts)
        xtr = [Trk("xd%d" % i) for i in range(NST)]
        t1 = [Trk("s1_%d" % i) for i in range(NST)]
        t2 = [Trk("s2_%d" % i) for i in range(NST)]
        otr = [Trk("od%d" % i) for i in range(NST)]
        mixer_a_phase(C, S, x, xtr, sc1, t1, mem, w["a_w_in"], w["a_b_igate"], w["a_b_fgate"], w["a_w_mem_kv"],
                      w["a_w_out"], w["ln1_g"][0], w["ln1_b"][0])
        ffn_phase(C, S, sc1, t1, sc2, t2, w["ffn_w_up"][0], w["ffn_w_down"][0], w["ln2_g"][0], w["ln2_b"][0], False)
        mixer_b_phase(C, S, sc2, t2, sc1, t1, mem, pos, w["kv_w_down"], w["kv_norm_g"], w["kv_w_uk"], w["kv_w_uv"],
                      w["b_w_in"], w["b_q_norm_g"], w["b_w_uq"], w["b_w_mem_kv"], w["b_w_out"], w["ln1_g"][1],
                      w["ln1_b"][1], cf2)
        ffn_phase(C, S, sc1, t1, out, otr, w["ffn_w_up"][1], w["ffn_w_down"][1], w["ln2_g"][1], w["ln2_b"][1], True)
        C.P.finish()
    return nc


def kernel(x, mem, positions, a_w_in, a_b_igate, a_b_fgate, a_w_mem_kv, a_w_out, kv_w_down, kv_norm_g, kv_w_uk,
           kv_w_uv, b_w_in, b_q_norm_g, b_w_uq, b_w_mem_kv, b_w_out, ln1_g, ln1_b, ffn_w_up, ffn_w_down, ln2_g,
           ln2_b):
    f32 = lambda a: np.ascontiguousarray(np.asarray(a), dtype=np.float32)
    shared = {
        "a_w_in": f32(a_w_in)[0], "a_b_igate": f32(a_b_igate)[0], "a_b_fgate": f32(a_b_fgate)[0],
        "a_w_mem_kv": f32(a_w_mem_kv)[0], "a_w_out": f32(a_w_out)[0], "kv_w_down": f32(kv_w_down),
        "kv_norm_g": f32(kv_norm_g), "kv_w_uk": f32(kv_w_uk), "kv_w_uv": f32(kv_w_uv), "b_w_in": f32(b_w_in)[0],
        "b_q_norm_g": f32(b_q_norm_g)[0], "b_w_uq": f32(b_w_uq)[0], "b_w_mem_kv": f32(b_w_mem_kv)[0],
        "b_w_out": f32(b_w_out)[0], "ln1_g": f32(ln1_g), "ln1_b": f32(ln1_b), "ffn_w_up": f32(ffn_w_up),
        "ffn_w_down": f32(ffn_w_down), "ln2_g": f32(ln2_g), "ln2_b": f32(ln2_b),
        "consts": _consts_np(), "cf2": _cf2_np(),
    }
    shared = {k: np.ascontiguousarray(v) for k, v in shared.items()}
    x = f32(x)
    mem = f32(mem)
    positions = np.ascontiguousarray(np.asarray(positions), dtype=np.int32)
    in_maps = []
    for c in range(NCORES):
        m = dict(shared)
        m["x"] = np.ascontiguousarray(x[c * NSEQ:(c + 1) * NSEQ].reshape(NTOK, D))
        m["mem"] = np.ascontiguousarray(mem[c * NSEQ:(c + 1) * NSEQ])
        m["positions"] = np.ascontiguousarray(positions[c * NSEQ:(c + 1) * NSEQ])
        in_maps.append(m)
    nc = build_program()
    res = run_bass_kernel_spmd(nc, in_maps, core_ids=list(range(NCORES)))
    outs = [np.asarray(r["out"], dtype=np.float32).reshape(NSEQ, SEQ, D) for r in res.results]
    return np.concatenate(outs, axis=0)
```
